# Optimizing a Trainium2 kernel written in Bass

```python
import math
import jax, jax.numpy as jnp
from jax import lax
import numpy as np

D_MODEL = 1024
BATCH = 16
SEQ = 2048
DEPTH = 1

N_META = 16
A_HEADS = 4
A_DK = 64
A_DV = 2 * A_DK
B_HEADS = 4
B_DQ = 128
B_DC = 256
B_DV = 128
IDX_HEADS = 8
IDX_DIM = 64
IDX_TOPK_MAX = 256
REL_BUCKETS = 32
REL_MAX_DIST = 128
N_BIAS_HEADS = A_HEADS + B_HEADS
D_FF = 2816
Q_BLOCK = 128
EPS = 1e-6
A_SCALE = A_DK ** -0.5
B_SCALE = B_DC ** -0.5

IN_SIZES = (
    A_HEADS * 2 * A_DK,
    A_HEADS * 2 * A_DK,
    A_HEADS * A_DV,
    B_HEADS * B_DQ,
    B_DC,
    IDX_HEADS * IDX_DIM,
    IDX_DIM,
    IDX_HEADS,
)
N_IN = sum(IN_SIZES)
IN_SPLITS = [int(v) for v in np.cumsum(IN_SIZES)[:-1]]
D_MIX = A_HEADS * A_DV + B_HEADS * B_DV

kernel_name = 'hymba_diffattn_dsa_macaron'


def rmsnorm(x, g):
    xf = x.astype(jnp.float32)
    y = xf * lax.rsqrt(jnp.mean(xf * xf, axis=-1, keepdims=True) + EPS)
    return (y * g.astype(jnp.float32)).astype(x.dtype)


def swiglu(x, w_gate, w_up, w_down):
    return (jax.nn.silu(x @ w_gate) * (x @ w_up)) @ w_down


def rel_bucket(q_pos, k_pos):
    max_exact = REL_BUCKETS // 2
    n = jnp.maximum(q_pos - k_pos, 0)
    nf = jnp.maximum(n, 1).astype(jnp.float32)
    large = max_exact + (jnp.log(nf / max_exact) / math.log(REL_MAX_DIST / max_exact)
                         * (REL_BUCKETS - max_exact)).astype(jnp.int32)
    large = jnp.minimum(large, REL_BUCKETS - 1)
    return jnp.where(n < max_exact, n, large)


def diff_attention(q, k, v, lam, bias_a, subln_g, lam_init):
    bsz, length = q.shape[0], q.shape[1]
    outs = []
    for start in range(0, length, Q_BLOCK):
        stop = min(start + Q_BLOCK, length)
        q_pos = jnp.arange(start, stop)
        k_pos = jnp.arange(stop)
        bias = bias_a[rel_bucket(q_pos[:, None], k_pos[None, :])].transpose(2, 0, 1)
        s = jnp.einsum('bqhmd,bkhmd->bhmqk', q[:, start:stop], k[:, :stop]).astype(jnp.float32)
        s = s * A_SCALE + bias[None, :, None].astype(jnp.float32)
        s = jnp.where(k_pos[None, :] <= q_pos[:, None], s, -jnp.inf)
        p = jax.nn.softmax(s, axis=-1)
        a = p[:, :, 0] - lam * p[:, :, 1]
        outs.append(jnp.einsum('bhqk,bkhd->bqhd', a.astype(v.dtype), v[:, :stop]))
    o = jnp.concatenate(outs, axis=1)
    o = rmsnorm(o, subln_g) * (1.0 - lam_init)
    return o.reshape(bsz, length, A_HEADS * A_DV)


def dsa_attention(q_lat, c, iq, ik, iw, w_uv, bias_b, topk):
    bsz, length = q_lat.shape[0], q_lat.shape[1]
    gather = jax.vmap(lambda cb, ib: cb[ib])
    outs = []
    for start in range(0, length, Q_BLOCK):
        stop = min(start + Q_BLOCK, length)
        q_pos = jnp.arange(start, stop)
        k_pos = jnp.arange(stop)
        dots = jnp.einsum('bqhd,bkd->bqhk', iq[:, start:stop], ik[:, :stop]).astype(jnp.float32)
        w = iw[:, start:stop].astype(jnp.float32) * (IDX_HEADS ** -0.5)
        score = jnp.einsum('bqh,bqhk->bqk', w, jax.nn.relu(dots * (IDX_DIM ** -0.5)))
        score = jnp.where(k_pos[None, None, :] <= q_pos[None, :, None], score, -jnp.inf)
        _, sel = lax.top_k(score, min(topk, stop))
        valid = sel <= q_pos[None, :, None]
        g = gather(c, sel)
        bias = bias_b[rel_bucket(q_pos[None, :, None], sel)].transpose(0, 3, 1, 2)
        s = jnp.einsum('bqhc,bqkc->bhqk', q_lat[:, start:stop], g).astype(jnp.float32)
        s = s * B_SCALE + bias.astype(jnp.float32)
        s = jnp.where(valid[:, None], s, -jnp.inf)
        p = jax.nn.softmax(s, axis=-1)
        o_lat = jnp.einsum('bhqk,bqkc->bqhc', p.astype(g.dtype), g)
        outs.append(jnp.einsum('bqhc,hcd->bqhd', o_lat, w_uv))
    o = jnp.concatenate(outs, axis=1)
    return o.reshape(bsz, length, B_HEADS * B_DV)


def setup_inputs(seed: int = 0) -> dict:
    key = jax.random.key(seed)
    ks = jax.random.split(key, 32)
    f32 = jnp.float32

    def nrm(k, shape, scale):
        return jax.random.normal(k, shape, f32) * scale

    def gain(k, shape):
        return 1.0 + 0.02 * jax.random.normal(k, shape, f32)

    L = DEPTH
    return {
        'x': nrm(ks[0], (BATCH, SEQ, D_MODEL), 1.0),
        'meta_tokens': nrm(ks[1], (N_META, D_MODEL), 1.0),
        'rel_bias': nrm(ks[2], (REL_BUCKETS, N_BIAS_HEADS), 0.1),
        'ffn1_norm': gain(ks[3], (L, D_MODEL)),
        'ffn1_w_gate': nrm(ks[4], (L, D_MODEL, D_FF), D_MODEL ** -0.5),
        'ffn1_w_up': nrm(ks[5], (L, D_MODEL, D_FF), D_MODEL ** -0.5),
        'ffn1_w_down': nrm(ks[6], (L, D_FF, D_MODEL), D_FF ** -0.5),
        'mix_norm': gain(ks[7], (L, D_MODEL)),
        'w_in': nrm(ks[8], (L, D_MODEL, N_IN), D_MODEL ** -0.5),
        'a_q_norm': gain(ks[9], (L, A_DK)),
        'a_k_norm': gain(ks[10], (L, A_DK)),
        'a_lambda_q1': nrm(ks[11], (L, A_DK), 0.1),
        'a_lambda_k1': nrm(ks[12], (L, A_DK), 0.1),
        'a_lambda_q2': nrm(ks[13], (L, A_DK), 0.1),
        'a_lambda_k2': nrm(ks[14], (L, A_DK), 0.1),
        'a_subln': gain(ks[15], (L, A_DV)),
        'b_kv_norm': gain(ks[16], (L, B_DC)),
        'b_w_uk': nrm(ks[17], (L, B_HEADS, B_DQ, B_DC), B_DQ ** -0.5),
        'b_q_norm': gain(ks[18], (L, B_DC)),
        'b_w_uv': nrm(ks[19], (L, B_HEADS, B_DC, B_DV), B_DC ** -0.5),
        'w_out': nrm(ks[20], (L, D_MIX, D_MODEL), D_MIX ** -0.5),
        'ffn2_norm': gain(ks[21], (L, D_MODEL)),
        'ffn2_w_gate': nrm(ks[22], (L, D_MODEL, D_FF), D_MODEL ** -0.5),
        'ffn2_w_up': nrm(ks[23], (L, D_MODEL, D_FF), D_MODEL ** -0.5),
        'ffn2_w_down': nrm(ks[24], (L, D_FF, D_MODEL), D_FF ** -0.5),
    }


def reference(x, meta_tokens, rel_bias, ffn1_norm, ffn1_w_gate, ffn1_w_up, ffn1_w_down,
              mix_norm, w_in, a_q_norm, a_k_norm, a_lambda_q1, a_lambda_k1, a_lambda_q2,
              a_lambda_k2, a_subln, b_kv_norm, b_w_uk, b_q_norm, b_w_uv, w_out,
              ffn2_norm, ffn2_w_gate, ffn2_w_up, ffn2_w_down):
    bsz, seq = x.shape[0], x.shape[1]
    topk = min(IDX_TOPK_MAX, seq // 4)
    meta = jnp.broadcast_to(meta_tokens.astype(x.dtype)[None], (bsz, N_META, D_MODEL))
    h = jnp.concatenate([meta, x], axis=1)
    length = h.shape[1]
    bias_a = rel_bias[:, :A_HEADS]
    bias_b = rel_bias[:, A_HEADS:]

    for l in range(DEPTH):
        h = h + 0.5 * swiglu(rmsnorm(h, ffn1_norm[l]), ffn1_w_gate[l], ffn1_w_up[l], ffn1_w_down[l])

        u = rmsnorm(h, mix_norm[l]) @ w_in[l]
        qa, ka, va, qb, ckv, iq, ik, iw = jnp.split(u, IN_SPLITS, axis=-1)

        qa = rmsnorm(qa.reshape(bsz, length, A_HEADS, 2, A_DK), a_q_norm[l])
        ka = rmsnorm(ka.reshape(bsz, length, A_HEADS, 2, A_DK), a_k_norm[l])
        va = va.reshape(bsz, length, A_HEADS, A_DV)
        lam_init = 0.8 - 0.6 * math.exp(-0.3 * l)
        lam = (jnp.exp(jnp.sum(a_lambda_q1[l].astype(jnp.float32) * a_lambda_k1[l].astype(jnp.float32)))
               - jnp.exp(jnp.sum(a_lambda_q2[l].astype(jnp.float32) * a_lambda_k2[l].astype(jnp.float32)))
               + lam_init)
        o_a = diff_attention(qa, ka, va, lam, bias_a, a_subln[l], lam_init)

        qb = qb.reshape(bsz, length, B_HEADS, B_DQ)
        q_lat = rmsnorm(jnp.einsum('blhd,hdc->blhc', qb, b_w_uk[l]), b_q_norm[l])
        c = rmsnorm(ckv, b_kv_norm[l])
        iq = iq.reshape(bsz, length, IDX_HEADS, IDX_DIM)
        o_b = dsa_attention(q_lat, c, iq, ik, iw, b_w_uv[l], bias_b, topk)

        h = h + jnp.concatenate([o_a, o_b], axis=-1) @ w_out[l]

        h = h + 0.5 * swiglu(rmsnorm(h, ffn2_norm[l]), ffn2_w_gate[l], ffn2_w_up[l], ffn2_w_down[l])

    return h[:, N_META:]
```

```python
import math
import os as _os
from contextlib import ExitStack

import numpy as np
import concourse.bass as bass
import concourse.mybir as mybir
from concourse.bass_utils import run_bass_kernel_spmd

F32 = mybir.dt.float32
BF16 = mybir.dt.bfloat16
ALU = mybir.AluOpType
AF = mybir.ActivationFunctionType
AX = mybir.AxisListType

D = 1024
SEQ = 2048
NMETA = 16
L = SEQ + NMETA
DFF = 2816
NSEQ = 2
NCORES = 8
NT = 17
TILES = [(i * 128, min(128, L - i * 128)) for i in range(NT)]
TGS = [(0, 512), (512, 512), (1024, 512), (1536, 512), (2048, 16)]
QGS = [[0, 1, 2, 3], [4, 5, 6, 7], [8, 9, 10, 11], [12, 13, 14, 15], [16]]
EPS = 1e-6
A_SCALE = 64 ** -0.5
B_SCALE = 256 ** -0.5
LAM_INIT = 0.8 - 0.6 * math.exp(0.0)
TOPK = 256
NEG = -30000.0
NBIS = 16
WIN_COLS = 4 * 384 + 264 + 9 * 128
STRICT = True
EPOCH = 2000


def _dsize(dt):
    return 4 if dt == F32 else 2


class Dep:
    __slots__ = ("name", "lw", "rd", "sem", "dcount")

    def __init__(self, name):
        self.name = name
        self.lw = None
        self.rd = []
        self.sem = None
        self.dcount = 0


class Op:
    __slots__ = ("eng", "fn", "deps", "dma", "signal", "seq", "sem", "waits", "wdep")

    def __init__(self, eng, fn, deps, dma, wdep):
        self.eng = eng
        self.fn = fn
        self.deps = deps
        self.dma = dma
        self.signal = False
        self.seq = 0
        self.sem = None
        self.waits = []
        self.wdep = wdep


class Prog:
    ENGS = ("sp", "pe", "act", "dve", "pool")

    def __init__(self, nc):
        self.nc = nc
        self.ops = []
        self.last = {e: None for e in self.ENGS}
        self.dmas_since_barrier = []

    def add(self, eng, fn, r=(), w=(), dma=False):
        idx = len(self.ops)
        deps = set()
        for t in r:
            if t.lw is not None:
                deps.add(t.lw)
        for t in w:
            if t.lw is not None:
                deps.add(t.lw)
            deps.update(t.rd)
        for t in r:
            t.rd.append(idx)
        for t in w:
            t.lw = idx
            t.rd = []
        wdep = None
        if dma:
            assert len(w) == 1
            wdep = w[0]
            self.dmas_since_barrier.append(idx)
        self.ops.append(Op(eng, fn, deps, dma, wdep))
        self.last[eng] = idx
        return idx

    def barrier(self):
        lasts = {v for v in self.last.values() if v is not None}
        lasts.update(self.dmas_since_barrier)
        self.dmas_since_barrier = []
        for e in self.ENGS:
            idx = len(self.ops)
            self.ops.append(Op(e, None, set(lasts), False, None))
            self.last[e] = idx

    def emit(self, stack):
        nc = self.nc
        ops = self.ops
        for op in ops:
            for d in sorted(op.deps):
                dop = ops[d]
                if not dop.dma and dop.eng == op.eng and (op.eng == "pe" or not STRICT):
                    continue
                if dop.fn is None:
                    continue
                dop.signal = True
                op.waits.append(d)
        esem = {e: stack.enter_context(nc.semaphore("s_" + e)) for e in self.ENGS}
        cnt = {e: 0 for e in self.ENGS}
        NCH = 8
        chsem = [stack.enter_context(nc.semaphore("dch%d" % i)) for i in range(NCH)]
        chcnt = [0] * NCH
        chlast = [None] * NCH
        ndma = 0
        for oi, op in enumerate(ops):
            if op.fn is None:
                continue
            if op.dma:
                c = ndma % NCH
                ndma += 1
                if chlast[c] is not None:
                    op.waits.append(chlast[c])
                chlast[c] = oi
                chcnt[c] += 16
                op.sem = chsem[c]
                op.seq = chcnt[c]
            elif op.signal:
                if cnt[op.eng] >= EPOCH:
                    esem[op.eng] = stack.enter_context(nc.semaphore("s_%s_%d" % (op.eng, oi)))
                    cnt[op.eng] = 0
                cnt[op.eng] += 1
                op.sem = esem[op.eng]
                op.seq = cnt[op.eng]
        per = {e: [op for op in ops if op.eng == e] for e in self.ENGS}

        def run(e, h):
            waited = {}
            for op in per[e]:
                need = {}
                for d in op.waits:
                    dop = ops[d]
                    k = id(dop.sem)
                    if k not in need or need[k][1] < dop.seq:
                        need[k] = (dop.sem, dop.seq)
                for k, (sem, val) in need.items():
                    if waited.get(k, 0) >= val:
                        continue
                    h.wait_ge(sem, val)
                    waited[k] = val
                if op.fn is None:
                    continue
                ins = op.fn(h)
                if op.dma:
                    ins.then_inc(op.sem, 16)
                elif op.signal:
                    ins.then_inc(op.sem, 1)

        with nc.Block() as block:
            @block.sync
            def _(h):
                run("sp", h)

            @block.tensor
            def _(h):
                run("pe", h)

            @block.scalar
            def _(h):
                run("act", h)

            @block.vector
            def _(h):
                run("dve", h)

            @block.gpsimd
            def _(h):
                run("pool", h)


class Builder:
    def __init__(self, stage=99, nseq=NSEQ, dbg=False, dbg_stop=False):
        self.dbg_stop = dbg_stop
        self.stage = stage
        self.nseq = nseq
        self.nc = nc = bass.Bass("TRN2", target_bir_lowering=False)
        self.P = Prog(nc)
        self.cur = 16640
        dt = nc.dram_tensor
        self.x = dt("x", [NSEQ, SEQ, D], F32, kind="ExternalInput").ap()
        self.meta = dt("meta", [NMETA, D], F32, kind="ExternalInput").ap()
        self.wg = [dt("w%dg" % i, [D, DFF], F32, kind="ExternalInput").ap() for i in (1, 2)]
        self.wu = [dt("w%du" % i, [D, DFF], F32, kind="ExternalInput").ap() for i in (1, 2)]
        self.wd = [dt("w%dd" % i, [DFF, D], F32, kind="ExternalInput").ap() for i in (1, 2)]
        self.win = dt("win", [D, WIN_COLS], F32, kind="ExternalInput").ap()
        self.wuk = dt("wuk", [128, 4, 256], F32, kind="ExternalInput").ap()
        self.wuv = dt("wuv", [128, 2, 4, 128], F32, kind="ExternalInput").ap()
        self.wout = dt("wout", [D, D], F32, kind="ExternalInput").ap()
        self.cols = dt("cols", [128, 40], F32, kind="ExternalInput").ap()
        self.rows = dt("rows", [128, 896], F32, kind="ExternalInput").ap()
        self.bias = dt("biasblk", [128, 8, 2, 128], F32, kind="ExternalInput").ap()
        self.cmask = dt("cmask", [128, 3, 128], F32, kind="ExternalInput").ap()
        self.out = dt("out", [NSEQ, SEQ, D], F32, kind="ExternalOutput").ap()
        self.hs = dt("hs", [L, D], F32, kind="ExternalOutput").ap()
        self.dbg = dt("dbg", [128, 4096], F32, kind="ExternalOutput").ap() if dbg else None
        self.psum = [nc.alloc_psum_tensor("pb%d" % i, [128, 512], F32).ap() for i in range(8)]
        self.pdep = [Dep("pb%d" % i) for i in range(8)]
        self.out_dep = Dep("out")

    def sb(self, name, shape, dtype, at=None):
        n = 1
        for s in shape[1:]:
            n *= s
        nbytes = (n * _dsize(dtype) + 63) // 64 * 64
        off = self.cur if at is None else at
        t = self.nc.alloc_sbuf_tensor_at(name, list(shape), dtype, offset=off)
        if at is None:
            self.cur = off + nbytes
        assert off + nbytes <= 229376, (name, off + nbytes)
        return t.ap()

    def mm(self, out, lhsT, rhs, start, stop, r, w):
        self.P.add("pe", lambda e: e.matmul(out, lhsT, rhs, start=start, stop=stop, skip_group_check=True), r, w)

    def tr(self, out, in_, ident, r, w):
        self.P.add("pe", lambda e: e.transpose(out, in_, ident), r, w)

    def act(self, out, in_, func, r, w, bias=0.0, scale=1.0, accum=None):
        if accum is None:
            self.P.add("act", lambda e: e.activation(out, in_, func, bias=bias, scale=scale), r, w)
        else:
            self.P.add("act", lambda e: e.activation(out, in_, func, bias=bias, scale=scale, accum_out=accum), r, w)

    def ts(self, eng, out, in0, s1, s2, op0, op1, r, w, accum=None):
        if accum is None:
            self.P.add(eng, lambda e: e.tensor_scalar(out, in0, s1, s2, op0, op1), r, w)
        else:
            self.P.add(eng, lambda e: e.tensor_scalar(out, in0, s1, s2, op0, op1, accum_out=accum), r, w)

    def tt(self, eng, out, in0, in1, op, r, w):
        self.P.add(eng, lambda e: e.tensor_tensor(out, in0, in1, op), r, w)

    def stt(self, out, in0, scalar, in1, op0, op1, r, w):
        self.P.add("dve", lambda e: e.scalar_tensor_tensor(out, in0, scalar, in1, op0, op1), r, w)

    def cp(self, eng, out, in_, r, w):
        self.P.add(eng, lambda e: e.tensor_copy(out, in_), r, w)

    def red(self, out, in_, op, r, w, absval=False):
        self.P.add("dve", lambda e: e.tensor_reduce(out, in_, AX.X, op, apply_absolute_value=absval), r, w)

    def recip(self, out, in_, r, w):
        self.P.add("dve", lambda e: e.reciprocal(out, in_), r, w)

    def memset(self, eng, ap, val, w):
        self.P.add(eng, lambda e: e.memset(ap, val), (), w)

    def dma(self, out, in_, r, w):
        self.P.add("sp", lambda e: e.dma_start(out=out, in_=in_), r, w, dma=True)

    def stat(self):
        i = self.st_i
        self.st_i = (i + 1) % 8
        return 4 * i, self.st_dep[i]

    def rstd(self, np_, k, nc_, inv_n, deps):
        ssd, td, rsd = deps
        self.ts("dve", self.tmp[0:np_, k:k + nc_], self.ss[0:np_, k:k + nc_], inv_n, EPS, ALU.mult, ALU.add, [ssd], [td])
        self.tt("pool", self.rs[0:np_, k:k + nc_], self.tmp[0:np_, k:k + nc_], self.neghalf[0:np_, 0:nc_], ALU.pow,
                [td, self.nh_dep], [rsd])

    def build(self):
        nc = self.nc
        stage = self.stage
        with ExitStack() as stack:
            self._alloc()
            self._setup()
            for s in range(self.nseq):
                self._sequence(s)
            self._finish()
            self.P.emit(stack)
        return nc

    def _alloc(self):
        sb = self.sb
        self.cols_sb = sb("cols", [128, 40], F32)
        self.rows_sb = sb("rows", [128, 896], F32)
        self.ident_f = sb("identf", [128, 128], F32)
        self.ident = sb("ident", [128, 128], BF16)
        self.neghalf = sb("neghalf", [128, 16], F32)
        self.smallc = sb("smallc", [128, 32], F32)
        self.gqk = self.smallc[:, 6:7]
        self.gqb = [self.smallc[:, 2:3], self.smallc[:, 10:11]]
        self.lamn = self.smallc[:, 14:15]
        self.abias = sb("abias", [128, 8], F32)
        self.gsub = self.smallc[:, 18:19]
        self.sm = sb("small", [128, 272], F32)
        self.biasbf = sb("biasbf", [128, 8, 2, 128], BF16)
        self.masktok = sb("masktok", [128, 128], F32)
        self.iw_sb = sb("iw", [128, NT, 8], F32)
        self.ss = sb("ss", [128, 32], F32)
        self.tmp = sb("tmpst", [128, 32], F32)
        self.rs = sb("rs", [128, 32], F32)
        self.st_dep = [(Dep("ss%d" % i), Dep("tm%d" % i), Dep("rs%d" % i)) for i in range(8)]
        self.st_i = 0
        self.wukb = sb("wukb", [128, 4, 256], BF16)
        self.wuvb = sb("wuvb", [128, 2, 4, 128], BF16)
        self.maskT = sb("maskT", [128, 128], F32)
        self.pw = sb("pw", [128, NBIS + 1], F32)
        self.thc = self.smallc[:, 22:23]
        self.xn_s = [sb("xn_s%d" % i, [128, D], BF16) for i in range(2)]
        self.xn_s_dep = [Dep("xn_s%d" % i) for i in range(2)]
        self.junk = sb("junk", [128, D], BF16)
        self.junk_dep = Dep("junk")
        self.c_const = self.cur
        self.xnT = sb("xnT", [128, 8, L], BF16)
        self.xn_dep = [Dep("xnT%d" % t) for t in range(NT)]
        self.h_off = self.cur
        self.h = sb("h", [128, NT, D], F32)
        self.h_dep = [Dep("h%d" % t) for t in range(NT)]
        self.big_off = self.cur
        self.phase_off = self.cur

    def _setup(self):
        P = self.P
        cd = self.cdep = Dep("consts")
        self.dma(self.cols_sb, self.cols, [], [cd])
        rd = Dep("rows")
        self.dma(self.rows_sb, self.rows, [], [rd])
        idd = Dep("identf")
        self.dma(self.ident_f, self.cmask[:, 0, :], [], [idd])
        mk = Dep("masktok")
        self.dma(self.masktok, self.cmask[:, 2, :], [], [mk])
        self.masktok_dep = mk
        self.ident_dep = Dep("ident")
        self.cp("dve", self.ident, self.ident_f, [idd], [self.ident_dep])
        self.nh_dep = Dep("neghalf")
        self.memset("dve", self.neghalf, -0.5, [self.nh_dep])
        if self.stage <= 1:
            return
        self._ffn_alloc()
        C = self.cols_sb
        R = self.rows_sb
        sd = self.setup_dep = Dep("setup")
        smd = Dep("sm")
        mtd = Dep("maskT")
        self.dma(self.maskT, self.cmask[:, 1, :], [], [mtd])
        self.stt(self.gqk, C[:, 24:25], A_SCALE, C[:, 25:26], ALU.mult, ALU.mult, [cd], [sd])
        for cc in range(2):
            self.ts("dve", self.gqb[cc], C[:, 28 + cc:29 + cc], B_SCALE, None, ALU.mult, ALU.bypass, [cd], [sd])
        self.ts("dve", self.gsub, C[:, 30:31], 1.0 - LAM_INIT, None, ALU.mult, ALU.bypass, [cd], [sd])
        sm = self.sm
        self.tt("dve", sm[:, 0:64], R[:, 0:64], R[:, 64:128], ALU.mult, [rd], [smd])
        self.red(sm[:, 256:257], sm[:, 0:64], ALU.add, [smd], [smd])
        self.tt("dve", sm[:, 64:128], R[:, 128:192], R[:, 192:256], ALU.mult, [rd], [smd])
        self.red(sm[:, 257:258], sm[:, 64:128], ALU.add, [smd], [smd])
        self.act(sm[:, 258:260], sm[:, 256:258], AF.Exp, [smd], [smd])
        self.tt("dve", sm[:, 260:261], sm[:, 258:259], sm[:, 259:260], ALU.subtract, [smd], [smd])
        self.ts("dve", self.lamn, sm[:, 260:261], -1.0, -LAM_INIT, ALU.mult, ALU.add, [smd], [sd])
        self.tt("dve", sm[:, 0:64], R[:, 256:320], R[:, 320:384], ALU.mult, [rd, smd], [smd])
        self.red(sm[:, 261:262], sm[:, 0:64], ALU.max, [smd], [smd], absval=True)
        self.ts("dve", sm[:, 262:263], sm[:, 261:262], 64.0 * A_SCALE, None, ALU.mult, ALU.bypass, [smd], [smd])
        self.ts("dve", self.abias[:, 0:4], C[:, 31:35], sm[:, 262:263], None, ALU.subtract, ALU.bypass, [smd, cd], [sd])
        self.tt("dve", sm[:, 0:256], R[:, 384:640], R[:, 640:896], ALU.mult, [rd, smd], [smd])
        self.red(sm[:, 263:264], sm[:, 0:256], ALU.max, [smd], [smd], absval=True)
        self.ts("dve", sm[:, 264:265], sm[:, 263:264], 256.0 * B_SCALE, None, ALU.mult, ALU.bypass, [smd], [smd])
        self.ts("dve", self.abias[:, 4:8], C[:, 35:39], sm[:, 264:265], None, ALU.subtract, ALU.bypass, [smd, cd], [sd])
        st = self.stg[0].rearrange("p (h b c) -> p h b c", h=8, b=2)
        self.dma(st, self.bias, [], [self.stg_dep[0]])
        for hh in range(8):
            self.stt(self.biasbf[:, hh, 0, :], st[:, hh, 0, :], C[:, 31 + hh:32 + hh], self.maskT, ALU.subtract, ALU.add,
                     [self.stg_dep[0], cd, mtd], [sd])
            self.ts("dve", self.biasbf[:, hh, 1, :], st[:, hh, 1, :], C[:, 31 + hh:32 + hh], None, ALU.subtract, ALU.bypass,
                    [self.stg_dep[0], cd], [sd])
        s1 = self.stg[1].rearrange("p (h c) -> p h c", h=4)[:, :, 0:256]
        self.dma(s1, self.wuk, [], [self.stg_dep[1]])
        self.cp("pool", self.wukb, s1, [self.stg_dep[1]], [sd])
        s2 = self.stg[2][:, 0:1024].rearrange("p (a h c) -> p a h c", a=2, h=4)
        self.dma(s2, self.wuv, [], [self.stg_dep[2]])
        self.cp("pool", self.wuvb, s2, [self.stg_dep[2]], [sd])
        for k in range(NBIS + 1):
            self.memset("pool", self.pw[:, k:k + 1], 2.0 ** -(k + 1), [sd])
        self.memset("pool", self.thc, -1e29, [sd])

    def _sequence(self, s):
        self._load_h(s, from_x=True)
        self._norm_pass(0)
        self._ffn(0)
        if self.stage <= 1:
            self._store_out(s)
            return
        self._norm_pass(1)
        self.hs_dep = getattr(self, "hs_dep", None) or [Dep("hs%d" % t) for t in range(NT)]
        for t, (t0, n) in enumerate(TILES):
            self.dma(self.hs[t0:t0 + n, :], self.h[0:n, t, :], [self.h_dep[t]], [self.hs_dep[t]])
        self.P.barrier()
        self._attn_alloc()
        if not _os.environ.get("K_SKIP_PROJ"):
            self._proj()
        self.P.barrier()
        if self.stage >= 3:
            self._attnA()
            self.P.barrier()
        if self.stage >= 4:
            self._attnB()
            self.P.barrier()
        if self.dbg_stop:
            return
        if not _os.environ.get("K_NO_RELOAD"):
            self._load_h(s, from_x=False)
        if not _os.environ.get("K_NO_FFN2"):
            self._norm_pass(2)
            self._ffn(1)
        self._store_out(s)

    def _load_h(self, s, from_x):
        for t, (t0, n) in enumerate(TILES):
            hd = self.h_dep[t]
            if from_x:
                if t == 0:
                    self.dma(self.h[0:NMETA, 0, :], self.meta, [], [hd])
                    self.dma(self.h[NMETA:128, 0, :], self.x[s, 0:128 - NMETA, :], [], [hd])
                else:
                    self.dma(self.h[0:n, t, :], self.x[s, t0 - NMETA:t0 - NMETA + n, :], [], [hd])
            else:
                self.dma(self.h[0:n, t, :], self.hs[t0:t0 + n, :], [self.hs_dep[t]], [hd])

    def _store_out(self, s):
        for t, (t0, n) in enumerate(TILES):
            if t == 0:
                self.dma(self.out[s, 0:128 - NMETA, :], self.h[NMETA:128, 0, :], [self.h_dep[0]], [self.out_dep])
            else:
                self.dma(self.out[s, t0 - NMETA:t0 - NMETA + n, :], self.h[0:n, t, :], [self.h_dep[t]], [self.out_dep])

    def _norm_pass(self, which):
        for t, (t0, n) in enumerate(TILES):
            b = t % 2
            k, (ssd, td, rsd) = self.stat()
            ss = self.ss[0:n, k:k + 1]
            self.act(self.junk[0:n, :], self.h[0:n, t, :], AF.Square, [self.h_dep[t]], [self.junk_dep, ssd], accum=ss)
            self.ts("dve", self.tmp[0:n, k:k + 1], ss, 1.0 / D, EPS, ALU.mult, ALU.add, [ssd], [td])
            self.tt("pool", self.rs[0:n, k:k + 1], self.tmp[0:n, k:k + 1], self.neghalf[0:n, 0:1], ALU.pow,
                    [td, self.nh_dep], [rsd])
            self.ts("dve", self.xn_s[b][0:n, :], self.h[0:n, t, :], self.rs[0:n, k:k + 1], None, ALU.mult, ALU.bypass,
                    [self.h_dep[t], rsd], [self.xn_s_dep[b]])
            pb = self.psum[b].bitcast(BF16)
            for kk in range(8):
                self.tr(pb[:, kk * 128:kk * 128 + n], self.xn_s[b][0:n, kk * 128:(kk + 1) * 128],
                        self.ident[0:n, 0:n], [self.xn_s_dep[b], self.ident_dep], [self.pdep[b]])
            src = pb.rearrange("p (k c) -> p k c", k=8)[:, :, 0:n]
            self.P.add("act", (lambda e, o=self.xnT[:, :, t0:t0 + n], i=src: e.activation(o, i, AF.Copy)),
                       [self.pdep[b]], [self.xn_dep[t]])

    def _ffn_alloc(self):
        if hasattr(self, "ffn_alloced"):
            return
        self.ffn_alloced = True
        self.cur = self.phase_off
        sb = self.sb
        self.stg = [sb("stg%d" % i, [128, 2048], F32) for i in range(3)]
        self.stg_dep = [Dep("stg%d" % i) for i in range(3)]
        self.stg_i = 0
        self.wgb = [sb("wgb%d" % i, [128, 8, 256], BF16) for i in range(2)]
        self.wub = [sb("wub%d" % i, [128, 8, 256], BF16) for i in range(2)]
        self.wgb_dep = [Dep("wgb%d" % i) for i in range(2)]
        self.wub_dep = [Dep("wub%d" % i) for i in range(2)]
        self.wdb = [sb("wdb%d" % i, [128, 2, D], BF16) for i in range(4)]
        self.wdb_dep = [Dep("wdb%d" % i) for i in range(4)]
        self.actb = sb("actb", [128, 4, L], BF16)
        self.actb_dep = [[Dep("act%d_%d" % (f, g)) for g in range(len(TGS))] for f in range(4)]
        self.sg = [sb("sg%d" % i, [128, 512], BF16) for i in range(2)]
        self.sg_dep = [Dep("sg%d" % i) for i in range(2)]
        self.ffn_end = self.cur
        self.slab_ctr = 0
        self.gu_ctr = 0
        self.dn_ctr = 0

    def _stage_slot(self):
        i = self.stg_i
        self.stg_i = (i + 1) % 3
        return i

    def _ffn(self, which):
        self._ffn_alloc()
        wg, wu, wd = self.wg[which], self.wu[which], self.wd[which]
        gcol = {0: 0, 1: 16}[which]
        gain = self.cols_sb[:, gcol:gcol + 8]
        slabs = list(range(11))
        groups = [slabs[i:i + 2] for i in range(0, 11, 2)]
        for grp in groups:
            for li, sl in enumerate(grp):
                c0 = sl * 256
                sw = self.slab_ctr % 2
                dslot = self.slab_ctr % 4
                self.slab_ctr += 1
                for (src, dst, ddep) in ((wg, self.wgb[sw], self.wgb_dep[sw]), (wu, self.wub[sw], self.wub_dep[sw])):
                    si = self._stage_slot()
                    st3 = self.stg[si].rearrange("p (k c) -> p k c", k=8)
                    self.dma(st3, src[:, c0:c0 + 256].rearrange("(k p) c -> p k c", p=128), [], [self.stg_dep[si]])
                    self.tt("pool", dst, st3, gain.unsqueeze(2).to_broadcast([128, 8, 256]), ALU.mult,
                            [self.stg_dep[si], self.cdep], [ddep])
                si = self._stage_slot()
                st3 = self.stg[si].rearrange("p (k c) -> p k c", k=2)
                self.dma(st3, wd[c0:c0 + 256, :].rearrange("(k p) c -> p k c", p=128), [], [self.stg_dep[si]])
                self.cp("pool", self.wdb[dslot], st3, [self.stg_dep[si]], [self.wdb_dep[dslot]])
                grp_dslot = dslot
                for c in range(2):
                    fl = li * 2 + c
                    for g, (g0, gn) in enumerate(TGS):
                        pbuf = self.gu_ctr % 2
                        self.gu_ctr += 1
                        pg, pu = 2 + 2 * pbuf, 3 + 2 * pbuf
                        xdeps = [self.xn_dep[t] for t in range(NT) if TILES[t][0] >= g0 and TILES[t][0] < g0 + gn]
                        for k in range(8):
                            self.mm(self.psum[pg][:, 0:gn], self.wgb[sw][:, k, c * 128:(c + 1) * 128],
                                    self.xnT[:, k, g0:g0 + gn], k == 0, k == 7,
                                    [self.wgb_dep[sw]] + xdeps, [self.pdep[pg]])
                        for k in range(8):
                            self.mm(self.psum[pu][:, 0:gn], self.wub[sw][:, k, c * 128:(c + 1) * 128],
                                    self.xnT[:, k, g0:g0 + gn], k == 0, k == 7,
                                    [self.wub_dep[sw]] + xdeps, [self.pdep[pu]])
                        sgi = pbuf
                        self.act(self.sg[sgi][:, 0:gn], self.psum[pg][:, 0:gn], AF.Silu, [self.pdep[pg]], [self.sg_dep[sgi]])
                        self.tt("dve", self.actb[:, fl, g0:g0 + gn], self.sg[sgi][:, 0:gn], self.psum[pu][:, 0:gn], ALU.mult,
                                [self.sg_dep[sgi], self.pdep[pu]], [self.actb_dep[fl][g]])
            nfl = len(grp) * 2
            first_dslot = (self.slab_ctr - len(grp)) % 4
            for t, (t0, n) in enumerate(TILES):
                g = min(t // 4, 4)
                for half in range(2):
                    pd = 6 + (self.dn_ctr % 2)
                    self.dn_ctr += 1
                    for fl in range(nfl):
                        dslot = (first_dslot + fl // 2) % 4
                        self.mm(self.psum[pd][0:n, :], self.actb[:, fl, t0:t0 + n],
                                self.wdb[dslot][:, fl % 2, half * 512:(half + 1) * 512], fl == 0, fl == nfl - 1,
                                [self.actb_dep[fl][g], self.wdb_dep[dslot]], [self.pdep[pd]])
                    hv = self.h[0:n, t, half * 512:(half + 1) * 512]
                    self.stt(hv, self.psum[pd][0:n, :], 0.5, hv, ALU.mult, ALU.add,
                             [self.pdep[pd], self.h_dep[t]], [self.h_dep[t]])


    def _attn_alloc(self):
        if hasattr(self, "attn_alloced"):
            return
        self.attn_alloced = True
        sb = self.sb
        save = self.cur
        self.cur = self.h_off
        r1 = self.cur
        self.qT = sb("qT", [128, 4, L], BF16)
        self.kT = sb("kT", [128, 4, L], BF16)
        self.VA = sb("VA", [128, NT, 4, 129], BF16)
        r1_end = self.cur
        self.cur = r1
        self.qlT = sb("qlT", [128, 4, 2, 512], BF16)
        self.rr = [sb("rr%d" % i, [128, 512], F32) for i in range(2)]
        self.qn1k = sb("qn1k", [128, 1024], BF16)
        self.ocT = sb("ocT", [128, 8, 128], BF16)
        self.woutb = sb("woutb", [128, 8, D], BF16)
        self.hst2 = sb("hst2", [128, 2, D], F32)
        self.hst = [self.hst2[:, 0, :], self.hst2[:, 1, :]]
        self.obG = sb("obG", [128, 4, 512], BF16)
        self.cntj = sb("cntj", [128, L], BF16)
        assert self.cur <= r1_end, (self.cur, r1_end)
        self.cur = r1_end
        self.qbT = sb("qbT", [128, 4, L], BF16)
        self.cT = sb("cT", [128, 2, L], BF16)
        self.VB = sb("VB", [128, NT, 4, 129], BF16)
        self.iqT = sb("iqT", [128, 4, L], BF16)
        self.ikT = sb("ikT", [128, L], BF16)
        r3 = self.cur
        self.wst = [sb("wst%d" % i, [128, 8, 384], F32) for i in range(2)]
        self.wb = [sb("wb%d" % i, [128, 8, 384], BF16) for i in range(2)]
        r3_end = self.cur
        self.cur = r3
        self.oa = sb("oa", [128, NT, 512], BF16)
        self.PT = [sb("PT%d" % i, [128, 512], BF16) for i in range(4)]
        self.t1 = [sb("t1_%d" % i, [128, 128], F32) for i in range(2)]
        self.ov = [sb("ov_%d" % i, [128, 128], F32) for i in range(2)]
        self.bst = sb("bst", [128, 4 * NBIS + 8], F32)
        assert self.cur <= r3_end, (self.cur, r3_end)
        self.cur = max(r3_end, save)
        self.sq = [sb("sq%d" % i, [128, 256], F32) for i in range(2)]
        self.qn = [sb("qn%d" % i, [128, 256], BF16) for i in range(2)]
        save2 = self.cur
        self.cur = self.c_const
        self.acc = sb("acc", [128, L], F32)
        self.maskb = sb("maskb", [128, L], BF16)
        self.mT = sb("mT", [128, NT, 512], BF16)
        assert self.cur <= self.h_off, (self.cur, self.h_off)
        self.cur = save2
        D_ = Dep
        self.qT_dep = [[D_("qT%d_%d" % (h, t)) for t in range(NT)] for h in range(4)]
        self.kT_dep = [[D_("kT%d_%d" % (h, t)) for t in range(NT)] for h in range(4)]
        self.VA_dep = [D_("VA%d" % t) for t in range(NT)]
        self.VB_dep = [D_("VB%d" % t) for t in range(NT)]
        self.cT_dep = [D_("cT%d" % t) for t in range(NT)]
        self.qbT_dep = [D_("qbT%d" % g) for g in range(5)]
        self.iqT_dep = [D_("iqT%d" % g) for g in range(5)]
        self.ikT_dep = [D_("ikT%d" % g) for g in range(5)]
        self.iw_dep = [D_("iw%d" % t) for t in range(NT)]
        self.wst_dep = [D_("wst%d" % i) for i in range(2)]
        self.wb_dep = [D_("wb%d" % i) for i in range(2)]
        self.sq_dep = [D_("sq%d" % i) for i in range(2)]
        self.qn_dep = [D_("qn%d" % i) for i in range(2)]
        self.oa_dep = [D_("oa%d" % t) for t in range(NT)]
        self.PT_dep = [D_("PT%d" % i) for i in range(4)]
        self.t1_dep = [D_("t1_%d" % i) for i in range(2)]
        self.ov_dep = [D_("ov_%d" % i) for i in range(2)]
        self.qlT_dep = [D_("qlT%d" % i) for i in range(4)]
        self.rr_dep = [D_("rr%d" % i) for i in range(2)]
        self.qn1k_dep = D_("qn1k")
        self.ocT_dep = D_("ocT")
        self.woutb_dep = D_("woutb")
        self.hst_dep = [D_("hst%d" % i) for i in range(2)]
        self.obG_dep = [D_("obG%d" % i) for i in range(4)]
        self.cntj_dep = D_("cntj")
        self.acc_dep = D_("acc")
        self.maskb_dep = D_("maskb")
        self.mT_dep = [D_("mT%d" % i) for i in range(4)]
        self.bst_dep = D_("bst")
        self.ctr = 0

    def _load_slab(self, c0, ncol, gain):
        slot = self.ctr % 2
        self.ctr += 1
        st = self.wst[slot][:, :, 0:ncol]
        self.dma(st, self.win[:, c0:c0 + ncol].rearrange("(k p) c -> p k c", p=128), [], [self.wst_dep[slot]])
        self.tt("pool", self.wb[slot][:, :, 0:ncol], st, gain.unsqueeze(2).to_broadcast([128, 8, ncol]), ALU.mult,
                [self.wst_dep[slot], self.cdep], [self.wb_dep[slot]])
        return slot

    def _proj(self):
        gain = self.cols_sb[:, 8:16]
        C = self.cols_sb
        parts = _os.environ.get("K_PROJ_PARTS", "ms,A,CK,FM").split(",")
        if "ms" in parts:
            self.memset("pool", self.VA[:, :, :, 128:129], 1.0, self.VA_dep)
            self.memset("pool", self.VB[:, :, :, 128:129], 1.0, self.VB_dep)
        tctr = 0
        for sl in range(5):
            if (sl < 4 and "A" not in parts) or (sl == 4 and "CK" not in parts):
                continue
            ncol = 384 if sl < 4 else 264
            slot = self._load_slab(sl * 384, ncol, gain)
            for t, (t0, n) in enumerate(TILES):
                pb = 2 + (tctr % 2)
                tb = tctr % 2
                b = tctr % 2
                tctr += 1
                ps = self.psum[pb]
                for k in range(8):
                    self.mm(ps[0:n, 0:ncol], self.xnT[:, k, t0:t0 + n], self.wb[slot][:, k, 0:ncol], k == 0, k == 7,
                            [self.xn_dep[t], self.wb_dep[slot]], [self.pdep[pb]])
                pT = self.psum[tb].bitcast(BF16)
                k4, sdeps = self.stat()
                ssd, td, rsd = sdeps
                if sl < 4:
                    h = sl
                    KA = _os.environ.get("K_A", "sq,red,rstd,qn,tr,evq,evk,va").split(",")
                    if "sq" in KA:
                        self.act(self.sq[b][0:n, :], ps[0:n, 0:256], AF.Square, [self.pdep[pb]], [self.sq_dep[b]])
                    if "red" in KA:
                        self.red(self.ss[0:n, k4:k4 + 4], self.sq[b][0:n, :].rearrange("p (g d) -> p g d", d=64), ALU.add,
                                 [self.sq_dep[b]], [ssd])
                    if "rstd" in KA:
                        self.rstd(n, k4, 4, 1.0 / 64, sdeps)
                    if "qn" in KA:
                        self.tt("dve", self.qn[b][0:n, :].rearrange("p (g d) -> p g d", d=64),
                                ps[0:n, 0:256].rearrange("p (g d) -> p g d", d=64),
                                self.rs[0:n, k4:k4 + 4].unsqueeze(2).to_broadcast([n, 4, 64]), ALU.mult,
                                [self.pdep[pb], rsd], [self.qn_dep[b]])
                    if "tr" in KA:
                        self.tr(pT[:, 0:n], self.qn[b][0:n, 0:128], self.ident[0:n, 0:n], [self.qn_dep[b], self.ident_dep],
                                [self.pdep[tb]])
                        self.tr(pT[:, 128:128 + n], self.qn[b][0:n, 128:256], self.ident[0:n, 0:n], [self.qn_dep[b], self.ident_dep],
                                [self.pdep[tb]])
                    if "evq" in KA:
                        qdst = self.junk[:, 0:n] if _os.environ.get("K_DEST") else self.qT[:, h, t0:t0 + n]
                        self.act(qdst, pT[:, 0:n], AF.Copy, [self.pdep[tb], self.setup_dep],
                                 [self.qT_dep[h][t]], scale=self.gqk)
                    if "evk" in KA:
                        kdst = self.junk[:, 128:128 + n] if _os.environ.get("K_DEST") else self.kT[:, h, t0:t0 + n]
                        self.act(kdst, pT[:, 128:128 + n], AF.Copy, [self.pdep[tb]], [self.kT_dep[h][t]])
                    if "va" in KA:
                        self.act(self.VA[0:n, t, h, 0:128], ps[0:n, 256:384], AF.Copy, [self.pdep[pb]], [self.VA_dep[t]])
                else:
                    self.act(self.sq[b][0:n, :], ps[0:n, 0:256], AF.Square, [self.pdep[pb]], [self.sq_dep[b], ssd],
                             accum=self.ss[0:n, k4:k4 + 1])
                    self.rstd(n, k4, 1, 1.0 / 256, sdeps)
                    self.ts("dve", self.qn[b][0:n, :], ps[0:n, 0:256], self.rs[0:n, k4:k4 + 1], None, ALU.mult, ALU.bypass,
                            [self.pdep[pb], rsd], [self.qn_dep[b]])
                    self.ts("dve", self.iw_sb[0:n, t, :], ps[0:n, 256:264], (64 ** -0.5) * (8 ** -0.5), None, ALU.mult, ALU.bypass,
                            [self.pdep[pb]], [self.iw_dep[t]])
                    for cc in range(2):
                        self.tr(pT[:, cc * 128:cc * 128 + n], self.qn[b][0:n, cc * 128:(cc + 1) * 128], self.ident[0:n, 0:n],
                                [self.qn_dep[b], self.ident_dep], [self.pdep[tb]])
                    self.ts("dve", self.cT[:, 0, t0:t0 + n], pT[:, 0:n], C[:, 26:27], None, ALU.mult, ALU.bypass,
                            [self.pdep[tb], self.cdep], [self.cT_dep[t]])
                    self.act(self.cT[:, 1, t0:t0 + n], pT[:, 128:128 + n], AF.Copy, [self.pdep[tb], self.cdep], [self.cT_dep[t]],
                             scale=C[:, 27:28])
                    vb = 6 + (t % 2)
                    for cc in range(2):
                        self.mm(self.psum[vb][0:n, :], self.cT[:, cc, t0:t0 + n],
                                self.wuvb[:, cc, :, :].rearrange("p h e -> p (h e)"), cc == 0, cc == 1,
                                [self.cT_dep[t], self.setup_dep], [self.pdep[vb]])
                    self.act(self.VB[0:n, t, :, 0:128], self.psum[vb][0:n, :].rearrange("p (h e) -> p h e", h=4), AF.Copy,
                             [self.pdep[vb]], [self.VB_dep[t]])
        ectr = 0
        for sl in range(3):
            if "FM" not in parts:
                continue
            slot = self._load_slab(1800 + sl * 384, 384, gain)
            for c in range(3):
                ch = sl * 3 + c
                for g, (g0, gn) in enumerate(TGS):
                    if ch < 4:
                        dst, ddep = self.qbT[:, ch, g0:g0 + gn], self.qbT_dep[g]
                    elif ch < 8:
                        dst, ddep = self.iqT[:, ch - 4, g0:g0 + gn], self.iqT_dep[g]
                    else:
                        dst, ddep = self.ikT[:, g0:g0 + gn], self.ikT_dep[g]
                    pb = 4 + (ectr % 2)
                    xdeps = [self.xn_dep[t] for t in range(NT) if g0 <= TILES[t][0] < g0 + gn]
                    for k in range(8):
                        self.mm(self.psum[pb][:, 0:gn], self.wb[slot][:, k, c * 128:(c + 1) * 128], self.xnT[:, k, g0:g0 + gn],
                                k == 0, k == 7, [self.wb_dep[slot]] + xdeps, [self.pdep[pb]])
                    if ectr % 2 == 0:
                        self.act(dst, self.psum[pb][:, 0:gn], AF.Copy, [self.pdep[pb]], [ddep])
                    else:
                        self.cp("dve", dst, self.psum[pb][:, 0:gn], [self.pdep[pb]], [ddep])
                    ectr += 1

    def _bias_blocks(self, ps, pbank, hh, j, tiles, c0):
        k0, kn = TILES[j]
        for blk, i in ((0, j), (1, j + 1)):
            if i in tiles:
                off = TILES[i][0] - c0
                ni = TILES[i][1]
                self.mm(ps[:, off:off + ni], self.ident[0:kn, 0:kn], self.biasbf[0:kn, hh, blk, 0:ni], False, True,
                        [self.ident_dep, self.setup_dep], [self.pdep[pbank]])

    def _attnA(self):
        sctr = 0
        for G, tiles in enumerate(QGS):
            q0 = TILES[tiles[0]][0]
            qn_ = sum(TILES[t][1] for t in tiles)
            for h in range(4):
                started = set()
                for j in range(tiles[-1] + 1):
                    k0, kn = TILES[j]
                    c0 = max(q0, k0)
                    ncol = q0 + qn_ - c0
                    qdeps = [self.qT_dep[h][t] for t in tiles if TILES[t][0] + TILES[t][1] > c0]
                    for m in range(2):
                        bank = (sctr % 2) * 2 + m
                        pbuf = (sctr % 2) * 2 + m
                        ps = self.psum[bank][0:kn, 0:ncol]
                        self.mm(ps, self.kT[m * 64:(m + 1) * 64, h, k0:k0 + kn], self.qT[m * 64:(m + 1) * 64, h, c0:c0 + ncol],
                                True, True, [self.kT_dep[h][j]] + qdeps, [self.pdep[bank]])
                        self._bias_blocks(ps, bank, h, j, tiles, c0)
                        self.act(self.PT[pbuf][0:kn, 0:ncol], ps, AF.Exp, [self.pdep[bank], self.setup_dep], [self.PT_dep[pbuf]],
                                 bias=self.abias[0:kn, h:h + 1])
                        for il, i in enumerate(tiles):
                            if i < j:
                                continue
                            off = TILES[i][0] - c0
                            ni = TILES[i][1]
                            slot = m * 4 + il
                            ab = 4 + slot // 3
                            o = (slot % 3) * 129
                            first = ab not in started
                            started.add(ab)
                            self.mm(self.psum[ab][0:ni, o:o + 129], self.PT[pbuf][0:kn, off:off + ni], self.VA[0:kn, j, h, :],
                                    first, j == i, [self.PT_dep[pbuf], self.VA_dep[j]], [self.pdep[ab]])
                    sctr += 1
                for il, i in enumerate(tiles):
                    ni = TILES[i][1]
                    b = il % 2
                    s0, s1 = il, 4 + il
                    b0, o0 = 4 + s0 // 3, (s0 % 3) * 129
                    b1, o1 = 4 + s1 // 3, (s1 % 3) * 129
                    k4, sdeps = self.stat()
                    ssd, td, rsd = sdeps
                    self.recip(self.rs[0:ni, k4 + 1:k4 + 2], self.psum[b0][0:ni, o0 + 128:o0 + 129], [self.pdep[b0]], [rsd])
                    self.recip(self.rs[0:ni, k4 + 2:k4 + 3], self.psum[b1][0:ni, o1 + 128:o1 + 129], [self.pdep[b1]], [rsd])
                    self.ts("dve", self.rs[0:ni, k4 + 3:k4 + 4], self.rs[0:ni, k4 + 2:k4 + 3], self.lamn[0:ni, 0:1], None,
                            ALU.mult, ALU.bypass, [rsd, self.setup_dep], [rsd])
                    self.ts("dve", self.t1[b][0:ni, :], self.psum[b1][0:ni, o1:o1 + 128], self.rs[0:ni, k4 + 3:k4 + 4], None,
                            ALU.mult, ALU.bypass, [self.pdep[b1], rsd], [self.t1_dep[b]])
                    self.stt(self.ov[b][0:ni, :], self.psum[b0][0:ni, o0:o0 + 128], self.rs[0:ni, k4 + 1:k4 + 2],
                             self.t1[b][0:ni, :], ALU.mult, ALU.add, [self.pdep[b0], rsd, self.t1_dep[b]], [self.ov_dep[b]])
                    self.act(self.junk[0:ni, 0:128], self.ov[b][0:ni, :], AF.Square, [self.ov_dep[b]], [self.junk_dep, ssd],
                             accum=self.ss[0:ni, k4:k4 + 1])
                    self.rstd(ni, k4, 1, 1.0 / 128, sdeps)
                    self.ts("dve", self.oa[0:ni, i, h * 128:(h + 1) * 128], self.ov[b][0:ni, :], self.rs[0:ni, k4:k4 + 1], None,
                            ALU.mult, ALU.bypass, [self.ov_dep[b], rsd], [self.oa_dep[i]])

    def _attnB(self):
        C = self.cols_sb
        for q in range(4):
            st = self.hst2
            hd = self.hst_dep
            self.dma(st, self.wout[q * 256:(q + 1) * 256, :].rearrange("(k p) c -> p k c", p=128), [hd[1]], [hd[0]])
            if q < 2:
                self.ts("pool", self.woutb[:, 2 * q:2 * q + 2, :], st, self.gsub[:, 0:1], None, ALU.mult, ALU.bypass,
                        [hd[0], hd[1], self.setup_dep], [self.woutb_dep])
            else:
                self.cp("pool", self.woutb[:, 2 * q:2 * q + 2, :], st, [hd[0], hd[1]], [self.woutb_dep])
        dctr = 0
        sctr = 0
        hctr = 0
        for G, tiles in enumerate(QGS):
            q0 = TILES[tiles[0]][0]
            qn_ = sum(TILES[t][1] for t in tiles)
            for il, i in enumerate(tiles):
                t0, ni = TILES[i]
                for h in range(4):
                    pbk = h // 2
                    self.mm(self.psum[pbk][0:ni, (h % 2) * 256:(h % 2) * 256 + 256], self.qbT[:, h, t0:t0 + ni], self.wukb[:, h, :],
                            True, True, [self.qbT_dep[G], self.setup_dep], [self.pdep[pbk]])
                k4, sdeps = self.stat()
                ssd, td, rsd = sdeps
                for pbk in range(2):
                    self.act(self.junk[0:ni, pbk * 512:(pbk + 1) * 512], self.psum[pbk][0:ni, :], AF.Square, [self.pdep[pbk]],
                             [self.junk_dep])
                self.red(self.ss[0:ni, k4:k4 + 4], self.junk[0:ni, :].rearrange("p (g d) -> p g d", d=256), ALU.add,
                         [self.junk_dep], [ssd])
                self.rstd(ni, k4, 4, 1.0 / 256, sdeps)
                for pbk in range(2):
                    self.tt("dve", self.qn1k[0:ni, pbk * 512:(pbk + 1) * 512].rearrange("p (g d) -> p g d", d=256),
                            self.psum[pbk][0:ni, :].rearrange("p (g d) -> p g d", d=256),
                            self.rs[0:ni, k4 + 2 * pbk:k4 + 2 * pbk + 2].unsqueeze(2).to_broadcast([ni, 2, 256]), ALU.mult,
                            [self.pdep[pbk], rsd], [self.qn1k_dep])
                pT = self.psum[2].bitcast(BF16)
                for ch in range(8):
                    self.tr(pT[:, ch * 128:ch * 128 + ni], self.qn1k[0:ni, ch * 128:(ch + 1) * 128], self.ident[0:ni, 0:ni],
                            [self.qn1k_dep, self.ident_dep], [self.pdep[2]])
                pv = pT.rearrange("p (h c t) -> p h c t", h=4, c=2)
                self.ts("dve", self.qlT[:, :, 0, il * 128:il * 128 + ni], pv[:, :, 0, 0:ni], self.gqb[0], None, ALU.mult,
                        ALU.bypass, [self.pdep[2], self.setup_dep], [self.qlT_dep[il]])
                self.ts("dve", self.qlT[:, :, 1, il * 128:il * 128 + ni], pv[:, :, 1, 0:ni], self.gqb[1], None, ALU.mult,
                        ALU.bypass, [self.pdep[2], self.setup_dep], [self.qlT_dep[il]])
            for il, i in enumerate(tiles):
                t0, ni = TILES[i]
                Lk = t0 + ni
                acc = self.acc
                for kc0 in range(0, Lk, 512):
                    kcn = min(512, Lk - kc0)
                    g = kc0 // 512
                    for hh in range(8):
                        pb = 3 + (dctr % 2)
                        rb = dctr % 2
                        dctr += 1
                        pr = (hh % 2) * 64
                        self.mm(self.psum[pb][0:ni, 0:kcn], self.iqT[pr:pr + 64, hh // 2, t0:t0 + ni], self.ikT[pr:pr + 64, kc0:kc0 + kcn],
                                True, True, [self.iqT_dep[G], self.ikT_dep[g]], [self.pdep[pb]])
                        self.act(self.rr[rb][0:ni, 0:kcn], self.psum[pb][0:ni, 0:kcn], AF.Relu, [self.pdep[pb]], [self.rr_dep[rb]])
                        if hh == 0:
                            self.ts("dve", acc[0:ni, kc0:kc0 + kcn], self.rr[rb][0:ni, 0:kcn], self.iw_sb[0:ni, i, 0:1], None,
                                    ALU.mult, ALU.bypass, [self.rr_dep[rb], self.iw_dep[i]], [self.acc_dep])
                        else:
                            self.stt(acc[0:ni, kc0:kc0 + kcn], self.rr[rb][0:ni, 0:kcn], self.iw_sb[0:ni, i, hh:hh + 1],
                                     acc[0:ni, kc0:kc0 + kcn], ALU.mult, ALU.add, [self.rr_dep[rb], self.iw_dep[i], self.acc_dep],
                                     [self.acc_dep])
                bs = self.bst
                bd = self.bst_dep
                if i >= 2:
                    self.red(bs[0:ni, 0:1], acc[0:ni, 0:Lk], ALU.max, [self.acc_dep], [bd])
                    self.red(bs[0:ni, 1:2], acc[0:ni, 0:Lk], ALU.min, [self.acc_dep], [bd])
                self.tt("dve", acc[0:ni, t0:t0 + ni], acc[0:ni, t0:t0 + ni], self.masktok[0:ni, 0:ni], ALU.add,
                        [self.acc_dep, self.masktok_dep], [self.acc_dep])
                if i >= 2:
                    self.tt("dve", bs[0:ni, 2:3], bs[0:ni, 0:1], bs[0:ni, 1:2], ALU.subtract, [bd], [bd])
                    W0 = 8
                    self.ts("dve", bs[0:ni, W0:W0 + NBIS + 1], self.pw[0:ni, :], bs[0:ni, 2:3], None, ALU.mult, ALU.bypass,
                            [bd, self.setup_dep], [bd])
                    M0 = W0 + NBIS + 1
                    self.tt("dve", bs[0:ni, M0:M0 + 1], bs[0:ni, 1:2], bs[0:ni, W0:W0 + 1], ALU.add, [bd], [bd])
                    C0 = M0 + NBIS + 1
                    for k in range(NBIS):
                        self.ts("dve", self.cntj[0:ni, 0:Lk], acc[0:ni, 0:Lk], bs[0:ni, M0 + k:M0 + k + 1], None, ALU.is_ge, ALU.add,
                                [self.acc_dep, bd], [self.cntj_dep, bd], accum=bs[0:ni, C0 + k:C0 + k + 1])
                        self.ts("dve", bs[0:ni, 3:4], bs[0:ni, C0 + k:C0 + k + 1], TOPK - 0.5, bs[0:ni, W0 + k:W0 + k + 1],
                                ALU.is_ge, ALU.mult, [bd], [bd])
                        wn = W0 + k + 1 if k < NBIS - 1 else W0 + k
                        self.stt(bs[0:ni, M0 + k + 1:M0 + k + 2], bs[0:ni, 3:4], bs[0:ni, wn:wn + 1], bs[0:ni, M0 + k:M0 + k + 1],
                                 ALU.subtract, ALU.add, [bd], [bd])
                    theta = bs[0:ni, M0 + NBIS:M0 + NBIS + 1]
                    thd = [bd]
                else:
                    theta = self.thc[0:ni, 0:1]
                    thd = [self.setup_dep]
                self.ts("dve", self.maskb[0:ni, 0:Lk], acc[0:ni, 0:Lk], theta, NEG, ALU.is_lt, ALU.mult, [self.acc_dep] + thd,
                        [self.maskb_dep])
                for j0 in range(0, i + 1, 8):
                    js = list(range(j0, min(j0 + 8, i + 1)))
                    tb = 5 + ((j0 // 8) % 2)
                    pT = self.psum[tb].bitcast(BF16)
                    for jj, j in enumerate(js):
                        k0, kn = TILES[j]
                        self.tr(pT[0:kn, jj * 128:jj * 128 + ni], self.maskb[0:ni, k0:k0 + kn], self.ident[0:ni, 0:ni],
                                [self.maskb_dep, self.ident_dep], [self.pdep[tb]])
                    src = pT.rearrange("p (j t) -> p j t", j=8)[:, 0:len(js), 0:ni]
                    self.cp("dve", self.mT[:, j0:j0 + len(js), il * 128:il * 128 + ni], src, [self.pdep[tb]], [self.mT_dep[il]])
            for h in range(4):
                started = set()
                for j in range(tiles[-1] + 1):
                    k0, kn = TILES[j]
                    c0 = max(q0, k0)
                    ncol = q0 + qn_ - c0
                    co = c0 - q0
                    bank = sctr % 2
                    pbuf = sctr % 2
                    sctr += 1
                    ps = self.psum[bank][0:kn, 0:ncol]
                    qd = [self.qlT_dep[il] for il, t in enumerate(tiles) if TILES[t][0] + TILES[t][1] > c0]
                    md = [self.mT_dep[il] for il, t in enumerate(tiles) if TILES[t][0] + TILES[t][1] > c0]
                    for cc in range(2):
                        self.mm(ps, self.cT[:, cc, k0:k0 + kn], self.qlT[:, h, cc, co:co + ncol], cc == 0, False,
                                [self.cT_dep[j]] + qd, [self.pdep[bank]])
                    self.mm(ps, self.ident[0:kn, 0:kn], self.mT[0:kn, j, co:co + ncol], False, True, [self.ident_dep] + md,
                            [self.pdep[bank]])
                    self._bias_blocks(ps, bank, 4 + h, j, tiles, c0)
                    self.act(self.PT[pbuf][0:kn, 0:ncol], ps, AF.Exp, [self.pdep[bank], self.setup_dep], [self.PT_dep[pbuf]],
                             bias=self.abias[0:kn, 4 + h:5 + h])
                    for il, i in enumerate(tiles):
                        if i < j:
                            continue
                        off = TILES[i][0] - c0
                        ni = TILES[i][1]
                        ab = 6 + il // 3
                        o = (il % 3) * 129
                        first = ab not in started
                        started.add(ab)
                        self.mm(self.psum[ab][0:ni, o:o + 129], self.PT[pbuf][0:kn, off:off + ni], self.VB[0:kn, j, h, :],
                                first, j == i, [self.PT_dep[pbuf], self.VB_dep[j]], [self.pdep[ab]])
                for il, i in enumerate(tiles):
                    ni = TILES[i][1]
                    ab = 6 + il // 3
                    o = (il % 3) * 129
                    k4, sdeps = self.stat()
                    ssd, td, rsd = sdeps
                    self.recip(self.rs[0:ni, k4:k4 + 1], self.psum[ab][0:ni, o + 128:o + 129], [self.pdep[ab]], [rsd])
                    self.ts("dve", self.obG[0:ni, il, h * 128:(h + 1) * 128], self.psum[ab][0:ni, o:o + 128], self.rs[0:ni, k4:k4 + 1],
                            None, ALU.mult, ALU.bypass, [self.pdep[ab], rsd], [self.obG_dep[il]])
            for il, i in enumerate(tiles):
                t0, ni = TILES[i]
                pT = self.psum[2].bitcast(BF16)
                for k in range(8):
                    src = self.oa[0:ni, i, k * 128:(k + 1) * 128] if k < 4 else self.obG[0:ni, il, (k - 4) * 128:(k - 3) * 128]
                    sd_ = self.oa_dep[i] if k < 4 else self.obG_dep[il]
                    self.tr(pT[:, k * 128:k * 128 + ni], src, self.ident[0:ni, 0:ni], [sd_, self.ident_dep], [self.pdep[2]])
                self.act(self.ocT[:, :, 0:ni], pT.rearrange("p (k t) -> p k t", k=8)[:, :, 0:ni], AF.Copy, [self.pdep[2]],
                         [self.ocT_dep])
                hb = hctr % 2
                hctr += 1
                self.dma(self.hst[hb][0:ni, :], self.hs[t0:t0 + ni, :], [self.hs_dep[i]], [self.hst_dep[hb]])
                for half in range(2):
                    pb = 3 + half
                    for k in range(8):
                        self.mm(self.psum[pb][0:ni, :], self.ocT[:, k, 0:ni], self.woutb[:, k, half * 512:(half + 1) * 512],
                                k == 0, k == 7, [self.ocT_dep, self.woutb_dep], [self.pdep[pb]])
                    hv = self.hst[hb][0:ni, half * 512:(half + 1) * 512]
                    self.tt("dve", hv, self.psum[pb][0:ni, :], hv, ALU.add, [self.pdep[pb], self.hst_dep[hb]], [self.hst_dep[hb]])
                self.dma(self.hs[t0:t0 + ni, :], self.hst[hb][0:ni, :], [self.hst_dep[hb]], [self.hs_dep[i]])

    def _finish(self):
        if _os.environ.get("K_DUMP"):
            dd = Dep("dump")
            self.P.barrier()
            self.dma(self.hs[0:128, 0:16], self.smallc[:, 0:16], [self.setup_dep], [dd])
            self.dma(self.hs[0:128, 16:24], self.abias, [self.setup_dep], [dd])
            self.P.add("sp", None, [dd], [])
        self.P.add("sp", None, [self.out_dep], [])
        if self.dbg is not None:
            pass


def _bucket(n):
    n = np.maximum(n, 0)
    nf = np.maximum(n, 1).astype(np.float32)
    large = 16 + (np.log(nf / np.float32(16)) / np.float32(math.log(128 / 16)) * np.float32(16)).astype(np.int32)
    large = np.minimum(large, 31)
    return np.where(n < 16, n, large)


def _prep_shared(inp):
    f = lambda a: np.ascontiguousarray(np.asarray(a, dtype=np.float32))
    sh = {}
    sh["meta"] = f(inp["meta_tokens"])
    for i, nm in ((1, "ffn1"), (2, "ffn2")):
        sh["w%dg" % i] = f(inp[nm + "_w_gate"][0])
        sh["w%du" % i] = f(inp[nm + "_w_up"][0])
        sh["w%dd" % i] = f(inp[nm + "_w_down"][0])
    w_in = f(inp["w_in"][0])
    qa, ka, va, qb, ckv, iq, ik, iw = np.split(w_in, np.cumsum([512, 512, 512, 512, 256, 512, 64, 8])[:-1], axis=1)
    parts = []
    for h in range(4):
        parts += [qa[:, h * 128:(h + 1) * 128], ka[:, h * 128:(h + 1) * 128], va[:, h * 128:(h + 1) * 128]]
    parts += [ckv, iw]
    parts += [qb, iq, ik, ik]
    sh["win"] = np.ascontiguousarray(np.concatenate(parts, axis=1))
    assert sh["win"].shape[1] == WIN_COLS
    sh["wuk"] = np.ascontiguousarray(f(inp["b_w_uk"][0]).transpose(1, 0, 2))
    sh["wuv"] = np.ascontiguousarray(f(inp["b_w_uv"][0]).reshape(4, 2, 128, 128).transpose(2, 1, 0, 3))
    sh["wout"] = f(inp["w_out"][0])
    cols = np.zeros((128, 40), np.float32)
    cols[:, 0:8] = f(inp["ffn1_norm"][0]).reshape(8, 128).T
    cols[:, 8:16] = f(inp["mix_norm"][0]).reshape(8, 128).T
    cols[:, 16:24] = f(inp["ffn2_norm"][0]).reshape(8, 128).T
    cols[:, 24] = np.tile(f(inp["a_q_norm"][0]), 2)
    cols[:, 25] = np.tile(f(inp["a_k_norm"][0]), 2)
    cols[:, 26:28] = f(inp["b_kv_norm"][0]).reshape(2, 128).T
    cols[:, 28:30] = f(inp["b_q_norm"][0]).reshape(2, 128).T
    cols[:, 30] = f(inp["a_subln"][0])
    rb = f(inp["rel_bias"])
    cols[:, 31:39] = np.broadcast_to(rb[31], (128, 8))
    sh["cols"] = cols
    rows = np.zeros((128, 896), np.float32)
    rows[:, 0:64] = f(inp["a_lambda_q1"][0])[None]
    rows[:, 64:128] = f(inp["a_lambda_k1"][0])[None]
    rows[:, 128:192] = f(inp["a_lambda_q2"][0])[None]
    rows[:, 192:256] = f(inp["a_lambda_k2"][0])[None]
    rows[:, 256:320] = f(inp["a_q_norm"][0])[None]
    rows[:, 320:384] = f(inp["a_k_norm"][0])[None]
    rows[:, 384:640] = f(inp["b_q_norm"][0])[None]
    rows[:, 640:896] = f(inp["b_kv_norm"][0])[None]
    sh["rows"] = rows
    tk = np.arange(128)[:, None]
    tq = np.arange(128)[None, :]
    bb = np.zeros((128, 8, 2, 128), np.float32)
    for blk in range(2):
        idx = _bucket(tq - tk + 128 * blk)
        bb[:, :, blk, :] = rb[idx].transpose(0, 2, 1)
    sh["biasblk"] = bb
    cm = np.zeros((128, 3, 128), np.float32)
    cm[:, 0, :] = np.eye(128, dtype=np.float32)
    cm[:, 1, :] = np.where(tq >= tk, 0.0, NEG)
    cm[:, 2, :] = np.where(tq <= tk, 0.0, -1e30)
    sh["cmask"] = cm
    return sh


_CACHE = {}


def kernel(**inputs):
    x = np.asarray(inputs["x"], dtype=np.float32)
    sh = _prep_shared(inputs)
    if "nc" not in _CACHE:
        _CACHE["nc"] = Builder().build()
    nc = _CACHE["nc"]
    in_maps = []
    for c in range(NCORES):
        m = dict(sh)
        m["x"] = np.ascontiguousarray(x[c * NSEQ:(c + 1) * NSEQ])
        in_maps.append(m)
    res = run_bass_kernel_spmd(nc, in_maps, core_ids=list(range(NCORES)))
    out = np.concatenate([np.asarray(r["out"]) for r in res.results], axis=0)
    return out.astype(np.float32)
```

```python
import math
import os as _os
from contextlib import ExitStack

import numpy as np
import concourse.bass as bass
import concourse.mybir as mybir
from concourse.bass_utils import run_bass_kernel_spmd

F32 = mybir.dt.float32
BF16 = mybir.dt.bfloat16
ALU = mybir.AluOpType
AF = mybir.ActivationFunctionType
AX = mybir.AxisListType

D = 1024
SEQ = 2048
NMETA = 16
L = SEQ + NMETA
DFF = 2816
NSEQ = 2
NCORES = 8
NT = 17
TILES = [(i * 128, min(128, L - i * 128)) for i in range(NT)]
TGS = [(0, 512), (512, 512), (1024, 512), (1536, 512), (2048, 16)]
QGS = [[0, 1, 2, 3], [4, 5, 6, 7], [8, 9, 10, 11], [12, 13, 14, 15], [16]]
EPS = 1e-6
A_SCALE = 64 ** -0.5
B_SCALE = 256 ** -0.5
LAM_INIT = 0.8 - 0.6 * math.exp(0.0)
TOPK = 256
NEG = -30000.0
NBIS = 12
WIN_COLS = 4 * 384 + 264 + 9 * 128
STRICT = True
EPOCH = 2000


def _dsize(dt):
    return 4 if dt == F32 else 2


class Dep:
    __slots__ = ("name", "lw", "rd", "sem", "dcount")

    def __init__(self, name):
        self.name = name
        self.lw = None
        self.rd = []
        self.sem = None
        self.dcount = 0


class Op:
    __slots__ = ("eng", "fn", "deps", "dma", "signal", "seq", "sem", "waits", "wdep")

    def __init__(self, eng, fn, deps, dma, wdep):
        self.eng = eng
        self.fn = fn
        self.deps = deps
        self.dma = dma
        self.signal = False
        self.seq = 0
        self.sem = None
        self.waits = []
        self.wdep = wdep


class Prog:
    ENGS = ("sp", "pe", "act", "dve", "pool")

    def __init__(self, nc):
        self.nc = nc
        self.ops = []
        self.last = {e: None for e in self.ENGS}
        self.dmas_since_barrier = []

    def add(self, eng, fn, r=(), w=(), dma=False):
        idx = len(self.ops)
        deps = set()
        for t in r:
            if t.lw is not None:
                deps.add(t.lw)
        for t in w:
            if t.lw is not None:
                deps.add(t.lw)
            deps.update(t.rd)
        for t in r:
            t.rd.append(idx)
        for t in w:
            t.lw = idx
            t.rd = []
        wdep = None
        if dma:
            assert len(w) == 1
            wdep = w[0]
            self.dmas_since_barrier.append(idx)
        self.ops.append(Op(eng, fn, deps, dma, wdep))
        self.last[eng] = idx
        return idx

    def barrier(self):
        lasts = {v for v in self.last.values() if v is not None}
        lasts.update(self.dmas_since_barrier)
        self.dmas_since_barrier = []
        for e in self.ENGS:
            idx = len(self.ops)
            self.ops.append(Op(e, None, set(lasts), False, None))
            self.last[e] = idx

    def emit(self, stack):
        nc = self.nc
        ops = self.ops
        for op in ops:
            for d in sorted(op.deps):
                dop = ops[d]
                if not dop.dma and dop.eng == op.eng and (op.eng == "pe" or not STRICT):
                    continue
                if dop.fn is None:
                    continue
                dop.signal = True
                op.waits.append(d)
        esem = {e: stack.enter_context(nc.semaphore("s_" + e)) for e in self.ENGS}
        cnt = {e: 0 for e in self.ENGS}
        NCH = 8
        chsem = [stack.enter_context(nc.semaphore("dch%d" % i)) for i in range(NCH)]
        chcnt = [0] * NCH
        chlast = [None] * NCH
        ndma = 0
        for oi, op in enumerate(ops):
            if op.fn is None:
                continue
            if op.dma:
                c = ndma % NCH
                ndma += 1
                if chlast[c] is not None:
                    op.waits.append(chlast[c])
                chlast[c] = oi
                chcnt[c] += 16
                op.sem = chsem[c]
                op.seq = chcnt[c]
            elif op.signal:
                if cnt[op.eng] >= EPOCH:
                    esem[op.eng] = stack.enter_context(nc.semaphore("s_%s_%d" % (op.eng, oi)))
                    cnt[op.eng] = 0
                cnt[op.eng] += 1
                op.sem = esem[op.eng]
                op.seq = cnt[op.eng]
        per = {e: [op for op in ops if op.eng == e] for e in self.ENGS}

        def run(e, h):
            waited = {}
            for op in per[e]:
                need = {}
                for d in op.waits:
                    dop = ops[d]
                    k = id(dop.sem)
                    if k not in need or need[k][1] < dop.seq:
                        need[k] = (dop.sem, dop.seq)
                for k, (sem, val) in need.items():
                    if waited.get(k, 0) >= val:
                        continue
                    h.wait_ge(sem, val)
                    waited[k] = val
                if op.fn is None:
                    continue
                ins = op.fn(h)
                if op.dma:
                    ins.then_inc(op.sem, 16)
                elif op.signal:
                    ins.then_inc(op.sem, 1)

        with nc.Block() as block:
            @block.sync
            def _(h):
                run("sp", h)

            @block.tensor
            def _(h):
                run("pe", h)

            @block.scalar
            def _(h):
                run("act", h)

            @block.vector
            def _(h):
                run("dve", h)

            @block.gpsimd
            def _(h):
                run("pool", h)


class Builder:
    def __init__(self, stage=99, nseq=NSEQ, dbg=False, dbg_stop=False):
        self.dbg_stop = dbg_stop
        self.stage = stage
        self.nseq = nseq
        self.nc = nc = bass.Bass("TRN2", target_bir_lowering=False)
        self.P = Prog(nc)
        self.cur = 16640
        dt = nc.dram_tensor
        self.x = dt("x", [NSEQ, SEQ, D], F32, kind="ExternalInput").ap()
        self.meta = dt("meta", [NMETA, D], F32, kind="ExternalInput").ap()
        self.wg = [dt("w%dg" % i, [D, DFF], F32, kind="ExternalInput").ap() for i in (1, 2)]
        self.wu = [dt("w%du" % i, [D, DFF], F32, kind="ExternalInput").ap() for i in (1, 2)]
        self.wd = [dt("w%dd" % i, [DFF, D], F32, kind="ExternalInput").ap() for i in (1, 2)]
        self.win = dt("win", [D, WIN_COLS], F32, kind="ExternalInput").ap()
        self.wuk = dt("wuk", [128, 4, 256], F32, kind="ExternalInput").ap()
        self.wuv = dt("wuv", [128, 2, 4, 128], F32, kind="ExternalInput").ap()
        self.wout = dt("wout", [D, D], F32, kind="ExternalInput").ap()
        self.cols = dt("cols", [128, 40], F32, kind="ExternalInput").ap()
        self.rows = dt("rows", [128, 896], F32, kind="ExternalInput").ap()
        self.bias = dt("biasblk", [128, 8, 2, 128], F32, kind="ExternalInput").ap()
        self.cmask = dt("cmask", [128, 3, 128], F32, kind="ExternalInput").ap()
        self.out = dt("out", [NSEQ, SEQ, D], F32, kind="ExternalOutput").ap()
        self.hs = dt("hs", [L, D], F32, kind="ExternalOutput").ap()
        self.dbg = dt("dbg", [128, 4096], F32, kind="ExternalOutput").ap() if dbg else None
        self.psum = [nc.alloc_psum_tensor("pb%d" % i, [128, 512], F32).ap() for i in range(8)]
        self.pdep = [Dep("pb%d" % i) for i in range(8)]
        self.out_dep = Dep("out")

    def sb(self, name, shape, dtype, at=None):
        n = 1
        for s in shape[1:]:
            n *= s
        nbytes = (n * _dsize(dtype) + 63) // 64 * 64
        off = self.cur if at is None else at
        t = self.nc.alloc_sbuf_tensor_at(name, list(shape), dtype, offset=off)
        if at is None:
            self.cur = off + nbytes
        assert off + nbytes <= 229376, (name, off + nbytes)
        return t.ap()

    def mm(self, out, lhsT, rhs, start, stop, r, w):
        self.P.add("pe", lambda e: e.matmul(out, lhsT, rhs, start=start, stop=stop, skip_group_check=True), r, w)

    def tr(self, out, in_, ident, r, w):
        self.P.add("pe", lambda e: e.transpose(out, in_, ident), r, w)

    def act(self, out, in_, func, r, w, bias=0.0, scale=1.0, accum=None):
        if accum is None:
            self.P.add("act", lambda e: e.activation(out, in_, func, bias=bias, scale=scale), r, w)
        else:
            self.P.add("act", lambda e: e.activation(out, in_, func, bias=bias, scale=scale, accum_out=accum), r, w)

    def ts(self, eng, out, in0, s1, s2, op0, op1, r, w, accum=None):
        if accum is None:
            self.P.add(eng, lambda e: e.tensor_scalar(out, in0, s1, s2, op0, op1), r, w)
        else:
            self.P.add(eng, lambda e: e.tensor_scalar(out, in0, s1, s2, op0, op1, accum_out=accum), r, w)

    def tt(self, eng, out, in0, in1, op, r, w):
        self.P.add(eng, lambda e: e.tensor_tensor(out, in0, in1, op), r, w)

    def stt(self, out, in0, scalar, in1, op0, op1, r, w):
        self.P.add("dve", lambda e: e.scalar_tensor_tensor(out, in0, scalar, in1, op0, op1), r, w)

    def cp(self, eng, out, in_, r, w):
        self.P.add(eng, lambda e: e.tensor_copy(out, in_), r, w)

    def red(self, out, in_, op, r, w, absval=False):
        self.P.add("dve", lambda e: e.tensor_reduce(out, in_, AX.X, op, apply_absolute_value=absval), r, w)

    def recip(self, out, in_, r, w):
        self.P.add("dve", lambda e: e.reciprocal(out, in_), r, w)

    def memset(self, eng, ap, val, w):
        self.P.add(eng, lambda e: e.memset(ap, val), (), w)

    def dma(self, out, in_, r, w):
        self.P.add("sp", lambda e: e.dma_start(out=out, in_=in_), r, w, dma=True)

    def stat(self):
        i = self.st_i
        self.st_i = (i + 1) % 8
        return 4 * i, self.st_dep[i]

    def rstd(self, np_, k, nc_, inv_n, deps):
        ssd, td, rsd = deps
        self.ts("dve", self.tmp[0:np_, k:k + nc_], self.ss[0:np_, k:k + nc_], inv_n, EPS, ALU.mult, ALU.add, [ssd], [td])
        self.tt("pool", self.rs[0:np_, k:k + nc_], self.tmp[0:np_, k:k + nc_], self.neghalf[0:np_, 0:nc_], ALU.pow,
                [td, self.nh_dep], [rsd])

    def build(self):
        nc = self.nc
        stage = self.stage
        with ExitStack() as stack:
            self._alloc()
            self._setup()
            for s in range(self.nseq):
                self._sequence(s)
            self._finish()
            self.P.emit(stack)
        return nc

    def _alloc(self):
        sb = self.sb
        self.cols_sb = sb("cols", [128, 40], F32)
        self.rows_sb = sb("rows", [128, 896], F32)
        self.ident_f = sb("identf", [128, 128], F32)
        self.ident = sb("ident", [128, 128], BF16)
        self.neghalf = sb("neghalf", [128, 16], F32)
        self.smallc = sb("smallc", [128, 32], F32)
        self.gqk = self.smallc[:, 6:7]
        self.gqb = [self.smallc[:, 2:3], self.smallc[:, 10:11]]
        self.lamn = self.smallc[:, 14:15]
        self.abias = sb("abias", [128, 8], F32)
        self.gsub = self.smallc[:, 18:19]
        self.sm = sb("small", [128, 272], F32)
        self.biasbf = sb("biasbf", [128, 8, 2, 128], BF16)
        self.masktok = sb("masktok", [128, 128], F32)
        self.iw_sb = sb("iw", [128, NT, 8], F32)
        self.ss = sb("ss", [128, 32], F32)
        self.tmp = sb("tmpst", [128, 32], F32)
        self.rs = sb("rs", [128, 32], F32)
        self.st_dep = [(Dep("ss%d" % i), Dep("tm%d" % i), Dep("rs%d" % i)) for i in range(8)]
        self.st_i = 0
        self.wukb = sb("wukb", [128, 4, 256], BF16)
        self.wuvb = sb("wuvb", [128, 2, 4, 128], BF16)
        self.maskT = sb("maskT", [128, 128], F32)
        self.pw = sb("pw", [128, NBIS + 1], F32)
        self.thc = self.smallc[:, 22:23]
        self.xn_s = [sb("xn_s%d" % i, [128, D], BF16) for i in range(2)]
        self.xn_s_dep = [Dep("xn_s%d" % i) for i in range(2)]
        self.junk = sb("junk", [128, D], BF16)
        self.junk_dep = Dep("junk")
        self.c_const = self.cur
        self.xnT = sb("xnT", [128, 8, L], BF16)
        self.xn_dep = [Dep("xnT%d" % t) for t in range(NT)]
        self.h_off = self.cur
        self.h = sb("h", [128, NT, D], F32)
        self.h_dep = [Dep("h%d" % t) for t in range(NT)]
        self.big_off = self.cur
        self.phase_off = self.cur

    def _setup(self):
        P = self.P
        cd = self.cdep = Dep("consts")
        self.dma(self.cols_sb, self.cols, [], [cd])
        rd = Dep("rows")
        self.dma(self.rows_sb, self.rows, [], [rd])
        idd = Dep("identf")
        self.dma(self.ident_f, self.cmask[:, 0, :], [], [idd])
        mk = Dep("masktok")
        self.dma(self.masktok, self.cmask[:, 2, :], [], [mk])
        self.masktok_dep = mk
        self.ident_dep = Dep("ident")
        self.cp("dve", self.ident, self.ident_f, [idd], [self.ident_dep])
        self.nh_dep = Dep("neghalf")
        self.memset("dve", self.neghalf, -0.5, [self.nh_dep])
        if self.stage <= 1:
            return
        self._ffn_alloc()
        C = self.cols_sb
        R = self.rows_sb
        sd = self.setup_dep = Dep("setup")
        smd = Dep("sm")
        mtd = Dep("maskT")
        self.dma(self.maskT, self.cmask[:, 1, :], [], [mtd])
        self.stt(self.gqk, C[:, 24:25], A_SCALE, C[:, 25:26], ALU.mult, ALU.mult, [cd], [sd])
        for cc in range(2):
            self.ts("dve", self.gqb[cc], C[:, 28 + cc:29 + cc], B_SCALE, None, ALU.mult, ALU.bypass, [cd], [sd])
        self.ts("dve", self.gsub, C[:, 30:31], 1.0 - LAM_INIT, None, ALU.mult, ALU.bypass, [cd], [sd])
        sm = self.sm
        self.tt("dve", sm[:, 0:64], R[:, 0:64], R[:, 64:128], ALU.mult, [rd], [smd])
        self.red(sm[:, 256:257], sm[:, 0:64], ALU.add, [smd], [smd])
        self.tt("dve", sm[:, 64:128], R[:, 128:192], R[:, 192:256], ALU.mult, [rd], [smd])
        self.red(sm[:, 257:258], sm[:, 64:128], ALU.add, [smd], [smd])
        self.act(sm[:, 258:260], sm[:, 256:258], AF.Exp, [smd], [smd])
        self.tt("dve", sm[:, 260:261], sm[:, 258:259], sm[:, 259:260], ALU.subtract, [smd], [smd])
        self.ts("dve", self.lamn, sm[:, 260:261], -1.0, -LAM_INIT, ALU.mult, ALU.add, [smd], [sd])
        self.tt("dve", sm[:, 0:64], R[:, 256:320], R[:, 320:384], ALU.mult, [rd, smd], [smd])
        self.red(sm[:, 261:262], sm[:, 0:64], ALU.max, [smd], [smd], absval=True)
        self.ts("dve", sm[:, 262:263], sm[:, 261:262], 64.0 * A_SCALE, None, ALU.mult, ALU.bypass, [smd], [smd])
        self.ts("dve", self.abias[:, 0:4], C[:, 31:35], sm[:, 262:263], None, ALU.subtract, ALU.bypass, [smd, cd], [sd])
        self.tt("dve", sm[:, 0:256], R[:, 384:640], R[:, 640:896], ALU.mult, [rd, smd], [smd])
        self.red(sm[:, 263:264], sm[:, 0:256], ALU.max, [smd], [smd], absval=True)
        self.ts("dve", sm[:, 264:265], sm[:, 263:264], 256.0 * B_SCALE, None, ALU.mult, ALU.bypass, [smd], [smd])
        self.ts("dve", self.abias[:, 4:8], C[:, 35:39], sm[:, 264:265], None, ALU.subtract, ALU.bypass, [smd, cd], [sd])
        st = self.stg[0].rearrange("p (h b c) -> p h b c", h=8, b=2)
        self.dma(st, self.bias, [], [self.stg_dep[0]])
        for hh in range(8):
            self.stt(self.biasbf[:, hh, 0, :], st[:, hh, 0, :], C[:, 31 + hh:32 + hh], self.maskT, ALU.subtract, ALU.add,
                     [self.stg_dep[0], cd, mtd], [sd])
            self.ts("dve", self.biasbf[:, hh, 1, :], st[:, hh, 1, :], C[:, 31 + hh:32 + hh], None, ALU.subtract, ALU.bypass,
                    [self.stg_dep[0], cd], [sd])
        s1 = self.stg[1].rearrange("p (h c) -> p h c", h=4)[:, :, 0:256]
        self.dma(s1, self.wuk, [], [self.stg_dep[1]])
        self.cp("pool", self.wukb, s1, [self.stg_dep[1]], [sd])
        s2 = self.stg[2][:, 0:1024].rearrange("p (a h c) -> p a h c", a=2, h=4)
        self.dma(s2, self.wuv, [], [self.stg_dep[2]])
        self.cp("pool", self.wuvb, s2, [self.stg_dep[2]], [sd])
        for k in range(NBIS + 1):
            self.memset("pool", self.pw[:, k:k + 1], 2.0 ** -(k + 1), [sd])
        self.memset("pool", self.thc, -1e29, [sd])

    def _sequence(self, s):
        self._load_h(s, from_x=True)
        self._norm_pass(0)
        self._ffn(0)
        if self.stage <= 1:
            self._store_out(s)
            return
        self._norm_pass(1)
        self.hs_dep = getattr(self, "hs_dep", None) or [Dep("hs%d" % t) for t in range(NT)]
        for t, (t0, n) in enumerate(TILES):
            self.dma(self.hs[t0:t0 + n, :], self.h[0:n, t, :], [self.h_dep[t]], [self.hs_dep[t]])
        self.P.barrier()
        self._attn_alloc()
        if not _os.environ.get("K_SKIP_PROJ"):
            self._proj()
        self.P.barrier()
        if self.stage >= 3:
            self._attnA()
            self.P.barrier()
        if self.stage >= 4:
            self._attnB()
            self.P.barrier()
        if self.dbg_stop:
            return
        if not _os.environ.get("K_NO_RELOAD"):
            self._load_h(s, from_x=False)
        if not _os.environ.get("K_NO_FFN2"):
            self._norm_pass(2)
            self._ffn(1)
        self._store_out(s)

    def _load_h(self, s, from_x):
        for t, (t0, n) in enumerate(TILES):
            hd = self.h_dep[t]
            if from_x:
                if t == 0:
                    self.dma(self.h[0:NMETA, 0, :], self.meta, [], [hd])
                    self.dma(self.h[NMETA:128, 0, :], self.x[s, 0:128 - NMETA, :], [], [hd])
                else:
                    self.dma(self.h[0:n, t, :], self.x[s, t0 - NMETA:t0 - NMETA + n, :], [], [hd])
            else:
                self.dma(self.h[0:n, t, :], self.hs[t0:t0 + n, :], [self.hs_dep[t]], [hd])

    def _store_out(self, s):
        for t, (t0, n) in enumerate(TILES):
            if t == 0:
                self.dma(self.out[s, 0:128 - NMETA, :], self.h[NMETA:128, 0, :], [self.h_dep[0]], [self.out_dep])
            else:
                self.dma(self.out[s, t0 - NMETA:t0 - NMETA + n, :], self.h[0:n, t, :], [self.h_dep[t]], [self.out_dep])

    def _norm_pass(self, which):
        for t, (t0, n) in enumerate(TILES):
            b = t % 2
            k, (ssd, td, rsd) = self.stat()
            ss = self.ss[0:n, k:k + 1]
            self.act(self.junk[0:n, :], self.h[0:n, t, :], AF.Square, [self.h_dep[t]], [self.junk_dep, ssd], accum=ss)
            self.ts("dve", self.tmp[0:n, k:k + 1], ss, 1.0 / D, EPS, ALU.mult, ALU.add, [ssd], [td])
            self.tt("pool", self.rs[0:n, k:k + 1], self.tmp[0:n, k:k + 1], self.neghalf[0:n, 0:1], ALU.pow,
                    [td, self.nh_dep], [rsd])
            self.ts("dve", self.xn_s[b][0:n, :], self.h[0:n, t, :], self.rs[0:n, k:k + 1], None, ALU.mult, ALU.bypass,
                    [self.h_dep[t], rsd], [self.xn_s_dep[b]])
            pb = self.psum[b].bitcast(BF16)
            for kk in range(8):
                self.tr(pb[:, kk * 128:kk * 128 + n], self.xn_s[b][0:n, kk * 128:(kk + 1) * 128],
                        self.ident[0:n, 0:n], [self.xn_s_dep[b], self.ident_dep], [self.pdep[b]])
            src = pb.rearrange("p (k c) -> p k c", k=8)[:, :, 0:n]
            self.P.add("act", (lambda e, o=self.xnT[:, :, t0:t0 + n], i=src: e.activation(o, i, AF.Copy)),
                       [self.pdep[b]], [self.xn_dep[t]])

    def _ffn_alloc(self):
        if hasattr(self, "ffn_alloced"):
            return
        self.ffn_alloced = True
        self.cur = self.phase_off
        sb = self.sb
        self.stg = [sb("stg%d" % i, [128, 2048], F32) for i in range(3)]
        self.stg_dep = [Dep("stg%d" % i) for i in range(3)]
        self.stg_i = 0
        self.wgb = [sb("wgb%d" % i, [128, 8, 256], BF16) for i in range(2)]
        self.wub = [sb("wub%d" % i, [128, 8, 256], BF16) for i in range(2)]
        self.wgb_dep = [Dep("wgb%d" % i) for i in range(2)]
        self.wub_dep = [Dep("wub%d" % i) for i in range(2)]
        self.wdb = [sb("wdb%d" % i, [128, 2, D], BF16) for i in range(4)]
        self.wdb_dep = [Dep("wdb%d" % i) for i in range(4)]
        self.actb = sb("actb", [128, 4, L], BF16)
        self.actb_dep = [[Dep("act%d_%d" % (f, g)) for g in range(len(TGS))] for f in range(4)]
        self.sg = [sb("sg%d" % i, [128, 512], BF16) for i in range(2)]
        self.sg_dep = [Dep("sg%d" % i) for i in range(2)]
        self.ffn_end = self.cur
        self.slab_ctr = 0
        self.gu_ctr = 0
        self.dn_ctr = 0

    def _stage_slot(self):
        i = self.stg_i
        self.stg_i = (i + 1) % 3
        return i

    def _ffn(self, which):
        self._ffn_alloc()
        wg, wu, wd = self.wg[which], self.wu[which], self.wd[which]
        gcol = {0: 0, 1: 16}[which]
        gain = self.cols_sb[:, gcol:gcol + 8]
        slabs = list(range(11))
        groups = [slabs[i:i + 2] for i in range(0, 11, 2)]
        for grp in groups:
            for li, sl in enumerate(grp):
                c0 = sl * 256
                sw = self.slab_ctr % 2
                dslot = self.slab_ctr % 4
                self.slab_ctr += 1
                for (src, dst, ddep) in ((wg, self.wgb[sw], self.wgb_dep[sw]), (wu, self.wub[sw], self.wub_dep[sw])):
                    si = self._stage_slot()
                    st3 = self.stg[si].rearrange("p (k c) -> p k c", k=8)
                    self.dma(st3, src[:, c0:c0 + 256].rearrange("(k p) c -> p k c", p=128), [], [self.stg_dep[si]])
                    self.tt("pool", dst, st3, gain.unsqueeze(2).to_broadcast([128, 8, 256]), ALU.mult,
                            [self.stg_dep[si], self.cdep], [ddep])
                si = self._stage_slot()
                st3 = self.stg[si].rearrange("p (k c) -> p k c", k=2)
                self.dma(st3, wd[c0:c0 + 256, :].rearrange("(k p) c -> p k c", p=128), [], [self.stg_dep[si]])
                self.cp("pool", self.wdb[dslot], st3, [self.stg_dep[si]], [self.wdb_dep[dslot]])
                grp_dslot = dslot
                for c in range(2):
                    fl = li * 2 + c
                    for g, (g0, gn) in enumerate(TGS):
                        pbuf = self.gu_ctr % 2
                        self.gu_ctr += 1
                        pg, pu = 2 + 2 * pbuf, 3 + 2 * pbuf
                        xdeps = [self.xn_dep[t] for t in range(NT) if TILES[t][0] >= g0 and TILES[t][0] < g0 + gn]
                        for k in range(8):
                            self.mm(self.psum[pg][:, 0:gn], self.wgb[sw][:, k, c * 128:(c + 1) * 128],
                                    self.xnT[:, k, g0:g0 + gn], k == 0, k == 7,
                                    [self.wgb_dep[sw]] + xdeps, [self.pdep[pg]])
                        for k in range(8):
                            self.mm(self.psum[pu][:, 0:gn], self.wub[sw][:, k, c * 128:(c + 1) * 128],
                                    self.xnT[:, k, g0:g0 + gn], k == 0, k == 7,
                                    [self.wub_dep[sw]] + xdeps, [self.pdep[pu]])
                        sgi = pbuf
                        self.act(self.sg[sgi][:, 0:gn], self.psum[pg][:, 0:gn], AF.Silu, [self.pdep[pg]], [self.sg_dep[sgi]])
                        self.tt("dve", self.actb[:, fl, g0:g0 + gn], self.sg[sgi][:, 0:gn], self.psum[pu][:, 0:gn], ALU.mult,
                                [self.sg_dep[sgi], self.pdep[pu]], [self.actb_dep[fl][g]])
            nfl = len(grp) * 2
            first_dslot = (self.slab_ctr - len(grp)) % 4
            for t, (t0, n) in enumerate(TILES):
                g = min(t // 4, 4)
                for half in range(2):
                    pd = 6 + (self.dn_ctr % 2)
                    self.dn_ctr += 1
                    for fl in range(nfl):
                        dslot = (first_dslot + fl // 2) % 4
                        self.mm(self.psum[pd][0:n, :], self.actb[:, fl, t0:t0 + n],
                                self.wdb[dslot][:, fl % 2, half * 512:(half + 1) * 512], fl == 0, fl == nfl - 1,
                                [self.actb_dep[fl][g], self.wdb_dep[dslot]], [self.pdep[pd]])
                    hv = self.h[0:n, t, half * 512:(half + 1) * 512]
                    self.stt(hv, self.psum[pd][0:n, :], 0.5, hv, ALU.mult, ALU.add,
                             [self.pdep[pd], self.h_dep[t]], [self.h_dep[t]])


    def _attn_alloc(self):
        if hasattr(self, "attn_alloced"):
            return
        self.attn_alloced = True
        sb = self.sb
        save = self.cur
        self.cur = self.h_off
        r1 = self.cur
        self.qT = sb("qT", [128, 4, L], BF16)
        self.kT = sb("kT", [128, 4, L], BF16)
        self.VA = sb("VA", [128, NT, 4, 129], BF16)
        r1_end = self.cur
        self.cur = r1
        self.qlT = sb("qlT", [128, 4, 2, 512], BF16)
        self.rr = [sb("rr%d" % i, [128, 512], F32) for i in range(2)]
        self.qn1k = sb("qn1k", [128, 1024], BF16)
        self.ocT = sb("ocT", [128, 8, 128], BF16)
        self.woutb = sb("woutb", [128, 8, D], BF16)
        self.hst2 = sb("hst2", [128, 2, D], F32)
        self.hst = [self.hst2[:, 0, :], self.hst2[:, 1, :]]
        self.obG = sb("obG", [128, 4, 512], BF16)
        self.cntj = sb("cntj", [128, L], BF16)
        assert self.cur <= r1_end, (self.cur, r1_end)
        self.cur = r1_end
        self.qbT = sb("qbT", [128, 4, L], BF16)
        self.cT = sb("cT", [128, 2, L], BF16)
        self.VB = sb("VB", [128, NT, 4, 129], BF16)
        self.iqT = sb("iqT", [128, 4, L], BF16)
        self.ikT = sb("ikT", [128, L], BF16)
        r3 = self.cur
        self.wst = [sb("wst%d" % i, [128, 8, 384], F32) for i in range(2)]
        self.wb = [sb("wb%d" % i, [128, 8, 384], BF16) for i in range(2)]
        r3_end = self.cur
        self.cur = r3
        self.oa = sb("oa", [128, NT, 512], BF16)
        self.PT = [sb("PT%d" % i, [128, 512], BF16) for i in range(4)]
        self.t1 = [sb("t1_%d" % i, [128, 128], F32) for i in range(2)]
        self.ov = [sb("ov_%d" % i, [128, 128], F32) for i in range(2)]
        self.bst = sb("bst", [128, 4 * NBIS + 8], F32)
        assert self.cur <= r3_end, (self.cur, r3_end)
        self.cur = max(r3_end, save)
        self.sq = [sb("sq%d" % i, [128, 256], F32) for i in range(2)]
        self.qn = [sb("qn%d" % i, [128, 256], BF16) for i in range(2)]
        save2 = self.cur
        self.cur = self.c_const
        self.acc = sb("acc", [128, L], F32)
        self.maskb = sb("maskb", [128, L], BF16)
        self.mT = sb("mT", [128, NT, 512], BF16)
        assert self.cur <= self.h_off, (self.cur, self.h_off)
        self.cur = save2
        D_ = Dep
        self.qT_dep = [[D_("qT%d_%d" % (h, t)) for t in range(NT)] for h in range(4)]
        self.kT_dep = [[D_("kT%d_%d" % (h, t)) for t in range(NT)] for h in range(4)]
        self.VA_dep = [D_("VA%d" % t) for t in range(NT)]
        self.VB_dep = [D_("VB%d" % t) for t in range(NT)]
        self.cT_dep = [D_("cT%d" % t) for t in range(NT)]
        self.qbT_dep = [D_("qbT%d" % g) for g in range(5)]
        self.iqT_dep = [D_("iqT%d" % g) for g in range(5)]
        self.ikT_dep = [D_("ikT%d" % g) for g in range(5)]
        self.iw_dep = [D_("iw%d" % t) for t in range(NT)]
        self.wst_dep = [D_("wst%d" % i) for i in range(2)]
        self.wb_dep = [D_("wb%d" % i) for i in range(2)]
        self.sq_dep = [D_("sq%d" % i) for i in range(2)]
        self.qn_dep = [D_("qn%d" % i) for i in range(2)]
        self.oa_dep = [D_("oa%d" % t) for t in range(NT)]
        self.PT_dep = [D_("PT%d" % i) for i in range(4)]
        self.t1_dep = [D_("t1_%d" % i) for i in range(2)]
        self.ov_dep = [D_("ov_%d" % i) for i in range(2)]
        self.qlT_dep = [D_("qlT%d" % i) for i in range(4)]
        self.rr_dep = [D_("rr%d" % i) for i in range(2)]
        self.qn1k_dep = D_("qn1k")
        self.ocT_dep = D_("ocT")
        self.woutb_dep = D_("woutb")
        self.hst_dep = [D_("hst%d" % i) for i in range(2)]
        self.obG_dep = [D_("obG%d" % i) for i in range(4)]
        self.cntj_dep = D_("cntj")
        self.acc_dep = D_("acc")
        self.maskb_dep = D_("maskb")
        self.mT_dep = [D_("mT%d" % i) for i in range(4)]
        self.bst_dep = D_("bst")
        self.ctr = 0

    def _load_slab(self, c0, ncol, gain):
        slot = self.ctr % 2
        self.ctr += 1
        st = self.wst[slot][:, :, 0:ncol]
        self.dma(st, self.win[:, c0:c0 + ncol].rearrange("(k p) c -> p k c", p=128), [], [self.wst_dep[slot]])
        self.tt("pool", self.wb[slot][:, :, 0:ncol], st, gain.unsqueeze(2).to_broadcast([128, 8, ncol]), ALU.mult,
                [self.wst_dep[slot], self.cdep], [self.wb_dep[slot]])
        return slot

    def _proj(self):
        gain = self.cols_sb[:, 8:16]
        C = self.cols_sb
        parts = _os.environ.get("K_PROJ_PARTS", "ms,A,CK,FM").split(",")
        if "ms" in parts:
            self.memset("pool", self.VA[:, :, :, 128:129], 1.0, self.VA_dep)
            self.memset("pool", self.VB[:, :, :, 128:129], 1.0, self.VB_dep)
        tctr = 0
        for sl in range(5):
            if (sl < 4 and "A" not in parts) or (sl == 4 and "CK" not in parts):
                continue
            ncol = 384 if sl < 4 else 264
            slot = self._load_slab(sl * 384, ncol, gain)

            def mmA(t, slot=slot, ncol=ncol):
                t0, n = TILES[t]
                pb = 2 + (t % 2)
                for k in range(8):
                    self.mm(self.psum[pb][0:n, 0:ncol], self.xnT[:, k, t0:t0 + n], self.wb[slot][:, k, 0:ncol], k == 0, k == 7,
                            [self.xn_dep[t], self.wb_dep[slot]], [self.pdep[pb]])

            mmA(0)
            for t, (t0, n) in enumerate(TILES):
                if t + 1 < NT:
                    mmA(t + 1)
                pb = 2 + (t % 2)
                tb = t % 2
                b = t % 2
                ps = self.psum[pb]
                pT = self.psum[tb].bitcast(BF16)
                k4, sdeps = self.stat()
                ssd, td, rsd = sdeps
                if sl < 4:
                    h = sl
                    KA = _os.environ.get("K_A", "sq,red,rstd,qn,tr,evq,evk,va").split(",")
                    if "sq" in KA:
                        self.act(self.sq[b][0:n, :], ps[0:n, 0:256], AF.Square, [self.pdep[pb]], [self.sq_dep[b]])
                    if "red" in KA:
                        self.red(self.ss[0:n, k4:k4 + 4], self.sq[b][0:n, :].rearrange("p (g d) -> p g d", d=64), ALU.add,
                                 [self.sq_dep[b]], [ssd])
                    if "rstd" in KA:
                        self.rstd(n, k4, 4, 1.0 / 64, sdeps)
                    if "qn" in KA:
                        self.tt("dve", self.qn[b][0:n, :].rearrange("p (g d) -> p g d", d=64),
                                ps[0:n, 0:256].rearrange("p (g d) -> p g d", d=64),
                                self.rs[0:n, k4:k4 + 4].unsqueeze(2).to_broadcast([n, 4, 64]), ALU.mult,
                                [self.pdep[pb], rsd], [self.qn_dep[b]])
                    if "tr" in KA:
                        self.tr(pT[:, 0:n], self.qn[b][0:n, 0:128], self.ident[0:n, 0:n], [self.qn_dep[b], self.ident_dep],
                                [self.pdep[tb]])
                        self.tr(pT[:, 128:128 + n], self.qn[b][0:n, 128:256], self.ident[0:n, 0:n], [self.qn_dep[b], self.ident_dep],
                                [self.pdep[tb]])
                    if "evq" in KA:
                        qdst = self.junk[:, 0:n] if _os.environ.get("K_DEST") else self.qT[:, h, t0:t0 + n]
                        self.act(qdst, pT[:, 0:n], AF.Copy, [self.pdep[tb], self.setup_dep],
                                 [self.qT_dep[h][t]], scale=self.gqk)
                    if "evk" in KA:
                        kdst = self.junk[:, 128:128 + n] if _os.environ.get("K_DEST") else self.kT[:, h, t0:t0 + n]
                        self.act(kdst, pT[:, 128:128 + n], AF.Copy, [self.pdep[tb]], [self.kT_dep[h][t]])
                    if "va" in KA:
                        self.act(self.VA[0:n, t, h, 0:128], ps[0:n, 256:384], AF.Copy, [self.pdep[pb]], [self.VA_dep[t]])
                else:
                    self.act(self.sq[b][0:n, :], ps[0:n, 0:256], AF.Square, [self.pdep[pb]], [self.sq_dep[b], ssd],
                             accum=self.ss[0:n, k4:k4 + 1])
                    self.rstd(n, k4, 1, 1.0 / 256, sdeps)
                    self.ts("dve", self.qn[b][0:n, :], ps[0:n, 0:256], self.rs[0:n, k4:k4 + 1], None, ALU.mult, ALU.bypass,
                            [self.pdep[pb], rsd], [self.qn_dep[b]])
                    self.ts("dve", self.iw_sb[0:n, t, :], ps[0:n, 256:264], (64 ** -0.5) * (8 ** -0.5), None, ALU.mult, ALU.bypass,
                            [self.pdep[pb]], [self.iw_dep[t]])
                    for cc in range(2):
                        self.tr(pT[:, cc * 128:cc * 128 + n], self.qn[b][0:n, cc * 128:(cc + 1) * 128], self.ident[0:n, 0:n],
                                [self.qn_dep[b], self.ident_dep], [self.pdep[tb]])
                    self.ts("dve", self.cT[:, 0, t0:t0 + n], pT[:, 0:n], C[:, 26:27], None, ALU.mult, ALU.bypass,
                            [self.pdep[tb], self.cdep], [self.cT_dep[t]])
                    self.act(self.cT[:, 1, t0:t0 + n], pT[:, 128:128 + n], AF.Copy, [self.pdep[tb], self.cdep], [self.cT_dep[t]],
                             scale=C[:, 27:28])
                    vb = 6 + (t % 2)
                    for cc in range(2):
                        self.mm(self.psum[vb][0:n, :], self.cT[:, cc, t0:t0 + n],
                                self.wuvb[:, cc, :, :].rearrange("p h e -> p (h e)"), cc == 0, cc == 1,
                                [self.cT_dep[t], self.setup_dep], [self.pdep[vb]])
                    self.act(self.VB[0:n, t, :, 0:128], self.psum[vb][0:n, :].rearrange("p (h e) -> p h e", h=4), AF.Copy,
                             [self.pdep[vb]], [self.VB_dep[t]])
        ectr = 0
        for sl in range(3):
            if "FM" not in parts:
                continue
            slot = self._load_slab(1800 + sl * 384, 384, gain)
            for c in range(3):
                ch = sl * 3 + c
                for g, (g0, gn) in enumerate(TGS):
                    if ch < 4:
                        dst, ddep = self.qbT[:, ch, g0:g0 + gn], self.qbT_dep[g]
                    elif ch < 8:
                        dst, ddep = self.iqT[:, ch - 4, g0:g0 + gn], self.iqT_dep[g]
                    else:
                        dst, ddep = self.ikT[:, g0:g0 + gn], self.ikT_dep[g]
                    pb = 4 + (ectr % 2)
                    xdeps = [self.xn_dep[t] for t in range(NT) if g0 <= TILES[t][0] < g0 + gn]
                    for k in range(8):
                        self.mm(self.psum[pb][:, 0:gn], self.wb[slot][:, k, c * 128:(c + 1) * 128], self.xnT[:, k, g0:g0 + gn],
                                k == 0, k == 7, [self.wb_dep[slot]] + xdeps, [self.pdep[pb]])
                    if ectr % 2 == 0:
                        self.act(dst, self.psum[pb][:, 0:gn], AF.Copy, [self.pdep[pb]], [ddep])
                    else:
                        self.cp("dve", dst, self.psum[pb][:, 0:gn], [self.pdep[pb]], [ddep])
                    ectr += 1

    def _bias_blocks(self, ps, pbank, hh, j, tiles, c0):
        k0, kn = TILES[j]
        for blk, i in ((0, j), (1, j + 1)):
            if i in tiles:
                off = TILES[i][0] - c0
                ni = TILES[i][1]
                self.mm(ps[:, off:off + ni], self.ident[0:kn, 0:kn], self.biasbf[0:kn, hh, blk, 0:ni], False, True,
                        [self.ident_dep, self.setup_dep], [self.pdep[pbank]])

    def _pipe(self, n, stage1, stage2, depth=2):
        for k in range(min(depth, n)):
            stage1(k)
        for k in range(n):
            if k + depth < n:
                stage1(k + depth)
            stage2(k)

    def _attnA(self):
        for G, tiles in enumerate(QGS):
            q0 = TILES[tiles[0]][0]
            qn_ = sum(TILES[t][1] for t in tiles)
            for h in range(4):
                started = set()
                blocks = [(j, m) for j in range(tiles[-1] + 1) for m in range(2)]

                def stage1(k, h=h, tiles=tiles, q0=q0, qn_=qn_, blocks=blocks):
                    j, m = blocks[k]
                    k0, kn = TILES[j]
                    c0 = max(q0, k0)
                    ncol = q0 + qn_ - c0
                    qdeps = [self.qT_dep[h][t] for t in tiles if TILES[t][0] + TILES[t][1] > c0]
                    bank = k % 4
                    ps = self.psum[bank][0:kn, 0:ncol]
                    self.mm(ps, self.kT[m * 64:(m + 1) * 64, h, k0:k0 + kn], self.qT[m * 64:(m + 1) * 64, h, c0:c0 + ncol],
                            True, True, [self.kT_dep[h][j]] + qdeps, [self.pdep[bank]])
                    self._bias_blocks(ps, bank, h, j, tiles, c0)
                    self.act(self.PT[bank][0:kn, 0:ncol], ps, AF.Exp, [self.pdep[bank], self.setup_dep], [self.PT_dep[bank]],
                             bias=self.abias[0:kn, h:h + 1])

                def stage2(k, h=h, tiles=tiles, q0=q0, blocks=blocks, started=started):
                    j, m = blocks[k]
                    k0, kn = TILES[j]
                    c0 = max(q0, k0)
                    pbuf = k % 4
                    for il, i in enumerate(tiles):
                        if i < j:
                            continue
                        off = TILES[i][0] - c0
                        ni = TILES[i][1]
                        slot = m * 4 + il
                        ab = 4 + slot // 3
                        o = (slot % 3) * 129
                        first = ab not in started
                        started.add(ab)
                        self.mm(self.psum[ab][0:ni, o:o + 129], self.PT[pbuf][0:kn, off:off + ni], self.VA[0:kn, j, h, :],
                                first, j == i, [self.PT_dep[pbuf], self.VA_dep[j]], [self.pdep[ab]])

                self._pipe(len(blocks), stage1, stage2)
                for il, i in enumerate(tiles):
                    ni = TILES[i][1]
                    b = il % 2
                    s0, s1 = il, 4 + il
                    b0, o0 = 4 + s0 // 3, (s0 % 3) * 129
                    b1, o1 = 4 + s1 // 3, (s1 % 3) * 129
                    k4, sdeps = self.stat()
                    ssd, td, rsd = sdeps
                    self.recip(self.rs[0:ni, k4 + 1:k4 + 2], self.psum[b0][0:ni, o0 + 128:o0 + 129], [self.pdep[b0]], [rsd])
                    self.recip(self.rs[0:ni, k4 + 2:k4 + 3], self.psum[b1][0:ni, o1 + 128:o1 + 129], [self.pdep[b1]], [rsd])
                    self.ts("dve", self.rs[0:ni, k4 + 3:k4 + 4], self.rs[0:ni, k4 + 2:k4 + 3], self.lamn[0:ni, 0:1], None,
                            ALU.mult, ALU.bypass, [rsd, self.setup_dep], [rsd])
                    self.ts("dve", self.t1[b][0:ni, :], self.psum[b1][0:ni, o1:o1 + 128], self.rs[0:ni, k4 + 3:k4 + 4], None,
                            ALU.mult, ALU.bypass, [self.pdep[b1], rsd], [self.t1_dep[b]])
                    self.stt(self.ov[b][0:ni, :], self.psum[b0][0:ni, o0:o0 + 128], self.rs[0:ni, k4 + 1:k4 + 2],
                             self.t1[b][0:ni, :], ALU.mult, ALU.add, [self.pdep[b0], rsd, self.t1_dep[b]], [self.ov_dep[b]])
                    self.act(self.junk[0:ni, 0:128], self.ov[b][0:ni, :], AF.Square, [self.ov_dep[b]], [self.junk_dep, ssd],
                             accum=self.ss[0:ni, k4:k4 + 1])
                    self.rstd(ni, k4, 1, 1.0 / 128, sdeps)
                    self.ts("dve", self.oa[0:ni, i, h * 128:(h + 1) * 128], self.ov[b][0:ni, :], self.rs[0:ni, k4:k4 + 1], None,
                            ALU.mult, ALU.bypass, [self.ov_dep[b], rsd], [self.oa_dep[i]])

    def _attnB(self):
        C = self.cols_sb
        for q in range(4):
            st = self.hst2
            hd = self.hst_dep
            self.dma(st, self.wout[q * 256:(q + 1) * 256, :].rearrange("(k p) c -> p k c", p=128), [hd[1]], [hd[0]])
            if q < 2:
                self.ts("pool", self.woutb[:, 2 * q:2 * q + 2, :], st, self.gsub[:, 0:1], None, ALU.mult, ALU.bypass,
                        [hd[0], hd[1], self.setup_dep], [self.woutb_dep])
            else:
                self.cp("pool", self.woutb[:, 2 * q:2 * q + 2, :], st, [hd[0], hd[1]], [self.woutb_dep])
        dctr = 0
        sctr = 0
        hctr = 0
        for G, tiles in enumerate(QGS):
            q0 = TILES[tiles[0]][0]
            qn_ = sum(TILES[t][1] for t in tiles)
            for il, i in enumerate(tiles):
                t0, ni = TILES[i]
                for h in range(4):
                    pbk = h // 2
                    self.mm(self.psum[pbk][0:ni, (h % 2) * 256:(h % 2) * 256 + 256], self.qbT[:, h, t0:t0 + ni], self.wukb[:, h, :],
                            True, True, [self.qbT_dep[G], self.setup_dep], [self.pdep[pbk]])
                k4, sdeps = self.stat()
                ssd, td, rsd = sdeps
                for pbk in range(2):
                    self.act(self.junk[0:ni, pbk * 512:(pbk + 1) * 512], self.psum[pbk][0:ni, :], AF.Square, [self.pdep[pbk]],
                             [self.junk_dep])
                self.red(self.ss[0:ni, k4:k4 + 4], self.junk[0:ni, :].rearrange("p (g d) -> p g d", d=256), ALU.add,
                         [self.junk_dep], [ssd])
                self.rstd(ni, k4, 4, 1.0 / 256, sdeps)
                for pbk in range(2):
                    self.tt("dve", self.qn1k[0:ni, pbk * 512:(pbk + 1) * 512].rearrange("p (g d) -> p g d", d=256),
                            self.psum[pbk][0:ni, :].rearrange("p (g d) -> p g d", d=256),
                            self.rs[0:ni, k4 + 2 * pbk:k4 + 2 * pbk + 2].unsqueeze(2).to_broadcast([ni, 2, 256]), ALU.mult,
                            [self.pdep[pbk], rsd], [self.qn1k_dep])
                pT = self.psum[2].bitcast(BF16)
                for ch in range(8):
                    self.tr(pT[:, ch * 128:ch * 128 + ni], self.qn1k[0:ni, ch * 128:(ch + 1) * 128], self.ident[0:ni, 0:ni],
                            [self.qn1k_dep, self.ident_dep], [self.pdep[2]])
                pv = pT.rearrange("p (h c t) -> p h c t", h=4, c=2)
                self.ts("dve", self.qlT[:, :, 0, il * 128:il * 128 + ni], pv[:, :, 0, 0:ni], self.gqb[0], None, ALU.mult,
                        ALU.bypass, [self.pdep[2], self.setup_dep], [self.qlT_dep[il]])
                self.ts("dve", self.qlT[:, :, 1, il * 128:il * 128 + ni], pv[:, :, 1, 0:ni], self.gqb[1], None, ALU.mult,
                        ALU.bypass, [self.pdep[2], self.setup_dep], [self.qlT_dep[il]])
            for il, i in enumerate(tiles):
                t0, ni = TILES[i]
                Lk = t0 + ni
                acc = self.acc
                for kc0 in range(0, Lk, 512):
                    kcn = min(512, Lk - kc0)
                    g = kc0 // 512
                    for hh in range(8):
                        pb = 3 + (dctr % 2)
                        rb = dctr % 2
                        dctr += 1
                        pr = (hh % 2) * 64
                        self.mm(self.psum[pb][0:ni, 0:kcn], self.iqT[pr:pr + 64, hh // 2, t0:t0 + ni], self.ikT[pr:pr + 64, kc0:kc0 + kcn],
                                True, True, [self.iqT_dep[G], self.ikT_dep[g]], [self.pdep[pb]])
                        self.act(self.rr[rb][0:ni, 0:kcn], self.psum[pb][0:ni, 0:kcn], AF.Relu, [self.pdep[pb]], [self.rr_dep[rb]])
                        if hh == 0:
                            self.ts("dve", acc[0:ni, kc0:kc0 + kcn], self.rr[rb][0:ni, 0:kcn], self.iw_sb[0:ni, i, 0:1], None,
                                    ALU.mult, ALU.bypass, [self.rr_dep[rb], self.iw_dep[i]], [self.acc_dep])
                        else:
                            self.stt(acc[0:ni, kc0:kc0 + kcn], self.rr[rb][0:ni, 0:kcn], self.iw_sb[0:ni, i, hh:hh + 1],
                                     acc[0:ni, kc0:kc0 + kcn], ALU.mult, ALU.add, [self.rr_dep[rb], self.iw_dep[i], self.acc_dep],
                                     [self.acc_dep])
                bs = self.bst
                bd = self.bst_dep
                if i >= 2:
                    self.red(bs[0:ni, 0:1], acc[0:ni, 0:Lk], ALU.max, [self.acc_dep], [bd])
                    self.red(bs[0:ni, 1:2], acc[0:ni, 0:Lk], ALU.min, [self.acc_dep], [bd])
                self.tt("dve", acc[0:ni, t0:t0 + ni], acc[0:ni, t0:t0 + ni], self.masktok[0:ni, 0:ni], ALU.add,
                        [self.acc_dep, self.masktok_dep], [self.acc_dep])
                if i >= 2:
                    self.tt("dve", bs[0:ni, 2:3], bs[0:ni, 0:1], bs[0:ni, 1:2], ALU.subtract, [bd], [bd])
                    W0 = 8
                    self.ts("dve", bs[0:ni, W0:W0 + NBIS + 1], self.pw[0:ni, :], bs[0:ni, 2:3], None, ALU.mult, ALU.bypass,
                            [bd, self.setup_dep], [bd])
                    M0 = W0 + NBIS + 1
                    self.tt("dve", bs[0:ni, M0:M0 + 1], bs[0:ni, 1:2], bs[0:ni, W0:W0 + 1], ALU.add, [bd], [bd])
                    C0 = M0 + NBIS + 1
                    for k in range(NBIS):
                        self.ts("dve", self.cntj[0:ni, 0:Lk], acc[0:ni, 0:Lk], bs[0:ni, M0 + k:M0 + k + 1], None, ALU.is_ge, ALU.add,
                                [self.acc_dep, bd], [self.cntj_dep, bd], accum=bs[0:ni, C0 + k:C0 + k + 1])
                        self.ts("dve", bs[0:ni, 3:4], bs[0:ni, C0 + k:C0 + k + 1], TOPK - 0.5, bs[0:ni, W0 + k:W0 + k + 1],
                                ALU.is_ge, ALU.mult, [bd], [bd])
                        wn = W0 + k + 1 if k < NBIS - 1 else W0 + k
                        self.stt(bs[0:ni, M0 + k + 1:M0 + k + 2], bs[0:ni, 3:4], bs[0:ni, wn:wn + 1], bs[0:ni, M0 + k:M0 + k + 1],
                                 ALU.subtract, ALU.add, [bd], [bd])
                    theta = bs[0:ni, M0 + NBIS:M0 + NBIS + 1]
                    thd = [bd]
                else:
                    theta = self.thc[0:ni, 0:1]
                    thd = [self.setup_dep]
                self.ts("dve", self.maskb[0:ni, 0:Lk], acc[0:ni, 0:Lk], theta, NEG, ALU.is_lt, ALU.mult, [self.acc_dep] + thd,
                        [self.maskb_dep])
                for j0 in range(0, i + 1, 8):
                    js = list(range(j0, min(j0 + 8, i + 1)))
                    tb = 5 + ((j0 // 8) % 2)
                    pT = self.psum[tb].bitcast(BF16)
                    for jj, j in enumerate(js):
                        k0, kn = TILES[j]
                        self.tr(pT[0:kn, jj * 128:jj * 128 + ni], self.maskb[0:ni, k0:k0 + kn], self.ident[0:ni, 0:ni],
                                [self.maskb_dep, self.ident_dep], [self.pdep[tb]])
                    src = pT.rearrange("p (j t) -> p j t", j=8)[:, 0:len(js), 0:ni]
                    self.cp("dve", self.mT[:, j0:j0 + len(js), il * 128:il * 128 + ni], src, [self.pdep[tb]], [self.mT_dep[il]])
            for h in range(4):
                started = set()
                nblk = tiles[-1] + 1

                def stage1(j, h=h, tiles=tiles, q0=q0, qn_=qn_):
                    k0, kn = TILES[j]
                    c0 = max(q0, k0)
                    ncol = q0 + qn_ - c0
                    co = c0 - q0
                    bank = j % 4
                    ps = self.psum[bank][0:kn, 0:ncol]
                    qd = [self.qlT_dep[il] for il, t in enumerate(tiles) if TILES[t][0] + TILES[t][1] > c0]
                    md = [self.mT_dep[il] for il, t in enumerate(tiles) if TILES[t][0] + TILES[t][1] > c0]
                    for cc in range(2):
                        self.mm(ps, self.cT[:, cc, k0:k0 + kn], self.qlT[:, h, cc, co:co + ncol], cc == 0, False,
                                [self.cT_dep[j]] + qd, [self.pdep[bank]])
                    self.mm(ps, self.ident[0:kn, 0:kn], self.mT[0:kn, j, co:co + ncol], False, True, [self.ident_dep] + md,
                            [self.pdep[bank]])
                    self._bias_blocks(ps, bank, 4 + h, j, tiles, c0)
                    self.act(self.PT[bank][0:kn, 0:ncol], ps, AF.Exp, [self.pdep[bank], self.setup_dep], [self.PT_dep[bank]],
                             bias=self.abias[0:kn, 4 + h:5 + h])

                def stage2(j, h=h, tiles=tiles, q0=q0, started=started):
                    k0, kn = TILES[j]
                    c0 = max(q0, k0)
                    pbuf = j % 4
                    for il, i in enumerate(tiles):
                        if i < j:
                            continue
                        off = TILES[i][0] - c0
                        ni = TILES[i][1]
                        ab = 6 + il // 3
                        o = (il % 3) * 129
                        first = ab not in started
                        started.add(ab)
                        self.mm(self.psum[ab][0:ni, o:o + 129], self.PT[pbuf][0:kn, off:off + ni], self.VB[0:kn, j, h, :],
                                first, j == i, [self.PT_dep[pbuf], self.VB_dep[j]], [self.pdep[ab]])

                self._pipe(nblk, stage1, stage2)
                for il, i in enumerate(tiles):
                    ni = TILES[i][1]
                    ab = 6 + il // 3
                    o = (il % 3) * 129
                    k4, sdeps = self.stat()
                    ssd, td, rsd = sdeps
                    self.recip(self.rs[0:ni, k4:k4 + 1], self.psum[ab][0:ni, o + 128:o + 129], [self.pdep[ab]], [rsd])
                    self.ts("dve", self.obG[0:ni, il, h * 128:(h + 1) * 128], self.psum[ab][0:ni, o:o + 128], self.rs[0:ni, k4:k4 + 1],
                            None, ALU.mult, ALU.bypass, [self.pdep[ab], rsd], [self.obG_dep[il]])
            for il, i in enumerate(tiles):
                t0, ni = TILES[i]
                pT = self.psum[2].bitcast(BF16)
                for k in range(8):
                    src = self.oa[0:ni, i, k * 128:(k + 1) * 128] if k < 4 else self.obG[0:ni, il, (k - 4) * 128:(k - 3) * 128]
                    sd_ = self.oa_dep[i] if k < 4 else self.obG_dep[il]
                    self.tr(pT[:, k * 128:k * 128 + ni], src, self.ident[0:ni, 0:ni], [sd_, self.ident_dep], [self.pdep[2]])
                self.act(self.ocT[:, :, 0:ni], pT.rearrange("p (k t) -> p k t", k=8)[:, :, 0:ni], AF.Copy, [self.pdep[2]],
                         [self.ocT_dep])
                hb = hctr % 2
                hctr += 1
                self.dma(self.hst[hb][0:ni, :], self.hs[t0:t0 + ni, :], [self.hs_dep[i]], [self.hst_dep[hb]])
                for half in range(2):
                    pb = 3 + half
                    for k in range(8):
                        self.mm(self.psum[pb][0:ni, :], self.ocT[:, k, 0:ni], self.woutb[:, k, half * 512:(half + 1) * 512],
                                k == 0, k == 7, [self.ocT_dep, self.woutb_dep], [self.pdep[pb]])
                    hv = self.hst[hb][0:ni, half * 512:(half + 1) * 512]
                    self.tt("dve", hv, self.psum[pb][0:ni, :], hv, ALU.add, [self.pdep[pb], self.hst_dep[hb]], [self.hst_dep[hb]])
                self.dma(self.hs[t0:t0 + ni, :], self.hst[hb][0:ni, :], [self.hst_dep[hb]], [self.hs_dep[i]])

    def _finish(self):
        if _os.environ.get("K_DUMP"):
            dd = Dep("dump")
            self.P.barrier()
            self.dma(self.hs[0:128, 0:16], self.smallc[:, 0:16], [self.setup_dep], [dd])
            self.dma(self.hs[0:128, 16:24], self.abias, [self.setup_dep], [dd])
            self.P.add("sp", None, [dd], [])
        self.P.add("sp", None, [self.out_dep], [])
        if self.dbg is not None:
            pass


def _bucket(n):
    n = np.maximum(n, 0)
    nf = np.maximum(n, 1).astype(np.float32)
    large = 16 + (np.log(nf / np.float32(16)) / np.float32(math.log(128 / 16)) * np.float32(16)).astype(np.int32)
    large = np.minimum(large, 31)
    return np.where(n < 16, n, large)


def _prep_shared(inp):
    f = lambda a: np.ascontiguousarray(np.asarray(a, dtype=np.float32))
    sh = {}
    sh["meta"] = f(inp["meta_tokens"])
    for i, nm in ((1, "ffn1"), (2, "ffn2")):
        sh["w%dg" % i] = f(inp[nm + "_w_gate"][0])
        sh["w%du" % i] = f(inp[nm + "_w_up"][0])
        sh["w%dd" % i] = f(inp[nm + "_w_down"][0])
    w_in = f(inp["w_in"][0])
    qa, ka, va, qb, ckv, iq, ik, iw = np.split(w_in, np.cumsum([512, 512, 512, 512, 256, 512, 64, 8])[:-1], axis=1)
    parts = []
    for h in range(4):
        parts += [qa[:, h * 128:(h + 1) * 128], ka[:, h * 128:(h + 1) * 128], va[:, h * 128:(h + 1) * 128]]
    parts += [ckv, iw]
    parts += [qb, iq, ik, ik]
    sh["win"] = np.ascontiguousarray(np.concatenate(parts, axis=1))
    assert sh["win"].shape[1] == WIN_COLS
    sh["wuk"] = np.ascontiguousarray(f(inp["b_w_uk"][0]).transpose(1, 0, 2))
    sh["wuv"] = np.ascontiguousarray(f(inp["b_w_uv"][0]).reshape(4, 2, 128, 128).transpose(2, 1, 0, 3))
    sh["wout"] = f(inp["w_out"][0])
    cols = np.zeros((128, 40), np.float32)
    cols[:, 0:8] = f(inp["ffn1_norm"][0]).reshape(8, 128).T
    cols[:, 8:16] = f(inp["mix_norm"][0]).reshape(8, 128).T
    cols[:, 16:24] = f(inp["ffn2_norm"][0]).reshape(8, 128).T
    cols[:, 24] = np.tile(f(inp["a_q_norm"][0]), 2)
    cols[:, 25] = np.tile(f(inp["a_k_norm"][0]), 2)
    cols[:, 26:28] = f(inp["b_kv_norm"][0]).reshape(2, 128).T
    cols[:, 28:30] = f(inp["b_q_norm"][0]).reshape(2, 128).T
    cols[:, 30] = f(inp["a_subln"][0])
    rb = f(inp["rel_bias"])
    cols[:, 31:39] = np.broadcast_to(rb[31], (128, 8))
    sh["cols"] = cols
    rows = np.zeros((128, 896), np.float32)
    rows[:, 0:64] = f(inp["a_lambda_q1"][0])[None]
    rows[:, 64:128] = f(inp["a_lambda_k1"][0])[None]
    rows[:, 128:192] = f(inp["a_lambda_q2"][0])[None]
    rows[:, 192:256] = f(inp["a_lambda_k2"][0])[None]
    rows[:, 256:320] = f(inp["a_q_norm"][0])[None]
    rows[:, 320:384] = f(inp["a_k_norm"][0])[None]
    rows[:, 384:640] = f(inp["b_q_norm"][0])[None]
    rows[:, 640:896] = f(inp["b_kv_norm"][0])[None]
    sh["rows"] = rows
    tk = np.arange(128)[:, None]
    tq = np.arange(128)[None, :]
    bb = np.zeros((128, 8, 2, 128), np.float32)
    for blk in range(2):
        idx = _bucket(tq - tk + 128 * blk)
        bb[:, :, blk, :] = rb[idx].transpose(0, 2, 1)
    sh["biasblk"] = bb
    cm = np.zeros((128, 3, 128), np.float32)
    cm[:, 0, :] = np.eye(128, dtype=np.float32)
    cm[:, 1, :] = np.where(tq >= tk, 0.0, NEG)
    cm[:, 2, :] = np.where(tq <= tk, 0.0, -1e30)
    sh["cmask"] = cm
    return sh


_CACHE = {}


def kernel(**inputs):
    x = np.asarray(inputs["x"], dtype=np.float32)
    sh = _prep_shared(inputs)
    if "nc" not in _CACHE:
        _CACHE["nc"] = Builder().build()
    nc = _CACHE["nc"]
    in_maps = []
    for c in range(NCORES):
        m = dict(sh)
        m["x"] = np.ascontiguousarray(x[c * NSEQ:(c + 1) * NSEQ])
        in_maps.append(m)
    res = run_bass_kernel_spmd(nc, in_maps, core_ids=list(range(NCORES)))
    out = np.concatenate([np.asarray(r["out"]) for r in res.results], axis=0)
    return out.astype(np.float32)
```

```python
import math
import os as _os
from contextlib import ExitStack

import numpy as np
import concourse.bass as bass
import concourse.mybir as mybir
from concourse.bass_utils import run_bass_kernel_spmd

F32 = mybir.dt.float32
BF16 = mybir.dt.bfloat16
ALU = mybir.AluOpType
AF = mybir.ActivationFunctionType
AX = mybir.AxisListType

D = 1024
SEQ = 2048
NMETA = 16
L = SEQ + NMETA
DFF = 2816
NSEQ = 2
NCORES = 8
NT = 17
TILES = [(i * 128, min(128, L - i * 128)) for i in range(NT)]
TGS = [(0, 512), (512, 512), (1024, 512), (1536, 512), (2048, 16)]
QGS = [[0, 1, 2, 3], [4, 5, 6, 7], [8, 9, 10, 11], [12, 13, 14, 15], [16]]
EPS = 1e-6
A_SCALE = 64 ** -0.5
B_SCALE = 256 ** -0.5
LAM_INIT = 0.8 - 0.6 * math.exp(0.0)
TOPK = 256
NEG = -30000.0
NBIS = 12
WIN_COLS = 4 * 384 + 264 + 9 * 128
STRICT = True
EPOCH = 2000


def _dsize(dt):
    return 4 if dt == F32 else 2


class Dep:
    __slots__ = ("name", "lw", "rd", "sem", "dcount")

    def __init__(self, name):
        self.name = name
        self.lw = None
        self.rd = []
        self.sem = None
        self.dcount = 0


class Op:
    __slots__ = ("eng", "fn", "deps", "dma", "signal", "seq", "sem", "waits", "wdep")

    def __init__(self, eng, fn, deps, dma, wdep):
        self.eng = eng
        self.fn = fn
        self.deps = deps
        self.dma = dma
        self.signal = False
        self.seq = 0
        self.sem = None
        self.waits = []
        self.wdep = wdep


class Prog:
    ENGS = ("sp", "pe", "act", "dve", "pool")

    def __init__(self, nc):
        self.nc = nc
        self.ops = []
        self.last = {e: None for e in self.ENGS}
        self.dmas_since_barrier = []

    def add(self, eng, fn, r=(), w=(), dma=False):
        idx = len(self.ops)
        deps = set()
        for t in r:
            if t.lw is not None:
                deps.add(t.lw)
        for t in w:
            if t.lw is not None:
                deps.add(t.lw)
            deps.update(t.rd)
        for t in r:
            t.rd.append(idx)
        for t in w:
            t.lw = idx
            t.rd = []
        wdep = None
        if dma:
            assert len(w) == 1
            wdep = w[0]
            self.dmas_since_barrier.append(idx)
        self.ops.append(Op(eng, fn, deps, dma, wdep))
        self.last[eng] = idx
        return idx

    def barrier(self):
        lasts = {v for v in self.last.values() if v is not None}
        lasts.update(self.dmas_since_barrier)
        self.dmas_since_barrier = []
        for e in self.ENGS:
            idx = len(self.ops)
            self.ops.append(Op(e, None, set(lasts), False, None))
            self.last[e] = idx

    def emit(self, stack):
        nc = self.nc
        ops = self.ops
        for op in ops:
            for d in sorted(op.deps):
                dop = ops[d]
                if not dop.dma and dop.eng == op.eng and (op.eng == "pe" or not STRICT):
                    continue
                if dop.fn is None:
                    continue
                dop.signal = True
                op.waits.append(d)
        esem = {e: stack.enter_context(nc.semaphore("s_" + e)) for e in self.ENGS}
        cnt = {e: 0 for e in self.ENGS}
        NCH = 8
        chsem = [stack.enter_context(nc.semaphore("dch%d" % i)) for i in range(NCH)]
        chcnt = [0] * NCH
        chlast = [None] * NCH
        ndma = 0
        for oi, op in enumerate(ops):
            if op.fn is None:
                continue
            if op.dma:
                c = ndma % NCH
                ndma += 1
                if chlast[c] is not None:
                    op.waits.append(chlast[c])
                chlast[c] = oi
                chcnt[c] += 16
                op.sem = chsem[c]
                op.seq = chcnt[c]
            elif op.signal:
                if cnt[op.eng] >= EPOCH:
                    esem[op.eng] = stack.enter_context(nc.semaphore("s_%s_%d" % (op.eng, oi)))
                    cnt[op.eng] = 0
                cnt[op.eng] += 1
                op.sem = esem[op.eng]
                op.seq = cnt[op.eng]
        per = {e: [op for op in ops if op.eng == e] for e in self.ENGS}

        def run(e, h):
            waited = {}
            for op in per[e]:
                need = {}
                for d in op.waits:
                    dop = ops[d]
                    k = id(dop.sem)
                    if k not in need or need[k][1] < dop.seq:
                        need[k] = (dop.sem, dop.seq)
                for k, (sem, val) in need.items():
                    if waited.get(k, 0) >= val:
                        continue
                    h.wait_ge(sem, val)
                    waited[k] = val
                if op.fn is None:
                    continue
                ins = op.fn(h)
                if op.dma:
                    ins.then_inc(op.sem, 16)
                elif op.signal:
                    ins.then_inc(op.sem, 1)

        with nc.Block() as block:
            @block.sync
            def _(h):
                run("sp", h)

            @block.tensor
            def _(h):
                run("pe", h)

            @block.scalar
            def _(h):
                run("act", h)

            @block.vector
            def _(h):
                run("dve", h)

            @block.gpsimd
            def _(h):
                run("pool", h)


class Builder:
    def __init__(self, stage=99, nseq=NSEQ, dbg=False, dbg_stop=False):
        self.dbg_stop = dbg_stop
        self.stage = stage
        self.nseq = nseq
        self.nc = nc = bass.Bass("TRN2", target_bir_lowering=False)
        self.P = Prog(nc)
        self.cur = 16640
        dt = nc.dram_tensor
        self.x = dt("x", [NSEQ, SEQ, D], F32, kind="ExternalInput").ap()
        self.meta = dt("meta", [NMETA, D], F32, kind="ExternalInput").ap()
        self.wg = [dt("w%dg" % i, [D, DFF], F32, kind="ExternalInput").ap() for i in (1, 2)]
        self.wu = [dt("w%du" % i, [D, DFF], F32, kind="ExternalInput").ap() for i in (1, 2)]
        self.wd = [dt("w%dd" % i, [DFF, D], F32, kind="ExternalInput").ap() for i in (1, 2)]
        self.win = dt("win", [D, WIN_COLS], F32, kind="ExternalInput").ap()
        self.wuk = dt("wuk", [128, 4, 256], F32, kind="ExternalInput").ap()
        self.wuv = dt("wuv", [128, 2, 4, 128], F32, kind="ExternalInput").ap()
        self.wout = dt("wout", [D, D], F32, kind="ExternalInput").ap()
        self.cols = dt("cols", [128, 40], F32, kind="ExternalInput").ap()
        self.rows = dt("rows", [128, 896], F32, kind="ExternalInput").ap()
        self.bias = dt("biasblk", [128, 8, 2, 128], F32, kind="ExternalInput").ap()
        self.cmask = dt("cmask", [128, 3, 128], F32, kind="ExternalInput").ap()
        self.out = dt("out", [NSEQ, SEQ, D], F32, kind="ExternalOutput").ap()
        self.hs = dt("hs", [L, D], F32, kind="ExternalOutput").ap()
        self.dbg = dt("dbg", [128, 4096], F32, kind="ExternalOutput").ap() if dbg else None
        self.psum = [nc.alloc_psum_tensor("pb%d" % i, [128, 512], F32).ap() for i in range(8)]
        self.pdep = [Dep("pb%d" % i) for i in range(8)]
        self.out_dep = Dep("out")

    def sb(self, name, shape, dtype, at=None):
        n = 1
        for s in shape[1:]:
            n *= s
        nbytes = (n * _dsize(dtype) + 63) // 64 * 64
        off = self.cur if at is None else at
        t = self.nc.alloc_sbuf_tensor_at(name, list(shape), dtype, offset=off)
        if at is None:
            self.cur = off + nbytes
        assert off + nbytes <= 229376, (name, off + nbytes)
        return t.ap()

    def mm(self, out, lhsT, rhs, start, stop, r, w):
        self.P.add("pe", lambda e: e.matmul(out, lhsT, rhs, start=start, stop=stop, skip_group_check=True), r, w)

    def tr(self, out, in_, ident, r, w):
        self.P.add("pe", lambda e: e.transpose(out, in_, ident), r, w)

    def act(self, out, in_, func, r, w, bias=0.0, scale=1.0, accum=None):
        if accum is None:
            self.P.add("act", lambda e: e.activation(out, in_, func, bias=bias, scale=scale), r, w)
        else:
            self.P.add("act", lambda e: e.activation(out, in_, func, bias=bias, scale=scale, accum_out=accum), r, w)

    def ts(self, eng, out, in0, s1, s2, op0, op1, r, w, accum=None):
        if accum is None:
            self.P.add(eng, lambda e: e.tensor_scalar(out, in0, s1, s2, op0, op1), r, w)
        else:
            self.P.add(eng, lambda e: e.tensor_scalar(out, in0, s1, s2, op0, op1, accum_out=accum), r, w)

    def tt(self, eng, out, in0, in1, op, r, w):
        self.P.add(eng, lambda e: e.tensor_tensor(out, in0, in1, op), r, w)

    def stt(self, out, in0, scalar, in1, op0, op1, r, w):
        self.P.add("dve", lambda e: e.scalar_tensor_tensor(out, in0, scalar, in1, op0, op1), r, w)

    def cp(self, eng, out, in_, r, w):
        self.P.add(eng, lambda e: e.tensor_copy(out, in_), r, w)

    def red(self, out, in_, op, r, w, absval=False):
        self.P.add("dve", lambda e: e.tensor_reduce(out, in_, AX.X, op, apply_absolute_value=absval), r, w)

    def recip(self, out, in_, r, w):
        self.P.add("dve", lambda e: e.reciprocal(out, in_), r, w)

    def memset(self, eng, ap, val, w):
        self.P.add(eng, lambda e: e.memset(ap, val), (), w)

    def dma(self, out, in_, r, w):
        self.P.add("sp", lambda e: e.dma_start(out=out, in_=in_), r, w, dma=True)

    def stat(self):
        i = self.st_i
        self.st_i = (i + 1) % 8
        return 4 * i, self.st_dep[i]

    def rstd(self, np_, k, nc_, inv_n, deps):
        ssd, td, rsd = deps
        self.ts("dve", self.tmp[0:np_, k:k + nc_], self.ss[0:np_, k:k + nc_], inv_n, EPS, ALU.mult, ALU.add, [ssd], [td])
        self.tt("pool", self.rs[0:np_, k:k + nc_], self.tmp[0:np_, k:k + nc_], self.neghalf[0:np_, 0:nc_], ALU.pow,
                [td, self.nh_dep], [rsd])

    def build(self):
        nc = self.nc
        stage = self.stage
        with ExitStack() as stack:
            self._alloc()
            self._setup()
            for s in range(self.nseq):
                self._sequence(s)
            self._finish()
            self.P.emit(stack)
        return nc

    def _alloc(self):
        sb = self.sb
        self.cols_sb = sb("cols", [128, 40], F32)
        self.rows_sb = sb("rows", [128, 896], F32)
        self.ident_f = sb("identf", [128, 128], F32)
        self.ident = sb("ident", [128, 128], BF16)
        self.neghalf = sb("neghalf", [128, 16], F32)
        self.smallc = sb("smallc", [128, 32], F32)
        self.gqk = self.smallc[:, 6:7]
        self.gqb = [self.smallc[:, 2:3], self.smallc[:, 10:11]]
        self.lamn = self.smallc[:, 14:15]
        self.abias = sb("abias", [128, 8], F32)
        self.gsub = self.smallc[:, 18:19]
        self.sm = sb("small", [128, 272], F32)
        self.biasbf = sb("biasbf", [128, 8, 2, 128], BF16)
        self.masktok = sb("masktok", [128, 128], F32)
        self.iw_sb = sb("iw", [128, NT, 8], F32)
        self.ss = sb("ss", [128, 32], F32)
        self.tmp = sb("tmpst", [128, 32], F32)
        self.rs = sb("rs", [128, 32], F32)
        self.st_dep = [(Dep("ss%d" % i), Dep("tm%d" % i), Dep("rs%d" % i)) for i in range(8)]
        self.st_i = 0
        self.wukb = sb("wukb", [128, 4, 256], BF16)
        self.wuvb = sb("wuvb", [128, 2, 4, 128], BF16)
        self.maskT = sb("maskT", [128, 128], F32)
        self.pw = sb("pw", [128, NBIS + 1], F32)
        self.thc = self.smallc[:, 22:23]
        self.xn_s = [sb("xn_s%d" % i, [128, D], BF16) for i in range(2)]
        self.xn_s_dep = [Dep("xn_s%d" % i) for i in range(2)]
        self.junk = sb("junk", [128, D], BF16)
        self.junk_dep = Dep("junk")
        self.c_const = self.cur
        self.xnT = sb("xnT", [128, 8, L], BF16)
        self.xn_dep = [Dep("xnT%d" % t) for t in range(NT)]
        self.h_off = self.cur
        self.h = sb("h", [128, NT, D], F32)
        self.h_dep = [Dep("h%d" % t) for t in range(NT)]
        self.big_off = self.cur
        self.phase_off = self.cur

    def _setup(self):
        P = self.P
        cd = self.cdep = Dep("consts")
        self.dma(self.cols_sb, self.cols, [], [cd])
        rd = Dep("rows")
        self.dma(self.rows_sb, self.rows, [], [rd])
        idd = Dep("identf")
        self.dma(self.ident_f, self.cmask[:, 0, :], [], [idd])
        mk = Dep("masktok")
        self.dma(self.masktok, self.cmask[:, 2, :], [], [mk])
        self.masktok_dep = mk
        self.ident_dep = Dep("ident")
        self.cp("dve", self.ident, self.ident_f, [idd], [self.ident_dep])
        self.nh_dep = Dep("neghalf")
        self.memset("dve", self.neghalf, -0.5, [self.nh_dep])
        if self.stage <= 1:
            return
        self._ffn_alloc()
        C = self.cols_sb
        R = self.rows_sb
        sd = self.setup_dep = Dep("setup")
        smd = Dep("sm")
        mtd = Dep("maskT")
        self.dma(self.maskT, self.cmask[:, 1, :], [], [mtd])
        self.stt(self.gqk, C[:, 24:25], A_SCALE, C[:, 25:26], ALU.mult, ALU.mult, [cd], [sd])
        for cc in range(2):
            self.ts("dve", self.gqb[cc], C[:, 28 + cc:29 + cc], B_SCALE, None, ALU.mult, ALU.bypass, [cd], [sd])
        self.ts("dve", self.gsub, C[:, 30:31], 1.0 - LAM_INIT, None, ALU.mult, ALU.bypass, [cd], [sd])
        sm = self.sm
        self.tt("dve", sm[:, 0:64], R[:, 0:64], R[:, 64:128], ALU.mult, [rd], [smd])
        self.red(sm[:, 256:257], sm[:, 0:64], ALU.add, [smd], [smd])
        self.tt("dve", sm[:, 64:128], R[:, 128:192], R[:, 192:256], ALU.mult, [rd], [smd])
        self.red(sm[:, 257:258], sm[:, 64:128], ALU.add, [smd], [smd])
        self.act(sm[:, 258:260], sm[:, 256:258], AF.Exp, [smd], [smd])
        self.tt("dve", sm[:, 260:261], sm[:, 258:259], sm[:, 259:260], ALU.subtract, [smd], [smd])
        self.ts("dve", self.lamn, sm[:, 260:261], -1.0, -LAM_INIT, ALU.mult, ALU.add, [smd], [sd])
        self.tt("dve", sm[:, 0:64], R[:, 256:320], R[:, 320:384], ALU.mult, [rd, smd], [smd])
        self.red(sm[:, 261:262], sm[:, 0:64], ALU.max, [smd], [smd], absval=True)
        self.ts("dve", sm[:, 262:263], sm[:, 261:262], 64.0 * A_SCALE, None, ALU.mult, ALU.bypass, [smd], [smd])
        self.ts("dve", self.abias[:, 0:4], C[:, 31:35], sm[:, 262:263], None, ALU.subtract, ALU.bypass, [smd, cd], [sd])
        self.tt("dve", sm[:, 0:256], R[:, 384:640], R[:, 640:896], ALU.mult, [rd, smd], [smd])
        self.red(sm[:, 263:264], sm[:, 0:256], ALU.max, [smd], [smd], absval=True)
        self.ts("dve", sm[:, 264:265], sm[:, 263:264], 256.0 * B_SCALE, None, ALU.mult, ALU.bypass, [smd], [smd])
        self.ts("dve", self.abias[:, 4:8], C[:, 35:39], sm[:, 264:265], None, ALU.subtract, ALU.bypass, [smd, cd], [sd])
        st = self.stg[0].rearrange("p (h b c) -> p h b c", h=8, b=2)
        self.dma(st, self.bias, [], [self.stg_dep[0]])
        for hh in range(8):
            self.stt(self.biasbf[:, hh, 0, :], st[:, hh, 0, :], C[:, 31 + hh:32 + hh], self.maskT, ALU.subtract, ALU.add,
                     [self.stg_dep[0], cd, mtd], [sd])
            self.ts("dve", self.biasbf[:, hh, 1, :], st[:, hh, 1, :], C[:, 31 + hh:32 + hh], None, ALU.subtract, ALU.bypass,
                    [self.stg_dep[0], cd], [sd])
        s1 = self.stg[1].rearrange("p (h c) -> p h c", h=4)[:, :, 0:256]
        self.dma(s1, self.wuk, [], [self.stg_dep[1]])
        self.cp("pool", self.wukb, s1, [self.stg_dep[1]], [sd])
        s2 = self.stg[2][:, 0:1024].rearrange("p (a h c) -> p a h c", a=2, h=4)
        self.dma(s2, self.wuv, [], [self.stg_dep[2]])
        self.cp("pool", self.wuvb, s2, [self.stg_dep[2]], [sd])
        for k in range(NBIS + 1):
            self.memset("pool", self.pw[:, k:k + 1], 2.0 ** -(k + 1), [sd])
        self.memset("pool", self.thc, -1e29, [sd])

    def _sequence(self, s):
        self._load_h(s, from_x=True)
        self._norm_pass(0)
        self._ffn(0)
        if self.stage <= 1:
            self._store_out(s)
            return
        self._norm_pass(1)
        self.hs_dep = getattr(self, "hs_dep", None) or [Dep("hs%d" % t) for t in range(NT)]
        for t, (t0, n) in enumerate(TILES):
            self.dma(self.hs[t0:t0 + n, :], self.h[0:n, t, :], [self.h_dep[t]], [self.hs_dep[t]])
        self.P.barrier()
        self._attn_alloc()
        if not _os.environ.get("K_SKIP_PROJ"):
            self._proj()
        self.P.barrier()
        if self.stage >= 3:
            self._attnA()
            self.P.barrier()
        if self.stage >= 4:
            self._attnB()
            self.P.barrier()
        if self.dbg_stop:
            return
        if not _os.environ.get("K_NO_RELOAD"):
            self._load_h(s, from_x=False)
        if not _os.environ.get("K_NO_FFN2"):
            self._norm_pass(2)
            self._ffn(1)
        self._store_out(s)

    def _load_h(self, s, from_x):
        for t, (t0, n) in enumerate(TILES):
            hd = self.h_dep[t]
            if from_x:
                if t == 0:
                    self.dma(self.h[0:NMETA, 0, :], self.meta, [], [hd])
                    self.dma(self.h[NMETA:128, 0, :], self.x[s, 0:128 - NMETA, :], [], [hd])
                else:
                    self.dma(self.h[0:n, t, :], self.x[s, t0 - NMETA:t0 - NMETA + n, :], [], [hd])
            else:
                self.dma(self.h[0:n, t, :], self.hs[t0:t0 + n, :], [self.hs_dep[t]], [hd])

    def _store_out(self, s):
        for t, (t0, n) in enumerate(TILES):
            if t == 0:
                self.dma(self.out[s, 0:128 - NMETA, :], self.h[NMETA:128, 0, :], [self.h_dep[0]], [self.out_dep])
            else:
                self.dma(self.out[s, t0 - NMETA:t0 - NMETA + n, :], self.h[0:n, t, :], [self.h_dep[t]], [self.out_dep])

    def _norm_pass(self, which):
        for t, (t0, n) in enumerate(TILES):
            b = t % 2
            k, (ssd, td, rsd) = self.stat()
            ss = self.ss[0:n, k:k + 1]
            self.act(self.junk[0:n, :], self.h[0:n, t, :], AF.Square, [self.h_dep[t]], [self.junk_dep, ssd], accum=ss)
            self.ts("dve", self.tmp[0:n, k:k + 1], ss, 1.0 / D, EPS, ALU.mult, ALU.add, [ssd], [td])
            self.tt("pool", self.rs[0:n, k:k + 1], self.tmp[0:n, k:k + 1], self.neghalf[0:n, 0:1], ALU.pow,
                    [td, self.nh_dep], [rsd])
            self.ts("dve", self.xn_s[b][0:n, :], self.h[0:n, t, :], self.rs[0:n, k:k + 1], None, ALU.mult, ALU.bypass,
                    [self.h_dep[t], rsd], [self.xn_s_dep[b]])
            pb = self.psum[b].bitcast(BF16)
            for kk in range(8):
                self.tr(pb[:, kk * 128:kk * 128 + n], self.xn_s[b][0:n, kk * 128:(kk + 1) * 128],
                        self.ident[0:n, 0:n], [self.xn_s_dep[b], self.ident_dep], [self.pdep[b]])
            src = pb.rearrange("p (k c) -> p k c", k=8)[:, :, 0:n]
            self.P.add("act", (lambda e, o=self.xnT[:, :, t0:t0 + n], i=src: e.activation(o, i, AF.Copy)),
                       [self.pdep[b]], [self.xn_dep[t]])

    def _ffn_alloc(self):
        if hasattr(self, "ffn_alloced"):
            return
        self.ffn_alloced = True
        self.cur = self.phase_off
        sb = self.sb
        self.stg = [sb("stg%d" % i, [128, 2048], F32) for i in range(3)]
        self.stg_dep = [Dep("stg%d" % i) for i in range(3)]
        self.stg_i = 0
        self.wgb = [sb("wgb%d" % i, [128, 8, 256], BF16) for i in range(2)]
        self.wub = [sb("wub%d" % i, [128, 8, 256], BF16) for i in range(2)]
        self.wgb_dep = [Dep("wgb%d" % i) for i in range(2)]
        self.wub_dep = [Dep("wub%d" % i) for i in range(2)]
        self.wdb = [sb("wdb%d" % i, [128, 2, D], BF16) for i in range(4)]
        self.wdb_dep = [Dep("wdb%d" % i) for i in range(4)]
        self.actb = sb("actb", [128, 4, L], BF16)
        self.actb_dep = [[Dep("act%d_%d" % (f, g)) for g in range(len(TGS))] for f in range(4)]
        self.sg = [sb("sg%d" % i, [128, 512], BF16) for i in range(2)]
        self.sg_dep = [Dep("sg%d" % i) for i in range(2)]
        self.ffn_end = self.cur
        self.slab_ctr = 0
        self.gu_ctr = 0
        self.dn_ctr = 0

    def _stage_slot(self):
        i = self.stg_i
        self.stg_i = (i + 1) % 3
        return i

    def _ffn(self, which):
        self._ffn_alloc()
        wg, wu, wd = self.wg[which], self.wu[which], self.wd[which]
        gcol = {0: 0, 1: 16}[which]
        gain = self.cols_sb[:, gcol:gcol + 8]
        slabs = list(range(11))
        groups = [slabs[i:i + 2] for i in range(0, 11, 2)]
        for grp in groups:
            for li, sl in enumerate(grp):
                c0 = sl * 256
                sw = self.slab_ctr % 2
                dslot = self.slab_ctr % 4
                self.slab_ctr += 1
                for (src, dst, ddep) in ((wg, self.wgb[sw], self.wgb_dep[sw]), (wu, self.wub[sw], self.wub_dep[sw])):
                    si = self._stage_slot()
                    st3 = self.stg[si].rearrange("p (k c) -> p k c", k=8)
                    self.dma(st3, src[:, c0:c0 + 256].rearrange("(k p) c -> p k c", p=128), [], [self.stg_dep[si]])
                    self.tt("pool", dst, st3, gain.unsqueeze(2).to_broadcast([128, 8, 256]), ALU.mult,
                            [self.stg_dep[si], self.cdep], [ddep])
                si = self._stage_slot()
                st3 = self.stg[si].rearrange("p (k c) -> p k c", k=2)
                self.dma(st3, wd[c0:c0 + 256, :].rearrange("(k p) c -> p k c", p=128), [], [self.stg_dep[si]])
                self.cp("pool", self.wdb[dslot], st3, [self.stg_dep[si]], [self.wdb_dep[dslot]])
                grp_dslot = dslot
                for c in range(2):
                    fl = li * 2 + c
                    for g, (g0, gn) in enumerate(TGS):
                        pbuf = self.gu_ctr % 2
                        self.gu_ctr += 1
                        pg, pu = 2 + 2 * pbuf, 3 + 2 * pbuf
                        xdeps = [self.xn_dep[t] for t in range(NT) if TILES[t][0] >= g0 and TILES[t][0] < g0 + gn]
                        for k in range(8):
                            self.mm(self.psum[pg][:, 0:gn], self.wgb[sw][:, k, c * 128:(c + 1) * 128],
                                    self.xnT[:, k, g0:g0 + gn], k == 0, k == 7,
                                    [self.wgb_dep[sw]] + xdeps, [self.pdep[pg]])
                        for k in range(8):
                            self.mm(self.psum[pu][:, 0:gn], self.wub[sw][:, k, c * 128:(c + 1) * 128],
                                    self.xnT[:, k, g0:g0 + gn], k == 0, k == 7,
                                    [self.wub_dep[sw]] + xdeps, [self.pdep[pu]])
                        sgi = pbuf
                        self.act(self.sg[sgi][:, 0:gn], self.psum[pg][:, 0:gn], AF.Silu, [self.pdep[pg]], [self.sg_dep[sgi]])
                        self.tt("dve", self.actb[:, fl, g0:g0 + gn], self.sg[sgi][:, 0:gn], self.psum[pu][:, 0:gn], ALU.mult,
                                [self.sg_dep[sgi], self.pdep[pu]], [self.actb_dep[fl][g]])
            nfl = len(grp) * 2
            first_dslot = (self.slab_ctr - len(grp)) % 4
            for t, (t0, n) in enumerate(TILES):
                g = min(t // 4, 4)
                for half in range(2):
                    pd = 6 + (self.dn_ctr % 2)
                    self.dn_ctr += 1
                    for fl in range(nfl):
                        dslot = (first_dslot + fl // 2) % 4
                        self.mm(self.psum[pd][0:n, :], self.actb[:, fl, t0:t0 + n],
                                self.wdb[dslot][:, fl % 2, half * 512:(half + 1) * 512], fl == 0, fl == nfl - 1,
                                [self.actb_dep[fl][g], self.wdb_dep[dslot]], [self.pdep[pd]])
                    hv = self.h[0:n, t, half * 512:(half + 1) * 512]
                    self.stt(hv, self.psum[pd][0:n, :], 0.5, hv, ALU.mult, ALU.add,
                             [self.pdep[pd], self.h_dep[t]], [self.h_dep[t]])


    def _attn_alloc(self):
        if hasattr(self, "attn_alloced"):
            return
        self.attn_alloced = True
        sb = self.sb
        save = self.cur
        self.cur = self.h_off
        r1 = self.cur
        self.qT = sb("qT", [128, 4, L], BF16)
        self.kT = sb("kT", [128, 4, L], BF16)
        self.VA = sb("VA", [128, NT, 4, 129], BF16)
        r1_end = self.cur
        self.cur = r1
        self.qlT = sb("qlT", [128, 4, 2, 512], BF16)
        self.rr = [sb("rr%d" % i, [128, 512], F32) for i in range(2)]
        self.qn1k = sb("qn1k", [128, 1024], BF16)
        self.ocT = sb("ocT", [128, 8, 128], BF16)
        self.woutb = sb("woutb", [128, 8, D], BF16)
        self.hst2 = sb("hst2", [128, 2, D], F32)
        self.hst = [self.hst2[:, 0, :], self.hst2[:, 1, :]]
        self.obG = sb("obG", [128, 4, 512], BF16)
        self.cntj = sb("cntj", [128, L], BF16)
        assert self.cur <= r1_end, (self.cur, r1_end)
        self.cur = r1_end
        self.qbT = sb("qbT", [128, 4, L], BF16)
        self.cT = sb("cT", [128, 2, L], BF16)
        self.VB = sb("VB", [128, NT, 4, 129], BF16)
        self.iqT = sb("iqT", [128, 4, L], BF16)
        self.ikT = sb("ikT", [128, L], BF16)
        r3 = self.cur
        self.wst = [sb("wst%d" % i, [128, 8, 384], F32) for i in range(2)]
        self.wb = [sb("wb%d" % i, [128, 8, 384], BF16) for i in range(2)]
        r3_end = self.cur
        self.cur = r3
        self.oa = sb("oa", [128, NT, 512], BF16)
        self.PT = [sb("PT%d" % i, [128, 512], BF16) for i in range(4)]
        self.t1 = [sb("t1_%d" % i, [128, 128], F32) for i in range(2)]
        self.ov = [sb("ov_%d" % i, [128, 128], F32) for i in range(2)]
        self.bst = sb("bst", [128, 4 * NBIS + 8], F32)
        self.mTa = sb("mTa", [128, 12, 512], BF16)
        assert self.cur <= r3_end, (self.cur, r3_end)
        self.cur = max(r3_end, save)
        self.sq = [sb("sq%d" % i, [128, 256], F32) for i in range(2)]
        self.qn = [sb("qn%d" % i, [128, 256], BF16) for i in range(2)]
        save2 = self.cur
        self.cur = self.c_const
        self.acc = sb("acc", [128, L], F32)
        self.maskb = sb("maskb", [128, L], BF16)
        self.mT = sb("mT", [128, NT, 512], BF16)
        assert self.cur <= self.h_off, (self.cur, self.h_off)
        self.cur = save2
        D_ = Dep
        self.qT_dep = [[D_("qT%d_%d" % (h, t)) for t in range(NT)] for h in range(4)]
        self.kT_dep = [[D_("kT%d_%d" % (h, t)) for t in range(NT)] for h in range(4)]
        self.VA_dep = [D_("VA%d" % t) for t in range(NT)]
        self.VB_dep = [D_("VB%d" % t) for t in range(NT)]
        self.cT_dep = [D_("cT%d" % t) for t in range(NT)]
        self.qbT_dep = [D_("qbT%d" % g) for g in range(5)]
        self.iqT_dep = [D_("iqT%d" % g) for g in range(5)]
        self.ikT_dep = [D_("ikT%d" % g) for g in range(5)]
        self.iw_dep = [D_("iw%d" % t) for t in range(NT)]
        self.wst_dep = [D_("wst%d" % i) for i in range(2)]
        self.wb_dep = [D_("wb%d" % i) for i in range(2)]
        self.sq_dep = [D_("sq%d" % i) for i in range(2)]
        self.qn_dep = [D_("qn%d" % i) for i in range(2)]
        self.oa_dep = [D_("oa%d" % t) for t in range(NT)]
        self.PT_dep = [D_("PT%d" % i) for i in range(4)]
        self.t1_dep = [D_("t1_%d" % i) for i in range(2)]
        self.ov_dep = [D_("ov_%d" % i) for i in range(2)]
        self.qlT_dep = [D_("qlT%d" % i) for i in range(4)]
        self.rr_dep = [D_("rr%d" % i) for i in range(2)]
        self.qn1k_dep = D_("qn1k")
        self.ocT_dep = D_("ocT")
        self.woutb_dep = D_("woutb")
        self.hst_dep = [D_("hst%d" % i) for i in range(2)]
        self.obG_dep = [D_("obG%d" % i) for i in range(4)]
        self.cntj_dep = D_("cntj")
        self.acc_dep = D_("acc")
        self.maskb_dep = D_("maskb")
        self.mT_dep = [D_("mT%d" % i) for i in range(4)]
        self.mTa_dep = [D_("mTa%d" % i) for i in range(4)]
        self.bst_dep = D_("bst")
        self.ctr = 0

    def _load_slab(self, c0, ncol, gain):
        slot = self.ctr % 2
        self.ctr += 1
        st = self.wst[slot][:, :, 0:ncol]
        self.dma(st, self.win[:, c0:c0 + ncol].rearrange("(k p) c -> p k c", p=128), [], [self.wst_dep[slot]])
        self.tt("pool", self.wb[slot][:, :, 0:ncol], st, gain.unsqueeze(2).to_broadcast([128, 8, ncol]), ALU.mult,
                [self.wst_dep[slot], self.cdep], [self.wb_dep[slot]])
        return slot

    def _proj(self):
        gain = self.cols_sb[:, 8:16]
        C = self.cols_sb
        parts = _os.environ.get("K_PROJ_PARTS", "ms,A,CK,FM").split(",")
        if "ms" in parts:
            self.memset("pool", self.VA[:, :, :, 128:129], 1.0, self.VA_dep)
            self.memset("pool", self.VB[:, :, :, 128:129], 1.0, self.VB_dep)
        tctr = 0
        for sl in range(5):
            if (sl < 4 and "A" not in parts) or (sl == 4 and "CK" not in parts):
                continue
            ncol = 384 if sl < 4 else 264
            slot = self._load_slab(sl * 384, ncol, gain)

            def mmA(t, slot=slot, ncol=ncol):
                t0, n = TILES[t]
                pb = 2 + (t % 2)
                for k in range(8):
                    self.mm(self.psum[pb][0:n, 0:ncol], self.xnT[:, k, t0:t0 + n], self.wb[slot][:, k, 0:ncol], k == 0, k == 7,
                            [self.xn_dep[t], self.wb_dep[slot]], [self.pdep[pb]])

            mmA(0)
            for t, (t0, n) in enumerate(TILES):
                if t + 1 < NT:
                    mmA(t + 1)
                pb = 2 + (t % 2)
                tb = t % 2
                b = t % 2
                ps = self.psum[pb]
                pT = self.psum[tb].bitcast(BF16)
                k4, sdeps = self.stat()
                ssd, td, rsd = sdeps
                if sl < 4:
                    h = sl
                    KA = _os.environ.get("K_A", "sq,red,rstd,qn,tr,evq,evk,va").split(",")
                    if "sq" in KA:
                        self.act(self.sq[b][0:n, :], ps[0:n, 0:256], AF.Square, [self.pdep[pb]], [self.sq_dep[b]])
                    if "red" in KA:
                        self.red(self.ss[0:n, k4:k4 + 4], self.sq[b][0:n, :].rearrange("p (g d) -> p g d", d=64), ALU.add,
                                 [self.sq_dep[b]], [ssd])
                    if "rstd" in KA:
                        self.rstd(n, k4, 4, 1.0 / 64, sdeps)
                    if "qn" in KA:
                        self.tt("dve", self.qn[b][0:n, :].rearrange("p (g d) -> p g d", d=64),
                                ps[0:n, 0:256].rearrange("p (g d) -> p g d", d=64),
                                self.rs[0:n, k4:k4 + 4].unsqueeze(2).to_broadcast([n, 4, 64]), ALU.mult,
                                [self.pdep[pb], rsd], [self.qn_dep[b]])
                    if "tr" in KA:
                        self.tr(pT[:, 0:n], self.qn[b][0:n, 0:128], self.ident[0:n, 0:n], [self.qn_dep[b], self.ident_dep],
                                [self.pdep[tb]])
                        self.tr(pT[:, 128:128 + n], self.qn[b][0:n, 128:256], self.ident[0:n, 0:n], [self.qn_dep[b], self.ident_dep],
                                [self.pdep[tb]])
                    if "evq" in KA:
                        qdst = self.junk[:, 0:n] if _os.environ.get("K_DEST") else self.qT[:, h, t0:t0 + n]
                        self.act(qdst, pT[:, 0:n], AF.Copy, [self.pdep[tb], self.setup_dep],
                                 [self.qT_dep[h][t]], scale=self.gqk)
                    if "evk" in KA:
                        kdst = self.junk[:, 128:128 + n] if _os.environ.get("K_DEST") else self.kT[:, h, t0:t0 + n]
                        self.act(kdst, pT[:, 128:128 + n], AF.Copy, [self.pdep[tb]], [self.kT_dep[h][t]])
                    if "va" in KA:
                        self.act(self.VA[0:n, t, h, 0:128], ps[0:n, 256:384], AF.Copy, [self.pdep[pb]], [self.VA_dep[t]])
                else:
                    self.act(self.sq[b][0:n, :], ps[0:n, 0:256], AF.Square, [self.pdep[pb]], [self.sq_dep[b], ssd],
                             accum=self.ss[0:n, k4:k4 + 1])
                    self.rstd(n, k4, 1, 1.0 / 256, sdeps)
                    self.ts("dve", self.qn[b][0:n, :], ps[0:n, 0:256], self.rs[0:n, k4:k4 + 1], None, ALU.mult, ALU.bypass,
                            [self.pdep[pb], rsd], [self.qn_dep[b]])
                    self.ts("dve", self.iw_sb[0:n, t, :], ps[0:n, 256:264], (64 ** -0.5) * (8 ** -0.5), None, ALU.mult, ALU.bypass,
                            [self.pdep[pb]], [self.iw_dep[t]])
                    for cc in range(2):
                        self.tr(pT[:, cc * 128:cc * 128 + n], self.qn[b][0:n, cc * 128:(cc + 1) * 128], self.ident[0:n, 0:n],
                                [self.qn_dep[b], self.ident_dep], [self.pdep[tb]])
                    self.ts("dve", self.cT[:, 0, t0:t0 + n], pT[:, 0:n], C[:, 26:27], None, ALU.mult, ALU.bypass,
                            [self.pdep[tb], self.cdep], [self.cT_dep[t]])
                    self.act(self.cT[:, 1, t0:t0 + n], pT[:, 128:128 + n], AF.Copy, [self.pdep[tb], self.cdep], [self.cT_dep[t]],
                             scale=C[:, 27:28])
                    vb = 6 + (t % 2)
                    for cc in range(2):
                        self.mm(self.psum[vb][0:n, :], self.cT[:, cc, t0:t0 + n],
                                self.wuvb[:, cc, :, :].rearrange("p h e -> p (h e)"), cc == 0, cc == 1,
                                [self.cT_dep[t], self.setup_dep], [self.pdep[vb]])
                    self.act(self.VB[0:n, t, :, 0:128], self.psum[vb][0:n, :].rearrange("p (h e) -> p h e", h=4), AF.Copy,
                             [self.pdep[vb]], [self.VB_dep[t]])
        ectr = 0
        for sl in range(3):
            if "FM" not in parts:
                continue
            slot = self._load_slab(1800 + sl * 384, 384, gain)
            for c in range(3):
                ch = sl * 3 + c
                for g, (g0, gn) in enumerate(TGS):
                    if ch < 4:
                        dst, ddep = self.qbT[:, ch, g0:g0 + gn], self.qbT_dep[g]
                    elif ch < 8:
                        dst, ddep = self.iqT[:, ch - 4, g0:g0 + gn], self.iqT_dep[g]
                    else:
                        dst, ddep = self.ikT[:, g0:g0 + gn], self.ikT_dep[g]
                    pb = 4 + (ectr % 2)
                    xdeps = [self.xn_dep[t] for t in range(NT) if g0 <= TILES[t][0] < g0 + gn]
                    for k in range(8):
                        self.mm(self.psum[pb][:, 0:gn], self.wb[slot][:, k, c * 128:(c + 1) * 128], self.xnT[:, k, g0:g0 + gn],
                                k == 0, k == 7, [self.wb_dep[slot]] + xdeps, [self.pdep[pb]])
                    if ectr % 2 == 0:
                        self.act(dst, self.psum[pb][:, 0:gn], AF.Copy, [self.pdep[pb]], [ddep])
                    else:
                        self.cp("dve", dst, self.psum[pb][:, 0:gn], [self.pdep[pb]], [ddep])
                    ectr += 1

    def _bias_blocks(self, ps, pbank, hh, j, tiles, c0):
        k0, kn = TILES[j]
        for blk, i in ((0, j), (1, j + 1)):
            if i in tiles:
                off = TILES[i][0] - c0
                ni = TILES[i][1]
                self.mm(ps[:, off:off + ni], self.ident[0:kn, 0:kn], self.biasbf[0:kn, hh, blk, 0:ni], False, True,
                        [self.ident_dep, self.setup_dep], [self.pdep[pbank]])

    def _pipe(self, n, stage1, stage2, depth=2):
        for k in range(min(depth, n)):
            stage1(k)
        for k in range(n):
            if k + depth < n:
                stage1(k + depth)
            stage2(k)

    def _attnA(self):
        for G, tiles in enumerate(QGS):
            q0 = TILES[tiles[0]][0]
            qn_ = sum(TILES[t][1] for t in tiles)
            for h in range(4):
                started = set()
                blocks = [(j, m) for j in range(tiles[-1] + 1) for m in range(2)]

                def stage1(k, h=h, tiles=tiles, q0=q0, qn_=qn_, blocks=blocks):
                    j, m = blocks[k]
                    k0, kn = TILES[j]
                    c0 = max(q0, k0)
                    ncol = q0 + qn_ - c0
                    qdeps = [self.qT_dep[h][t] for t in tiles if TILES[t][0] + TILES[t][1] > c0]
                    bank = k % 4
                    ps = self.psum[bank][0:kn, 0:ncol]
                    self.mm(ps, self.kT[m * 64:(m + 1) * 64, h, k0:k0 + kn], self.qT[m * 64:(m + 1) * 64, h, c0:c0 + ncol],
                            True, True, [self.kT_dep[h][j]] + qdeps, [self.pdep[bank]])
                    self._bias_blocks(ps, bank, h, j, tiles, c0)
                    self.act(self.PT[bank][0:kn, 0:ncol], ps, AF.Exp, [self.pdep[bank], self.setup_dep], [self.PT_dep[bank]],
                             bias=self.abias[0:kn, h:h + 1])

                def stage2(k, h=h, tiles=tiles, q0=q0, blocks=blocks, started=started):
                    j, m = blocks[k]
                    k0, kn = TILES[j]
                    c0 = max(q0, k0)
                    pbuf = k % 4
                    for il, i in enumerate(tiles):
                        if i < j:
                            continue
                        off = TILES[i][0] - c0
                        ni = TILES[i][1]
                        slot = m * 4 + il
                        ab = 4 + slot // 3
                        o = (slot % 3) * 129
                        first = ab not in started
                        started.add(ab)
                        self.mm(self.psum[ab][0:ni, o:o + 129], self.PT[pbuf][0:kn, off:off + ni], self.VA[0:kn, j, h, :],
                                first, j == i, [self.PT_dep[pbuf], self.VA_dep[j]], [self.pdep[ab]])

                self._pipe(len(blocks), stage1, stage2)
                for il, i in enumerate(tiles):
                    ni = TILES[i][1]
                    b = il % 2
                    s0, s1 = il, 4 + il
                    b0, o0 = 4 + s0 // 3, (s0 % 3) * 129
                    b1, o1 = 4 + s1 // 3, (s1 % 3) * 129
                    k4, sdeps = self.stat()
                    ssd, td, rsd = sdeps
                    self.recip(self.rs[0:ni, k4 + 1:k4 + 2], self.psum[b0][0:ni, o0 + 128:o0 + 129], [self.pdep[b0]], [rsd])
                    self.recip(self.rs[0:ni, k4 + 2:k4 + 3], self.psum[b1][0:ni, o1 + 128:o1 + 129], [self.pdep[b1]], [rsd])
                    self.ts("dve", self.rs[0:ni, k4 + 3:k4 + 4], self.rs[0:ni, k4 + 2:k4 + 3], self.lamn[0:ni, 0:1], None,
                            ALU.mult, ALU.bypass, [rsd, self.setup_dep], [rsd])
                    self.ts("dve", self.t1[b][0:ni, :], self.psum[b1][0:ni, o1:o1 + 128], self.rs[0:ni, k4 + 3:k4 + 4], None,
                            ALU.mult, ALU.bypass, [self.pdep[b1], rsd], [self.t1_dep[b]])
                    self.stt(self.ov[b][0:ni, :], self.psum[b0][0:ni, o0:o0 + 128], self.rs[0:ni, k4 + 1:k4 + 2],
                             self.t1[b][0:ni, :], ALU.mult, ALU.add, [self.pdep[b0], rsd, self.t1_dep[b]], [self.ov_dep[b]])
                    self.act(self.junk[0:ni, 0:128], self.ov[b][0:ni, :], AF.Square, [self.ov_dep[b]], [self.junk_dep, ssd],
                             accum=self.ss[0:ni, k4:k4 + 1])
                    self.rstd(ni, k4, 1, 1.0 / 128, sdeps)
                    self.ts("dve", self.oa[0:ni, i, h * 128:(h + 1) * 128], self.ov[b][0:ni, :], self.rs[0:ni, k4:k4 + 1], None,
                            ALU.mult, ALU.bypass, [self.ov_dep[b], rsd], [self.oa_dep[i]])

    def _attnB(self):
        C = self.cols_sb
        for q in range(4):
            st = self.hst2
            hd = self.hst_dep
            self.dma(st, self.wout[q * 256:(q + 1) * 256, :].rearrange("(k p) c -> p k c", p=128), [hd[1]], [hd[0]])
            if q < 2:
                self.ts("pool", self.woutb[:, 2 * q:2 * q + 2, :], st, self.gsub[:, 0:1], None, ALU.mult, ALU.bypass,
                        [hd[0], hd[1], self.setup_dep], [self.woutb_dep])
            else:
                self.cp("pool", self.woutb[:, 2 * q:2 * q + 2, :], st, [hd[0], hd[1]], [self.woutb_dep])
        self.dctr = 0
        self.hctr = 0
        ng = len(QGS)
        for il in range(len(QGS[0])):
            self._b_idx1(0, il)
            self._b_idx2(0, il)
        for G in range(ng):
            self._b_qlat(G)
            nxt = G + 1 if G + 1 < ng - 1 else None
            for h in range(4):
                if nxt is not None and h < len(QGS[nxt]):
                    self._b_idx1(nxt, h)
                self._b_attn_mm(G, h)
                if nxt is not None and h < len(QGS[nxt]):
                    self._b_idx2(nxt, h)
                self._b_attn_norm(G, h)
            if G + 1 == ng - 1:
                for il in range(len(QGS[G + 1])):
                    self._b_idx1(G + 1, il)
                    self._b_idx2(G + 1, il)
            self._b_wout(G)

    def _b_ctx(self, G):
        tiles = QGS[G]
        q0 = TILES[tiles[0]][0]
        qn_ = sum(TILES[t][1] for t in tiles)
        use_a = G in (0, 2)
        mT = self.mTa if use_a else self.mT
        mT_dep = self.mTa_dep if use_a else self.mT_dep
        return tiles, q0, qn_, mT, mT_dep

    def _b_qlat(self, G):
        tiles, q0, qn_, mT, mT_dep = self._b_ctx(G)
        for il, i in enumerate(tiles):
            t0, ni = TILES[i]
            for h in range(4):
                pbk = h // 2
                self.mm(self.psum[pbk][0:ni, (h % 2) * 256:(h % 2) * 256 + 256], self.qbT[:, h, t0:t0 + ni], self.wukb[:, h, :],
                        True, True, [self.qbT_dep[G], self.setup_dep], [self.pdep[pbk]])
            k4, sdeps = self.stat()
            ssd, td, rsd = sdeps
            for pbk in range(2):
                self.act(self.junk[0:ni, pbk * 512:(pbk + 1) * 512], self.psum[pbk][0:ni, :], AF.Square, [self.pdep[pbk]],
                         [self.junk_dep])
            self.red(self.ss[0:ni, k4:k4 + 4], self.junk[0:ni, :].rearrange("p (g d) -> p g d", d=256), ALU.add,
                     [self.junk_dep], [ssd])
            self.rstd(ni, k4, 4, 1.0 / 256, sdeps)
            for pbk in range(2):
                self.tt("dve", self.qn1k[0:ni, pbk * 512:(pbk + 1) * 512].rearrange("p (g d) -> p g d", d=256),
                        self.psum[pbk][0:ni, :].rearrange("p (g d) -> p g d", d=256),
                        self.rs[0:ni, k4 + 2 * pbk:k4 + 2 * pbk + 2].unsqueeze(2).to_broadcast([ni, 2, 256]), ALU.mult,
                        [self.pdep[pbk], rsd], [self.qn1k_dep])
            pT = self.psum[2].bitcast(BF16)
            for ch in range(8):
                self.tr(pT[:, ch * 128:ch * 128 + ni], self.qn1k[0:ni, ch * 128:(ch + 1) * 128], self.ident[0:ni, 0:ni],
                        [self.qn1k_dep, self.ident_dep], [self.pdep[2]])
            pv = pT.rearrange("p (h c t) -> p h c t", h=4, c=2)
            self.ts("dve", self.qlT[:, :, 0, il * 128:il * 128 + ni], pv[:, :, 0, 0:ni], self.gqb[0], None, ALU.mult,
                    ALU.bypass, [self.pdep[2], self.setup_dep], [self.qlT_dep[il]])
            self.ts("dve", self.qlT[:, :, 1, il * 128:il * 128 + ni], pv[:, :, 1, 0:ni], self.gqb[1], None, ALU.mult,
                    ALU.bypass, [self.pdep[2], self.setup_dep], [self.qlT_dep[il]])

    def _b_idx1(self, G, il):
        tiles, q0, qn_, mT, mT_dep = self._b_ctx(G)
        i = tiles[il]
        dctr = self.dctr
        t0, ni = TILES[i]
        Lk = t0 + ni
        acc = self.acc
        for kc0 in range(0, Lk, 512):
            kcn = min(512, Lk - kc0)
            g = kc0 // 512
            for hh in range(8):
                pb = 3 + (dctr % 2)
                rb = dctr % 2
                dctr += 1
                pr = (hh % 2) * 64
                self.mm(self.psum[pb][0:ni, 0:kcn], self.iqT[pr:pr + 64, hh // 2, t0:t0 + ni], self.ikT[pr:pr + 64, kc0:kc0 + kcn],
                        True, True, [self.iqT_dep[G], self.ikT_dep[g]], [self.pdep[pb]])
                self.act(self.rr[rb][0:ni, 0:kcn], self.psum[pb][0:ni, 0:kcn], AF.Relu, [self.pdep[pb]], [self.rr_dep[rb]])
                if hh == 0:
                    self.ts("dve", acc[0:ni, kc0:kc0 + kcn], self.rr[rb][0:ni, 0:kcn], self.iw_sb[0:ni, i, 0:1], None,
                            ALU.mult, ALU.bypass, [self.rr_dep[rb], self.iw_dep[i]], [self.acc_dep])
                else:
                    self.stt(acc[0:ni, kc0:kc0 + kcn], self.rr[rb][0:ni, 0:kcn], self.iw_sb[0:ni, i, hh:hh + 1],
                             acc[0:ni, kc0:kc0 + kcn], ALU.mult, ALU.add, [self.rr_dep[rb], self.iw_dep[i], self.acc_dep],
                             [self.acc_dep])
        self.dctr = dctr

    def _b_idx2(self, G, il):
        tiles, q0, qn_, mT, mT_dep = self._b_ctx(G)
        i = tiles[il]
        t0, ni = TILES[i]
        Lk = t0 + ni
        acc = self.acc
        bs = self.bst
        bd = self.bst_dep
        if i >= 2:
            self.red(bs[0:ni, 0:1], acc[0:ni, 0:Lk], ALU.max, [self.acc_dep], [bd])
            self.red(bs[0:ni, 1:2], acc[0:ni, 0:Lk], ALU.min, [self.acc_dep], [bd])
        self.tt("dve", acc[0:ni, t0:t0 + ni], acc[0:ni, t0:t0 + ni], self.masktok[0:ni, 0:ni], ALU.add,
                [self.acc_dep, self.masktok_dep], [self.acc_dep])
        if i >= 2:
            self.tt("dve", bs[0:ni, 2:3], bs[0:ni, 0:1], bs[0:ni, 1:2], ALU.subtract, [bd], [bd])
            W0 = 8
            self.ts("dve", bs[0:ni, W0:W0 + NBIS + 1], self.pw[0:ni, :], bs[0:ni, 2:3], None, ALU.mult, ALU.bypass,
                    [bd, self.setup_dep], [bd])
            M0 = W0 + NBIS + 1
            self.tt("dve", bs[0:ni, M0:M0 + 1], bs[0:ni, 1:2], bs[0:ni, W0:W0 + 1], ALU.add, [bd], [bd])
            C0 = M0 + NBIS + 1
            for k in range(NBIS):
                self.ts("dve", self.cntj[0:ni, 0:Lk], acc[0:ni, 0:Lk], bs[0:ni, M0 + k:M0 + k + 1], None, ALU.is_ge, ALU.add,
                        [self.acc_dep, bd], [self.cntj_dep, bd], accum=bs[0:ni, C0 + k:C0 + k + 1])
                self.ts("dve", bs[0:ni, 3:4], bs[0:ni, C0 + k:C0 + k + 1], TOPK - 0.5, bs[0:ni, W0 + k:W0 + k + 1],
                        ALU.is_ge, ALU.mult, [bd], [bd])
                wn = W0 + k + 1 if k < NBIS - 1 else W0 + k
                self.stt(bs[0:ni, M0 + k + 1:M0 + k + 2], bs[0:ni, 3:4], bs[0:ni, wn:wn + 1], bs[0:ni, M0 + k:M0 + k + 1],
                         ALU.subtract, ALU.add, [bd], [bd])
            theta = bs[0:ni, M0 + NBIS:M0 + NBIS + 1]
            thd = [bd]
        else:
            theta = self.thc[0:ni, 0:1]
            thd = [self.setup_dep]
        self.ts("dve", self.maskb[0:ni, 0:Lk], acc[0:ni, 0:Lk], theta, NEG, ALU.is_lt, ALU.mult, [self.acc_dep] + thd,
                [self.maskb_dep])
        for j0 in range(0, i + 1, 8):
            js = list(range(j0, min(j0 + 8, i + 1)))
            tb = 5
            pT = self.psum[tb].bitcast(BF16)
            for jj, j in enumerate(js):
                k0, kn = TILES[j]
                self.tr(pT[0:kn, jj * 128:jj * 128 + ni], self.maskb[0:ni, k0:k0 + kn], self.ident[0:ni, 0:ni],
                        [self.maskb_dep, self.ident_dep], [self.pdep[tb]])
            src = pT.rearrange("p (j t) -> p j t", j=8)[:, 0:len(js), 0:ni]
            self.cp("dve", mT[:, j0:j0 + len(js), il * 128:il * 128 + ni], src, [self.pdep[tb]], [mT_dep[il]])

    def _b_attn_mm(self, G, h):
        tiles, q0, qn_, mT, mT_dep = self._b_ctx(G)
        started = set()
        nblk = tiles[-1] + 1

        def stage1(j, h=h, tiles=tiles, q0=q0, qn_=qn_, mT=mT, mT_dep=mT_dep):
            k0, kn = TILES[j]
            c0 = max(q0, k0)
            ncol = q0 + qn_ - c0
            co = c0 - q0
            bank = j % 3
            ps = self.psum[bank][0:kn, 0:ncol]
            qd = [self.qlT_dep[il] for il, t in enumerate(tiles) if TILES[t][0] + TILES[t][1] > c0]
            md = [mT_dep[il] for il, t in enumerate(tiles) if TILES[t][0] + TILES[t][1] > c0]
            for cc in range(2):
                self.mm(ps, self.cT[:, cc, k0:k0 + kn], self.qlT[:, h, cc, co:co + ncol], cc == 0, False,
                        [self.cT_dep[j]] + qd, [self.pdep[bank]])
            self.mm(ps, self.ident[0:kn, 0:kn], mT[0:kn, j, co:co + ncol], False, True, [self.ident_dep] + md,
                    [self.pdep[bank]])
            self._bias_blocks(ps, bank, 4 + h, j, tiles, c0)
            self.act(self.PT[j % 4][0:kn, 0:ncol], ps, AF.Exp, [self.pdep[bank], self.setup_dep], [self.PT_dep[j % 4]],
                     bias=self.abias[0:kn, 4 + h:5 + h])

        def stage2(j, h=h, tiles=tiles, q0=q0, started=started):
            k0, kn = TILES[j]
            c0 = max(q0, k0)
            pbuf = j % 4
            for il, i in enumerate(tiles):
                if i < j:
                    continue
                off = TILES[i][0] - c0
                ni = TILES[i][1]
                ab = 6 + il // 3
                o = (il % 3) * 129
                first = ab not in started
                started.add(ab)
                self.mm(self.psum[ab][0:ni, o:o + 129], self.PT[pbuf][0:kn, off:off + ni], self.VB[0:kn, j, h, :],
                        first, j == i, [self.PT_dep[pbuf], self.VB_dep[j]], [self.pdep[ab]])

        self._pipe(nblk, stage1, stage2)

    def _b_attn_norm(self, G, h):
        tiles, q0, qn_, mT, mT_dep = self._b_ctx(G)
        for il, i in enumerate(tiles):
            ni = TILES[i][1]
            ab = 6 + il // 3
            o = (il % 3) * 129
            k4, sdeps = self.stat()
            ssd, td, rsd = sdeps
            self.recip(self.rs[0:ni, k4:k4 + 1], self.psum[ab][0:ni, o + 128:o + 129], [self.pdep[ab]], [rsd])
            self.ts("dve", self.obG[0:ni, il, h * 128:(h + 1) * 128], self.psum[ab][0:ni, o:o + 128], self.rs[0:ni, k4:k4 + 1],
                    None, ALU.mult, ALU.bypass, [self.pdep[ab], rsd], [self.obG_dep[il]])

    def _b_wout(self, G):
        tiles, q0, qn_, mT, mT_dep = self._b_ctx(G)
        hctr = self.hctr
        for il, i in enumerate(tiles):
            t0, ni = TILES[i]
            pT = self.psum[2].bitcast(BF16)
            for k in range(8):
                src = self.oa[0:ni, i, k * 128:(k + 1) * 128] if k < 4 else self.obG[0:ni, il, (k - 4) * 128:(k - 3) * 128]
                sd_ = self.oa_dep[i] if k < 4 else self.obG_dep[il]
                self.tr(pT[:, k * 128:k * 128 + ni], src, self.ident[0:ni, 0:ni], [sd_, self.ident_dep], [self.pdep[2]])
            self.act(self.ocT[:, :, 0:ni], pT.rearrange("p (k t) -> p k t", k=8)[:, :, 0:ni], AF.Copy, [self.pdep[2]],
                     [self.ocT_dep])
            hb = hctr % 2
            hctr += 1
            self.dma(self.hst[hb][0:ni, :], self.hs[t0:t0 + ni, :], [self.hs_dep[i]], [self.hst_dep[hb]])
            for half in range(2):
                pb = 3 + half
                for k in range(8):
                    self.mm(self.psum[pb][0:ni, :], self.ocT[:, k, 0:ni], self.woutb[:, k, half * 512:(half + 1) * 512],
                            k == 0, k == 7, [self.ocT_dep, self.woutb_dep], [self.pdep[pb]])
                hv = self.hst[hb][0:ni, half * 512:(half + 1) * 512]
                self.tt("dve", hv, self.psum[pb][0:ni, :], hv, ALU.add, [self.pdep[pb], self.hst_dep[hb]], [self.hst_dep[hb]])
            self.dma(self.hs[t0:t0 + ni, :], self.hst[hb][0:ni, :], [self.hst_dep[hb]], [self.hs_dep[i]])
        self.hctr = hctr

    def _finish(self):
        if _os.environ.get("K_DUMP"):
            dd = Dep("dump")
            self.P.barrier()
            self.dma(self.hs[0:128, 0:16], self.smallc[:, 0:16], [self.setup_dep], [dd])
            self.dma(self.hs[0:128, 16:24], self.abias, [self.setup_dep], [dd])
            self.P.add("sp", None, [dd], [])
        self.P.add("sp", None, [self.out_dep], [])
        if self.dbg is not None:
            pass


def _bucket(n):
    n = np.maximum(n, 0)
    nf = np.maximum(n, 1).astype(np.float32)
    large = 16 + (np.log(nf / np.float32(16)) / np.float32(math.log(128 / 16)) * np.float32(16)).astype(np.int32)
    large = np.minimum(large, 31)
    return np.where(n < 16, n, large)


def _prep_shared(inp):
    f = lambda a: np.ascontiguousarray(np.asarray(a, dtype=np.float32))
    sh = {}
    sh["meta"] = f(inp["meta_tokens"])
    for i, nm in ((1, "ffn1"), (2, "ffn2")):
        sh["w%dg" % i] = f(inp[nm + "_w_gate"][0])
        sh["w%du" % i] = f(inp[nm + "_w_up"][0])
        sh["w%dd" % i] = f(inp[nm + "_w_down"][0])
    w_in = f(inp["w_in"][0])
    qa, ka, va, qb, ckv, iq, ik, iw = np.split(w_in, np.cumsum([512, 512, 512, 512, 256, 512, 64, 8])[:-1], axis=1)
    parts = []
    for h in range(4):
        parts += [qa[:, h * 128:(h + 1) * 128], ka[:, h * 128:(h + 1) * 128], va[:, h * 128:(h + 1) * 128]]
    parts += [ckv, iw]
    parts += [qb, iq, ik, ik]
    sh["win"] = np.ascontiguousarray(np.concatenate(parts, axis=1))
    assert sh["win"].shape[1] == WIN_COLS
    sh["wuk"] = np.ascontiguousarray(f(inp["b_w_uk"][0]).transpose(1, 0, 2))
    sh["wuv"] = np.ascontiguousarray(f(inp["b_w_uv"][0]).reshape(4, 2, 128, 128).transpose(2, 1, 0, 3))
    sh["wout"] = f(inp["w_out"][0])
    cols = np.zeros((128, 40), np.float32)
    cols[:, 0:8] = f(inp["ffn1_norm"][0]).reshape(8, 128).T
    cols[:, 8:16] = f(inp["mix_norm"][0]).reshape(8, 128).T
    cols[:, 16:24] = f(inp["ffn2_norm"][0]).reshape(8, 128).T
    cols[:, 24] = np.tile(f(inp["a_q_norm"][0]), 2)
    cols[:, 25] = np.tile(f(inp["a_k_norm"][0]), 2)
    cols[:, 26:28] = f(inp["b_kv_norm"][0]).reshape(2, 128).T
    cols[:, 28:30] = f(inp["b_q_norm"][0]).reshape(2, 128).T
    cols[:, 30] = f(inp["a_subln"][0])
    rb = f(inp["rel_bias"])
    cols[:, 31:39] = np.broadcast_to(rb[31], (128, 8))
    sh["cols"] = cols
    rows = np.zeros((128, 896), np.float32)
    rows[:, 0:64] = f(inp["a_lambda_q1"][0])[None]
    rows[:, 64:128] = f(inp["a_lambda_k1"][0])[None]
    rows[:, 128:192] = f(inp["a_lambda_q2"][0])[None]
    rows[:, 192:256] = f(inp["a_lambda_k2"][0])[None]
    rows[:, 256:320] = f(inp["a_q_norm"][0])[None]
    rows[:, 320:384] = f(inp["a_k_norm"][0])[None]
    rows[:, 384:640] = f(inp["b_q_norm"][0])[None]
    rows[:, 640:896] = f(inp["b_kv_norm"][0])[None]
    sh["rows"] = rows
    tk = np.arange(128)[:, None]
    tq = np.arange(128)[None, :]
    bb = np.zeros((128, 8, 2, 128), np.float32)
    for blk in range(2):
        idx = _bucket(tq - tk + 128 * blk)
        bb[:, :, blk, :] = rb[idx].transpose(0, 2, 1)
    sh["biasblk"] = bb
    cm = np.zeros((128, 3, 128), np.float32)
    cm[:, 0, :] = np.eye(128, dtype=np.float32)
    cm[:, 1, :] = np.where(tq >= tk, 0.0, NEG)
    cm[:, 2, :] = np.where(tq <= tk, 0.0, -1e30)
    sh["cmask"] = cm
    return sh


_CACHE = {}


def kernel(**inputs):
    x = np.asarray(inputs["x"], dtype=np.float32)
    sh = _prep_shared(inputs)
    if "nc" not in _CACHE:
        _CACHE["nc"] = Builder().build()
    nc = _CACHE["nc"]
    in_maps = []
    for c in range(NCORES):
        m = dict(sh)
        m["x"] = np.ascontiguousarray(x[c * NSEQ:(c + 1) * NSEQ])
        in_maps.append(m)
    res = run_bass_kernel_spmd(nc, in_maps, core_ids=list(range(NCORES)))
    out = np.concatenate([np.asarray(r["out"]) for r in res.results], axis=0)
    return out.astype(np.float32)
```

```python
import math
import os as _os
from contextlib import ExitStack

import numpy as np
import concourse.bass as bass
import concourse.mybir as mybir
from concourse.bass_utils import run_bass_kernel_spmd

F32 = mybir.dt.float32
BF16 = mybir.dt.bfloat16
ALU = mybir.AluOpType
AF = mybir.ActivationFunctionType
AX = mybir.AxisListType

D = 1024
SEQ = 2048
NMETA = 16
L = SEQ + NMETA
DFF = 2816
NSEQ = 2
NCORES = 8
NT = 17
TILES = [(i * 128, min(128, L - i * 128)) for i in range(NT)]
TGS = [(0, 512), (512, 512), (1024, 512), (1536, 512), (2048, 16)]
QGS = [[0, 1, 2, 3], [4, 5, 6, 7], [8, 9, 10, 11], [12, 13, 14, 15], [16]]
EPS = 1e-6
A_SCALE = 64 ** -0.5
B_SCALE = 256 ** -0.5
LAM_INIT = 0.8 - 0.6 * math.exp(0.0)
TOPK = 256
NEG = -30000.0
NBIS = 10
WIN_COLS = 4 * 384 + 264 + 9 * 128
STRICT = True
EPOCH = 2000


def _dsize(dt):
    return 4 if dt == F32 else 2


class Dep:
    __slots__ = ("name", "lw", "rd", "sem", "dcount")

    def __init__(self, name):
        self.name = name
        self.lw = None
        self.rd = []
        self.sem = None
        self.dcount = 0


class Op:
    __slots__ = ("eng", "fn", "deps", "dma", "signal", "seq", "sem", "waits", "wdep")

    def __init__(self, eng, fn, deps, dma, wdep):
        self.eng = eng
        self.fn = fn
        self.deps = deps
        self.dma = dma
        self.signal = False
        self.seq = 0
        self.sem = None
        self.waits = []
        self.wdep = wdep


class Prog:
    ENGS = ("sp", "pe", "act", "dve", "pool")

    def __init__(self, nc):
        self.nc = nc
        self.ops = []
        self.last = {e: None for e in self.ENGS}
        self.dmas_since_barrier = []

    def add(self, eng, fn, r=(), w=(), dma=False):
        idx = len(self.ops)
        deps = set()
        for t in r:
            if t.lw is not None:
                deps.add(t.lw)
        for t in w:
            if t.lw is not None:
                deps.add(t.lw)
            deps.update(t.rd)
        for t in r:
            t.rd.append(idx)
        for t in w:
            t.lw = idx
            t.rd = []
        wdep = None
        if dma:
            assert len(w) == 1
            wdep = w[0]
            self.dmas_since_barrier.append(idx)
        self.ops.append(Op(eng, fn, deps, dma, wdep))
        self.last[eng] = idx
        return idx

    def barrier(self):
        lasts = {v for v in self.last.values() if v is not None}
        lasts.update(self.dmas_since_barrier)
        self.dmas_since_barrier = []
        for e in self.ENGS:
            idx = len(self.ops)
            self.ops.append(Op(e, None, set(lasts), False, None))
            self.last[e] = idx

    def emit(self, stack):
        nc = self.nc
        ops = self.ops
        for op in ops:
            for d in sorted(op.deps):
                dop = ops[d]
                if not dop.dma and dop.eng == op.eng and (op.eng == "pe" or not STRICT):
                    continue
                if dop.fn is None:
                    continue
                dop.signal = True
                op.waits.append(d)
        esem = {e: stack.enter_context(nc.semaphore("s_" + e)) for e in self.ENGS}
        cnt = {e: 0 for e in self.ENGS}
        NCH = 8
        chsem = [stack.enter_context(nc.semaphore("dch%d" % i)) for i in range(NCH)]
        chcnt = [0] * NCH
        chlast = [None] * NCH
        ndma = 0
        for oi, op in enumerate(ops):
            if op.fn is None:
                continue
            if op.dma:
                c = ndma % NCH
                ndma += 1
                if chlast[c] is not None:
                    op.waits.append(chlast[c])
                chlast[c] = oi
                chcnt[c] += 16
                op.sem = chsem[c]
                op.seq = chcnt[c]
            elif op.signal:
                if cnt[op.eng] >= EPOCH:
                    esem[op.eng] = stack.enter_context(nc.semaphore("s_%s_%d" % (op.eng, oi)))
                    cnt[op.eng] = 0
                cnt[op.eng] += 1
                op.sem = esem[op.eng]
                op.seq = cnt[op.eng]
        per = {e: [op for op in ops if op.eng == e] for e in self.ENGS}

        def run(e, h):
            waited = {}
            for op in per[e]:
                need = {}
                for d in op.waits:
                    dop = ops[d]
                    k = id(dop.sem)
                    if k not in need or need[k][1] < dop.seq:
                        need[k] = (dop.sem, dop.seq)
                for k, (sem, val) in need.items():
                    if waited.get(k, 0) >= val:
                        continue
                    h.wait_ge(sem, val)
                    waited[k] = val
                if op.fn is None:
                    continue
                ins = op.fn(h)
                if op.dma:
                    ins.then_inc(op.sem, 16)
                elif op.signal:
                    ins.then_inc(op.sem, 1)

        with nc.Block() as block:
            @block.sync
            def _(h):
                run("sp", h)

            @block.tensor
            def _(h):
                run("pe", h)

            @block.scalar
            def _(h):
                run("act", h)

            @block.vector
            def _(h):
                run("dve", h)

            @block.gpsimd
            def _(h):
                run("pool", h)


class Builder:
    def __init__(self, stage=99, nseq=NSEQ, dbg=False, dbg_stop=False):
        self.dbg_stop = dbg_stop
        self.stage = stage
        self.nseq = nseq
        self.nc = nc = bass.Bass("TRN2", target_bir_lowering=False)
        self.P = Prog(nc)
        self.cur = 16640
        dt = nc.dram_tensor
        self.x = dt("x", [NSEQ, SEQ, D], F32, kind="ExternalInput").ap()
        self.meta = dt("meta", [NMETA, D], F32, kind="ExternalInput").ap()
        self.wg = [dt("w%dg" % i, [D, DFF], F32, kind="ExternalInput").ap() for i in (1, 2)]
        self.wu = [dt("w%du" % i, [D, DFF], F32, kind="ExternalInput").ap() for i in (1, 2)]
        self.wd = [dt("w%dd" % i, [DFF, D], F32, kind="ExternalInput").ap() for i in (1, 2)]
        self.win = dt("win", [D, WIN_COLS], F32, kind="ExternalInput").ap()
        self.wuk = dt("wuk", [128, 4, 256], F32, kind="ExternalInput").ap()
        self.wuv = dt("wuv", [128, 2, 4, 128], F32, kind="ExternalInput").ap()
        self.wout = dt("wout", [D, D], F32, kind="ExternalInput").ap()
        self.cols = dt("cols", [128, 40], F32, kind="ExternalInput").ap()
        self.rows = dt("rows", [128, 896], F32, kind="ExternalInput").ap()
        self.bias = dt("biasblk", [128, 8, 2, 128], F32, kind="ExternalInput").ap()
        self.cmask = dt("cmask", [128, 3, 128], F32, kind="ExternalInput").ap()
        self.out = dt("out", [NSEQ, SEQ, D], F32, kind="ExternalOutput").ap()
        self.hs = dt("hs", [L, D], F32, kind="ExternalOutput").ap()
        self.dbg = dt("dbg", [128, 4096], F32, kind="ExternalOutput").ap() if dbg else None
        self.psum = [nc.alloc_psum_tensor("pb%d" % i, [128, 512], F32).ap() for i in range(8)]
        self.pdep = [Dep("pb%d" % i) for i in range(8)]
        self.out_dep = Dep("out")

    def sb(self, name, shape, dtype, at=None):
        n = 1
        for s in shape[1:]:
            n *= s
        nbytes = (n * _dsize(dtype) + 63) // 64 * 64
        off = self.cur if at is None else at
        t = self.nc.alloc_sbuf_tensor_at(name, list(shape), dtype, offset=off)
        if at is None:
            self.cur = off + nbytes
        assert off + nbytes <= 229376, (name, off + nbytes)
        return t.ap()

    def mm(self, out, lhsT, rhs, start, stop, r, w):
        self.P.add("pe", lambda e: e.matmul(out, lhsT, rhs, start=start, stop=stop, skip_group_check=True), r, w)

    def tr(self, out, in_, ident, r, w):
        self.P.add("pe", lambda e: e.transpose(out, in_, ident), r, w)

    def act(self, out, in_, func, r, w, bias=0.0, scale=1.0, accum=None):
        if accum is None:
            self.P.add("act", lambda e: e.activation(out, in_, func, bias=bias, scale=scale), r, w)
        else:
            self.P.add("act", lambda e: e.activation(out, in_, func, bias=bias, scale=scale, accum_out=accum), r, w)

    def ts(self, eng, out, in0, s1, s2, op0, op1, r, w, accum=None):
        if accum is None:
            self.P.add(eng, lambda e: e.tensor_scalar(out, in0, s1, s2, op0, op1), r, w)
        else:
            self.P.add(eng, lambda e: e.tensor_scalar(out, in0, s1, s2, op0, op1, accum_out=accum), r, w)

    def tt(self, eng, out, in0, in1, op, r, w):
        self.P.add(eng, lambda e: e.tensor_tensor(out, in0, in1, op), r, w)

    def stt(self, out, in0, scalar, in1, op0, op1, r, w):
        self.P.add("dve", lambda e: e.scalar_tensor_tensor(out, in0, scalar, in1, op0, op1), r, w)

    def cp(self, eng, out, in_, r, w):
        self.P.add(eng, lambda e: e.tensor_copy(out, in_), r, w)

    def red(self, out, in_, op, r, w, absval=False):
        self.P.add("dve", lambda e: e.tensor_reduce(out, in_, AX.X, op, apply_absolute_value=absval), r, w)

    def recip(self, out, in_, r, w):
        self.P.add("dve", lambda e: e.reciprocal(out, in_), r, w)

    def memset(self, eng, ap, val, w):
        self.P.add(eng, lambda e: e.memset(ap, val), (), w)

    def dma(self, out, in_, r, w):
        self.P.add("sp", lambda e: e.dma_start(out=out, in_=in_), r, w, dma=True)

    def stat(self):
        i = self.st_i
        self.st_i = (i + 1) % 8
        return 4 * i, self.st_dep[i]

    def rstd(self, np_, k, nc_, inv_n, deps):
        ssd, td, rsd = deps
        self.ts("dve", self.tmp[0:np_, k:k + nc_], self.ss[0:np_, k:k + nc_], inv_n, EPS, ALU.mult, ALU.add, [ssd], [td])
        self.tt("pool", self.rs[0:np_, k:k + nc_], self.tmp[0:np_, k:k + nc_], self.neghalf[0:np_, 0:nc_], ALU.pow,
                [td, self.nh_dep], [rsd])

    def build(self):
        nc = self.nc
        stage = self.stage
        with ExitStack() as stack:
            self._alloc()
            self._setup()
            for s in range(self.nseq):
                self._sequence(s)
            self._finish()
            self.P.emit(stack)
        return nc

    def _alloc(self):
        sb = self.sb
        self.cols_sb = sb("cols", [128, 40], F32)
        self.rows_sb = sb("rows", [128, 896], F32)
        self.ident_f = sb("identf", [128, 128], F32)
        self.ident = sb("ident", [128, 128], BF16)
        self.neghalf = sb("neghalf", [128, 16], F32)
        self.smallc = sb("smallc", [128, 32], F32)
        self.gqk = self.smallc[:, 6:7]
        self.gqb = [self.smallc[:, 2:3], self.smallc[:, 10:11]]
        self.lamn = self.smallc[:, 14:15]
        self.abias = sb("abias", [128, 8], F32)
        self.gsub = self.smallc[:, 18:19]
        self.sm = sb("small", [128, 272], F32)
        self.biasbf = sb("biasbf", [128, 8, 2, 128], BF16)
        self.masktok = sb("masktok", [128, 128], F32)
        self.iw_sb = sb("iw", [128, NT, 8], F32)
        self.ss = sb("ss", [128, 32], F32)
        self.tmp = sb("tmpst", [128, 32], F32)
        self.rs = sb("rs", [128, 32], F32)
        self.st_dep = [(Dep("ss%d" % i), Dep("tm%d" % i), Dep("rs%d" % i)) for i in range(8)]
        self.st_i = 0
        self.wukb = sb("wukb", [128, 4, 256], BF16)
        self.wuvb = sb("wuvb", [128, 2, 4, 128], BF16)
        self.maskT = sb("maskT", [128, 128], F32)
        self.pw = sb("pw", [128, NBIS + 1], F32)
        self.thc = self.smallc[:, 22:23]
        self.xn_s = [sb("xn_s%d" % i, [128, D], BF16) for i in range(2)]
        self.xn_s_dep = [Dep("xn_s%d" % i) for i in range(2)]
        self.junk = sb("junk", [128, D], BF16)
        self.junk_dep = Dep("junk")
        self.c_const = self.cur
        self.xnT = sb("xnT", [128, 8, L], BF16)
        self.xn_dep = [Dep("xnT%d" % t) for t in range(NT)]
        self.h_off = self.cur
        self.h = sb("h", [128, NT, D], F32)
        self.h_dep = [Dep("h%d" % t) for t in range(NT)]
        self.big_off = self.cur
        self.phase_off = self.cur

    def _setup(self):
        P = self.P
        cd = self.cdep = Dep("consts")
        self.dma(self.cols_sb, self.cols, [], [cd])
        rd = Dep("rows")
        self.dma(self.rows_sb, self.rows, [], [rd])
        idd = Dep("identf")
        self.dma(self.ident_f, self.cmask[:, 0, :], [], [idd])
        mk = Dep("masktok")
        self.dma(self.masktok, self.cmask[:, 2, :], [], [mk])
        self.masktok_dep = mk
        self.ident_dep = Dep("ident")
        self.cp("dve", self.ident, self.ident_f, [idd], [self.ident_dep])
        self.nh_dep = Dep("neghalf")
        self.memset("dve", self.neghalf, -0.5, [self.nh_dep])
        if self.stage <= 1:
            return
        self._ffn_alloc()
        C = self.cols_sb
        R = self.rows_sb
        sd = self.setup_dep = Dep("setup")
        smd = Dep("sm")
        mtd = Dep("maskT")
        self.dma(self.maskT, self.cmask[:, 1, :], [], [mtd])
        self.stt(self.gqk, C[:, 24:25], A_SCALE, C[:, 25:26], ALU.mult, ALU.mult, [cd], [sd])
        for cc in range(2):
            self.ts("dve", self.gqb[cc], C[:, 28 + cc:29 + cc], B_SCALE, None, ALU.mult, ALU.bypass, [cd], [sd])
        self.ts("dve", self.gsub, C[:, 30:31], 1.0 - LAM_INIT, None, ALU.mult, ALU.bypass, [cd], [sd])
        sm = self.sm
        self.tt("dve", sm[:, 0:64], R[:, 0:64], R[:, 64:128], ALU.mult, [rd], [smd])
        self.red(sm[:, 256:257], sm[:, 0:64], ALU.add, [smd], [smd])
        self.tt("dve", sm[:, 64:128], R[:, 128:192], R[:, 192:256], ALU.mult, [rd], [smd])
        self.red(sm[:, 257:258], sm[:, 64:128], ALU.add, [smd], [smd])
        self.act(sm[:, 258:260], sm[:, 256:258], AF.Exp, [smd], [smd])
        self.tt("dve", sm[:, 260:261], sm[:, 258:259], sm[:, 259:260], ALU.subtract, [smd], [smd])
        self.ts("dve", self.lamn, sm[:, 260:261], -1.0, -LAM_INIT, ALU.mult, ALU.add, [smd], [sd])
        self.tt("dve", sm[:, 0:64], R[:, 256:320], R[:, 320:384], ALU.mult, [rd, smd], [smd])
        self.red(sm[:, 261:262], sm[:, 0:64], ALU.max, [smd], [smd], absval=True)
        self.ts("dve", sm[:, 262:263], sm[:, 261:262], 64.0 * A_SCALE, None, ALU.mult, ALU.bypass, [smd], [smd])
        self.ts("dve", self.abias[:, 0:4], C[:, 31:35], sm[:, 262:263], None, ALU.subtract, ALU.bypass, [smd, cd], [sd])
        self.tt("dve", sm[:, 0:256], R[:, 384:640], R[:, 640:896], ALU.mult, [rd, smd], [smd])
        self.red(sm[:, 263:264], sm[:, 0:256], ALU.max, [smd], [smd], absval=True)
        self.ts("dve", sm[:, 264:265], sm[:, 263:264], 256.0 * B_SCALE, None, ALU.mult, ALU.bypass, [smd], [smd])
        self.ts("dve", self.abias[:, 4:8], C[:, 35:39], sm[:, 264:265], None, ALU.subtract, ALU.bypass, [smd, cd], [sd])
        st = self.stg[0].rearrange("p (h b c) -> p h b c", h=8, b=2)
        self.dma(st, self.bias, [], [self.stg_dep[0]])
        for hh in range(8):
            self.stt(self.biasbf[:, hh, 0, :], st[:, hh, 0, :], C[:, 31 + hh:32 + hh], self.maskT, ALU.subtract, ALU.add,
                     [self.stg_dep[0], cd, mtd], [sd])
            self.ts("dve", self.biasbf[:, hh, 1, :], st[:, hh, 1, :], C[:, 31 + hh:32 + hh], None, ALU.subtract, ALU.bypass,
                    [self.stg_dep[0], cd], [sd])
        s1 = self.stg[1].rearrange("p (h c) -> p h c", h=4)[:, :, 0:256]
        self.dma(s1, self.wuk, [], [self.stg_dep[1]])
        self.cp("pool", self.wukb, s1, [self.stg_dep[1]], [sd])
        s2 = self.stg[2][:, 0:1024].rearrange("p (a h c) -> p a h c", a=2, h=4)
        self.dma(s2, self.wuv, [], [self.stg_dep[2]])
        self.cp("pool", self.wuvb, s2, [self.stg_dep[2]], [sd])
        for k in range(NBIS + 1):
            self.memset("pool", self.pw[:, k:k + 1], 2.0 ** -(k + 1), [sd])
        self.memset("pool", self.thc, -1e29, [sd])

    def _sequence(self, s):
        self._load_h(s, from_x=True)
        self._norm_pass(0)
        self._ffn(0)
        if self.stage <= 1:
            self._store_out(s)
            return
        self._norm_pass(1)
        self.hs_dep = getattr(self, "hs_dep", None) or [Dep("hs%d" % t) for t in range(NT)]
        for t, (t0, n) in enumerate(TILES):
            self.dma(self.hs[t0:t0 + n, :], self.h[0:n, t, :], [self.h_dep[t]], [self.hs_dep[t]])
        self.P.barrier()
        self._attn_alloc()
        if not _os.environ.get("K_SKIP_PROJ"):
            self._proj()
        self.P.barrier()
        if self.stage >= 3:
            self._attnA()
            self.P.barrier()
        if self.stage >= 4:
            self._attnB()
            self.P.barrier()
        if self.dbg_stop:
            return
        if not _os.environ.get("K_NO_RELOAD"):
            self._load_h(s, from_x=False)
        if not _os.environ.get("K_NO_FFN2"):
            self._norm_pass(2)
            self._ffn(1)
        self._store_out(s)

    def _load_h(self, s, from_x):
        for t, (t0, n) in enumerate(TILES):
            hd = self.h_dep[t]
            if from_x:
                if t == 0:
                    self.dma(self.h[0:NMETA, 0, :], self.meta, [], [hd])
                    self.dma(self.h[NMETA:128, 0, :], self.x[s, 0:128 - NMETA, :], [], [hd])
                else:
                    self.dma(self.h[0:n, t, :], self.x[s, t0 - NMETA:t0 - NMETA + n, :], [], [hd])
            else:
                self.dma(self.h[0:n, t, :], self.hs[t0:t0 + n, :], [self.hs_dep[t]], [hd])

    def _store_out(self, s):
        for t, (t0, n) in enumerate(TILES):
            if t == 0:
                self.dma(self.out[s, 0:128 - NMETA, :], self.h[NMETA:128, 0, :], [self.h_dep[0]], [self.out_dep])
            else:
                self.dma(self.out[s, t0 - NMETA:t0 - NMETA + n, :], self.h[0:n, t, :], [self.h_dep[t]], [self.out_dep])

    def _norm_pass(self, which):
        for t, (t0, n) in enumerate(TILES):
            b = t % 2
            k, (ssd, td, rsd) = self.stat()
            ss = self.ss[0:n, k:k + 1]
            self.act(self.junk[0:n, :], self.h[0:n, t, :], AF.Square, [self.h_dep[t]], [self.junk_dep, ssd], accum=ss)
            self.ts("dve", self.tmp[0:n, k:k + 1], ss, 1.0 / D, EPS, ALU.mult, ALU.add, [ssd], [td])
            self.tt("pool", self.rs[0:n, k:k + 1], self.tmp[0:n, k:k + 1], self.neghalf[0:n, 0:1], ALU.pow,
                    [td, self.nh_dep], [rsd])
            self.ts("dve", self.xn_s[b][0:n, :], self.h[0:n, t, :], self.rs[0:n, k:k + 1], None, ALU.mult, ALU.bypass,
                    [self.h_dep[t], rsd], [self.xn_s_dep[b]])
            pb = self.psum[b].bitcast(BF16)
            for kk in range(8):
                self.tr(pb[:, kk * 128:kk * 128 + n], self.xn_s[b][0:n, kk * 128:(kk + 1) * 128],
                        self.ident[0:n, 0:n], [self.xn_s_dep[b], self.ident_dep], [self.pdep[b]])
            src = pb.rearrange("p (k c) -> p k c", k=8)[:, :, 0:n]
            self.P.add("act", (lambda e, o=self.xnT[:, :, t0:t0 + n], i=src: e.activation(o, i, AF.Copy)),
                       [self.pdep[b]], [self.xn_dep[t]])

    def _ffn_alloc(self):
        if hasattr(self, "ffn_alloced"):
            return
        self.ffn_alloced = True
        self.cur = self.phase_off
        sb = self.sb
        self.stg = [sb("stg%d" % i, [128, 2048], F32) for i in range(3)]
        self.stg_dep = [Dep("stg%d" % i) for i in range(3)]
        self.stg_i = 0
        self.wgb = [sb("wgb%d" % i, [128, 8, 256], BF16) for i in range(2)]
        self.wub = [sb("wub%d" % i, [128, 8, 256], BF16) for i in range(2)]
        self.wgb_dep = [Dep("wgb%d" % i) for i in range(2)]
        self.wub_dep = [Dep("wub%d" % i) for i in range(2)]
        self.wdb = [sb("wdb%d" % i, [128, 2, D], BF16) for i in range(4)]
        self.wdb_dep = [Dep("wdb%d" % i) for i in range(4)]
        self.actb = sb("actb", [128, 4, L], BF16)
        self.actb_dep = [[Dep("act%d_%d" % (f, g)) for g in range(len(TGS))] for f in range(4)]
        self.sg = [sb("sg%d" % i, [128, 512], BF16) for i in range(2)]
        self.sg_dep = [Dep("sg%d" % i) for i in range(2)]
        self.ffn_end = self.cur
        self.slab_ctr = 0
        self.gu_ctr = 0
        self.dn_ctr = 0

    def _stage_slot(self):
        i = self.stg_i
        self.stg_i = (i + 1) % 3
        return i

    def _ffn(self, which):
        self._ffn_alloc()
        wg, wu, wd = self.wg[which], self.wu[which], self.wd[which]
        gcol = {0: 0, 1: 16}[which]
        gain = self.cols_sb[:, gcol:gcol + 8]
        slabs = list(range(11))
        groups = [slabs[i:i + 2] for i in range(0, 11, 2)]
        for grp in groups:
            for li, sl in enumerate(grp):
                c0 = sl * 256
                sw = self.slab_ctr % 2
                dslot = self.slab_ctr % 4
                self.slab_ctr += 1
                for (src, dst, ddep) in ((wg, self.wgb[sw], self.wgb_dep[sw]), (wu, self.wub[sw], self.wub_dep[sw])):
                    si = self._stage_slot()
                    st3 = self.stg[si].rearrange("p (k c) -> p k c", k=8)
                    self.dma(st3, src[:, c0:c0 + 256].rearrange("(k p) c -> p k c", p=128), [], [self.stg_dep[si]])
                    self.tt("pool", dst, st3, gain.unsqueeze(2).to_broadcast([128, 8, 256]), ALU.mult,
                            [self.stg_dep[si], self.cdep], [ddep])
                si = self._stage_slot()
                st3 = self.stg[si].rearrange("p (k c) -> p k c", k=2)
                self.dma(st3, wd[c0:c0 + 256, :].rearrange("(k p) c -> p k c", p=128), [], [self.stg_dep[si]])
                self.cp("pool", self.wdb[dslot], st3, [self.stg_dep[si]], [self.wdb_dep[dslot]])
                grp_dslot = dslot
                for c in range(2):
                    fl = li * 2 + c
                    for g, (g0, gn) in enumerate(TGS):
                        pbuf = self.gu_ctr % 2
                        self.gu_ctr += 1
                        pg, pu = 2 + 2 * pbuf, 3 + 2 * pbuf
                        xdeps = [self.xn_dep[t] for t in range(NT) if TILES[t][0] >= g0 and TILES[t][0] < g0 + gn]
                        for k in range(8):
                            self.mm(self.psum[pg][:, 0:gn], self.wgb[sw][:, k, c * 128:(c + 1) * 128],
                                    self.xnT[:, k, g0:g0 + gn], k == 0, k == 7,
                                    [self.wgb_dep[sw]] + xdeps, [self.pdep[pg]])
                        for k in range(8):
                            self.mm(self.psum[pu][:, 0:gn], self.wub[sw][:, k, c * 128:(c + 1) * 128],
                                    self.xnT[:, k, g0:g0 + gn], k == 0, k == 7,
                                    [self.wub_dep[sw]] + xdeps, [self.pdep[pu]])
                        sgi = pbuf
                        self.act(self.sg[sgi][:, 0:gn], self.psum[pg][:, 0:gn], AF.Silu, [self.pdep[pg]], [self.sg_dep[sgi]])
                        self.tt("dve", self.actb[:, fl, g0:g0 + gn], self.sg[sgi][:, 0:gn], self.psum[pu][:, 0:gn], ALU.mult,
                                [self.sg_dep[sgi], self.pdep[pu]], [self.actb_dep[fl][g]])
            nfl = len(grp) * 2
            first_dslot = (self.slab_ctr - len(grp)) % 4
            for t, (t0, n) in enumerate(TILES):
                g = min(t // 4, 4)
                for half in range(2):
                    pd = 6 + (self.dn_ctr % 2)
                    self.dn_ctr += 1
                    for fl in range(nfl):
                        dslot = (first_dslot + fl // 2) % 4
                        self.mm(self.psum[pd][0:n, :], self.actb[:, fl, t0:t0 + n],
                                self.wdb[dslot][:, fl % 2, half * 512:(half + 1) * 512], fl == 0, fl == nfl - 1,
                                [self.actb_dep[fl][g], self.wdb_dep[dslot]], [self.pdep[pd]])
                    hv = self.h[0:n, t, half * 512:(half + 1) * 512]
                    self.stt(hv, self.psum[pd][0:n, :], 0.5, hv, ALU.mult, ALU.add,
                             [self.pdep[pd], self.h_dep[t]], [self.h_dep[t]])


    def _attn_alloc(self):
        if hasattr(self, "attn_alloced"):
            return
        self.attn_alloced = True
        sb = self.sb
        save = self.cur
        self.cur = self.h_off
        r1 = self.cur
        self.qT = sb("qT", [128, 4, L], BF16)
        self.kT = sb("kT", [128, 4, L], BF16)
        self.VA = sb("VA", [128, NT, 4, 129], BF16)
        r1_end = self.cur
        self.cur = r1
        self.qlT = sb("qlT", [128, 4, 2, 512], BF16)
        self.rr = [sb("rr%d" % i, [128, 512], F32) for i in range(2)]
        self.qn1k = sb("qn1k", [128, 1024], BF16)
        self.ocT = sb("ocT", [128, 8, 128], BF16)
        self.woutb = sb("woutb", [128, 8, D], BF16)
        self.hst2 = sb("hst2", [128, 2, D], F32)
        self.hst = [self.hst2[:, 0, :], self.hst2[:, 1, :]]
        self.obG = sb("obG", [128, 4, 512], BF16)
        self.cntj = sb("cntj", [128, L], BF16)
        assert self.cur <= r1_end, (self.cur, r1_end)
        self.cur = r1_end
        self.qbT = sb("qbT", [128, 4, L], BF16)
        self.cT = sb("cT", [128, 2, L], BF16)
        self.VB = sb("VB", [128, NT, 4, 129], BF16)
        self.iqT = sb("iqT", [128, 4, L], BF16)
        self.ikT = sb("ikT", [128, L], BF16)
        r3 = self.cur
        self.wst = [sb("wst%d" % i, [128, 8, 384], F32) for i in range(2)]
        self.wb = [sb("wb%d" % i, [128, 8, 384], BF16) for i in range(2)]
        r3_end = self.cur
        self.cur = r3
        self.oa = sb("oa", [128, NT, 512], BF16)
        self.PT = [sb("PT%d" % i, [128, 512], BF16) for i in range(4)]
        self.t1 = [sb("t1_%d" % i, [128, 128], F32) for i in range(2)]
        self.ov = [sb("ov_%d" % i, [128, 128], F32) for i in range(2)]
        self.bst = sb("bst", [128, 4 * NBIS + 8], F32)
        self.mTa = sb("mTa", [128, 12, 512], BF16)
        assert self.cur <= r3_end, (self.cur, r3_end)
        self.cur = max(r3_end, save)
        self.sq = [sb("sq%d" % i, [128, 256], F32) for i in range(2)]
        self.qn = [sb("qn%d" % i, [128, 256], BF16) for i in range(2)]
        save2 = self.cur
        self.cur = self.c_const
        self.acc = sb("acc", [128, L], F32)
        self.maskb = sb("maskb", [128, L], BF16)
        self.mT = sb("mT", [128, NT, 512], BF16)
        assert self.cur <= self.h_off, (self.cur, self.h_off)
        self.cur = save2
        D_ = Dep
        self.qT_dep = [[D_("qT%d_%d" % (h, t)) for t in range(NT)] for h in range(4)]
        self.kT_dep = [[D_("kT%d_%d" % (h, t)) for t in range(NT)] for h in range(4)]
        self.VA_dep = [D_("VA%d" % t) for t in range(NT)]
        self.VB_dep = [D_("VB%d" % t) for t in range(NT)]
        self.cT_dep = [D_("cT%d" % t) for t in range(NT)]
        self.qbT_dep = [D_("qbT%d" % g) for g in range(5)]
        self.iqT_dep = [D_("iqT%d" % g) for g in range(5)]
        self.ikT_dep = [D_("ikT%d" % g) for g in range(5)]
        self.iw_dep = [D_("iw%d" % t) for t in range(NT)]
        self.wst_dep = [D_("wst%d" % i) for i in range(2)]
        self.wb_dep = [D_("wb%d" % i) for i in range(2)]
        self.sq_dep = [D_("sq%d" % i) for i in range(2)]
        self.qn_dep = [D_("qn%d" % i) for i in range(2)]
        self.oa_dep = [D_("oa%d" % t) for t in range(NT)]
        self.PT_dep = [D_("PT%d" % i) for i in range(4)]
        self.t1_dep = [D_("t1_%d" % i) for i in range(2)]
        self.ov_dep = [D_("ov_%d" % i) for i in range(2)]
        self.qlT_dep = [D_("qlT%d" % i) for i in range(4)]
        self.rr_dep = [D_("rr%d" % i) for i in range(2)]
        self.qn1k_dep = D_("qn1k")
        self.ocT_dep = D_("ocT")
        self.woutb_dep = D_("woutb")
        self.hst_dep = [D_("hst%d" % i) for i in range(2)]
        self.obG_dep = [D_("obG%d" % i) for i in range(4)]
        self.cntj_dep = D_("cntj")
        self.acc_dep = D_("acc")
        self.maskb_dep = D_("maskb")
        self.mT_dep = [D_("mT%d" % i) for i in range(4)]
        self.mTa_dep = [D_("mTa%d" % i) for i in range(4)]
        self.bst_dep = D_("bst")
        self.ctr = 0

    def _load_slab(self, c0, ncol, gain):
        slot = self.ctr % 2
        self.ctr += 1
        st = self.wst[slot][:, :, 0:ncol]
        self.dma(st, self.win[:, c0:c0 + ncol].rearrange("(k p) c -> p k c", p=128), [], [self.wst_dep[slot]])
        self.tt("pool", self.wb[slot][:, :, 0:ncol], st, gain.unsqueeze(2).to_broadcast([128, 8, ncol]), ALU.mult,
                [self.wst_dep[slot], self.cdep], [self.wb_dep[slot]])
        return slot

    def _proj(self):
        gain = self.cols_sb[:, 8:16]
        C = self.cols_sb
        parts = _os.environ.get("K_PROJ_PARTS", "ms,A,CK,FM").split(",")
        if "ms" in parts:
            self.memset("pool", self.VA[:, :, :, 128:129], 1.0, self.VA_dep)
            self.memset("pool", self.VB[:, :, :, 128:129], 1.0, self.VB_dep)
        tctr = 0
        for sl in range(5):
            if (sl < 4 and "A" not in parts) or (sl == 4 and "CK" not in parts):
                continue
            ncol = 384 if sl < 4 else 264
            slot = self._load_slab(sl * 384, ncol, gain)

            def mmA(t, slot=slot, ncol=ncol):
                t0, n = TILES[t]
                pb = 2 + (t % 4)
                for k in range(8):
                    self.mm(self.psum[pb][0:n, 0:ncol], self.xnT[:, k, t0:t0 + n], self.wb[slot][:, k, 0:ncol], k == 0, k == 7,
                            [self.xn_dep[t], self.wb_dep[slot]], [self.pdep[pb]])

            for t in range(3):
                mmA(t)
            for t, (t0, n) in enumerate(TILES):
                if t + 3 < NT:
                    mmA(t + 3)
                pb = 2 + (t % 4)
                tb = t % 2
                b = t % 2
                ps = self.psum[pb]
                pT = self.psum[tb].bitcast(BF16)
                k4, sdeps = self.stat()
                ssd, td, rsd = sdeps
                if sl < 4:
                    h = sl
                    KA = _os.environ.get("K_A", "sq,red,rstd,qn,tr,evq,evk,va").split(",")
                    if "sq" in KA:
                        self.act(self.sq[b][0:n, :], ps[0:n, 0:256], AF.Square, [self.pdep[pb]], [self.sq_dep[b]])
                    if "red" in KA:
                        self.red(self.ss[0:n, k4:k4 + 4], self.sq[b][0:n, :].rearrange("p (g d) -> p g d", d=64), ALU.add,
                                 [self.sq_dep[b]], [ssd])
                    if "rstd" in KA:
                        self.rstd(n, k4, 4, 1.0 / 64, sdeps)
                    if "qn" in KA:
                        self.tt("dve", self.qn[b][0:n, :].rearrange("p (g d) -> p g d", d=64),
                                ps[0:n, 0:256].rearrange("p (g d) -> p g d", d=64),
                                self.rs[0:n, k4:k4 + 4].unsqueeze(2).to_broadcast([n, 4, 64]), ALU.mult,
                                [self.pdep[pb], rsd], [self.qn_dep[b]])
                    if "tr" in KA:
                        self.tr(pT[:, 0:n], self.qn[b][0:n, 0:128], self.ident[0:n, 0:n], [self.qn_dep[b], self.ident_dep],
                                [self.pdep[tb]])
                        self.tr(pT[:, 128:128 + n], self.qn[b][0:n, 128:256], self.ident[0:n, 0:n], [self.qn_dep[b], self.ident_dep],
                                [self.pdep[tb]])
                    if "evq" in KA:
                        qdst = self.junk[:, 0:n] if _os.environ.get("K_DEST") else self.qT[:, h, t0:t0 + n]
                        self.act(qdst, pT[:, 0:n], AF.Copy, [self.pdep[tb], self.setup_dep],
                                 [self.qT_dep[h][t]], scale=self.gqk)
                    if "evk" in KA:
                        kdst = self.junk[:, 128:128 + n] if _os.environ.get("K_DEST") else self.kT[:, h, t0:t0 + n]
                        self.act(kdst, pT[:, 128:128 + n], AF.Copy, [self.pdep[tb]], [self.kT_dep[h][t]])
                    if "va" in KA:
                        self.act(self.VA[0:n, t, h, 0:128], ps[0:n, 256:384], AF.Copy, [self.pdep[pb]], [self.VA_dep[t]])
                else:
                    self.act(self.sq[b][0:n, :], ps[0:n, 0:256], AF.Square, [self.pdep[pb]], [self.sq_dep[b], ssd],
                             accum=self.ss[0:n, k4:k4 + 1])
                    self.rstd(n, k4, 1, 1.0 / 256, sdeps)
                    self.ts("dve", self.qn[b][0:n, :], ps[0:n, 0:256], self.rs[0:n, k4:k4 + 1], None, ALU.mult, ALU.bypass,
                            [self.pdep[pb], rsd], [self.qn_dep[b]])
                    self.ts("dve", self.iw_sb[0:n, t, :], ps[0:n, 256:264], (64 ** -0.5) * (8 ** -0.5), None, ALU.mult, ALU.bypass,
                            [self.pdep[pb]], [self.iw_dep[t]])
                    for cc in range(2):
                        self.tr(pT[:, cc * 128:cc * 128 + n], self.qn[b][0:n, cc * 128:(cc + 1) * 128], self.ident[0:n, 0:n],
                                [self.qn_dep[b], self.ident_dep], [self.pdep[tb]])
                    self.ts("dve", self.cT[:, 0, t0:t0 + n], pT[:, 0:n], C[:, 26:27], None, ALU.mult, ALU.bypass,
                            [self.pdep[tb], self.cdep], [self.cT_dep[t]])
                    self.act(self.cT[:, 1, t0:t0 + n], pT[:, 128:128 + n], AF.Copy, [self.pdep[tb], self.cdep], [self.cT_dep[t]],
                             scale=C[:, 27:28])
                    vb = 6 + (t % 2)
                    for cc in range(2):
                        self.mm(self.psum[vb][0:n, :], self.cT[:, cc, t0:t0 + n],
                                self.wuvb[:, cc, :, :].rearrange("p h e -> p (h e)"), cc == 0, cc == 1,
                                [self.cT_dep[t], self.setup_dep], [self.pdep[vb]])
                    self.act(self.VB[0:n, t, :, 0:128], self.psum[vb][0:n, :].rearrange("p (h e) -> p h e", h=4), AF.Copy,
                             [self.pdep[vb]], [self.VB_dep[t]])
        ectr = 0
        for sl in range(3):
            if "FM" not in parts:
                continue
            slot = self._load_slab(1800 + sl * 384, 384, gain)
            for c in range(3):
                ch = sl * 3 + c
                for g, (g0, gn) in enumerate(TGS):
                    if ch < 4:
                        dst, ddep = self.qbT[:, ch, g0:g0 + gn], self.qbT_dep[g]
                    elif ch < 8:
                        dst, ddep = self.iqT[:, ch - 4, g0:g0 + gn], self.iqT_dep[g]
                    else:
                        dst, ddep = self.ikT[:, g0:g0 + gn], self.ikT_dep[g]
                    pb = 4 + (ectr % 2)
                    xdeps = [self.xn_dep[t] for t in range(NT) if g0 <= TILES[t][0] < g0 + gn]
                    for k in range(8):
                        self.mm(self.psum[pb][:, 0:gn], self.wb[slot][:, k, c * 128:(c + 1) * 128], self.xnT[:, k, g0:g0 + gn],
                                k == 0, k == 7, [self.wb_dep[slot]] + xdeps, [self.pdep[pb]])
                    if ectr % 2 == 0:
                        self.act(dst, self.psum[pb][:, 0:gn], AF.Copy, [self.pdep[pb]], [ddep])
                    else:
                        self.cp("dve", dst, self.psum[pb][:, 0:gn], [self.pdep[pb]], [ddep])
                    ectr += 1

    def _bias_blocks(self, ps, pbank, hh, j, tiles, c0):
        k0, kn = TILES[j]
        for blk, i in ((0, j), (1, j + 1)):
            if i in tiles:
                off = TILES[i][0] - c0
                ni = TILES[i][1]
                self.mm(ps[:, off:off + ni], self.ident[0:kn, 0:kn], self.biasbf[0:kn, hh, blk, 0:ni], False, True,
                        [self.ident_dep, self.setup_dep], [self.pdep[pbank]])

    def _pipe(self, n, stage1, stage2, depth=2):
        for k in range(min(depth, n)):
            stage1(k)
        for k in range(n):
            if k + depth < n:
                stage1(k + depth)
            stage2(k)

    def _attnA(self):
        for G, tiles in enumerate(QGS):
            q0 = TILES[tiles[0]][0]
            qn_ = sum(TILES[t][1] for t in tiles)
            for h in range(4):
                started = set()
                nblk = tiles[-1] + 1

                def stage1(j, h=h, tiles=tiles, q0=q0, qn_=qn_):
                    k0, kn = TILES[j]
                    c0 = max(q0, k0)
                    ncol = q0 + qn_ - c0
                    qdeps = [self.qT_dep[h][t] for t in tiles if TILES[t][0] + TILES[t][1] > c0]
                    for m in range(2):
                        bank = (j % 2) * 2 + m
                        ps = self.psum[bank][0:kn, 0:ncol]
                        self.mm(ps, self.kT[m * 64:(m + 1) * 64, h, k0:k0 + kn], self.qT[m * 64:(m + 1) * 64, h, c0:c0 + ncol],
                                True, True, [self.kT_dep[h][j]] + qdeps, [self.pdep[bank]])
                    for m in range(2):
                        bank = (j % 2) * 2 + m
                        ps = self.psum[bank][0:kn, 0:ncol]
                        self._bias_blocks(ps, bank, h, j, tiles, c0)
                        self.act(self.PT[bank][0:kn, 0:ncol], ps, AF.Exp, [self.pdep[bank], self.setup_dep], [self.PT_dep[bank]],
                                 bias=self.abias[0:kn, h:h + 1])

                def stage2(j, h=h, tiles=tiles, q0=q0, started=started):
                    k0, kn = TILES[j]
                    c0 = max(q0, k0)
                    for m in range(2):
                        pbuf = (j % 2) * 2 + m
                        for il, i in enumerate(tiles):
                            if i < j:
                                continue
                            off = TILES[i][0] - c0
                            ni = TILES[i][1]
                            slot = m * 4 + il
                            ab = 4 + slot // 3
                            o = (slot % 3) * 129
                            first = ab not in started
                            started.add(ab)
                            self.mm(self.psum[ab][0:ni, o:o + 129], self.PT[pbuf][0:kn, off:off + ni], self.VA[0:kn, j, h, :],
                                    first, j == i, [self.PT_dep[pbuf], self.VA_dep[j]], [self.pdep[ab]])

                self._pipe(nblk, stage1, stage2, depth=1)
                for il, i in enumerate(tiles):
                    ni = TILES[i][1]
                    b = il % 2
                    s0, s1 = il, 4 + il
                    b0, o0 = 4 + s0 // 3, (s0 % 3) * 129
                    b1, o1 = 4 + s1 // 3, (s1 % 3) * 129
                    k4, sdeps = self.stat()
                    ssd, td, rsd = sdeps
                    self.recip(self.rs[0:ni, k4 + 1:k4 + 2], self.psum[b0][0:ni, o0 + 128:o0 + 129], [self.pdep[b0]], [rsd])
                    self.recip(self.rs[0:ni, k4 + 2:k4 + 3], self.psum[b1][0:ni, o1 + 128:o1 + 129], [self.pdep[b1]], [rsd])
                    self.ts("dve", self.rs[0:ni, k4 + 3:k4 + 4], self.rs[0:ni, k4 + 2:k4 + 3], self.lamn[0:ni, 0:1], None,
                            ALU.mult, ALU.bypass, [rsd, self.setup_dep], [rsd])
                    self.ts("dve", self.t1[b][0:ni, :], self.psum[b1][0:ni, o1:o1 + 128], self.rs[0:ni, k4 + 3:k4 + 4], None,
                            ALU.mult, ALU.bypass, [self.pdep[b1], rsd], [self.t1_dep[b]])
                    self.stt(self.ov[b][0:ni, :], self.psum[b0][0:ni, o0:o0 + 128], self.rs[0:ni, k4 + 1:k4 + 2],
                             self.t1[b][0:ni, :], ALU.mult, ALU.add, [self.pdep[b0], rsd, self.t1_dep[b]], [self.ov_dep[b]])
                    self.act(self.junk[0:ni, 0:128], self.ov[b][0:ni, :], AF.Square, [self.ov_dep[b]], [self.junk_dep, ssd],
                             accum=self.ss[0:ni, k4:k4 + 1])
                    self.rstd(ni, k4, 1, 1.0 / 128, sdeps)
                    self.ts("dve", self.oa[0:ni, i, h * 128:(h + 1) * 128], self.ov[b][0:ni, :], self.rs[0:ni, k4:k4 + 1], None,
                            ALU.mult, ALU.bypass, [self.ov_dep[b], rsd], [self.oa_dep[i]])

    def _attnB(self):
        C = self.cols_sb
        for q in range(4):
            st = self.hst2
            hd = self.hst_dep
            self.dma(st, self.wout[q * 256:(q + 1) * 256, :].rearrange("(k p) c -> p k c", p=128), [hd[1]], [hd[0]])
            if q < 2:
                self.ts("pool", self.woutb[:, 2 * q:2 * q + 2, :], st, self.gsub[:, 0:1], None, ALU.mult, ALU.bypass,
                        [hd[0], hd[1], self.setup_dep], [self.woutb_dep])
            else:
                self.cp("pool", self.woutb[:, 2 * q:2 * q + 2, :], st, [hd[0], hd[1]], [self.woutb_dep])
        self.dctr = 0
        self.hctr = 0
        ng = len(QGS)
        for il in range(len(QGS[0])):
            self._b_idx1(0, il)
            self._b_idx2(0, il)
        for G in range(ng):
            self._b_qlat(G)
            nxt = G + 1 if G + 1 < ng - 1 else None
            for h in range(4):
                if nxt is not None and h < len(QGS[nxt]):
                    self._b_idx1(nxt, h)
                self._b_attn_mm(G, h)
                if nxt is not None and h < len(QGS[nxt]):
                    self._b_idx2(nxt, h)
                self._b_attn_norm(G, h)
            if G + 1 == ng - 1:
                for il in range(len(QGS[G + 1])):
                    self._b_idx1(G + 1, il)
                    self._b_idx2(G + 1, il)
            self._b_wout(G)

    def _b_ctx(self, G):
        tiles = QGS[G]
        q0 = TILES[tiles[0]][0]
        qn_ = sum(TILES[t][1] for t in tiles)
        use_a = G in (0, 2)
        mT = self.mTa if use_a else self.mT
        mT_dep = self.mTa_dep if use_a else self.mT_dep
        return tiles, q0, qn_, mT, mT_dep

    def _b_qlat(self, G):
        tiles, q0, qn_, mT, mT_dep = self._b_ctx(G)
        for il, i in enumerate(tiles):
            t0, ni = TILES[i]
            for h in range(4):
                pbk = h // 2
                self.mm(self.psum[pbk][0:ni, (h % 2) * 256:(h % 2) * 256 + 256], self.qbT[:, h, t0:t0 + ni], self.wukb[:, h, :],
                        True, True, [self.qbT_dep[G], self.setup_dep], [self.pdep[pbk]])
            k4, sdeps = self.stat()
            ssd, td, rsd = sdeps
            for pbk in range(2):
                self.act(self.junk[0:ni, pbk * 512:(pbk + 1) * 512], self.psum[pbk][0:ni, :], AF.Square, [self.pdep[pbk]],
                         [self.junk_dep])
            self.red(self.ss[0:ni, k4:k4 + 4], self.junk[0:ni, :].rearrange("p (g d) -> p g d", d=256), ALU.add,
                     [self.junk_dep], [ssd])
            self.rstd(ni, k4, 4, 1.0 / 256, sdeps)
            for pbk in range(2):
                self.tt("dve", self.qn1k[0:ni, pbk * 512:(pbk + 1) * 512].rearrange("p (g d) -> p g d", d=256),
                        self.psum[pbk][0:ni, :].rearrange("p (g d) -> p g d", d=256),
                        self.rs[0:ni, k4 + 2 * pbk:k4 + 2 * pbk + 2].unsqueeze(2).to_broadcast([ni, 2, 256]), ALU.mult,
                        [self.pdep[pbk], rsd], [self.qn1k_dep])
            pT = self.psum[2].bitcast(BF16)
            for ch in range(8):
                self.tr(pT[:, ch * 128:ch * 128 + ni], self.qn1k[0:ni, ch * 128:(ch + 1) * 128], self.ident[0:ni, 0:ni],
                        [self.qn1k_dep, self.ident_dep], [self.pdep[2]])
            pv = pT.rearrange("p (h c t) -> p h c t", h=4, c=2)
            self.ts("dve", self.qlT[:, :, 0, il * 128:il * 128 + ni], pv[:, :, 0, 0:ni], self.gqb[0], None, ALU.mult,
                    ALU.bypass, [self.pdep[2], self.setup_dep], [self.qlT_dep[il]])
            self.ts("dve", self.qlT[:, :, 1, il * 128:il * 128 + ni], pv[:, :, 1, 0:ni], self.gqb[1], None, ALU.mult,
                    ALU.bypass, [self.pdep[2], self.setup_dep], [self.qlT_dep[il]])

    def _b_idx1(self, G, il):
        tiles, q0, qn_, mT, mT_dep = self._b_ctx(G)
        i = tiles[il]
        dctr = self.dctr
        t0, ni = TILES[i]
        Lk = t0 + ni
        acc = self.acc
        for kc0 in range(0, Lk, 512):
            kcn = min(512, Lk - kc0)
            g = kc0 // 512
            for hh in range(8):
                pb = 3 + (dctr % 2)
                rb = dctr % 2
                dctr += 1
                pr = (hh % 2) * 64
                self.mm(self.psum[pb][0:ni, 0:kcn], self.iqT[pr:pr + 64, hh // 2, t0:t0 + ni], self.ikT[pr:pr + 64, kc0:kc0 + kcn],
                        True, True, [self.iqT_dep[G], self.ikT_dep[g]], [self.pdep[pb]])
                self.act(self.rr[rb][0:ni, 0:kcn], self.psum[pb][0:ni, 0:kcn], AF.Relu, [self.pdep[pb]], [self.rr_dep[rb]])
                if hh == 0:
                    self.ts("dve", acc[0:ni, kc0:kc0 + kcn], self.rr[rb][0:ni, 0:kcn], self.iw_sb[0:ni, i, 0:1], None,
                            ALU.mult, ALU.bypass, [self.rr_dep[rb], self.iw_dep[i]], [self.acc_dep])
                else:
                    self.stt(acc[0:ni, kc0:kc0 + kcn], self.rr[rb][0:ni, 0:kcn], self.iw_sb[0:ni, i, hh:hh + 1],
                             acc[0:ni, kc0:kc0 + kcn], ALU.mult, ALU.add, [self.rr_dep[rb], self.iw_dep[i], self.acc_dep],
                             [self.acc_dep])
        self.dctr = dctr

    def _b_idx2(self, G, il):
        tiles, q0, qn_, mT, mT_dep = self._b_ctx(G)
        i = tiles[il]
        t0, ni = TILES[i]
        Lk = t0 + ni
        acc = self.acc
        bs = self.bst
        bd = self.bst_dep
        if i >= 2:
            self.red(bs[0:ni, 0:1], acc[0:ni, 0:Lk], ALU.max, [self.acc_dep], [bd])
            self.red(bs[0:ni, 1:2], acc[0:ni, 0:Lk], ALU.min, [self.acc_dep], [bd])
        self.tt("dve", acc[0:ni, t0:t0 + ni], acc[0:ni, t0:t0 + ni], self.masktok[0:ni, 0:ni], ALU.add,
                [self.acc_dep, self.masktok_dep], [self.acc_dep])
        if i >= 2:
            self.tt("dve", bs[0:ni, 2:3], bs[0:ni, 0:1], bs[0:ni, 1:2], ALU.subtract, [bd], [bd])
            W0 = 8
            self.ts("dve", bs[0:ni, W0:W0 + NBIS + 1], self.pw[0:ni, :], bs[0:ni, 2:3], None, ALU.mult, ALU.bypass,
                    [bd, self.setup_dep], [bd])
            M0 = W0 + NBIS + 1
            self.tt("dve", bs[0:ni, M0:M0 + 1], bs[0:ni, 1:2], bs[0:ni, W0:W0 + 1], ALU.add, [bd], [bd])
            C0 = M0 + NBIS + 1
            for k in range(NBIS):
                self.ts("dve", self.cntj[0:ni, 0:Lk], acc[0:ni, 0:Lk], bs[0:ni, M0 + k:M0 + k + 1], None, ALU.is_ge, ALU.add,
                        [self.acc_dep, bd], [self.cntj_dep, bd], accum=bs[0:ni, C0 + k:C0 + k + 1])
                self.ts("dve", bs[0:ni, 3:4], bs[0:ni, C0 + k:C0 + k + 1], TOPK - 0.5, bs[0:ni, W0 + k:W0 + k + 1],
                        ALU.is_ge, ALU.mult, [bd], [bd])
                wn = W0 + k + 1 if k < NBIS - 1 else W0 + k
                self.stt(bs[0:ni, M0 + k + 1:M0 + k + 2], bs[0:ni, 3:4], bs[0:ni, wn:wn + 1], bs[0:ni, M0 + k:M0 + k + 1],
                         ALU.subtract, ALU.add, [bd], [bd])
            theta = bs[0:ni, M0 + NBIS:M0 + NBIS + 1]
            thd = [bd]
        else:
            theta = self.thc[0:ni, 0:1]
            thd = [self.setup_dep]
        self.ts("dve", self.maskb[0:ni, 0:Lk], acc[0:ni, 0:Lk], theta, NEG, ALU.is_lt, ALU.mult, [self.acc_dep] + thd,
                [self.maskb_dep])
        for j0 in range(0, i + 1, 8):
            js = list(range(j0, min(j0 + 8, i + 1)))
            tb = 5
            pT = self.psum[tb].bitcast(BF16)
            for jj, j in enumerate(js):
                k0, kn = TILES[j]
                self.tr(pT[0:kn, jj * 128:jj * 128 + ni], self.maskb[0:ni, k0:k0 + kn], self.ident[0:ni, 0:ni],
                        [self.maskb_dep, self.ident_dep], [self.pdep[tb]])
            src = pT.rearrange("p (j t) -> p j t", j=8)[:, 0:len(js), 0:ni]
            self.cp("dve", mT[:, j0:j0 + len(js), il * 128:il * 128 + ni], src, [self.pdep[tb]], [mT_dep[il]])

    def _b_attn_mm(self, G, h):
        tiles, q0, qn_, mT, mT_dep = self._b_ctx(G)
        started = set()
        nblk = tiles[-1] + 1

        def stage1(j, h=h, tiles=tiles, q0=q0, qn_=qn_, mT=mT, mT_dep=mT_dep):
            k0, kn = TILES[j]
            c0 = max(q0, k0)
            ncol = q0 + qn_ - c0
            co = c0 - q0
            bank = j % 3
            ps = self.psum[bank][0:kn, 0:ncol]
            qd = [self.qlT_dep[il] for il, t in enumerate(tiles) if TILES[t][0] + TILES[t][1] > c0]
            md = [mT_dep[il] for il, t in enumerate(tiles) if TILES[t][0] + TILES[t][1] > c0]
            for cc in range(2):
                self.mm(ps, self.cT[:, cc, k0:k0 + kn], self.qlT[:, h, cc, co:co + ncol], cc == 0, False,
                        [self.cT_dep[j]] + qd, [self.pdep[bank]])
            self.mm(ps, self.ident[0:kn, 0:kn], mT[0:kn, j, co:co + ncol], False, True, [self.ident_dep] + md,
                    [self.pdep[bank]])
            self._bias_blocks(ps, bank, 4 + h, j, tiles, c0)
            self.act(self.PT[j % 4][0:kn, 0:ncol], ps, AF.Exp, [self.pdep[bank], self.setup_dep], [self.PT_dep[j % 4]],
                     bias=self.abias[0:kn, 4 + h:5 + h])

        def stage2(j, h=h, tiles=tiles, q0=q0, started=started):
            k0, kn = TILES[j]
            c0 = max(q0, k0)
            pbuf = j % 4
            for il, i in enumerate(tiles):
                if i < j:
                    continue
                off = TILES[i][0] - c0
                ni = TILES[i][1]
                ab = 6 + il // 3
                o = (il % 3) * 129
                first = ab not in started
                started.add(ab)
                self.mm(self.psum[ab][0:ni, o:o + 129], self.PT[pbuf][0:kn, off:off + ni], self.VB[0:kn, j, h, :],
                        first, j == i, [self.PT_dep[pbuf], self.VB_dep[j]], [self.pdep[ab]])

        self._pipe(nblk, stage1, stage2)

    def _b_attn_norm(self, G, h):
        tiles, q0, qn_, mT, mT_dep = self._b_ctx(G)
        for il, i in enumerate(tiles):
            ni = TILES[i][1]
            ab = 6 + il // 3
            o = (il % 3) * 129
            k4, sdeps = self.stat()
            ssd, td, rsd = sdeps
            self.recip(self.rs[0:ni, k4:k4 + 1], self.psum[ab][0:ni, o + 128:o + 129], [self.pdep[ab]], [rsd])
            self.ts("dve", self.obG[0:ni, il, h * 128:(h + 1) * 128], self.psum[ab][0:ni, o:o + 128], self.rs[0:ni, k4:k4 + 1],
                    None, ALU.mult, ALU.bypass, [self.pdep[ab], rsd], [self.obG_dep[il]])

    def _b_wout(self, G):
        tiles, q0, qn_, mT, mT_dep = self._b_ctx(G)
        hctr = self.hctr
        for il, i in enumerate(tiles):
            t0, ni = TILES[i]
            pT = self.psum[2].bitcast(BF16)
            for k in range(8):
                src = self.oa[0:ni, i, k * 128:(k + 1) * 128] if k < 4 else self.obG[0:ni, il, (k - 4) * 128:(k - 3) * 128]
                sd_ = self.oa_dep[i] if k < 4 else self.obG_dep[il]
                self.tr(pT[:, k * 128:k * 128 + ni], src, self.ident[0:ni, 0:ni], [sd_, self.ident_dep], [self.pdep[2]])
            self.act(self.ocT[:, :, 0:ni], pT.rearrange("p (k t) -> p k t", k=8)[:, :, 0:ni], AF.Copy, [self.pdep[2]],
                     [self.ocT_dep])
            hb = hctr % 2
            hctr += 1
            self.dma(self.hst[hb][0:ni, :], self.hs[t0:t0 + ni, :], [self.hs_dep[i]], [self.hst_dep[hb]])
            for half in range(2):
                pb = 3 + half
                for k in range(8):
                    self.mm(self.psum[pb][0:ni, :], self.ocT[:, k, 0:ni], self.woutb[:, k, half * 512:(half + 1) * 512],
                            k == 0, k == 7, [self.ocT_dep, self.woutb_dep], [self.pdep[pb]])
                hv = self.hst[hb][0:ni, half * 512:(half + 1) * 512]
                self.tt("dve", hv, self.psum[pb][0:ni, :], hv, ALU.add, [self.pdep[pb], self.hst_dep[hb]], [self.hst_dep[hb]])
            self.dma(self.hs[t0:t0 + ni, :], self.hst[hb][0:ni, :], [self.hst_dep[hb]], [self.hs_dep[i]])
        self.hctr = hctr

    def _finish(self):
        if _os.environ.get("K_DUMP"):
            dd = Dep("dump")
            self.P.barrier()
            self.dma(self.hs[0:128, 0:16], self.smallc[:, 0:16], [self.setup_dep], [dd])
            self.dma(self.hs[0:128, 16:24], self.abias, [self.setup_dep], [dd])
            self.P.add("sp", None, [dd], [])
        self.P.add("sp", None, [self.out_dep], [])
        if self.dbg is not None:
            pass


def _bucket(n):
    n = np.maximum(n, 0)
    nf = np.maximum(n, 1).astype(np.float32)
    large = 16 + (np.log(nf / np.float32(16)) / np.float32(math.log(128 / 16)) * np.float32(16)).astype(np.int32)
    large = np.minimum(large, 31)
    return np.where(n < 16, n, large)


def _prep_shared(inp):
    f = lambda a: np.ascontiguousarray(np.asarray(a, dtype=np.float32))
    sh = {}
    sh["meta"] = f(inp["meta_tokens"])
    for i, nm in ((1, "ffn1"), (2, "ffn2")):
        sh["w%dg" % i] = f(inp[nm + "_w_gate"][0])
        sh["w%du" % i] = f(inp[nm + "_w_up"][0])
        sh["w%dd" % i] = f(inp[nm + "_w_down"][0])
    w_in = f(inp["w_in"][0])
    qa, ka, va, qb, ckv, iq, ik, iw = np.split(w_in, np.cumsum([512, 512, 512, 512, 256, 512, 64, 8])[:-1], axis=1)
    parts = []
    for h in range(4):
        parts += [qa[:, h * 128:(h + 1) * 128], ka[:, h * 128:(h + 1) * 128], va[:, h * 128:(h + 1) * 128]]
    parts += [ckv, iw]
    parts += [qb, iq, ik, ik]
    sh["win"] = np.ascontiguousarray(np.concatenate(parts, axis=1))
    assert sh["win"].shape[1] == WIN_COLS
    sh["wuk"] = np.ascontiguousarray(f(inp["b_w_uk"][0]).transpose(1, 0, 2))
    sh["wuv"] = np.ascontiguousarray(f(inp["b_w_uv"][0]).reshape(4, 2, 128, 128).transpose(2, 1, 0, 3))
    sh["wout"] = f(inp["w_out"][0])
    cols = np.zeros((128, 40), np.float32)
    cols[:, 0:8] = f(inp["ffn1_norm"][0]).reshape(8, 128).T
    cols[:, 8:16] = f(inp["mix_norm"][0]).reshape(8, 128).T
    cols[:, 16:24] = f(inp["ffn2_norm"][0]).reshape(8, 128).T
    cols[:, 24] = np.tile(f(inp["a_q_norm"][0]), 2)
    cols[:, 25] = np.tile(f(inp["a_k_norm"][0]), 2)
    cols[:, 26:28] = f(inp["b_kv_norm"][0]).reshape(2, 128).T
    cols[:, 28:30] = f(inp["b_q_norm"][0]).reshape(2, 128).T
    cols[:, 30] = f(inp["a_subln"][0])
    rb = f(inp["rel_bias"])
    cols[:, 31:39] = np.broadcast_to(rb[31], (128, 8))
    sh["cols"] = cols
    rows = np.zeros((128, 896), np.float32)
    rows[:, 0:64] = f(inp["a_lambda_q1"][0])[None]
    rows[:, 64:128] = f(inp["a_lambda_k1"][0])[None]
    rows[:, 128:192] = f(inp["a_lambda_q2"][0])[None]
    rows[:, 192:256] = f(inp["a_lambda_k2"][0])[None]
    rows[:, 256:320] = f(inp["a_q_norm"][0])[None]
    rows[:, 320:384] = f(inp["a_k_norm"][0])[None]
    rows[:, 384:640] = f(inp["b_q_norm"][0])[None]
    rows[:, 640:896] = f(inp["b_kv_norm"][0])[None]
    sh["rows"] = rows
    tk = np.arange(128)[:, None]
    tq = np.arange(128)[None, :]
    bb = np.zeros((128, 8, 2, 128), np.float32)
    for blk in range(2):
        idx = _bucket(tq - tk + 128 * blk)
        bb[:, :, blk, :] = rb[idx].transpose(0, 2, 1)
    sh["biasblk"] = bb
    cm = np.zeros((128, 3, 128), np.float32)
    cm[:, 0, :] = np.eye(128, dtype=np.float32)
    cm[:, 1, :] = np.where(tq >= tk, 0.0, NEG)
    cm[:, 2, :] = np.where(tq <= tk, 0.0, -1e30)
    sh["cmask"] = cm
    return sh


_CACHE = {}


def kernel(**inputs):
    x = np.asarray(inputs["x"], dtype=np.float32)
    sh = _prep_shared(inputs)
    if "nc" not in _CACHE:
        _CACHE["nc"] = Builder().build()
    nc = _CACHE["nc"]
    in_maps = []
    for c in range(NCORES):
        m = dict(sh)
        m["x"] = np.ascontiguousarray(x[c * NSEQ:(c + 1) * NSEQ])
        in_maps.append(m)
    res = run_bass_kernel_spmd(nc, in_maps, core_ids=list(range(NCORES)))
    out = np.concatenate([np.asarray(r["out"]) for r in res.results], axis=0)
    return out.astype(np.float32)
```

```python
import math
import os as _os
from contextlib import ExitStack

import numpy as np
import concourse.bass as bass
import concourse.mybir as mybir
from concourse.bass_utils import run_bass_kernel_spmd

F32 = mybir.dt.float32
BF16 = mybir.dt.bfloat16
ALU = mybir.AluOpType
AF = mybir.ActivationFunctionType
AX = mybir.AxisListType

D = 1024
SEQ = 2048
NMETA = 16
L = SEQ + NMETA
DFF = 2816
NSEQ = 2
NCORES = 8
NT = 17
TILES = [(i * 128, min(128, L - i * 128)) for i in range(NT)]
TGS = [(0, 512), (512, 512), (1024, 512), (1536, 512), (2048, 16)]
QGS = [[0, 1, 2, 3], [4, 5, 6, 7], [8, 9, 10, 11], [12, 13, 14, 15], [16]]
EPS = 1e-6
A_SCALE = 64 ** -0.5
B_SCALE = 256 ** -0.5
LAM_INIT = 0.8 - 0.6 * math.exp(0.0)
TOPK = 256
NEG = -30000.0
NBIS = 10
WIN_COLS = 4 * 384 + 264 + 9 * 128
STRICT = True
EPOCH = 2000


def _dsize(dt):
    return 4 if dt == F32 else 2


class Dep:
    __slots__ = ("name", "lw", "rd", "sem", "dcount")

    def __init__(self, name):
        self.name = name
        self.lw = None
        self.rd = []
        self.sem = None
        self.dcount = 0


class Op:
    __slots__ = ("eng", "fn", "deps", "dma", "signal", "seq", "sem", "waits", "wdep")

    def __init__(self, eng, fn, deps, dma, wdep):
        self.eng = eng
        self.fn = fn
        self.deps = deps
        self.dma = dma
        self.signal = False
        self.seq = 0
        self.sem = None
        self.waits = []
        self.wdep = wdep


class Prog:
    ENGS = ("sp", "pe", "act", "dve", "pool")

    def __init__(self, nc):
        self.nc = nc
        self.ops = []
        self.last = {e: None for e in self.ENGS}
        self.dmas_since_barrier = []

    def add(self, eng, fn, r=(), w=(), dma=False):
        idx = len(self.ops)
        deps = set()
        for t in r:
            if t.lw is not None:
                deps.add(t.lw)
        for t in w:
            if t.lw is not None:
                deps.add(t.lw)
            deps.update(t.rd)
        for t in r:
            t.rd.append(idx)
        for t in w:
            t.lw = idx
            t.rd = []
        wdep = None
        if dma:
            assert len(w) == 1
            wdep = w[0]
            self.dmas_since_barrier.append(idx)
        self.ops.append(Op(eng, fn, deps, dma, wdep))
        self.last[eng] = idx
        return idx

    def barrier(self):
        lasts = {v for v in self.last.values() if v is not None}
        lasts.update(self.dmas_since_barrier)
        self.dmas_since_barrier = []
        for e in self.ENGS:
            idx = len(self.ops)
            self.ops.append(Op(e, None, set(lasts), False, None))
            self.last[e] = idx

    def emit(self, stack):
        nc = self.nc
        ops = self.ops
        for op in ops:
            for d in sorted(op.deps):
                dop = ops[d]
                if not dop.dma and dop.eng == op.eng and (op.eng == "pe" or not STRICT):
                    continue
                if dop.fn is None:
                    continue
                dop.signal = True
                op.waits.append(d)
        esem = {e: stack.enter_context(nc.semaphore("s_" + e)) for e in self.ENGS}
        cnt = {e: 0 for e in self.ENGS}
        NCH = 8
        chsem = [stack.enter_context(nc.semaphore("dch%d" % i)) for i in range(NCH)]
        chcnt = [0] * NCH
        chlast = [None] * NCH
        ndma = 0
        for oi, op in enumerate(ops):
            if op.fn is None:
                continue
            if op.dma:
                c = ndma % NCH
                ndma += 1
                if chlast[c] is not None:
                    op.waits.append(chlast[c])
                chlast[c] = oi
                chcnt[c] += 16
                op.sem = chsem[c]
                op.seq = chcnt[c]
            elif op.signal:
                if cnt[op.eng] >= EPOCH:
                    esem[op.eng] = stack.enter_context(nc.semaphore("s_%s_%d" % (op.eng, oi)))
                    cnt[op.eng] = 0
                cnt[op.eng] += 1
                op.sem = esem[op.eng]
                op.seq = cnt[op.eng]
        per = {e: [op for op in ops if op.eng == e] for e in self.ENGS}

        def run(e, h):
            waited = {}
            for op in per[e]:
                need = {}
                for d in op.waits:
                    dop = ops[d]
                    k = id(dop.sem)
                    if k not in need or need[k][1] < dop.seq:
                        need[k] = (dop.sem, dop.seq)
                for k, (sem, val) in need.items():
                    if waited.get(k, 0) >= val:
                        continue
                    h.wait_ge(sem, val)
                    waited[k] = val
                if op.fn is None:
                    continue
                ins = op.fn(h)
                if op.dma:
                    ins.then_inc(op.sem, 16)
                elif op.signal:
                    ins.then_inc(op.sem, 1)

        with nc.Block() as block:
            @block.sync
            def _(h):
                run("sp", h)

            @block.tensor
            def _(h):
                run("pe", h)

            @block.scalar
            def _(h):
                run("act", h)

            @block.vector
            def _(h):
                run("dve", h)

            @block.gpsimd
            def _(h):
                run("pool", h)


class Builder:
    def __init__(self, stage=99, nseq=NSEQ, dbg=False, dbg_stop=False):
        self.dbg_stop = dbg_stop
        self.stage = stage
        self.nseq = nseq
        self.nc = nc = bass.Bass("TRN2", target_bir_lowering=False)
        self.P = Prog(nc)
        self.cur = 16640
        dt = nc.dram_tensor
        self.x = dt("x", [NSEQ, SEQ, D], F32, kind="ExternalInput").ap()
        self.meta = dt("meta", [NMETA, D], F32, kind="ExternalInput").ap()
        self.wg = [dt("w%dg" % i, [D, DFF], F32, kind="ExternalInput").ap() for i in (1, 2)]
        self.wu = [dt("w%du" % i, [D, DFF], F32, kind="ExternalInput").ap() for i in (1, 2)]
        self.wd = [dt("w%dd" % i, [DFF, D], F32, kind="ExternalInput").ap() for i in (1, 2)]
        self.win = dt("win", [D, WIN_COLS], F32, kind="ExternalInput").ap()
        self.wuk = dt("wuk", [128, 4, 256], F32, kind="ExternalInput").ap()
        self.wuv = dt("wuv", [128, 2, 4, 128], F32, kind="ExternalInput").ap()
        self.wout = dt("wout", [D, D], F32, kind="ExternalInput").ap()
        self.cols = dt("cols", [128, 40], F32, kind="ExternalInput").ap()
        self.rows = dt("rows", [128, 896], F32, kind="ExternalInput").ap()
        self.bias = dt("biasblk", [128, 8, 2, 128], F32, kind="ExternalInput").ap()
        self.cmask = dt("cmask", [128, 3, 128], F32, kind="ExternalInput").ap()
        self.out = dt("out", [NSEQ, SEQ, D], F32, kind="ExternalOutput").ap()
        self.hs = dt("hs", [L, D], F32, kind="ExternalOutput").ap()
        self.dbg = dt("dbg", [128, 4096], F32, kind="ExternalOutput").ap() if dbg else None
        self.psum = [nc.alloc_psum_tensor("pb%d" % i, [128, 512], F32).ap() for i in range(8)]
        self.pdep = [Dep("pb%d" % i) for i in range(8)]
        self.out_dep = Dep("out")

    def sb(self, name, shape, dtype, at=None):
        n = 1
        for s in shape[1:]:
            n *= s
        nbytes = (n * _dsize(dtype) + 63) // 64 * 64
        off = self.cur if at is None else at
        t = self.nc.alloc_sbuf_tensor_at(name, list(shape), dtype, offset=off)
        if at is None:
            self.cur = off + nbytes
        assert off + nbytes <= 229376, (name, off + nbytes)
        return t.ap()

    def mm(self, out, lhsT, rhs, start, stop, r, w):
        self.P.add("pe", lambda e: e.matmul(out, lhsT, rhs, start=start, stop=stop, skip_group_check=True), r, w)

    def tr(self, out, in_, ident, r, w):
        self.P.add("pe", lambda e: e.transpose(out, in_, ident), r, w)

    def act(self, out, in_, func, r, w, bias=0.0, scale=1.0, accum=None):
        if accum is None:
            self.P.add("act", lambda e: e.activation(out, in_, func, bias=bias, scale=scale), r, w)
        else:
            self.P.add("act", lambda e: e.activation(out, in_, func, bias=bias, scale=scale, accum_out=accum), r, w)

    def ts(self, eng, out, in0, s1, s2, op0, op1, r, w, accum=None):
        if accum is None:
            self.P.add(eng, lambda e: e.tensor_scalar(out, in0, s1, s2, op0, op1), r, w)
        else:
            self.P.add(eng, lambda e: e.tensor_scalar(out, in0, s1, s2, op0, op1, accum_out=accum), r, w)

    def tt(self, eng, out, in0, in1, op, r, w):
        self.P.add(eng, lambda e: e.tensor_tensor(out, in0, in1, op), r, w)

    def stt(self, out, in0, scalar, in1, op0, op1, r, w):
        self.P.add("dve", lambda e: e.scalar_tensor_tensor(out, in0, scalar, in1, op0, op1), r, w)

    def cp(self, eng, out, in_, r, w):
        self.P.add(eng, lambda e: e.tensor_copy(out, in_), r, w)

    def red(self, out, in_, op, r, w, absval=False):
        self.P.add("dve", lambda e: e.tensor_reduce(out, in_, AX.X, op, apply_absolute_value=absval), r, w)

    def recip(self, out, in_, r, w):
        self.P.add("dve", lambda e: e.reciprocal(out, in_), r, w)

    def memset(self, eng, ap, val, w):
        self.P.add(eng, lambda e: e.memset(ap, val), (), w)

    def dma(self, out, in_, r, w):
        self.P.add("sp", lambda e: e.dma_start(out=out, in_=in_), r, w, dma=True)

    def stat(self):
        i = self.st_i
        self.st_i = (i + 1) % 8
        return 4 * i, self.st_dep[i]

    def rstd(self, np_, k, nc_, inv_n, deps):
        ssd, td, rsd = deps
        self.ts("dve", self.tmp[0:np_, k:k + nc_], self.ss[0:np_, k:k + nc_], inv_n, EPS, ALU.mult, ALU.add, [ssd], [td])
        self.tt("pool", self.rs[0:np_, k:k + nc_], self.tmp[0:np_, k:k + nc_], self.neghalf[0:np_, 0:nc_], ALU.pow,
                [td, self.nh_dep], [rsd])

    def build(self):
        nc = self.nc
        stage = self.stage
        with ExitStack() as stack:
            self._alloc()
            self._setup()
            for s in range(self.nseq):
                self._sequence(s)
            self._finish()
            self.P.emit(stack)
        return nc

    def _alloc(self):
        sb = self.sb
        self.cols_sb = sb("cols", [128, 40], F32)
        self.rows_sb = sb("rows", [128, 896], F32)
        self.ident_f = sb("identf", [128, 128], F32)
        self.ident = sb("ident", [128, 128], BF16)
        self.neghalf = sb("neghalf", [128, 16], F32)
        self.smallc = sb("smallc", [128, 32], F32)
        self.gqk = self.smallc[:, 6:7]
        self.gqb = [self.smallc[:, 2:3], self.smallc[:, 10:11]]
        self.lamn = self.smallc[:, 14:15]
        self.abias = sb("abias", [128, 8], F32)
        self.gsub = self.smallc[:, 18:19]
        self.sm = sb("small", [128, 272], F32)
        self.biasbf = sb("biasbf", [128, 8, 2, 128], BF16)
        self.masktok = sb("masktok", [128, 128], F32)
        self.iw_sb = sb("iw", [128, NT, 8], F32)
        self.ss = sb("ss", [128, 32], F32)
        self.tmp = sb("tmpst", [128, 32], F32)
        self.rs = sb("rs", [128, 32], F32)
        self.st_dep = [(Dep("ss%d" % i), Dep("tm%d" % i), Dep("rs%d" % i)) for i in range(8)]
        self.st_i = 0
        self.wukb = sb("wukb", [128, 4, 256], BF16)
        self.wuvb = sb("wuvb", [128, 2, 4, 128], BF16)
        self.maskT = sb("maskT", [128, 128], F32)
        self.pw = sb("pw", [128, NBIS + 1], F32)
        self.thc = self.smallc[:, 22:23]
        self.xn_s = [sb("xn_s%d" % i, [128, D], BF16) for i in range(2)]
        self.xn_s_dep = [Dep("xn_s%d" % i) for i in range(2)]
        self.junk = sb("junk", [128, D], BF16)
        self.junk_dep = Dep("junk")
        self.c_const = self.cur
        self.xnT = sb("xnT", [128, 8, L], BF16)
        self.xn_dep = [Dep("xnT%d" % t) for t in range(NT)]
        self.h_off = self.cur
        self.h = sb("h", [128, NT, D], F32)
        self.h_dep = [Dep("h%d" % t) for t in range(NT)]
        self.big_off = self.cur
        self.phase_off = self.cur

    def _setup(self):
        P = self.P
        cd = self.cdep = Dep("consts")
        self.dma(self.cols_sb, self.cols, [], [cd])
        rd = Dep("rows")
        self.dma(self.rows_sb, self.rows, [], [rd])
        idd = Dep("identf")
        self.dma(self.ident_f, self.cmask[:, 0, :], [], [idd])
        mk = Dep("masktok")
        self.dma(self.masktok, self.cmask[:, 2, :], [], [mk])
        self.masktok_dep = mk
        self.ident_dep = Dep("ident")
        self.cp("dve", self.ident, self.ident_f, [idd], [self.ident_dep])
        self.nh_dep = Dep("neghalf")
        self.memset("dve", self.neghalf, -0.5, [self.nh_dep])
        if self.stage <= 1:
            return
        self._ffn_alloc()
        C = self.cols_sb
        R = self.rows_sb
        sd = self.setup_dep = Dep("setup")
        smd = Dep("sm")
        mtd = Dep("maskT")
        self.dma(self.maskT, self.cmask[:, 1, :], [], [mtd])
        self.stt(self.gqk, C[:, 24:25], A_SCALE, C[:, 25:26], ALU.mult, ALU.mult, [cd], [sd])
        for cc in range(2):
            self.ts("dve", self.gqb[cc], C[:, 28 + cc:29 + cc], B_SCALE, None, ALU.mult, ALU.bypass, [cd], [sd])
        self.ts("dve", self.gsub, C[:, 30:31], 1.0 - LAM_INIT, None, ALU.mult, ALU.bypass, [cd], [sd])
        sm = self.sm
        self.tt("dve", sm[:, 0:64], R[:, 0:64], R[:, 64:128], ALU.mult, [rd], [smd])
        self.red(sm[:, 256:257], sm[:, 0:64], ALU.add, [smd], [smd])
        self.tt("dve", sm[:, 64:128], R[:, 128:192], R[:, 192:256], ALU.mult, [rd], [smd])
        self.red(sm[:, 257:258], sm[:, 64:128], ALU.add, [smd], [smd])
        self.act(sm[:, 258:260], sm[:, 256:258], AF.Exp, [smd], [smd])
        self.tt("dve", sm[:, 260:261], sm[:, 258:259], sm[:, 259:260], ALU.subtract, [smd], [smd])
        self.ts("dve", self.lamn, sm[:, 260:261], -1.0, -LAM_INIT, ALU.mult, ALU.add, [smd], [sd])
        self.tt("dve", sm[:, 0:64], R[:, 256:320], R[:, 320:384], ALU.mult, [rd, smd], [smd])
        self.red(sm[:, 261:262], sm[:, 0:64], ALU.max, [smd], [smd], absval=True)
        self.ts("dve", sm[:, 262:263], sm[:, 261:262], 64.0 * A_SCALE, None, ALU.mult, ALU.bypass, [smd], [smd])
        self.ts("dve", self.abias[:, 0:4], C[:, 31:35], sm[:, 262:263], None, ALU.subtract, ALU.bypass, [smd, cd], [sd])
        self.tt("dve", sm[:, 0:256], R[:, 384:640], R[:, 640:896], ALU.mult, [rd, smd], [smd])
        self.red(sm[:, 263:264], sm[:, 0:256], ALU.max, [smd], [smd], absval=True)
        self.ts("dve", sm[:, 264:265], sm[:, 263:264], 256.0 * B_SCALE, None, ALU.mult, ALU.bypass, [smd], [smd])
        self.ts("dve", self.abias[:, 4:8], C[:, 35:39], sm[:, 264:265], None, ALU.subtract, ALU.bypass, [smd, cd], [sd])
        st = self.stg[0].rearrange("p (h b c) -> p h b c", h=8, b=2)
        self.dma(st, self.bias, [], [self.stg_dep[0]])
        for hh in range(8):
            self.stt(self.biasbf[:, hh, 0, :], st[:, hh, 0, :], C[:, 31 + hh:32 + hh], self.maskT, ALU.subtract, ALU.add,
                     [self.stg_dep[0], cd, mtd], [sd])
            self.ts("dve", self.biasbf[:, hh, 1, :], st[:, hh, 1, :], C[:, 31 + hh:32 + hh], None, ALU.subtract, ALU.bypass,
                    [self.stg_dep[0], cd], [sd])
        s1 = self.stg[1].rearrange("p (h c) -> p h c", h=4)[:, :, 0:256]
        self.dma(s1, self.wuk, [], [self.stg_dep[1]])
        self.cp("pool", self.wukb, s1, [self.stg_dep[1]], [sd])
        s2 = self.stg[2][:, 0:1024].rearrange("p (a h c) -> p a h c", a=2, h=4)
        self.dma(s2, self.wuv, [], [self.stg_dep[2]])
        self.cp("pool", self.wuvb, s2, [self.stg_dep[2]], [sd])
        for k in range(NBIS + 1):
            self.memset("pool", self.pw[:, k:k + 1], 2.0 ** -(k + 1), [sd])
        self.memset("pool", self.thc, -1e29, [sd])

    def _sequence(self, s):
        self._load_h(s, from_x=True)
        self._norm_pass(0)
        self._ffn(0)
        if self.stage <= 1:
            self._store_out(s)
            return
        self._norm_pass(1)
        self.hs_dep = getattr(self, "hs_dep", None) or [Dep("hs%d" % t) for t in range(NT)]
        for t, (t0, n) in enumerate(TILES):
            self.dma(self.hs[t0:t0 + n, :], self.h[0:n, t, :], [self.h_dep[t]], [self.hs_dep[t]])
        self.P.barrier()
        self._attn_alloc()
        if not _os.environ.get("K_SKIP_PROJ"):
            self._proj()
        self.P.barrier()
        if self.stage >= 3:
            self._attnA()
            self.P.barrier()
        if self.stage >= 4:
            self._attnB()
            self.P.barrier()
        if self.dbg_stop:
            return
        if not _os.environ.get("K_NO_RELOAD"):
            self._load_h(s, from_x=False)
        if not _os.environ.get("K_NO_FFN2"):
            self._norm_pass(2)
            self._ffn(1)
        self._store_out(s)

    def _load_h(self, s, from_x):
        for t, (t0, n) in enumerate(TILES):
            hd = self.h_dep[t]
            if from_x:
                if t == 0:
                    self.dma(self.h[0:NMETA, 0, :], self.meta, [], [hd])
                    self.dma(self.h[NMETA:128, 0, :], self.x[s, 0:128 - NMETA, :], [], [hd])
                else:
                    self.dma(self.h[0:n, t, :], self.x[s, t0 - NMETA:t0 - NMETA + n, :], [], [hd])
            else:
                self.dma(self.h[0:n, t, :], self.hs[t0:t0 + n, :], [self.hs_dep[t]], [hd])

    def _store_out(self, s):
        for t, (t0, n) in enumerate(TILES):
            if t == 0:
                self.dma(self.out[s, 0:128 - NMETA, :], self.h[NMETA:128, 0, :], [self.h_dep[0]], [self.out_dep])
            else:
                self.dma(self.out[s, t0 - NMETA:t0 - NMETA + n, :], self.h[0:n, t, :], [self.h_dep[t]], [self.out_dep])

    def _norm_pass(self, which):
        for t, (t0, n) in enumerate(TILES):
            b = t % 2
            k, (ssd, td, rsd) = self.stat()
            ss = self.ss[0:n, k:k + 1]
            self.act(self.junk[0:n, :], self.h[0:n, t, :], AF.Square, [self.h_dep[t]], [self.junk_dep, ssd], accum=ss)
            self.ts("dve", self.tmp[0:n, k:k + 1], ss, 1.0 / D, EPS, ALU.mult, ALU.add, [ssd], [td])
            self.tt("pool", self.rs[0:n, k:k + 1], self.tmp[0:n, k:k + 1], self.neghalf[0:n, 0:1], ALU.pow,
                    [td, self.nh_dep], [rsd])
            self.ts("dve", self.xn_s[b][0:n, :], self.h[0:n, t, :], self.rs[0:n, k:k + 1], None, ALU.mult, ALU.bypass,
                    [self.h_dep[t], rsd], [self.xn_s_dep[b]])
            pb = self.psum[b].bitcast(BF16)
            for kk in range(8):
                self.tr(pb[:, kk * 128:kk * 128 + n], self.xn_s[b][0:n, kk * 128:(kk + 1) * 128],
                        self.ident[0:n, 0:n], [self.xn_s_dep[b], self.ident_dep], [self.pdep[b]])
            src = pb.rearrange("p (k c) -> p k c", k=8)[:, :, 0:n]
            self.P.add("act", (lambda e, o=self.xnT[:, :, t0:t0 + n], i=src: e.activation(o, i, AF.Copy)),
                       [self.pdep[b]], [self.xn_dep[t]])

    def _ffn_alloc(self):
        if hasattr(self, "ffn_alloced"):
            return
        self.ffn_alloced = True
        self.cur = self.phase_off
        sb = self.sb
        self.stg = [sb("stg%d" % i, [128, 2048], F32) for i in range(3)]
        self.stg_dep = [Dep("stg%d" % i) for i in range(3)]
        self.stg_i = 0
        self.wgb = [sb("wgb%d" % i, [128, 8, 256], BF16) for i in range(2)]
        self.wub = [sb("wub%d" % i, [128, 8, 256], BF16) for i in range(2)]
        self.wgb_dep = [Dep("wgb%d" % i) for i in range(2)]
        self.wub_dep = [Dep("wub%d" % i) for i in range(2)]
        self.wdb = [sb("wdb%d" % i, [128, 2, D], BF16) for i in range(4)]
        self.wdb_dep = [Dep("wdb%d" % i) for i in range(4)]
        self.actb = sb("actb", [128, 4, L], BF16)
        self.actb_dep = [[Dep("act%d_%d" % (f, g)) for g in range(len(TGS))] for f in range(4)]
        self.sg = [sb("sg%d" % i, [128, 512], BF16) for i in range(2)]
        self.sg_dep = [Dep("sg%d" % i) for i in range(2)]
        self.ffn_end = self.cur
        self.slab_ctr = 0
        self.gu_ctr = 0
        self.dn_ctr = 0

    def _stage_slot(self):
        i = self.stg_i
        self.stg_i = (i + 1) % 3
        return i

    def _ffn(self, which):
        self._ffn_alloc()
        wg, wu, wd = self.wg[which], self.wu[which], self.wd[which]
        gcol = {0: 0, 1: 16}[which]
        gain = self.cols_sb[:, gcol:gcol + 8]
        slabs = list(range(11))
        groups = [slabs[i:i + 2] for i in range(0, 11, 2)]
        for grp in groups:
            for li, sl in enumerate(grp):
                c0 = sl * 256
                sw = self.slab_ctr % 2
                dslot = self.slab_ctr % 4
                self.slab_ctr += 1
                for (src, dst, ddep) in ((wg, self.wgb[sw], self.wgb_dep[sw]), (wu, self.wub[sw], self.wub_dep[sw])):
                    si = self._stage_slot()
                    st3 = self.stg[si].rearrange("p (k c) -> p k c", k=8)
                    self.dma(st3, src[:, c0:c0 + 256].rearrange("(k p) c -> p k c", p=128), [], [self.stg_dep[si]])
                    self.tt("pool", dst, st3, gain.unsqueeze(2).to_broadcast([128, 8, 256]), ALU.mult,
                            [self.stg_dep[si], self.cdep], [ddep])
                si = self._stage_slot()
                st3 = self.stg[si].rearrange("p (k c) -> p k c", k=2)
                self.dma(st3, wd[c0:c0 + 256, :].rearrange("(k p) c -> p k c", p=128), [], [self.stg_dep[si]])
                self.cp("pool", self.wdb[dslot], st3, [self.stg_dep[si]], [self.wdb_dep[dslot]])
                grp_dslot = dslot
                for c in range(2):
                    fl = li * 2 + c
                    for g, (g0, gn) in enumerate(TGS):
                        pbuf = self.gu_ctr % 2
                        self.gu_ctr += 1
                        pg, pu = 2 + 2 * pbuf, 3 + 2 * pbuf
                        xdeps = [self.xn_dep[t] for t in range(NT) if TILES[t][0] >= g0 and TILES[t][0] < g0 + gn]
                        for k in range(8):
                            self.mm(self.psum[pg][:, 0:gn], self.wgb[sw][:, k, c * 128:(c + 1) * 128],
                                    self.xnT[:, k, g0:g0 + gn], k == 0, k == 7,
                                    [self.wgb_dep[sw]] + xdeps, [self.pdep[pg]])
                        for k in range(8):
                            self.mm(self.psum[pu][:, 0:gn], self.wub[sw][:, k, c * 128:(c + 1) * 128],
                                    self.xnT[:, k, g0:g0 + gn], k == 0, k == 7,
                                    [self.wub_dep[sw]] + xdeps, [self.pdep[pu]])
                        sgi = pbuf
                        self.act(self.sg[sgi][:, 0:gn], self.psum[pg][:, 0:gn], AF.Silu, [self.pdep[pg]], [self.sg_dep[sgi]])
                        self.tt("dve", self.actb[:, fl, g0:g0 + gn], self.sg[sgi][:, 0:gn], self.psum[pu][:, 0:gn], ALU.mult,
                                [self.sg_dep[sgi], self.pdep[pu]], [self.actb_dep[fl][g]])
            nfl = len(grp) * 2
            first_dslot = (self.slab_ctr - len(grp)) % 4
            for t, (t0, n) in enumerate(TILES):
                g = min(t // 4, 4)
                for half in range(2):
                    pd = 6 + (self.dn_ctr % 2)
                    self.dn_ctr += 1
                    for fl in range(nfl):
                        dslot = (first_dslot + fl // 2) % 4
                        self.mm(self.psum[pd][0:n, :], self.actb[:, fl, t0:t0 + n],
                                self.wdb[dslot][:, fl % 2, half * 512:(half + 1) * 512], fl == 0, fl == nfl - 1,
                                [self.actb_dep[fl][g], self.wdb_dep[dslot]], [self.pdep[pd]])
                    hv = self.h[0:n, t, half * 512:(half + 1) * 512]
                    self.stt(hv, self.psum[pd][0:n, :], 0.5, hv, ALU.mult, ALU.add,
                             [self.pdep[pd], self.h_dep[t]], [self.h_dep[t]])


    def _attn_alloc(self):
        if hasattr(self, "attn_alloced"):
            return
        self.attn_alloced = True
        sb = self.sb
        save = self.cur
        self.cur = self.h_off
        r1 = self.cur
        self.qT = sb("qT", [128, 4, L], BF16)
        self.kT = sb("kT", [128, 4, L], BF16)
        self.VA = sb("VA", [128, NT, 4, 129], BF16)
        r1_end = self.cur
        self.cur = r1
        self.qlT = sb("qlT", [128, 4, 2, 512], BF16)
        self.rr = [sb("rr%d" % i, [128, 512], F32) for i in range(2)]
        self.qn1k = sb("qn1k", [128, 1024], BF16)
        self.ocT = sb("ocT", [128, 8, 128], BF16)
        self.woutb = sb("woutb", [128, 8, D], BF16)
        self.hst2 = sb("hst2", [128, 2, D], F32)
        self.hst = [self.hst2[:, 0, :], self.hst2[:, 1, :]]
        self.obG = sb("obG", [128, 4, 512], BF16)
        self.cntj = sb("cntj", [128, L], BF16)
        assert self.cur <= r1_end, (self.cur, r1_end)
        self.cur = r1_end
        self.qbT = sb("qbT", [128, 4, L], BF16)
        self.cT = sb("cT", [128, 2, L], BF16)
        self.VB = sb("VB", [128, NT, 4, 129], BF16)
        self.iqT = sb("iqT", [128, 4, L], BF16)
        self.ikT = sb("ikT", [128, L], BF16)
        r3 = self.cur
        self.wst = [sb("wst%d" % i, [128, 8, 384], F32) for i in range(2)]
        self.wb = [sb("wb%d" % i, [128, 8, 384], BF16) for i in range(2)]
        r3_end = self.cur
        self.cur = r3
        self.oa = sb("oa", [128, NT, 512], BF16)
        self.PT = [sb("PT%d" % i, [128, 512], BF16) for i in range(4)]
        self.t1 = [sb("t1_%d" % i, [128, 128], F32) for i in range(2)]
        self.ov = [sb("ov_%d" % i, [128, 128], F32) for i in range(2)]
        self.bst = sb("bst", [128, 4 * NBIS + 8], F32)
        self.mTa = sb("mTa", [128, 12, 512], BF16)
        assert self.cur <= r3_end, (self.cur, r3_end)
        self.cur = max(r3_end, save)
        self.sq = [sb("sq%d" % i, [128, 256], F32) for i in range(2)]
        self.qn = [sb("qn%d" % i, [128, 256], BF16) for i in range(2)]
        save2 = self.cur
        self.cur = self.c_const
        self.acc = sb("acc", [128, L], F32)
        self.maskb = sb("maskb", [128, L], BF16)
        self.mT = sb("mT", [128, NT, 512], BF16)
        assert self.cur <= self.h_off, (self.cur, self.h_off)
        self.cur = save2
        D_ = Dep
        self.qT_dep = [[D_("qT%d_%d" % (h, t)) for t in range(NT)] for h in range(4)]
        self.kT_dep = [[D_("kT%d_%d" % (h, t)) for t in range(NT)] for h in range(4)]
        self.VA_dep = [D_("VA%d" % t) for t in range(NT)]
        self.VB_dep = [D_("VB%d" % t) for t in range(NT)]
        self.cT_dep = [D_("cT%d" % t) for t in range(NT)]
        self.qbT_dep = [D_("qbT%d" % g) for g in range(5)]
        self.iqT_dep = [D_("iqT%d" % g) for g in range(5)]
        self.ikT_dep = [D_("ikT%d" % g) for g in range(5)]
        self.iw_dep = [D_("iw%d" % t) for t in range(NT)]
        self.wst_dep = [D_("wst%d" % i) for i in range(2)]
        self.wb_dep = [D_("wb%d" % i) for i in range(2)]
        self.sq_dep = [D_("sq%d" % i) for i in range(2)]
        self.qn_dep = [D_("qn%d" % i) for i in range(2)]
        self.oa_dep = [D_("oa%d" % t) for t in range(NT)]
        self.PT_dep = [D_("PT%d" % i) for i in range(4)]
        self.t1_dep = [D_("t1_%d" % i) for i in range(2)]
        self.ov_dep = [D_("ov_%d" % i) for i in range(2)]
        self.qlT_dep = [D_("qlT%d" % i) for i in range(4)]
        self.rr_dep = [D_("rr%d" % i) for i in range(2)]
        self.qn1k_dep = D_("qn1k")
        self.ocT_dep = D_("ocT")
        self.woutb_dep = D_("woutb")
        self.hst_dep = [D_("hst%d" % i) for i in range(2)]
        self.obG_dep = [D_("obG%d" % i) for i in range(4)]
        self.cntj_dep = D_("cntj")
        self.acc_dep = D_("acc")
        self.maskb_dep = D_("maskb")
        self.mT_dep = [D_("mT%d" % i) for i in range(4)]
        self.mTa_dep = [D_("mTa%d" % i) for i in range(4)]
        self.bst_dep = D_("bst")
        self.ctr = 0

    def _load_slab(self, c0, ncol, gain):
        slot = self.ctr % 2
        self.ctr += 1
        st = self.wst[slot][:, :, 0:ncol]
        self.dma(st, self.win[:, c0:c0 + ncol].rearrange("(k p) c -> p k c", p=128), [], [self.wst_dep[slot]])
        self.tt("pool", self.wb[slot][:, :, 0:ncol], st, gain.unsqueeze(2).to_broadcast([128, 8, ncol]), ALU.mult,
                [self.wst_dep[slot], self.cdep], [self.wb_dep[slot]])
        return slot

    def _proj(self):
        gain = self.cols_sb[:, 8:16]
        C = self.cols_sb
        parts = _os.environ.get("K_PROJ_PARTS", "ms,A,CK,FM").split(",")
        if "ms" in parts:
            self.memset("pool", self.VA[:, :, :, 128:129], 1.0, self.VA_dep)
            self.memset("pool", self.VB[:, :, :, 128:129], 1.0, self.VB_dep)
        tctr = 0
        for sl in range(5):
            if (sl < 4 and "A" not in parts) or (sl == 4 and "CK" not in parts):
                continue
            ncol = 384 if sl < 4 else 264
            slot = self._load_slab(sl * 384, ncol, gain)

            def mmA(t, slot=slot, ncol=ncol):
                t0, n = TILES[t]
                pb = 2 + (t % 4)
                for k in range(8):
                    self.mm(self.psum[pb][0:n, 0:ncol], self.xnT[:, k, t0:t0 + n], self.wb[slot][:, k, 0:ncol], k == 0, k == 7,
                            [self.xn_dep[t], self.wb_dep[slot]], [self.pdep[pb]])

            for t in range(3):
                mmA(t)
            for t, (t0, n) in enumerate(TILES):
                if t + 3 < NT:
                    mmA(t + 3)
                pb = 2 + (t % 4)
                tb = t % 2
                b = t % 2
                ps = self.psum[pb]
                pT = self.psum[tb].bitcast(BF16)
                k4, sdeps = self.stat()
                ssd, td, rsd = sdeps
                if sl < 4:
                    h = sl
                    KA = _os.environ.get("K_A", "sq,red,rstd,qn,tr,evq,evk,va").split(",")
                    if "sq" in KA:
                        self.act(self.sq[b][0:n, :], ps[0:n, 0:256], AF.Square, [self.pdep[pb]], [self.sq_dep[b]])
                    if "red" in KA:
                        self.red(self.ss[0:n, k4:k4 + 4], self.sq[b][0:n, :].rearrange("p (g d) -> p g d", d=64), ALU.add,
                                 [self.sq_dep[b]], [ssd])
                    if "rstd" in KA:
                        self.rstd(n, k4, 4, 1.0 / 64, sdeps)
                    if "qn" in KA:
                        self.tt("dve", self.qn[b][0:n, :].rearrange("p (g d) -> p g d", d=64),
                                ps[0:n, 0:256].rearrange("p (g d) -> p g d", d=64),
                                self.rs[0:n, k4:k4 + 4].unsqueeze(2).to_broadcast([n, 4, 64]), ALU.mult,
                                [self.pdep[pb], rsd], [self.qn_dep[b]])
                    if "tr" in KA:
                        self.tr(pT[:, 0:n], self.qn[b][0:n, 0:128], self.ident[0:n, 0:n], [self.qn_dep[b], self.ident_dep],
                                [self.pdep[tb]])
                        self.tr(pT[:, 128:128 + n], self.qn[b][0:n, 128:256], self.ident[0:n, 0:n], [self.qn_dep[b], self.ident_dep],
                                [self.pdep[tb]])
                    if "evq" in KA:
                        qdst = self.junk[:, 0:n] if _os.environ.get("K_DEST") else self.qT[:, h, t0:t0 + n]
                        self.act(qdst, pT[:, 0:n], AF.Copy, [self.pdep[tb], self.setup_dep],
                                 [self.qT_dep[h][t]], scale=self.gqk)
                    if "evk" in KA:
                        kdst = self.junk[:, 128:128 + n] if _os.environ.get("K_DEST") else self.kT[:, h, t0:t0 + n]
                        self.act(kdst, pT[:, 128:128 + n], AF.Copy, [self.pdep[tb]], [self.kT_dep[h][t]])
                    if "va" in KA:
                        self.act(self.VA[0:n, t, h, 0:128], ps[0:n, 256:384], AF.Copy, [self.pdep[pb]], [self.VA_dep[t]])
                else:
                    self.act(self.sq[b][0:n, :], ps[0:n, 0:256], AF.Square, [self.pdep[pb]], [self.sq_dep[b], ssd],
                             accum=self.ss[0:n, k4:k4 + 1])
                    self.rstd(n, k4, 1, 1.0 / 256, sdeps)
                    self.ts("dve", self.qn[b][0:n, :], ps[0:n, 0:256], self.rs[0:n, k4:k4 + 1], None, ALU.mult, ALU.bypass,
                            [self.pdep[pb], rsd], [self.qn_dep[b]])
                    self.ts("dve", self.iw_sb[0:n, t, :], ps[0:n, 256:264], (64 ** -0.5) * (8 ** -0.5), None, ALU.mult, ALU.bypass,
                            [self.pdep[pb]], [self.iw_dep[t]])
                    for cc in range(2):
                        self.tr(pT[:, cc * 128:cc * 128 + n], self.qn[b][0:n, cc * 128:(cc + 1) * 128], self.ident[0:n, 0:n],
                                [self.qn_dep[b], self.ident_dep], [self.pdep[tb]])
                    self.ts("dve", self.cT[:, 0, t0:t0 + n], pT[:, 0:n], C[:, 26:27], None, ALU.mult, ALU.bypass,
                            [self.pdep[tb], self.cdep], [self.cT_dep[t]])
                    self.act(self.cT[:, 1, t0:t0 + n], pT[:, 128:128 + n], AF.Copy, [self.pdep[tb], self.cdep], [self.cT_dep[t]],
                             scale=C[:, 27:28])
                    vb = 6 + (t % 2)
                    for cc in range(2):
                        self.mm(self.psum[vb][0:n, :], self.cT[:, cc, t0:t0 + n],
                                self.wuvb[:, cc, :, :].rearrange("p h e -> p (h e)"), cc == 0, cc == 1,
                                [self.cT_dep[t], self.setup_dep], [self.pdep[vb]])
                    self.act(self.VB[0:n, t, :, 0:128], self.psum[vb][0:n, :].rearrange("p (h e) -> p h e", h=4), AF.Copy,
                             [self.pdep[vb]], [self.VB_dep[t]])
        ectr = 0
        for sl in range(3):
            if "FM" not in parts:
                continue
            slot = self._load_slab(1800 + sl * 384, 384, gain)
            for c in range(3):
                ch = sl * 3 + c
                for g, (g0, gn) in enumerate(TGS):
                    if ch < 4:
                        dst, ddep = self.qbT[:, ch, g0:g0 + gn], self.qbT_dep[g]
                    elif ch < 8:
                        dst, ddep = self.iqT[:, ch - 4, g0:g0 + gn], self.iqT_dep[g]
                    else:
                        dst, ddep = self.ikT[:, g0:g0 + gn], self.ikT_dep[g]
                    pb = 4 + (ectr % 2)
                    xdeps = [self.xn_dep[t] for t in range(NT) if g0 <= TILES[t][0] < g0 + gn]
                    for k in range(8):
                        self.mm(self.psum[pb][:, 0:gn], self.wb[slot][:, k, c * 128:(c + 1) * 128], self.xnT[:, k, g0:g0 + gn],
                                k == 0, k == 7, [self.wb_dep[slot]] + xdeps, [self.pdep[pb]])
                    if ectr % 2 == 0:
                        self.act(dst, self.psum[pb][:, 0:gn], AF.Copy, [self.pdep[pb]], [ddep])
                    else:
                        self.cp("dve", dst, self.psum[pb][:, 0:gn], [self.pdep[pb]], [ddep])
                    ectr += 1

    def _bias_blocks(self, ps, pbank, hh, j, tiles, c0):
        k0, kn = TILES[j]
        for blk, i in ((0, j), (1, j + 1)):
            if i in tiles:
                off = TILES[i][0] - c0
                ni = TILES[i][1]
                self.mm(ps[:, off:off + ni], self.ident[0:kn, 0:kn], self.biasbf[0:kn, hh, blk, 0:ni], False, True,
                        [self.ident_dep, self.setup_dep], [self.pdep[pbank]])

    def _pipe(self, n, stage1, stage2, depth=2):
        for k in range(min(depth, n)):
            stage1(k)
        for k in range(n):
            if k + depth < n:
                stage1(k + depth)
            stage2(k)

    def _attnA(self):
        for G, tiles in enumerate(QGS):
            q0 = TILES[tiles[0]][0]
            qn_ = sum(TILES[t][1] for t in tiles)
            for h in range(4):
                started = set()
                nblk = tiles[-1] + 1

                def stage1(j, h=h, tiles=tiles, q0=q0, qn_=qn_):
                    k0, kn = TILES[j]
                    c0 = max(q0, k0)
                    ncol = q0 + qn_ - c0
                    qdeps = [self.qT_dep[h][t] for t in tiles if TILES[t][0] + TILES[t][1] > c0]
                    for m in range(2):
                        bank = (j % 2) * 2 + m
                        ps = self.psum[bank][0:kn, 0:ncol]
                        self.mm(ps, self.kT[m * 64:(m + 1) * 64, h, k0:k0 + kn], self.qT[m * 64:(m + 1) * 64, h, c0:c0 + ncol],
                                True, True, [self.kT_dep[h][j]] + qdeps, [self.pdep[bank]])
                    for m in range(2):
                        bank = (j % 2) * 2 + m
                        ps = self.psum[bank][0:kn, 0:ncol]
                        self._bias_blocks(ps, bank, h, j, tiles, c0)
                        self.act(self.PT[bank][0:kn, 0:ncol], ps, AF.Exp, [self.pdep[bank], self.setup_dep], [self.PT_dep[bank]],
                                 bias=self.abias[0:kn, h:h + 1])

                def stage2(j, h=h, tiles=tiles, q0=q0, started=started):
                    k0, kn = TILES[j]
                    c0 = max(q0, k0)
                    for m in range(2):
                        pbuf = (j % 2) * 2 + m
                        for il, i in enumerate(tiles):
                            if i < j:
                                continue
                            off = TILES[i][0] - c0
                            ni = TILES[i][1]
                            slot = m * 4 + il
                            ab = 4 + slot // 3
                            o = (slot % 3) * 129
                            first = ab not in started
                            started.add(ab)
                            self.mm(self.psum[ab][0:ni, o:o + 129], self.PT[pbuf][0:kn, off:off + ni], self.VA[0:kn, j, h, :],
                                    first, j == i, [self.PT_dep[pbuf], self.VA_dep[j]], [self.pdep[ab]])

                self._pipe(nblk, stage1, stage2, depth=1)
                for il, i in enumerate(tiles):
                    ni = TILES[i][1]
                    b = il % 2
                    s0, s1 = il, 4 + il
                    b0, o0 = 4 + s0 // 3, (s0 % 3) * 129
                    b1, o1 = 4 + s1 // 3, (s1 % 3) * 129
                    k4, sdeps = self.stat()
                    ssd, td, rsd = sdeps
                    self.recip(self.rs[0:ni, k4 + 1:k4 + 2], self.psum[b0][0:ni, o0 + 128:o0 + 129], [self.pdep[b0]], [rsd])
                    self.recip(self.rs[0:ni, k4 + 2:k4 + 3], self.psum[b1][0:ni, o1 + 128:o1 + 129], [self.pdep[b1]], [rsd])
                    self.ts("dve", self.rs[0:ni, k4 + 3:k4 + 4], self.rs[0:ni, k4 + 2:k4 + 3], self.lamn[0:ni, 0:1], None,
                            ALU.mult, ALU.bypass, [rsd, self.setup_dep], [rsd])
                    self.ts("dve", self.t1[b][0:ni, :], self.psum[b1][0:ni, o1:o1 + 128], self.rs[0:ni, k4 + 3:k4 + 4], None,
                            ALU.mult, ALU.bypass, [self.pdep[b1], rsd], [self.t1_dep[b]])
                    self.stt(self.ov[b][0:ni, :], self.psum[b0][0:ni, o0:o0 + 128], self.rs[0:ni, k4 + 1:k4 + 2],
                             self.t1[b][0:ni, :], ALU.mult, ALU.add, [self.pdep[b0], rsd, self.t1_dep[b]], [self.ov_dep[b]])
                    self.act(self.junk[0:ni, 0:128], self.ov[b][0:ni, :], AF.Square, [self.ov_dep[b]], [self.junk_dep, ssd],
                             accum=self.ss[0:ni, k4:k4 + 1])
                    self.rstd(ni, k4, 1, 1.0 / 128, sdeps)
                    self.ts("dve", self.oa[0:ni, i, h * 128:(h + 1) * 128], self.ov[b][0:ni, :], self.rs[0:ni, k4:k4 + 1], None,
                            ALU.mult, ALU.bypass, [self.ov_dep[b], rsd], [self.oa_dep[i]])

    def _attnB(self):
        C = self.cols_sb
        for q in range(4):
            st = self.hst2
            hd = self.hst_dep
            self.dma(st, self.wout[q * 256:(q + 1) * 256, :].rearrange("(k p) c -> p k c", p=128), [hd[1]], [hd[0]])
            if q < 2:
                self.ts("pool", self.woutb[:, 2 * q:2 * q + 2, :], st, self.gsub[:, 0:1], None, ALU.mult, ALU.bypass,
                        [hd[0], hd[1], self.setup_dep], [self.woutb_dep])
            else:
                self.cp("pool", self.woutb[:, 2 * q:2 * q + 2, :], st, [hd[0], hd[1]], [self.woutb_dep])
        self.dctr = 0
        self.hctr = 0
        ng = len(QGS)
        for il in range(len(QGS[0])):
            self._b_idx1(0, il)
            self._b_idx2(0, il)
        for G in range(ng):
            self._b_qlat(G)
            nxt = G + 1 if G + 1 < ng - 1 else None
            for h in range(4):
                if nxt is not None and h < len(QGS[nxt]):
                    self._b_idx1(nxt, h)
                self._b_attn_mm(G, h)
                if nxt is not None and h < len(QGS[nxt]):
                    self._b_idx2(nxt, h)
                self._b_attn_norm(G, h)
            if G + 1 == ng - 1:
                for il in range(len(QGS[G + 1])):
                    self._b_idx1(G + 1, il)
                    self._b_idx2(G + 1, il)
            self._b_wout(G)

    def _b_ctx(self, G):
        tiles = QGS[G]
        q0 = TILES[tiles[0]][0]
        qn_ = sum(TILES[t][1] for t in tiles)
        use_a = G in (0, 2)
        mT = self.mTa if use_a else self.mT
        mT_dep = self.mTa_dep if use_a else self.mT_dep
        return tiles, q0, qn_, mT, mT_dep

    def _b_qlat(self, G):
        tiles, q0, qn_, mT, mT_dep = self._b_ctx(G)
        for il, i in enumerate(tiles):
            t0, ni = TILES[i]
            for h in range(4):
                pbk = h // 2
                self.mm(self.psum[pbk][0:ni, (h % 2) * 256:(h % 2) * 256 + 256], self.qbT[:, h, t0:t0 + ni], self.wukb[:, h, :],
                        True, True, [self.qbT_dep[G], self.setup_dep], [self.pdep[pbk]])
            k4, sdeps = self.stat()
            ssd, td, rsd = sdeps
            for pbk in range(2):
                self.act(self.junk[0:ni, pbk * 512:(pbk + 1) * 512], self.psum[pbk][0:ni, :], AF.Square, [self.pdep[pbk]],
                         [self.junk_dep])
            self.red(self.ss[0:ni, k4:k4 + 4], self.junk[0:ni, :].rearrange("p (g d) -> p g d", d=256), ALU.add,
                     [self.junk_dep], [ssd])
            self.rstd(ni, k4, 4, 1.0 / 256, sdeps)
            for pbk in range(2):
                self.tt("dve", self.qn1k[0:ni, pbk * 512:(pbk + 1) * 512].rearrange("p (g d) -> p g d", d=256),
                        self.psum[pbk][0:ni, :].rearrange("p (g d) -> p g d", d=256),
                        self.rs[0:ni, k4 + 2 * pbk:k4 + 2 * pbk + 2].unsqueeze(2).to_broadcast([ni, 2, 256]), ALU.mult,
                        [self.pdep[pbk], rsd], [self.qn1k_dep])
            pT = self.psum[2].bitcast(BF16)
            for ch in range(8):
                self.tr(pT[:, ch * 128:ch * 128 + ni], self.qn1k[0:ni, ch * 128:(ch + 1) * 128], self.ident[0:ni, 0:ni],
                        [self.qn1k_dep, self.ident_dep], [self.pdep[2]])
            pv = pT.rearrange("p (h c t) -> p h c t", h=4, c=2)
            self.ts("dve", self.qlT[:, :, 0, il * 128:il * 128 + ni], pv[:, :, 0, 0:ni], self.gqb[0], None, ALU.mult,
                    ALU.bypass, [self.pdep[2], self.setup_dep], [self.qlT_dep[il]])
            self.ts("dve", self.qlT[:, :, 1, il * 128:il * 128 + ni], pv[:, :, 1, 0:ni], self.gqb[1], None, ALU.mult,
                    ALU.bypass, [self.pdep[2], self.setup_dep], [self.qlT_dep[il]])

    def _b_idx1(self, G, il):
        tiles, q0, qn_, mT, mT_dep = self._b_ctx(G)
        i = tiles[il]
        dctr = self.dctr
        t0, ni = TILES[i]
        Lk = t0 + ni
        acc = self.acc
        for kc0 in range(0, Lk, 512):
            kcn = min(512, Lk - kc0)
            g = kc0 // 512
            for hh in range(8):
                pb = 3 + (dctr % 2)
                rb = dctr % 2
                dctr += 1
                pr = (hh % 2) * 64
                self.mm(self.psum[pb][0:ni, 0:kcn], self.iqT[pr:pr + 64, hh // 2, t0:t0 + ni], self.ikT[pr:pr + 64, kc0:kc0 + kcn],
                        True, True, [self.iqT_dep[G], self.ikT_dep[g]], [self.pdep[pb]])
                self.act(self.rr[rb][0:ni, 0:kcn], self.psum[pb][0:ni, 0:kcn], AF.Relu, [self.pdep[pb]], [self.rr_dep[rb]])
                if hh == 0:
                    self.ts("dve", acc[0:ni, kc0:kc0 + kcn], self.rr[rb][0:ni, 0:kcn], self.iw_sb[0:ni, i, 0:1], None,
                            ALU.mult, ALU.bypass, [self.rr_dep[rb], self.iw_dep[i]], [self.acc_dep])
                else:
                    self.stt(acc[0:ni, kc0:kc0 + kcn], self.rr[rb][0:ni, 0:kcn], self.iw_sb[0:ni, i, hh:hh + 1],
                             acc[0:ni, kc0:kc0 + kcn], ALU.mult, ALU.add, [self.rr_dep[rb], self.iw_dep[i], self.acc_dep],
                             [self.acc_dep])
        self.dctr = dctr

    def _b_idx2(self, G, il):
        tiles, q0, qn_, mT, mT_dep = self._b_ctx(G)
        i = tiles[il]
        t0, ni = TILES[i]
        Lk = t0 + ni
        acc = self.acc
        bs = self.bst
        bd = self.bst_dep
        if i >= 2:
            self.red(bs[0:ni, 0:1], acc[0:ni, 0:Lk], ALU.max, [self.acc_dep], [bd])
            self.red(bs[0:ni, 1:2], acc[0:ni, 0:Lk], ALU.min, [self.acc_dep], [bd])
        self.tt("dve", acc[0:ni, t0:t0 + ni], acc[0:ni, t0:t0 + ni], self.masktok[0:ni, 0:ni], ALU.add,
                [self.acc_dep, self.masktok_dep], [self.acc_dep])
        if i >= 2:
            self.tt("dve", bs[0:ni, 2:3], bs[0:ni, 0:1], bs[0:ni, 1:2], ALU.subtract, [bd], [bd])
            W0 = 8
            self.ts("dve", bs[0:ni, W0:W0 + NBIS + 1], self.pw[0:ni, :], bs[0:ni, 2:3], None, ALU.mult, ALU.bypass,
                    [bd, self.setup_dep], [bd])
            M0 = W0 + NBIS + 1
            C0 = M0 + NBIS + 2
            self.ts("dve", bs[0:ni, M0:M0 + 1], bs[0:ni, 1:2], -1.0, bs[0:ni, W0:W0 + 1], ALU.mult, ALU.subtract, [bd], [bd])
            for k in range(NBIS):
                self.act(self.cntj[0:ni, 0:Lk], acc[0:ni, 0:Lk], AF.Sign, [self.acc_dep, bd], [self.cntj_dep, bd],
                         bias=bs[0:ni, M0 + k:M0 + k + 1], accum=bs[0:ni, C0 + k:C0 + k + 1])
                self.ts("dve", bs[0:ni, 3:4], bs[0:ni, C0 + k:C0 + k + 1], float(2 * TOPK - 1 - Lk), bs[0:ni, W0 + k:W0 + k + 1],
                        ALU.is_ge, ALU.mult, [bd], [bd])
                wn = W0 + k + 1 if k < NBIS - 1 else W0 + k
                self.stt(bs[0:ni, M0 + k + 1:M0 + k + 2], bs[0:ni, wn:wn + 1], bs[0:ni, 3:4], bs[0:ni, M0 + k:M0 + k + 1],
                         ALU.subtract, ALU.add, [bd], [bd])
            self.ts("dve", bs[0:ni, M0 + NBIS + 1:M0 + NBIS + 2], bs[0:ni, M0 + NBIS:M0 + NBIS + 1], -1.0, None, ALU.mult,
                    ALU.bypass, [bd], [bd])
            theta = bs[0:ni, M0 + NBIS + 1:M0 + NBIS + 2]
            thd = [bd]
        else:
            theta = self.thc[0:ni, 0:1]
            thd = [self.setup_dep]
        self.ts("dve", self.maskb[0:ni, 0:Lk], acc[0:ni, 0:Lk], theta, NEG, ALU.is_lt, ALU.mult, [self.acc_dep] + thd,
                [self.maskb_dep])
        for j0 in range(0, i + 1, 8):
            js = list(range(j0, min(j0 + 8, i + 1)))
            tb = 5
            pT = self.psum[tb].bitcast(BF16)
            for jj, j in enumerate(js):
                k0, kn = TILES[j]
                self.tr(pT[0:kn, jj * 128:jj * 128 + ni], self.maskb[0:ni, k0:k0 + kn], self.ident[0:ni, 0:ni],
                        [self.maskb_dep, self.ident_dep], [self.pdep[tb]])
            src = pT.rearrange("p (j t) -> p j t", j=8)[:, 0:len(js), 0:ni]
            self.cp("dve", mT[:, j0:j0 + len(js), il * 128:il * 128 + ni], src, [self.pdep[tb]], [mT_dep[il]])

    def _b_attn_mm(self, G, h):
        tiles, q0, qn_, mT, mT_dep = self._b_ctx(G)
        started = set()
        nblk = tiles[-1] + 1

        def stage1(j, h=h, tiles=tiles, q0=q0, qn_=qn_, mT=mT, mT_dep=mT_dep):
            k0, kn = TILES[j]
            c0 = max(q0, k0)
            ncol = q0 + qn_ - c0
            co = c0 - q0
            bank = j % 3
            ps = self.psum[bank][0:kn, 0:ncol]
            qd = [self.qlT_dep[il] for il, t in enumerate(tiles) if TILES[t][0] + TILES[t][1] > c0]
            md = [mT_dep[il] for il, t in enumerate(tiles) if TILES[t][0] + TILES[t][1] > c0]
            for cc in range(2):
                self.mm(ps, self.cT[:, cc, k0:k0 + kn], self.qlT[:, h, cc, co:co + ncol], cc == 0, False,
                        [self.cT_dep[j]] + qd, [self.pdep[bank]])
            self.mm(ps, self.ident[0:kn, 0:kn], mT[0:kn, j, co:co + ncol], False, True, [self.ident_dep] + md,
                    [self.pdep[bank]])
            self._bias_blocks(ps, bank, 4 + h, j, tiles, c0)
            self.act(self.PT[j % 4][0:kn, 0:ncol], ps, AF.Exp, [self.pdep[bank], self.setup_dep], [self.PT_dep[j % 4]],
                     bias=self.abias[0:kn, 4 + h:5 + h])

        def stage2(j, h=h, tiles=tiles, q0=q0, started=started):
            k0, kn = TILES[j]
            c0 = max(q0, k0)
            pbuf = j % 4
            for il, i in enumerate(tiles):
                if i < j:
                    continue
                off = TILES[i][0] - c0
                ni = TILES[i][1]
                ab = 6 + il // 3
                o = (il % 3) * 129
                first = ab not in started
                started.add(ab)
                self.mm(self.psum[ab][0:ni, o:o + 129], self.PT[pbuf][0:kn, off:off + ni], self.VB[0:kn, j, h, :],
                        first, j == i, [self.PT_dep[pbuf], self.VB_dep[j]], [self.pdep[ab]])

        self._pipe(nblk, stage1, stage2)

    def _b_attn_norm(self, G, h):
        tiles, q0, qn_, mT, mT_dep = self._b_ctx(G)
        for il, i in enumerate(tiles):
            ni = TILES[i][1]
            ab = 6 + il // 3
            o = (il % 3) * 129
            k4, sdeps = self.stat()
            ssd, td, rsd = sdeps
            self.recip(self.rs[0:ni, k4:k4 + 1], self.psum[ab][0:ni, o + 128:o + 129], [self.pdep[ab]], [rsd])
            self.ts("dve", self.obG[0:ni, il, h * 128:(h + 1) * 128], self.psum[ab][0:ni, o:o + 128], self.rs[0:ni, k4:k4 + 1],
                    None, ALU.mult, ALU.bypass, [self.pdep[ab], rsd], [self.obG_dep[il]])

    def _b_wout(self, G):
        tiles, q0, qn_, mT, mT_dep = self._b_ctx(G)
        hctr = self.hctr
        for il, i in enumerate(tiles):
            t0, ni = TILES[i]
            pT = self.psum[2].bitcast(BF16)
            for k in range(8):
                src = self.oa[0:ni, i, k * 128:(k + 1) * 128] if k < 4 else self.obG[0:ni, il, (k - 4) * 128:(k - 3) * 128]
                sd_ = self.oa_dep[i] if k < 4 else self.obG_dep[il]
                self.tr(pT[:, k * 128:k * 128 + ni], src, self.ident[0:ni, 0:ni], [sd_, self.ident_dep], [self.pdep[2]])
            self.act(self.ocT[:, :, 0:ni], pT.rearrange("p (k t) -> p k t", k=8)[:, :, 0:ni], AF.Copy, [self.pdep[2]],
                     [self.ocT_dep])
            hb = hctr % 2
            hctr += 1
            self.dma(self.hst[hb][0:ni, :], self.hs[t0:t0 + ni, :], [self.hs_dep[i]], [self.hst_dep[hb]])
            for half in range(2):
                pb = 3 + half
                for k in range(8):
                    self.mm(self.psum[pb][0:ni, :], self.ocT[:, k, 0:ni], self.woutb[:, k, half * 512:(half + 1) * 512],
                            k == 0, k == 7, [self.ocT_dep, self.woutb_dep], [self.pdep[pb]])
                hv = self.hst[hb][0:ni, half * 512:(half + 1) * 512]
                self.tt("dve", hv, self.psum[pb][0:ni, :], hv, ALU.add, [self.pdep[pb], self.hst_dep[hb]], [self.hst_dep[hb]])
            self.dma(self.hs[t0:t0 + ni, :], self.hst[hb][0:ni, :], [self.hst_dep[hb]], [self.hs_dep[i]])
        self.hctr = hctr

    def _finish(self):
        if _os.environ.get("K_DUMP"):
            dd = Dep("dump")
            self.P.barrier()
            self.dma(self.hs[0:128, 0:16], self.smallc[:, 0:16], [self.setup_dep], [dd])
            self.dma(self.hs[0:128, 16:24], self.abias, [self.setup_dep], [dd])
            self.P.add("sp", None, [dd], [])
        self.P.add("sp", None, [self.out_dep], [])
        if self.dbg is not None:
            pass


def _bucket(n):
    n = np.maximum(n, 0)
    nf = np.maximum(n, 1).astype(np.float32)
    large = 16 + (np.log(nf / np.float32(16)) / np.float32(math.log(128 / 16)) * np.float32(16)).astype(np.int32)
    large = np.minimum(large, 31)
    return np.where(n < 16, n, large)


def _prep_shared(inp):
    f = lambda a: np.ascontiguousarray(np.asarray(a, dtype=np.float32))
    sh = {}
    sh["meta"] = f(inp["meta_tokens"])
    for i, nm in ((1, "ffn1"), (2, "ffn2")):
        sh["w%dg" % i] = f(inp[nm + "_w_gate"][0])
        sh["w%du" % i] = f(inp[nm + "_w_up"][0])
        sh["w%dd" % i] = f(inp[nm + "_w_down"][0])
    w_in = f(inp["w_in"][0])
    qa, ka, va, qb, ckv, iq, ik, iw = np.split(w_in, np.cumsum([512, 512, 512, 512, 256, 512, 64, 8])[:-1], axis=1)
    parts = []
    for h in range(4):
        parts += [qa[:, h * 128:(h + 1) * 128], ka[:, h * 128:(h + 1) * 128], va[:, h * 128:(h + 1) * 128]]
    parts += [ckv, iw]
    parts += [qb, iq, ik, ik]
    sh["win"] = np.ascontiguousarray(np.concatenate(parts, axis=1))
    assert sh["win"].shape[1] == WIN_COLS
    sh["wuk"] = np.ascontiguousarray(f(inp["b_w_uk"][0]).transpose(1, 0, 2))
    sh["wuv"] = np.ascontiguousarray(f(inp["b_w_uv"][0]).reshape(4, 2, 128, 128).transpose(2, 1, 0, 3))
    sh["wout"] = f(inp["w_out"][0])
    cols = np.zeros((128, 40), np.float32)
    cols[:, 0:8] = f(inp["ffn1_norm"][0]).reshape(8, 128).T
    cols[:, 8:16] = f(inp["mix_norm"][0]).reshape(8, 128).T
    cols[:, 16:24] = f(inp["ffn2_norm"][0]).reshape(8, 128).T
    cols[:, 24] = np.tile(f(inp["a_q_norm"][0]), 2)
    cols[:, 25] = np.tile(f(inp["a_k_norm"][0]), 2)
    cols[:, 26:28] = f(inp["b_kv_norm"][0]).reshape(2, 128).T
    cols[:, 28:30] = f(inp["b_q_norm"][0]).reshape(2, 128).T
    cols[:, 30] = f(inp["a_subln"][0])
    rb = f(inp["rel_bias"])
    cols[:, 31:39] = np.broadcast_to(rb[31], (128, 8))
    sh["cols"] = cols
    rows = np.zeros((128, 896), np.float32)
    rows[:, 0:64] = f(inp["a_lambda_q1"][0])[None]
    rows[:, 64:128] = f(inp["a_lambda_k1"][0])[None]
    rows[:, 128:192] = f(inp["a_lambda_q2"][0])[None]
    rows[:, 192:256] = f(inp["a_lambda_k2"][0])[None]
    rows[:, 256:320] = f(inp["a_q_norm"][0])[None]
    rows[:, 320:384] = f(inp["a_k_norm"][0])[None]
    rows[:, 384:640] = f(inp["b_q_norm"][0])[None]
    rows[:, 640:896] = f(inp["b_kv_norm"][0])[None]
    sh["rows"] = rows
    tk = np.arange(128)[:, None]
    tq = np.arange(128)[None, :]
    bb = np.zeros((128, 8, 2, 128), np.float32)
    for blk in range(2):
        idx = _bucket(tq - tk + 128 * blk)
        bb[:, :, blk, :] = rb[idx].transpose(0, 2, 1)
    sh["biasblk"] = bb
    cm = np.zeros((128, 3, 128), np.float32)
    cm[:, 0, :] = np.eye(128, dtype=np.float32)
    cm[:, 1, :] = np.where(tq >= tk, 0.0, NEG)
    cm[:, 2, :] = np.where(tq <= tk, 0.0, -1e30)
    sh["cmask"] = cm
    return sh


_CACHE = {}


def kernel(**inputs):
    x = np.asarray(inputs["x"], dtype=np.float32)
    sh = _prep_shared(inputs)
    if "nc" not in _CACHE:
        _CACHE["nc"] = Builder().build()
    nc = _CACHE["nc"]
    in_maps = []
    for c in range(NCORES):
        m = dict(sh)
        m["x"] = np.ascontiguousarray(x[c * NSEQ:(c + 1) * NSEQ])
        in_maps.append(m)
    res = run_bass_kernel_spmd(nc, in_maps, core_ids=list(range(NCORES)))
    out = np.concatenate([np.asarray(r["out"]) for r in res.results], axis=0)
    return out.astype(np.float32)
```

```python
import math
import os as _os
from contextlib import ExitStack

import numpy as np
import concourse.bass as bass
import concourse.mybir as mybir
from concourse.bass_utils import run_bass_kernel_spmd

F32 = mybir.dt.float32
BF16 = mybir.dt.bfloat16
ALU = mybir.AluOpType
AF = mybir.ActivationFunctionType
AX = mybir.AxisListType

D = 1024
SEQ = 2048
NMETA = 16
L = SEQ + NMETA
DFF = 2816
NSEQ = 2
NCORES = 8
NT = 17
TILES = [(i * 128, min(128, L - i * 128)) for i in range(NT)]
TGS = [(0, 512), (512, 512), (1024, 512), (1536, 512), (2048, 16)]
QGS = [[0, 1, 2, 3], [4, 5, 6, 7], [8, 9, 10, 11], [12, 13, 14, 15], [16]]
EPS = 1e-6
A_SCALE = 64 ** -0.5
B_SCALE = 256 ** -0.5
LAM_INIT = 0.8 - 0.6 * math.exp(0.0)
TOPK = 256
NEG = -30000.0
NBIS = 10
WIN_COLS = 4 * 384 + 264 + 9 * 128
STRICT = True
EPOCH = 2000


def _dsize(dt):
    return 4 if dt == F32 else 2


class Dep:
    __slots__ = ("name", "lw", "rd", "sem", "dcount")

    def __init__(self, name):
        self.name = name
        self.lw = None
        self.rd = []
        self.sem = None
        self.dcount = 0


class Op:
    __slots__ = ("eng", "fn", "deps", "dma", "signal", "seq", "sem", "waits", "wdep")

    def __init__(self, eng, fn, deps, dma, wdep):
        self.eng = eng
        self.fn = fn
        self.deps = deps
        self.dma = dma
        self.signal = False
        self.seq = 0
        self.sem = None
        self.waits = []
        self.wdep = wdep


class Prog:
    ENGS = ("sp", "pe", "act", "dve", "pool")

    def __init__(self, nc):
        self.nc = nc
        self.ops = []
        self.last = {e: None for e in self.ENGS}
        self.dmas_since_barrier = []

    def add(self, eng, fn, r=(), w=(), dma=False):
        idx = len(self.ops)
        deps = set()
        for t in r:
            if t.lw is not None:
                deps.add(t.lw)
        for t in w:
            if t.lw is not None:
                deps.add(t.lw)
            deps.update(t.rd)
        for t in r:
            t.rd.append(idx)
        for t in w:
            t.lw = idx
            t.rd = []
        wdep = None
        if dma:
            assert len(w) == 1
            wdep = w[0]
            self.dmas_since_barrier.append(idx)
        self.ops.append(Op(eng, fn, deps, dma, wdep))
        self.last[eng] = idx
        return idx

    def barrier(self):
        lasts = {v for v in self.last.values() if v is not None}
        lasts.update(self.dmas_since_barrier)
        self.dmas_since_barrier = []
        for e in self.ENGS:
            idx = len(self.ops)
            self.ops.append(Op(e, None, set(lasts), False, None))
            self.last[e] = idx

    def emit(self, stack):
        nc = self.nc
        ops = self.ops
        for op in ops:
            for d in sorted(op.deps):
                dop = ops[d]
                if not dop.dma and dop.eng == op.eng and (op.eng == "pe" or not STRICT):
                    continue
                if dop.fn is None:
                    continue
                dop.signal = True
                op.waits.append(d)
        esem = {e: stack.enter_context(nc.semaphore("s_" + e)) for e in self.ENGS}
        cnt = {e: 0 for e in self.ENGS}
        NCH = 8
        chsem = [stack.enter_context(nc.semaphore("dch%d" % i)) for i in range(NCH)]
        chcnt = [0] * NCH
        chlast = [None] * NCH
        ndma = 0
        for oi, op in enumerate(ops):
            if op.fn is None:
                continue
            if op.dma:
                c = ndma % NCH
                ndma += 1
                if chlast[c] is not None:
                    op.waits.append(chlast[c])
                chlast[c] = oi
                chcnt[c] += 16
                op.sem = chsem[c]
                op.seq = chcnt[c]
            elif op.signal:
                if cnt[op.eng] >= EPOCH:
                    esem[op.eng] = stack.enter_context(nc.semaphore("s_%s_%d" % (op.eng, oi)))
                    cnt[op.eng] = 0
                cnt[op.eng] += 1
                op.sem = esem[op.eng]
                op.seq = cnt[op.eng]
        per = {e: [op for op in ops if op.eng == e] for e in self.ENGS}

        def run(e, h):
            waited = {}
            for op in per[e]:
                need = {}
                for d in op.waits:
                    dop = ops[d]
                    k = id(dop.sem)
                    if k not in need or need[k][1] < dop.seq:
                        need[k] = (dop.sem, dop.seq)
                for k, (sem, val) in need.items():
                    if waited.get(k, 0) >= val:
                        continue
                    h.wait_ge(sem, val)
                    waited[k] = val
                if op.fn is None:
                    continue
                ins = op.fn(h)
                if op.dma:
                    ins.then_inc(op.sem, 16)
                elif op.signal:
                    ins.then_inc(op.sem, 1)

        with nc.Block() as block:
            @block.sync
            def _(h):
                run("sp", h)

            @block.tensor
            def _(h):
                run("pe", h)

            @block.scalar
            def _(h):
                run("act", h)

            @block.vector
            def _(h):
                run("dve", h)

            @block.gpsimd
            def _(h):
                run("pool", h)


class Builder:
    def __init__(self, stage=99, nseq=NSEQ, dbg=False, dbg_stop=False):
        self.dbg_stop = dbg_stop
        self.stage = stage
        self.nseq = nseq
        self.nc = nc = bass.Bass("TRN2", target_bir_lowering=False)
        self.P = Prog(nc)
        self.cur = 16640
        dt = nc.dram_tensor
        self.x = dt("x", [NSEQ, SEQ, D], F32, kind="ExternalInput").ap()
        self.meta = dt("meta", [NMETA, D], F32, kind="ExternalInput").ap()
        self.wg = [dt("w%dg" % i, [D, DFF], F32, kind="ExternalInput").ap() for i in (1, 2)]
        self.wu = [dt("w%du" % i, [D, DFF], F32, kind="ExternalInput").ap() for i in (1, 2)]
        self.wd = [dt("w%dd" % i, [DFF, D], F32, kind="ExternalInput").ap() for i in (1, 2)]
        self.win = dt("win", [D, WIN_COLS], F32, kind="ExternalInput").ap()
        self.wuk = dt("wuk", [128, 4, 256], F32, kind="ExternalInput").ap()
        self.wuv = dt("wuv", [128, 2, 4, 128], F32, kind="ExternalInput").ap()
        self.wout = dt("wout", [D, D], F32, kind="ExternalInput").ap()
        self.cols = dt("cols", [128, 40], F32, kind="ExternalInput").ap()
        self.rows = dt("rows", [128, 896], F32, kind="ExternalInput").ap()
        self.bias = dt("biasblk", [128, 8, 2, 128], F32, kind="ExternalInput").ap()
        self.cmask = dt("cmask", [128, 3, 128], F32, kind="ExternalInput").ap()
        self.out = dt("out", [NSEQ, SEQ, D], F32, kind="ExternalOutput").ap()
        self.hs = dt("hs", [L, D], F32, kind="ExternalOutput").ap()
        self.dbg = dt("dbg", [128, 4096], F32, kind="ExternalOutput").ap() if dbg else None
        self.psum = [nc.alloc_psum_tensor("pb%d" % i, [128, 512], F32).ap() for i in range(8)]
        self.pdep = [Dep("pb%d" % i) for i in range(8)]
        self.out_dep = Dep("out")

    def sb(self, name, shape, dtype, at=None):
        n = 1
        for s in shape[1:]:
            n *= s
        nbytes = (n * _dsize(dtype) + 63) // 64 * 64
        off = self.cur if at is None else at
        t = self.nc.alloc_sbuf_tensor_at(name, list(shape), dtype, offset=off)
        if at is None:
            self.cur = off + nbytes
        assert off + nbytes <= 229376, (name, off + nbytes)
        return t.ap()

    def mm(self, out, lhsT, rhs, start, stop, r, w):
        self.P.add("pe", lambda e: e.matmul(out, lhsT, rhs, start=start, stop=stop, skip_group_check=True), r, w)

    def tr(self, out, in_, ident, r, w):
        self.P.add("pe", lambda e: e.transpose(out, in_, ident), r, w)

    def act(self, out, in_, func, r, w, bias=0.0, scale=1.0, accum=None):
        if accum is None:
            self.P.add("act", lambda e: e.activation(out, in_, func, bias=bias, scale=scale), r, w)
        else:
            self.P.add("act", lambda e: e.activation(out, in_, func, bias=bias, scale=scale, accum_out=accum), r, w)

    def ts(self, eng, out, in0, s1, s2, op0, op1, r, w, accum=None):
        if accum is None:
            self.P.add(eng, lambda e: e.tensor_scalar(out, in0, s1, s2, op0, op1), r, w)
        else:
            self.P.add(eng, lambda e: e.tensor_scalar(out, in0, s1, s2, op0, op1, accum_out=accum), r, w)

    def tt(self, eng, out, in0, in1, op, r, w):
        self.P.add(eng, lambda e: e.tensor_tensor(out, in0, in1, op), r, w)

    def stt(self, out, in0, scalar, in1, op0, op1, r, w):
        self.P.add("dve", lambda e: e.scalar_tensor_tensor(out, in0, scalar, in1, op0, op1), r, w)

    def cp(self, eng, out, in_, r, w):
        self.P.add(eng, lambda e: e.tensor_copy(out, in_), r, w)

    def red(self, out, in_, op, r, w, absval=False):
        self.P.add("dve", lambda e: e.tensor_reduce(out, in_, AX.X, op, apply_absolute_value=absval), r, w)

    def recip(self, out, in_, r, w):
        self.P.add("dve", lambda e: e.reciprocal(out, in_), r, w)

    def memset(self, eng, ap, val, w):
        self.P.add(eng, lambda e: e.memset(ap, val), (), w)

    def dma(self, out, in_, r, w):
        self.P.add("sp", lambda e: e.dma_start(out=out, in_=in_), r, w, dma=True)

    def stat(self):
        i = self.st_i
        self.st_i = (i + 1) % 8
        return 4 * i, self.st_dep[i]

    def rstd(self, np_, k, nc_, inv_n, deps):
        ssd, td, rsd = deps
        self.ts("dve", self.tmp[0:np_, k:k + nc_], self.ss[0:np_, k:k + nc_], inv_n, EPS, ALU.mult, ALU.add, [ssd], [td])
        self.tt("pool", self.rs[0:np_, k:k + nc_], self.tmp[0:np_, k:k + nc_], self.neghalf[0:np_, 0:nc_], ALU.pow,
                [td, self.nh_dep], [rsd])

    def build(self):
        nc = self.nc
        stage = self.stage
        with ExitStack() as stack:
            self._alloc()
            self._setup()
            for s in range(self.nseq):
                self._sequence(s)
            self._finish()
            self.P.emit(stack)
        return nc

    def _alloc(self):
        sb = self.sb
        self.cols_sb = sb("cols", [128, 40], F32)
        self.rows_sb = sb("rows", [128, 896], F32)
        self.ident_f = sb("identf", [128, 128], F32)
        self.ident = sb("ident", [128, 128], BF16)
        self.neghalf = sb("neghalf", [128, 16], F32)
        self.smallc = sb("smallc", [128, 32], F32)
        self.gqk = self.smallc[:, 6:7]
        self.gqb = [self.smallc[:, 2:3], self.smallc[:, 10:11]]
        self.lamn = self.smallc[:, 14:15]
        self.abias = sb("abias", [128, 8], F32)
        self.gsub = self.smallc[:, 18:19]
        self.sm = sb("small", [128, 272], F32)
        self.biasbf = sb("biasbf", [128, 8, 2, 128], BF16)
        self.masktok = sb("masktok", [128, 128], F32)
        self.iw_sb = sb("iw", [128, NT, 8], F32)
        self.ss = sb("ss", [128, 32], F32)
        self.tmp = sb("tmpst", [128, 32], F32)
        self.rs = sb("rs", [128, 32], F32)
        self.st_dep = [(Dep("ss%d" % i), Dep("tm%d" % i), Dep("rs%d" % i)) for i in range(8)]
        self.st_i = 0
        self.wukb = sb("wukb", [128, 4, 256], BF16)
        self.wuvb = sb("wuvb", [128, 2, 4, 128], BF16)
        self.maskT = sb("maskT", [128, 128], F32)
        self.pw = sb("pw", [128, NBIS + 1], F32)
        self.thc = self.smallc[:, 22:23]
        self.xn_s = [sb("xn_s%d" % i, [128, D], BF16) for i in range(2)]
        self.xn_s_dep = [Dep("xn_s%d" % i) for i in range(2)]
        self.junk = sb("junk", [128, D], BF16)
        self.junk_dep = Dep("junk")
        self.c_const = self.cur
        self.xnT = sb("xnT", [128, 8, L], BF16)
        self.xn_dep = [Dep("xnT%d" % t) for t in range(NT)]
        self.h_off = self.cur
        self.h = sb("h", [128, NT, D], F32)
        self.h_dep = [Dep("h%d" % t) for t in range(NT)]
        self.big_off = self.cur
        self.phase_off = self.cur

    def _setup(self):
        P = self.P
        cd = self.cdep = Dep("consts")
        self.dma(self.cols_sb, self.cols, [], [cd])
        rd = Dep("rows")
        self.dma(self.rows_sb, self.rows, [], [rd])
        idd = Dep("identf")
        self.dma(self.ident_f, self.cmask[:, 0, :], [], [idd])
        mk = Dep("masktok")
        self.dma(self.masktok, self.cmask[:, 2, :], [], [mk])
        self.masktok_dep = mk
        self.ident_dep = Dep("ident")
        self.cp("dve", self.ident, self.ident_f, [idd], [self.ident_dep])
        self.nh_dep = Dep("neghalf")
        self.memset("dve", self.neghalf, -0.5, [self.nh_dep])
        if self.stage <= 1:
            return
        self._ffn_alloc()
        C = self.cols_sb
        R = self.rows_sb
        sd = self.setup_dep = Dep("setup")
        smd = Dep("sm")
        mtd = Dep("maskT")
        self.dma(self.maskT, self.cmask[:, 1, :], [], [mtd])
        self.stt(self.gqk, C[:, 24:25], A_SCALE, C[:, 25:26], ALU.mult, ALU.mult, [cd], [sd])
        for cc in range(2):
            self.ts("dve", self.gqb[cc], C[:, 28 + cc:29 + cc], B_SCALE, None, ALU.mult, ALU.bypass, [cd], [sd])
        self.ts("dve", self.gsub, C[:, 30:31], 1.0 - LAM_INIT, None, ALU.mult, ALU.bypass, [cd], [sd])
        sm = self.sm
        self.tt("dve", sm[:, 0:64], R[:, 0:64], R[:, 64:128], ALU.mult, [rd], [smd])
        self.red(sm[:, 256:257], sm[:, 0:64], ALU.add, [smd], [smd])
        self.tt("dve", sm[:, 64:128], R[:, 128:192], R[:, 192:256], ALU.mult, [rd], [smd])
        self.red(sm[:, 257:258], sm[:, 64:128], ALU.add, [smd], [smd])
        self.act(sm[:, 258:260], sm[:, 256:258], AF.Exp, [smd], [smd])
        self.tt("dve", sm[:, 260:261], sm[:, 258:259], sm[:, 259:260], ALU.subtract, [smd], [smd])
        self.ts("dve", self.lamn, sm[:, 260:261], -1.0, -LAM_INIT, ALU.mult, ALU.add, [smd], [sd])
        self.tt("dve", sm[:, 0:64], R[:, 256:320], R[:, 320:384], ALU.mult, [rd, smd], [smd])
        self.red(sm[:, 261:262], sm[:, 0:64], ALU.max, [smd], [smd], absval=True)
        self.ts("dve", sm[:, 262:263], sm[:, 261:262], 64.0 * A_SCALE, None, ALU.mult, ALU.bypass, [smd], [smd])
        self.ts("dve", self.abias[:, 0:4], C[:, 31:35], sm[:, 262:263], None, ALU.subtract, ALU.bypass, [smd, cd], [sd])
        self.tt("dve", sm[:, 0:256], R[:, 384:640], R[:, 640:896], ALU.mult, [rd, smd], [smd])
        self.red(sm[:, 263:264], sm[:, 0:256], ALU.max, [smd], [smd], absval=True)
        self.ts("dve", sm[:, 264:265], sm[:, 263:264], 256.0 * B_SCALE, None, ALU.mult, ALU.bypass, [smd], [smd])
        self.ts("dve", self.abias[:, 4:8], C[:, 35:39], sm[:, 264:265], None, ALU.subtract, ALU.bypass, [smd, cd], [sd])
        st = self.stg[0].rearrange("p (h b c) -> p h b c", h=8, b=2)
        self.dma(st, self.bias, [], [self.stg_dep[0]])
        for hh in range(8):
            self.stt(self.biasbf[:, hh, 0, :], st[:, hh, 0, :], C[:, 31 + hh:32 + hh], self.maskT, ALU.subtract, ALU.add,
                     [self.stg_dep[0], cd, mtd], [sd])
            self.ts("dve", self.biasbf[:, hh, 1, :], st[:, hh, 1, :], C[:, 31 + hh:32 + hh], None, ALU.subtract, ALU.bypass,
                    [self.stg_dep[0], cd], [sd])
        s1 = self.stg[1].rearrange("p (h c) -> p h c", h=4)[:, :, 0:256]
        self.dma(s1, self.wuk, [], [self.stg_dep[1]])
        self.cp("pool", self.wukb, s1, [self.stg_dep[1]], [sd])
        s2 = self.stg[2][:, 0:1024].rearrange("p (a h c) -> p a h c", a=2, h=4)
        self.dma(s2, self.wuv, [], [self.stg_dep[2]])
        self.cp("pool", self.wuvb, s2, [self.stg_dep[2]], [sd])
        for k in range(NBIS + 1):
            self.memset("pool", self.pw[:, k:k + 1], 2.0 ** -(k + 1), [sd])
        self.memset("pool", self.thc, -1e29, [sd])

    def _sequence(self, s):
        self._load_h(s, from_x=True)
        self._norm_pass(0)
        self._ffn(0)
        if self.stage <= 1:
            self._store_out(s)
            return
        self._norm_pass(1)
        self.hs_dep = getattr(self, "hs_dep", None) or [Dep("hs%d" % t) for t in range(NT)]
        for t, (t0, n) in enumerate(TILES):
            self.dma(self.hs[t0:t0 + n, :], self.h[0:n, t, :], [self.h_dep[t]], [self.hs_dep[t]])
        self.P.barrier()
        self._attn_alloc()
        if not _os.environ.get("K_SKIP_PROJ"):
            self._proj()
        self.P.barrier()
        if self.stage >= 3:
            self._attnA()
            self.P.barrier()
        if self.stage >= 4:
            self._attnB()
            self.P.barrier()
        if self.dbg_stop:
            return
        if not _os.environ.get("K_NO_RELOAD"):
            self._load_h(s, from_x=False)
        if not _os.environ.get("K_NO_FFN2"):
            self._norm_pass(2)
            self._ffn(1)
        self._store_out(s)

    def _load_h(self, s, from_x):
        for t, (t0, n) in enumerate(TILES):
            hd = self.h_dep[t]
            if from_x:
                if t == 0:
                    self.dma(self.h[0:NMETA, 0, :], self.meta, [], [hd])
                    self.dma(self.h[NMETA:128, 0, :], self.x[s, 0:128 - NMETA, :], [], [hd])
                else:
                    self.dma(self.h[0:n, t, :], self.x[s, t0 - NMETA:t0 - NMETA + n, :], [], [hd])
            else:
                self.dma(self.h[0:n, t, :], self.hs[t0:t0 + n, :], [self.hs_dep[t]], [hd])

    def _store_out(self, s):
        for t, (t0, n) in enumerate(TILES):
            if t == 0:
                self.dma(self.out[s, 0:128 - NMETA, :], self.h[NMETA:128, 0, :], [self.h_dep[0]], [self.out_dep])
            else:
                self.dma(self.out[s, t0 - NMETA:t0 - NMETA + n, :], self.h[0:n, t, :], [self.h_dep[t]], [self.out_dep])

    def _norm_pass(self, which):
        slots = {}

        def stats(t):
            t0, n = TILES[t]
            k, (ssd, td, rsd) = self.stat()
            slots[t] = (k, rsd)
            ss = self.ss[0:n, k:k + 1]
            self.act(self.junk[0:n, :], self.h[0:n, t, :], AF.Square, [self.h_dep[t]], [self.junk_dep, ssd], accum=ss)
            self.ts("dve", self.tmp[0:n, k:k + 1], ss, 1.0 / D, EPS, ALU.mult, ALU.add, [ssd], [td])
            self.tt("pool", self.rs[0:n, k:k + 1], self.tmp[0:n, k:k + 1], self.neghalf[0:n, 0:1], ALU.pow,
                    [td, self.nh_dep], [rsd])

        stats(0)
        stats(1)
        for t, (t0, n) in enumerate(TILES):
            if t + 2 < NT:
                stats(t + 2)
            b = t % 2
            k, rsd = slots[t]
            self.ts("dve", self.xn_s[b][0:n, :], self.h[0:n, t, :], self.rs[0:n, k:k + 1], None, ALU.mult, ALU.bypass,
                    [self.h_dep[t], rsd], [self.xn_s_dep[b]])
            pb = self.psum[b].bitcast(BF16)
            for kk in range(8):
                self.tr(pb[:, kk * 128:kk * 128 + n], self.xn_s[b][0:n, kk * 128:(kk + 1) * 128],
                        self.ident[0:n, 0:n], [self.xn_s_dep[b], self.ident_dep], [self.pdep[b]])
            src = pb.rearrange("p (k c) -> p k c", k=8)[:, :, 0:n]
            self.P.add("act", (lambda e, o=self.xnT[:, :, t0:t0 + n], i=src: e.activation(o, i, AF.Copy)),
                       [self.pdep[b]], [self.xn_dep[t]])

    def _ffn_alloc(self):
        if hasattr(self, "ffn_alloced"):
            return
        self.ffn_alloced = True
        self.cur = self.phase_off
        sb = self.sb
        self.stg = [sb("stg%d" % i, [128, 2048], F32) for i in range(3)]
        self.stg_dep = [Dep("stg%d" % i) for i in range(3)]
        self.stg_i = 0
        self.wgb = [sb("wgb%d" % i, [128, 8, 256], BF16) for i in range(2)]
        self.wub = [sb("wub%d" % i, [128, 8, 256], BF16) for i in range(2)]
        self.wgb_dep = [Dep("wgb%d" % i) for i in range(2)]
        self.wub_dep = [Dep("wub%d" % i) for i in range(2)]
        self.wdb = [sb("wdb%d" % i, [128, 2, D], BF16) for i in range(4)]
        self.wdb_dep = [Dep("wdb%d" % i) for i in range(4)]
        self.actb = sb("actb", [128, 4, L], BF16)
        self.actb_dep = [[Dep("act%d_%d" % (f, g)) for g in range(len(TGS))] for f in range(4)]
        self.sg = [sb("sg%d" % i, [128, 512], BF16) for i in range(2)]
        self.sg_dep = [Dep("sg%d" % i) for i in range(2)]
        self.ffn_end = self.cur
        self.slab_ctr = 0
        self.gu_ctr = 0
        self.dn_ctr = 0

    def _stage_slot(self):
        i = self.stg_i
        self.stg_i = (i + 1) % 3
        return i

    def _ffn(self, which):
        self._ffn_alloc()
        wg, wu, wd = self.wg[which], self.wu[which], self.wd[which]
        gcol = {0: 0, 1: 16}[which]
        gain = self.cols_sb[:, gcol:gcol + 8]
        slabs = list(range(11))
        groups = [slabs[i:i + 2] for i in range(0, 11, 2)]
        for grp in groups:
            for li, sl in enumerate(grp):
                c0 = sl * 256
                sw = self.slab_ctr % 2
                dslot = self.slab_ctr % 4
                self.slab_ctr += 1
                for (src, dst, ddep) in ((wg, self.wgb[sw], self.wgb_dep[sw]), (wu, self.wub[sw], self.wub_dep[sw])):
                    si = self._stage_slot()
                    st3 = self.stg[si].rearrange("p (k c) -> p k c", k=8)
                    self.dma(st3, src[:, c0:c0 + 256].rearrange("(k p) c -> p k c", p=128), [], [self.stg_dep[si]])
                    self.tt("pool", dst, st3, gain.unsqueeze(2).to_broadcast([128, 8, 256]), ALU.mult,
                            [self.stg_dep[si], self.cdep], [ddep])
                si = self._stage_slot()
                st3 = self.stg[si].rearrange("p (k c) -> p k c", k=2)
                self.dma(st3, wd[c0:c0 + 256, :].rearrange("(k p) c -> p k c", p=128), [], [self.stg_dep[si]])
                self.cp("pool", self.wdb[dslot], st3, [self.stg_dep[si]], [self.wdb_dep[dslot]])
                grp_dslot = dslot
                for c in range(2):
                    fl = li * 2 + c
                    for g, (g0, gn) in enumerate(TGS):
                        pbuf = self.gu_ctr % 2
                        self.gu_ctr += 1
                        pg, pu = 2 + 2 * pbuf, 3 + 2 * pbuf
                        xdeps = [self.xn_dep[t] for t in range(NT) if TILES[t][0] >= g0 and TILES[t][0] < g0 + gn]
                        for k in range(8):
                            self.mm(self.psum[pg][:, 0:gn], self.wgb[sw][:, k, c * 128:(c + 1) * 128],
                                    self.xnT[:, k, g0:g0 + gn], k == 0, k == 7,
                                    [self.wgb_dep[sw]] + xdeps, [self.pdep[pg]])
                        for k in range(8):
                            self.mm(self.psum[pu][:, 0:gn], self.wub[sw][:, k, c * 128:(c + 1) * 128],
                                    self.xnT[:, k, g0:g0 + gn], k == 0, k == 7,
                                    [self.wub_dep[sw]] + xdeps, [self.pdep[pu]])
                        sgi = pbuf
                        self.act(self.sg[sgi][:, 0:gn], self.psum[pg][:, 0:gn], AF.Silu, [self.pdep[pg]], [self.sg_dep[sgi]])
                        self.tt("dve", self.actb[:, fl, g0:g0 + gn], self.sg[sgi][:, 0:gn], self.psum[pu][:, 0:gn], ALU.mult,
                                [self.sg_dep[sgi], self.pdep[pu]], [self.actb_dep[fl][g]])
            nfl = len(grp) * 2
            first_dslot = (self.slab_ctr - len(grp)) % 4
            for t, (t0, n) in enumerate(TILES):
                g = min(t // 4, 4)
                for half in range(2):
                    pd = 6 + (self.dn_ctr % 2)
                    self.dn_ctr += 1
                    for fl in range(nfl):
                        dslot = (first_dslot + fl // 2) % 4
                        self.mm(self.psum[pd][0:n, :], self.actb[:, fl, t0:t0 + n],
                                self.wdb[dslot][:, fl % 2, half * 512:(half + 1) * 512], fl == 0, fl == nfl - 1,
                                [self.actb_dep[fl][g], self.wdb_dep[dslot]], [self.pdep[pd]])
                    hv = self.h[0:n, t, half * 512:(half + 1) * 512]
                    self.stt(hv, self.psum[pd][0:n, :], 0.5, hv, ALU.mult, ALU.add,
                             [self.pdep[pd], self.h_dep[t]], [self.h_dep[t]])


    def _attn_alloc(self):
        if hasattr(self, "attn_alloced"):
            return
        self.attn_alloced = True
        sb = self.sb
        save = self.cur
        self.cur = self.h_off
        r1 = self.cur
        self.qT = sb("qT", [128, 4, L], BF16)
        self.kT = sb("kT", [128, 4, L], BF16)
        self.VA = sb("VA", [128, NT, 4, 129], BF16)
        r1_end = self.cur
        self.cur = r1
        self.qlT = sb("qlT", [128, 4, 2, 512], BF16)
        self.rr = [sb("rr%d" % i, [128, 512], F32) for i in range(2)]
        self.qn1k = sb("qn1k", [128, 1024], BF16)
        self.ocT = sb("ocT", [128, 8, 128], BF16)
        self.woutb = sb("woutb", [128, 8, D], BF16)
        self.hst2 = sb("hst2", [128, 2, D], F32)
        self.hst = [self.hst2[:, 0, :], self.hst2[:, 1, :]]
        self.obG = sb("obG", [128, 4, 512], BF16)
        self.cntj = sb("cntj", [128, L], BF16)
        assert self.cur <= r1_end, (self.cur, r1_end)
        self.cur = r1_end
        self.qbT = sb("qbT", [128, 4, L], BF16)
        self.cT = sb("cT", [128, 2, L], BF16)
        self.VB = sb("VB", [128, NT, 4, 129], BF16)
        self.iqT = sb("iqT", [128, 4, L], BF16)
        self.ikT = sb("ikT", [128, L], BF16)
        r3 = self.cur
        self.wst = [sb("wst%d" % i, [128, 8, 384], F32) for i in range(2)]
        self.wb = [sb("wb%d" % i, [128, 8, 384], BF16) for i in range(2)]
        r3_end = self.cur
        self.cur = r3
        self.oa = sb("oa", [128, NT, 512], BF16)
        self.PT = [sb("PT%d" % i, [128, 512], BF16) for i in range(4)]
        self.t1 = [sb("t1_%d" % i, [128, 128], F32) for i in range(2)]
        self.ov = [sb("ov_%d" % i, [128, 128], F32) for i in range(2)]
        self.bst = sb("bst", [128, 4 * NBIS + 8], F32)
        self.mTa = sb("mTa", [128, 12, 512], BF16)
        assert self.cur <= r3_end, (self.cur, r3_end)
        self.cur = max(r3_end, save)
        self.sq = [sb("sq%d" % i, [128, 256], F32) for i in range(2)]
        self.qn = [sb("qn%d" % i, [128, 256], BF16) for i in range(2)]
        save2 = self.cur
        self.cur = self.c_const
        self.acc = sb("acc", [128, L], F32)
        self.maskb = sb("maskb", [128, L], BF16)
        self.mT = sb("mT", [128, NT, 512], BF16)
        assert self.cur <= self.h_off, (self.cur, self.h_off)
        self.cur = save2
        D_ = Dep
        self.qT_dep = [[D_("qT%d_%d" % (h, t)) for t in range(NT)] for h in range(4)]
        self.kT_dep = [[D_("kT%d_%d" % (h, t)) for t in range(NT)] for h in range(4)]
        self.VA_dep = [D_("VA%d" % t) for t in range(NT)]
        self.VB_dep = [D_("VB%d" % t) for t in range(NT)]
        self.cT_dep = [D_("cT%d" % t) for t in range(NT)]
        self.qbT_dep = [D_("qbT%d" % g) for g in range(5)]
        self.iqT_dep = [D_("iqT%d" % g) for g in range(5)]
        self.ikT_dep = [D_("ikT%d" % g) for g in range(5)]
        self.iw_dep = [D_("iw%d" % t) for t in range(NT)]
        self.wst_dep = [D_("wst%d" % i) for i in range(2)]
        self.wb_dep = [D_("wb%d" % i) for i in range(2)]
        self.sq_dep = [D_("sq%d" % i) for i in range(2)]
        self.qn_dep = [D_("qn%d" % i) for i in range(2)]
        self.oa_dep = [D_("oa%d" % t) for t in range(NT)]
        self.PT_dep = [D_("PT%d" % i) for i in range(4)]
        self.t1_dep = [D_("t1_%d" % i) for i in range(2)]
        self.ov_dep = [D_("ov_%d" % i) for i in range(2)]
        self.qlT_dep = [D_("qlT%d" % i) for i in range(4)]
        self.rr_dep = [D_("rr%d" % i) for i in range(2)]
        self.qn1k_dep = D_("qn1k")
        self.ocT_dep = D_("ocT")
        self.woutb_dep = D_("woutb")
        self.hst_dep = [D_("hst%d" % i) for i in range(2)]
        self.obG_dep = [D_("obG%d" % i) for i in range(4)]
        self.cntj_dep = D_("cntj")
        self.acc_dep = D_("acc")
        self.maskb_dep = D_("maskb")
        self.mT_dep = [D_("mT%d" % i) for i in range(4)]
        self.mTa_dep = [D_("mTa%d" % i) for i in range(4)]
        self.bst_dep = D_("bst")
        self.ctr = 0

    def _load_slab(self, c0, ncol, gain):
        slot = self.ctr % 2
        self.ctr += 1
        st = self.wst[slot][:, :, 0:ncol]
        self.dma(st, self.win[:, c0:c0 + ncol].rearrange("(k p) c -> p k c", p=128), [], [self.wst_dep[slot]])
        self.tt("pool", self.wb[slot][:, :, 0:ncol], st, gain.unsqueeze(2).to_broadcast([128, 8, ncol]), ALU.mult,
                [self.wst_dep[slot], self.cdep], [self.wb_dep[slot]])
        return slot

    def _proj(self):
        gain = self.cols_sb[:, 8:16]
        C = self.cols_sb
        parts = _os.environ.get("K_PROJ_PARTS", "ms,A,CK,FM").split(",")
        if "ms" in parts:
            self.memset("pool", self.VA[:, :, :, 128:129], 1.0, self.VA_dep)
            self.memset("pool", self.VB[:, :, :, 128:129], 1.0, self.VB_dep)
        tctr = 0
        for sl in range(5):
            if (sl < 4 and "A" not in parts) or (sl == 4 and "CK" not in parts):
                continue
            ncol = 384 if sl < 4 else 264
            slot = self._load_slab(sl * 384, ncol, gain)

            def mmA(t, slot=slot, ncol=ncol):
                t0, n = TILES[t]
                pb = 2 + (t % 4)
                for k in range(8):
                    self.mm(self.psum[pb][0:n, 0:ncol], self.xnT[:, k, t0:t0 + n], self.wb[slot][:, k, 0:ncol], k == 0, k == 7,
                            [self.xn_dep[t], self.wb_dep[slot]], [self.pdep[pb]])

            for t in range(3):
                mmA(t)
            for t, (t0, n) in enumerate(TILES):
                if t + 3 < NT:
                    mmA(t + 3)
                pb = 2 + (t % 4)
                tb = t % 2
                b = t % 2
                ps = self.psum[pb]
                pT = self.psum[tb].bitcast(BF16)
                k4, sdeps = self.stat()
                ssd, td, rsd = sdeps
                if sl < 4:
                    h = sl
                    KA = _os.environ.get("K_A", "sq,red,rstd,qn,tr,evq,evk,va").split(",")
                    if "sq" in KA:
                        self.act(self.sq[b][0:n, :], ps[0:n, 0:256], AF.Square, [self.pdep[pb]], [self.sq_dep[b]])
                    if "red" in KA:
                        self.red(self.ss[0:n, k4:k4 + 4], self.sq[b][0:n, :].rearrange("p (g d) -> p g d", d=64), ALU.add,
                                 [self.sq_dep[b]], [ssd])
                    if "rstd" in KA:
                        self.rstd(n, k4, 4, 1.0 / 64, sdeps)
                    if "qn" in KA:
                        self.tt("dve", self.qn[b][0:n, :].rearrange("p (g d) -> p g d", d=64),
                                ps[0:n, 0:256].rearrange("p (g d) -> p g d", d=64),
                                self.rs[0:n, k4:k4 + 4].unsqueeze(2).to_broadcast([n, 4, 64]), ALU.mult,
                                [self.pdep[pb], rsd], [self.qn_dep[b]])
                    if "tr" in KA:
                        self.tr(pT[:, 0:n], self.qn[b][0:n, 0:128], self.ident[0:n, 0:n], [self.qn_dep[b], self.ident_dep],
                                [self.pdep[tb]])
                        self.tr(pT[:, 128:128 + n], self.qn[b][0:n, 128:256], self.ident[0:n, 0:n], [self.qn_dep[b], self.ident_dep],
                                [self.pdep[tb]])
                    if "evq" in KA:
                        qdst = self.junk[:, 0:n] if _os.environ.get("K_DEST") else self.qT[:, h, t0:t0 + n]
                        self.act(qdst, pT[:, 0:n], AF.Copy, [self.pdep[tb], self.setup_dep],
                                 [self.qT_dep[h][t]], scale=self.gqk)
                    if "evk" in KA:
                        kdst = self.junk[:, 128:128 + n] if _os.environ.get("K_DEST") else self.kT[:, h, t0:t0 + n]
                        self.act(kdst, pT[:, 128:128 + n], AF.Copy, [self.pdep[tb]], [self.kT_dep[h][t]])
                    if "va" in KA:
                        self.act(self.VA[0:n, t, h, 0:128], ps[0:n, 256:384], AF.Copy, [self.pdep[pb]], [self.VA_dep[t]])
                else:
                    self.act(self.sq[b][0:n, :], ps[0:n, 0:256], AF.Square, [self.pdep[pb]], [self.sq_dep[b], ssd],
                             accum=self.ss[0:n, k4:k4 + 1])
                    self.rstd(n, k4, 1, 1.0 / 256, sdeps)
                    self.ts("dve", self.qn[b][0:n, :], ps[0:n, 0:256], self.rs[0:n, k4:k4 + 1], None, ALU.mult, ALU.bypass,
                            [self.pdep[pb], rsd], [self.qn_dep[b]])
                    self.ts("dve", self.iw_sb[0:n, t, :], ps[0:n, 256:264], (64 ** -0.5) * (8 ** -0.5), None, ALU.mult, ALU.bypass,
                            [self.pdep[pb]], [self.iw_dep[t]])
                    for cc in range(2):
                        self.tr(pT[:, cc * 128:cc * 128 + n], self.qn[b][0:n, cc * 128:(cc + 1) * 128], self.ident[0:n, 0:n],
                                [self.qn_dep[b], self.ident_dep], [self.pdep[tb]])
                    self.ts("dve", self.cT[:, 0, t0:t0 + n], pT[:, 0:n], C[:, 26:27], None, ALU.mult, ALU.bypass,
                            [self.pdep[tb], self.cdep], [self.cT_dep[t]])
                    self.act(self.cT[:, 1, t0:t0 + n], pT[:, 128:128 + n], AF.Copy, [self.pdep[tb], self.cdep], [self.cT_dep[t]],
                             scale=C[:, 27:28])
                    vb = 6 + (t % 2)
                    for cc in range(2):
                        self.mm(self.psum[vb][0:n, :], self.cT[:, cc, t0:t0 + n],
                                self.wuvb[:, cc, :, :].rearrange("p h e -> p (h e)"), cc == 0, cc == 1,
                                [self.cT_dep[t], self.setup_dep], [self.pdep[vb]])
                    self.act(self.VB[0:n, t, :, 0:128], self.psum[vb][0:n, :].rearrange("p (h e) -> p h e", h=4), AF.Copy,
                             [self.pdep[vb]], [self.VB_dep[t]])
        ectr = 0
        for sl in range(3):
            if "FM" not in parts:
                continue
            slot = self._load_slab(1800 + sl * 384, 384, gain)
            for c in range(3):
                ch = sl * 3 + c
                for g, (g0, gn) in enumerate(TGS):
                    if ch < 4:
                        dst, ddep = self.qbT[:, ch, g0:g0 + gn], self.qbT_dep[g]
                    elif ch < 8:
                        dst, ddep = self.iqT[:, ch - 4, g0:g0 + gn], self.iqT_dep[g]
                    else:
                        dst, ddep = self.ikT[:, g0:g0 + gn], self.ikT_dep[g]
                    pb = 4 + (ectr % 2)
                    xdeps = [self.xn_dep[t] for t in range(NT) if g0 <= TILES[t][0] < g0 + gn]
                    for k in range(8):
                        self.mm(self.psum[pb][:, 0:gn], self.wb[slot][:, k, c * 128:(c + 1) * 128], self.xnT[:, k, g0:g0 + gn],
                                k == 0, k == 7, [self.wb_dep[slot]] + xdeps, [self.pdep[pb]])
                    if ectr % 2 == 0:
                        self.act(dst, self.psum[pb][:, 0:gn], AF.Copy, [self.pdep[pb]], [ddep])
                    else:
                        self.cp("dve", dst, self.psum[pb][:, 0:gn], [self.pdep[pb]], [ddep])
                    ectr += 1

    def _bias_blocks(self, ps, pbank, hh, j, tiles, c0):
        k0, kn = TILES[j]
        for blk, i in ((0, j), (1, j + 1)):
            if i in tiles:
                off = TILES[i][0] - c0
                ni = TILES[i][1]
                self.mm(ps[:, off:off + ni], self.ident[0:kn, 0:kn], self.biasbf[0:kn, hh, blk, 0:ni], False, True,
                        [self.ident_dep, self.setup_dep], [self.pdep[pbank]])

    def _pipe(self, n, stage1, stage2, depth=2):
        for k in range(min(depth, n)):
            stage1(k)
        for k in range(n):
            if k + depth < n:
                stage1(k + depth)
            stage2(k)

    def _attnA(self):
        for G, tiles in enumerate(QGS):
            q0 = TILES[tiles[0]][0]
            qn_ = sum(TILES[t][1] for t in tiles)
            for h in range(4):
                started = set()
                nblk = tiles[-1] + 1

                def stage1(j, h=h, tiles=tiles, q0=q0, qn_=qn_):
                    k0, kn = TILES[j]
                    c0 = max(q0, k0)
                    ncol = q0 + qn_ - c0
                    qdeps = [self.qT_dep[h][t] for t in tiles if TILES[t][0] + TILES[t][1] > c0]
                    for m in range(2):
                        bank = (j % 2) * 2 + m
                        ps = self.psum[bank][0:kn, 0:ncol]
                        self.mm(ps, self.kT[m * 64:(m + 1) * 64, h, k0:k0 + kn], self.qT[m * 64:(m + 1) * 64, h, c0:c0 + ncol],
                                True, True, [self.kT_dep[h][j]] + qdeps, [self.pdep[bank]])
                    for m in range(2):
                        bank = (j % 2) * 2 + m
                        ps = self.psum[bank][0:kn, 0:ncol]
                        self._bias_blocks(ps, bank, h, j, tiles, c0)
                        self.act(self.PT[bank][0:kn, 0:ncol], ps, AF.Exp, [self.pdep[bank], self.setup_dep], [self.PT_dep[bank]],
                                 bias=self.abias[0:kn, h:h + 1])

                def stage2(j, h=h, tiles=tiles, q0=q0, started=started):
                    k0, kn = TILES[j]
                    c0 = max(q0, k0)
                    for m in range(2):
                        pbuf = (j % 2) * 2 + m
                        for il, i in enumerate(tiles):
                            if i < j:
                                continue
                            off = TILES[i][0] - c0
                            ni = TILES[i][1]
                            slot = m * 4 + il
                            ab = 4 + slot // 3
                            o = (slot % 3) * 129
                            first = ab not in started
                            started.add(ab)
                            self.mm(self.psum[ab][0:ni, o:o + 129], self.PT[pbuf][0:kn, off:off + ni], self.VA[0:kn, j, h, :],
                                    first, j == i, [self.PT_dep[pbuf], self.VA_dep[j]], [self.pdep[ab]])

                self._pipe(nblk, stage1, stage2, depth=1)
                for il, i in enumerate(tiles):
                    ni = TILES[i][1]
                    b = il % 2
                    s0, s1 = il, 4 + il
                    b0, o0 = 4 + s0 // 3, (s0 % 3) * 129
                    b1, o1 = 4 + s1 // 3, (s1 % 3) * 129
                    k4, sdeps = self.stat()
                    ssd, td, rsd = sdeps
                    self.recip(self.rs[0:ni, k4 + 1:k4 + 2], self.psum[b0][0:ni, o0 + 128:o0 + 129], [self.pdep[b0]], [rsd])
                    self.recip(self.rs[0:ni, k4 + 2:k4 + 3], self.psum[b1][0:ni, o1 + 128:o1 + 129], [self.pdep[b1]], [rsd])
                    self.ts("dve", self.rs[0:ni, k4 + 3:k4 + 4], self.rs[0:ni, k4 + 2:k4 + 3], self.lamn[0:ni, 0:1], None,
                            ALU.mult, ALU.bypass, [rsd, self.setup_dep], [rsd])
                    self.ts("dve", self.t1[b][0:ni, :], self.psum[b1][0:ni, o1:o1 + 128], self.rs[0:ni, k4 + 3:k4 + 4], None,
                            ALU.mult, ALU.bypass, [self.pdep[b1], rsd], [self.t1_dep[b]])
                    self.stt(self.ov[b][0:ni, :], self.psum[b0][0:ni, o0:o0 + 128], self.rs[0:ni, k4 + 1:k4 + 2],
                             self.t1[b][0:ni, :], ALU.mult, ALU.add, [self.pdep[b0], rsd, self.t1_dep[b]], [self.ov_dep[b]])
                    self.act(self.junk[0:ni, 0:128], self.ov[b][0:ni, :], AF.Square, [self.ov_dep[b]], [self.junk_dep, ssd],
                             accum=self.ss[0:ni, k4:k4 + 1])
                    self.rstd(ni, k4, 1, 1.0 / 128, sdeps)
                    self.ts("dve", self.oa[0:ni, i, h * 128:(h + 1) * 128], self.ov[b][0:ni, :], self.rs[0:ni, k4:k4 + 1], None,
                            ALU.mult, ALU.bypass, [self.ov_dep[b], rsd], [self.oa_dep[i]])

    def _attnB(self):
        C = self.cols_sb
        for q in range(4):
            st = self.hst2
            hd = self.hst_dep
            self.dma(st, self.wout[q * 256:(q + 1) * 256, :].rearrange("(k p) c -> p k c", p=128), [hd[1]], [hd[0]])
            if q < 2:
                self.ts("pool", self.woutb[:, 2 * q:2 * q + 2, :], st, self.gsub[:, 0:1], None, ALU.mult, ALU.bypass,
                        [hd[0], hd[1], self.setup_dep], [self.woutb_dep])
            else:
                self.cp("pool", self.woutb[:, 2 * q:2 * q + 2, :], st, [hd[0], hd[1]], [self.woutb_dep])
        self.dctr = 0
        self.hctr = 0
        ng = len(QGS)
        for il in range(len(QGS[0])):
            self._b_idx1(0, il)
            self._b_idx2(0, il)
        for G in range(ng):
            self._b_qlat(G)
            nxt = G + 1 if G + 1 < ng - 1 else None
            for h in range(4):
                if nxt is not None and h < len(QGS[nxt]):
                    self._b_idx1(nxt, h)
                self._b_attn_mm(G, h)
                if nxt is not None and h < len(QGS[nxt]):
                    self._b_idx2(nxt, h)
                self._b_attn_norm(G, h)
            if G + 1 == ng - 1:
                for il in range(len(QGS[G + 1])):
                    self._b_idx1(G + 1, il)
                    self._b_idx2(G + 1, il)
            self._b_wout(G)

    def _b_ctx(self, G):
        tiles = QGS[G]
        q0 = TILES[tiles[0]][0]
        qn_ = sum(TILES[t][1] for t in tiles)
        use_a = G in (0, 2)
        mT = self.mTa if use_a else self.mT
        mT_dep = self.mTa_dep if use_a else self.mT_dep
        return tiles, q0, qn_, mT, mT_dep

    def _b_qlat(self, G):
        tiles, q0, qn_, mT, mT_dep = self._b_ctx(G)
        for il, i in enumerate(tiles):
            t0, ni = TILES[i]
            for h in range(4):
                pbk = h // 2
                self.mm(self.psum[pbk][0:ni, (h % 2) * 256:(h % 2) * 256 + 256], self.qbT[:, h, t0:t0 + ni], self.wukb[:, h, :],
                        True, True, [self.qbT_dep[G], self.setup_dep], [self.pdep[pbk]])
            k4, sdeps = self.stat()
            ssd, td, rsd = sdeps
            for pbk in range(2):
                self.act(self.junk[0:ni, pbk * 512:(pbk + 1) * 512], self.psum[pbk][0:ni, :], AF.Square, [self.pdep[pbk]],
                         [self.junk_dep])
            self.red(self.ss[0:ni, k4:k4 + 4], self.junk[0:ni, :].rearrange("p (g d) -> p g d", d=256), ALU.add,
                     [self.junk_dep], [ssd])
            self.rstd(ni, k4, 4, 1.0 / 256, sdeps)
            for pbk in range(2):
                self.tt("dve", self.qn1k[0:ni, pbk * 512:(pbk + 1) * 512].rearrange("p (g d) -> p g d", d=256),
                        self.psum[pbk][0:ni, :].rearrange("p (g d) -> p g d", d=256),
                        self.rs[0:ni, k4 + 2 * pbk:k4 + 2 * pbk + 2].unsqueeze(2).to_broadcast([ni, 2, 256]), ALU.mult,
                        [self.pdep[pbk], rsd], [self.qn1k_dep])
            pT = self.psum[2].bitcast(BF16)
            for ch in range(8):
                self.tr(pT[:, ch * 128:ch * 128 + ni], self.qn1k[0:ni, ch * 128:(ch + 1) * 128], self.ident[0:ni, 0:ni],
                        [self.qn1k_dep, self.ident_dep], [self.pdep[2]])
            pv = pT.rearrange("p (h c t) -> p h c t", h=4, c=2)
            self.ts("dve", self.qlT[:, :, 0, il * 128:il * 128 + ni], pv[:, :, 0, 0:ni], self.gqb[0], None, ALU.mult,
                    ALU.bypass, [self.pdep[2], self.setup_dep], [self.qlT_dep[il]])
            self.ts("dve", self.qlT[:, :, 1, il * 128:il * 128 + ni], pv[:, :, 1, 0:ni], self.gqb[1], None, ALU.mult,
                    ALU.bypass, [self.pdep[2], self.setup_dep], [self.qlT_dep[il]])

    def _b_idx1(self, G, il):
        tiles, q0, qn_, mT, mT_dep = self._b_ctx(G)
        i = tiles[il]
        dctr = self.dctr
        t0, ni = TILES[i]
        Lk = t0 + ni
        acc = self.acc
        for kc0 in range(0, Lk, 512):
            kcn = min(512, Lk - kc0)
            g = kc0 // 512
            for hh in range(8):
                pb = 3 + (dctr % 2)
                rb = dctr % 2
                dctr += 1
                pr = (hh % 2) * 64
                self.mm(self.psum[pb][0:ni, 0:kcn], self.iqT[pr:pr + 64, hh // 2, t0:t0 + ni], self.ikT[pr:pr + 64, kc0:kc0 + kcn],
                        True, True, [self.iqT_dep[G], self.ikT_dep[g]], [self.pdep[pb]])
                self.act(self.rr[rb][0:ni, 0:kcn], self.psum[pb][0:ni, 0:kcn], AF.Relu, [self.pdep[pb]], [self.rr_dep[rb]])
                if hh == 0:
                    self.ts("dve", acc[0:ni, kc0:kc0 + kcn], self.rr[rb][0:ni, 0:kcn], self.iw_sb[0:ni, i, 0:1], None,
                            ALU.mult, ALU.bypass, [self.rr_dep[rb], self.iw_dep[i]], [self.acc_dep])
                else:
                    self.stt(acc[0:ni, kc0:kc0 + kcn], self.rr[rb][0:ni, 0:kcn], self.iw_sb[0:ni, i, hh:hh + 1],
                             acc[0:ni, kc0:kc0 + kcn], ALU.mult, ALU.add, [self.rr_dep[rb], self.iw_dep[i], self.acc_dep],
                             [self.acc_dep])
        self.dctr = dctr

    def _b_idx2(self, G, il):
        tiles, q0, qn_, mT, mT_dep = self._b_ctx(G)
        i = tiles[il]
        t0, ni = TILES[i]
        Lk = t0 + ni
        acc = self.acc
        bs = self.bst
        bd = self.bst_dep
        if i >= 2:
            self.red(bs[0:ni, 0:1], acc[0:ni, 0:Lk], ALU.max, [self.acc_dep], [bd])
            self.red(bs[0:ni, 1:2], acc[0:ni, 0:TOPK], ALU.min, [self.acc_dep], [bd])
        self.tt("dve", acc[0:ni, t0:t0 + ni], acc[0:ni, t0:t0 + ni], self.masktok[0:ni, 0:ni], ALU.add,
                [self.acc_dep, self.masktok_dep], [self.acc_dep])
        if i >= 2:
            self.tt("dve", bs[0:ni, 2:3], bs[0:ni, 0:1], bs[0:ni, 1:2], ALU.subtract, [bd], [bd])
            W0 = 8
            self.ts("dve", bs[0:ni, W0:W0 + NBIS + 1], self.pw[0:ni, :], bs[0:ni, 2:3], None, ALU.mult, ALU.bypass,
                    [bd, self.setup_dep], [bd])
            M0 = W0 + NBIS + 1
            self.tt("dve", bs[0:ni, M0:M0 + 1], bs[0:ni, 1:2], bs[0:ni, W0:W0 + 1], ALU.add, [bd], [bd])
            C0 = M0 + NBIS + 1
            for k in range(NBIS):
                self.ts("dve", self.cntj[0:ni, 0:Lk], acc[0:ni, 0:Lk], bs[0:ni, M0 + k:M0 + k + 1], None, ALU.is_ge, ALU.add,
                        [self.acc_dep, bd], [self.cntj_dep, bd], accum=bs[0:ni, C0 + k:C0 + k + 1])
                self.ts("dve", bs[0:ni, 3:4], bs[0:ni, C0 + k:C0 + k + 1], TOPK - 0.5, bs[0:ni, W0 + k:W0 + k + 1],
                        ALU.is_ge, ALU.mult, [bd], [bd])
                wn = W0 + k + 1 if k < NBIS - 1 else W0 + k
                self.stt(bs[0:ni, M0 + k + 1:M0 + k + 2], bs[0:ni, 3:4], bs[0:ni, wn:wn + 1], bs[0:ni, M0 + k:M0 + k + 1],
                         ALU.subtract, ALU.add, [bd], [bd])
            theta = bs[0:ni, M0 + NBIS:M0 + NBIS + 1]
            thd = [bd]
        else:
            theta = self.thc[0:ni, 0:1]
            thd = [self.setup_dep]
        self.ts("dve", self.maskb[0:ni, 0:Lk], acc[0:ni, 0:Lk], theta, NEG, ALU.is_lt, ALU.mult, [self.acc_dep] + thd,
                [self.maskb_dep])
        for j0 in range(0, i + 1, 8):
            js = list(range(j0, min(j0 + 8, i + 1)))
            tb = 5
            pT = self.psum[tb].bitcast(BF16)
            for jj, j in enumerate(js):
                k0, kn = TILES[j]
                self.tr(pT[0:kn, jj * 128:jj * 128 + ni], self.maskb[0:ni, k0:k0 + kn], self.ident[0:ni, 0:ni],
                        [self.maskb_dep, self.ident_dep], [self.pdep[tb]])
            src = pT.rearrange("p (j t) -> p j t", j=8)[:, 0:len(js), 0:ni]
            self.cp("dve", mT[:, j0:j0 + len(js), il * 128:il * 128 + ni], src, [self.pdep[tb]], [mT_dep[il]])

    def _b_attn_mm(self, G, h):
        tiles, q0, qn_, mT, mT_dep = self._b_ctx(G)
        started = set()
        nblk = tiles[-1] + 1

        def stage1(j, h=h, tiles=tiles, q0=q0, qn_=qn_, mT=mT, mT_dep=mT_dep):
            k0, kn = TILES[j]
            c0 = max(q0, k0)
            ncol = q0 + qn_ - c0
            co = c0 - q0
            bank = j % 3
            ps = self.psum[bank][0:kn, 0:ncol]
            qd = [self.qlT_dep[il] for il, t in enumerate(tiles) if TILES[t][0] + TILES[t][1] > c0]
            md = [mT_dep[il] for il, t in enumerate(tiles) if TILES[t][0] + TILES[t][1] > c0]
            for cc in range(2):
                self.mm(ps, self.cT[:, cc, k0:k0 + kn], self.qlT[:, h, cc, co:co + ncol], cc == 0, False,
                        [self.cT_dep[j]] + qd, [self.pdep[bank]])
            self.mm(ps, self.ident[0:kn, 0:kn], mT[0:kn, j, co:co + ncol], False, True, [self.ident_dep] + md,
                    [self.pdep[bank]])
            self._bias_blocks(ps, bank, 4 + h, j, tiles, c0)
            self.act(self.PT[j % 4][0:kn, 0:ncol], ps, AF.Exp, [self.pdep[bank], self.setup_dep], [self.PT_dep[j % 4]],
                     bias=self.abias[0:kn, 4 + h:5 + h])

        def stage2(j, h=h, tiles=tiles, q0=q0, started=started):
            k0, kn = TILES[j]
            c0 = max(q0, k0)
            pbuf = j % 4
            for il, i in enumerate(tiles):
                if i < j:
                    continue
                off = TILES[i][0] - c0
                ni = TILES[i][1]
                ab = 6 + il // 3
                o = (il % 3) * 129
                first = ab not in started
                started.add(ab)
                self.mm(self.psum[ab][0:ni, o:o + 129], self.PT[pbuf][0:kn, off:off + ni], self.VB[0:kn, j, h, :],
                        first, j == i, [self.PT_dep[pbuf], self.VB_dep[j]], [self.pdep[ab]])

        self._pipe(nblk, stage1, stage2)

    def _b_attn_norm(self, G, h):
        tiles, q0, qn_, mT, mT_dep = self._b_ctx(G)
        for il, i in enumerate(tiles):
            ni = TILES[i][1]
            ab = 6 + il // 3
            o = (il % 3) * 129
            k4, sdeps = self.stat()
            ssd, td, rsd = sdeps
            self.recip(self.rs[0:ni, k4:k4 + 1], self.psum[ab][0:ni, o + 128:o + 129], [self.pdep[ab]], [rsd])
            self.ts("dve", self.obG[0:ni, il, h * 128:(h + 1) * 128], self.psum[ab][0:ni, o:o + 128], self.rs[0:ni, k4:k4 + 1],
                    None, ALU.mult, ALU.bypass, [self.pdep[ab], rsd], [self.obG_dep[il]])

    def _b_wout(self, G):
        tiles, q0, qn_, mT, mT_dep = self._b_ctx(G)
        hctr = self.hctr
        for il, i in enumerate(tiles):
            t0, ni = TILES[i]
            pT = self.psum[2].bitcast(BF16)
            for k in range(8):
                src = self.oa[0:ni, i, k * 128:(k + 1) * 128] if k < 4 else self.obG[0:ni, il, (k - 4) * 128:(k - 3) * 128]
                sd_ = self.oa_dep[i] if k < 4 else self.obG_dep[il]
                self.tr(pT[:, k * 128:k * 128 + ni], src, self.ident[0:ni, 0:ni], [sd_, self.ident_dep], [self.pdep[2]])
            self.act(self.ocT[:, :, 0:ni], pT.rearrange("p (k t) -> p k t", k=8)[:, :, 0:ni], AF.Copy, [self.pdep[2]],
                     [self.ocT_dep])
            hb = hctr % 2
            hctr += 1
            self.dma(self.hst[hb][0:ni, :], self.hs[t0:t0 + ni, :], [self.hs_dep[i]], [self.hst_dep[hb]])
            for half in range(2):
                pb = 3 + half
                for k in range(8):
                    self.mm(self.psum[pb][0:ni, :], self.ocT[:, k, 0:ni], self.woutb[:, k, half * 512:(half + 1) * 512],
                            k == 0, k == 7, [self.ocT_dep, self.woutb_dep], [self.pdep[pb]])
                hv = self.hst[hb][0:ni, half * 512:(half + 1) * 512]
                self.tt("dve", hv, self.psum[pb][0:ni, :], hv, ALU.add, [self.pdep[pb], self.hst_dep[hb]], [self.hst_dep[hb]])
            self.dma(self.hs[t0:t0 + ni, :], self.hst[hb][0:ni, :], [self.hst_dep[hb]], [self.hs_dep[i]])
        self.hctr = hctr

    def _finish(self):
        if _os.environ.get("K_DUMP"):
            dd = Dep("dump")
            self.P.barrier()
            self.dma(self.hs[0:128, 0:16], self.smallc[:, 0:16], [self.setup_dep], [dd])
            self.dma(self.hs[0:128, 16:24], self.abias, [self.setup_dep], [dd])
            self.P.add("sp", None, [dd], [])
        self.P.add("sp", None, [self.out_dep], [])
        if self.dbg is not None:
            pass


def _bucket(n):
    n = np.maximum(n, 0)
    nf = np.maximum(n, 1).astype(np.float32)
    large = 16 + (np.log(nf / np.float32(16)) / np.float32(math.log(128 / 16)) * np.float32(16)).astype(np.int32)
    large = np.minimum(large, 31)
    return np.where(n < 16, n, large)


def _prep_shared(inp):
    f = lambda a: np.ascontiguousarray(np.asarray(a, dtype=np.float32))
    sh = {}
    sh["meta"] = f(inp["meta_tokens"])
    for i, nm in ((1, "ffn1"), (2, "ffn2")):
        sh["w%dg" % i] = f(inp[nm + "_w_gate"][0])
        sh["w%du" % i] = f(inp[nm + "_w_up"][0])
        sh["w%dd" % i] = f(inp[nm + "_w_down"][0])
    w_in = f(inp["w_in"][0])
    qa, ka, va, qb, ckv, iq, ik, iw = np.split(w_in, np.cumsum([512, 512, 512, 512, 256, 512, 64, 8])[:-1], axis=1)
    parts = []
    for h in range(4):
        parts += [qa[:, h * 128:(h + 1) * 128], ka[:, h * 128:(h + 1) * 128], va[:, h * 128:(h + 1) * 128]]
    parts += [ckv, iw]
    parts += [qb, iq, ik, ik]
    sh["win"] = np.ascontiguousarray(np.concatenate(parts, axis=1))
    assert sh["win"].shape[1] == WIN_COLS
    sh["wuk"] = np.ascontiguousarray(f(inp["b_w_uk"][0]).transpose(1, 0, 2))
    sh["wuv"] = np.ascontiguousarray(f(inp["b_w_uv"][0]).reshape(4, 2, 128, 128).transpose(2, 1, 0, 3))
    sh["wout"] = f(inp["w_out"][0])
    cols = np.zeros((128, 40), np.float32)
    cols[:, 0:8] = f(inp["ffn1_norm"][0]).reshape(8, 128).T
    cols[:, 8:16] = f(inp["mix_norm"][0]).reshape(8, 128).T
    cols[:, 16:24] = f(inp["ffn2_norm"][0]).reshape(8, 128).T
    cols[:, 24] = np.tile(f(inp["a_q_norm"][0]), 2)
    cols[:, 25] = np.tile(f(inp["a_k_norm"][0]), 2)
    cols[:, 26:28] = f(inp["b_kv_norm"][0]).reshape(2, 128).T
    cols[:, 28:30] = f(inp["b_q_norm"][0]).reshape(2, 128).T
    cols[:, 30] = f(inp["a_subln"][0])
    rb = f(inp["rel_bias"])
    cols[:, 31:39] = np.broadcast_to(rb[31], (128, 8))
    sh["cols"] = cols
    rows = np.zeros((128, 896), np.float32)
    rows[:, 0:64] = f(inp["a_lambda_q1"][0])[None]
    rows[:, 64:128] = f(inp["a_lambda_k1"][0])[None]
    rows[:, 128:192] = f(inp["a_lambda_q2"][0])[None]
    rows[:, 192:256] = f(inp["a_lambda_k2"][0])[None]
    rows[:, 256:320] = f(inp["a_q_norm"][0])[None]
    rows[:, 320:384] = f(inp["a_k_norm"][0])[None]
    rows[:, 384:640] = f(inp["b_q_norm"][0])[None]
    rows[:, 640:896] = f(inp["b_kv_norm"][0])[None]
    sh["rows"] = rows
    tk = np.arange(128)[:, None]
    tq = np.arange(128)[None, :]
    bb = np.zeros((128, 8, 2, 128), np.float32)
    for blk in range(2):
        idx = _bucket(tq - tk + 128 * blk)
        bb[:, :, blk, :] = rb[idx].transpose(0, 2, 1)
    sh["biasblk"] = bb
    cm = np.zeros((128, 3, 128), np.float32)
    cm[:, 0, :] = np.eye(128, dtype=np.float32)
    cm[:, 1, :] = np.where(tq >= tk, 0.0, NEG)
    cm[:, 2, :] = np.where(tq <= tk, 0.0, -1e30)
    sh["cmask"] = cm
    return sh


_CACHE = {}


def kernel(**inputs):
    x = np.asarray(inputs["x"], dtype=np.float32)
    sh = _prep_shared(inputs)
    if "nc" not in _CACHE:
        _CACHE["nc"] = Builder().build()
    nc = _CACHE["nc"]
    in_maps = []
    for c in range(NCORES):
        m = dict(sh)
        m["x"] = np.ascontiguousarray(x[c * NSEQ:(c + 1) * NSEQ])
        in_maps.append(m)
    res = run_bass_kernel_spmd(nc, in_maps, core_ids=list(range(NCORES)))
    out = np.concatenate([np.asarray(r["out"]) for r in res.results], axis=0)
    return out.astype(np.float32)
```

```python
import math
import os as _os
from contextlib import ExitStack

import numpy as np
import concourse.bass as bass
import concourse.mybir as mybir
from concourse.bass_utils import run_bass_kernel_spmd

F32 = mybir.dt.float32
BF16 = mybir.dt.bfloat16
ALU = mybir.AluOpType
AF = mybir.ActivationFunctionType
AX = mybir.AxisListType

D = 1024
SEQ = 2048
NMETA = 16
L = SEQ + NMETA
DFF = 2816
NSEQ = 2
NCORES = 8
NT = 17
TILES = [(i * 128, min(128, L - i * 128)) for i in range(NT)]
TGS = [(0, 512), (512, 512), (1024, 512), (1536, 512), (2048, 16)]
QGS = [[0, 1, 2, 3], [4, 5, 6, 7], [8, 9, 10, 11], [12, 13, 14, 15], [16]]
EPS = 1e-6
A_SCALE = 64 ** -0.5
B_SCALE = 256 ** -0.5
LAM_INIT = 0.8 - 0.6 * math.exp(0.0)
TOPK = 256
NEG = -30000.0
NBIS = 9
WIN_COLS = 4 * 384 + 264 + 9 * 128
STRICT = True
EPOCH = 2000


def _dsize(dt):
    return 4 if dt == F32 else 2


class Dep:
    __slots__ = ("name", "lw", "rd", "sem", "dcount")

    def __init__(self, name):
        self.name = name
        self.lw = None
        self.rd = []
        self.sem = None
        self.dcount = 0


class Op:
    __slots__ = ("eng", "fn", "deps", "dma", "signal", "seq", "sem", "waits", "wdep")

    def __init__(self, eng, fn, deps, dma, wdep):
        self.eng = eng
        self.fn = fn
        self.deps = deps
        self.dma = dma
        self.signal = False
        self.seq = 0
        self.sem = None
        self.waits = []
        self.wdep = wdep


class Prog:
    ENGS = ("sp", "pe", "act", "dve", "pool")

    def __init__(self, nc):
        self.nc = nc
        self.ops = []
        self.last = {e: None for e in self.ENGS}
        self.dmas_since_barrier = []

    def add(self, eng, fn, r=(), w=(), dma=False):
        idx = len(self.ops)
        deps = set()
        for t in r:
            if t.lw is not None:
                deps.add(t.lw)
        for t in w:
            if t.lw is not None:
                deps.add(t.lw)
            deps.update(t.rd)
        for t in r:
            t.rd.append(idx)
        for t in w:
            t.lw = idx
            t.rd = []
        wdep = None
        if dma:
            assert len(w) == 1
            wdep = w[0]
            self.dmas_since_barrier.append(idx)
        self.ops.append(Op(eng, fn, deps, dma, wdep))
        self.last[eng] = idx
        return idx

    def barrier(self):
        lasts = {v for v in self.last.values() if v is not None}
        lasts.update(self.dmas_since_barrier)
        self.dmas_since_barrier = []
        for e in self.ENGS:
            idx = len(self.ops)
            self.ops.append(Op(e, None, set(lasts), False, None))
            self.last[e] = idx

    def emit(self, stack):
        nc = self.nc
        ops = self.ops
        for op in ops:
            for d in sorted(op.deps):
                dop = ops[d]
                if not dop.dma and dop.eng == op.eng and (op.eng == "pe" or not STRICT):
                    continue
                if dop.fn is None:
                    continue
                dop.signal = True
                op.waits.append(d)
        esem = {e: stack.enter_context(nc.semaphore("s_" + e)) for e in self.ENGS}
        cnt = {e: 0 for e in self.ENGS}
        NCH = 8
        chsem = [stack.enter_context(nc.semaphore("dch%d" % i)) for i in range(NCH)]
        chcnt = [0] * NCH
        chlast = [None] * NCH
        ndma = 0
        for oi, op in enumerate(ops):
            if op.fn is None:
                continue
            if op.dma:
                c = ndma % NCH
                ndma += 1
                if chlast[c] is not None:
                    op.waits.append(chlast[c])
                chlast[c] = oi
                chcnt[c] += 16
                op.sem = chsem[c]
                op.seq = chcnt[c]
            elif op.signal:
                if cnt[op.eng] >= EPOCH:
                    esem[op.eng] = stack.enter_context(nc.semaphore("s_%s_%d" % (op.eng, oi)))
                    cnt[op.eng] = 0
                cnt[op.eng] += 1
                op.sem = esem[op.eng]
                op.seq = cnt[op.eng]
        per = {e: [op for op in ops if op.eng == e] for e in self.ENGS}

        def run(e, h):
            waited = {}
            for op in per[e]:
                need = {}
                for d in op.waits:
                    dop = ops[d]
                    k = id(dop.sem)
                    if k not in need or need[k][1] < dop.seq:
                        need[k] = (dop.sem, dop.seq)
                for k, (sem, val) in need.items():
                    if waited.get(k, 0) >= val:
                        continue
                    h.wait_ge(sem, val)
                    waited[k] = val
                if op.fn is None:
                    continue
                ins = op.fn(h)
                if op.dma:
                    ins.then_inc(op.sem, 16)
                elif op.signal:
                    ins.then_inc(op.sem, 1)

        with nc.Block() as block:
            @block.sync
            def _(h):
                run("sp", h)

            @block.tensor
            def _(h):
                run("pe", h)

            @block.scalar
            def _(h):
                run("act", h)

            @block.vector
            def _(h):
                run("dve", h)

            @block.gpsimd
            def _(h):
                run("pool", h)


class Builder:
    def __init__(self, stage=99, nseq=NSEQ, dbg=False, dbg_stop=False):
        self.dbg_stop = dbg_stop
        self.stage = stage
        self.nseq = nseq
        self.nc = nc = bass.Bass("TRN2", target_bir_lowering=False)
        self.P = Prog(nc)
        self.cur = 16640
        dt = nc.dram_tensor
        self.x = dt("x", [NSEQ, SEQ, D], F32, kind="ExternalInput").ap()
        self.meta = dt("meta", [NMETA, D], F32, kind="ExternalInput").ap()
        self.wg = [dt("w%dg" % i, [D, DFF], F32, kind="ExternalInput").ap() for i in (1, 2)]
        self.wu = [dt("w%du" % i, [D, DFF], F32, kind="ExternalInput").ap() for i in (1, 2)]
        self.wd = [dt("w%dd" % i, [DFF, D], F32, kind="ExternalInput").ap() for i in (1, 2)]
        self.win = dt("win", [D, WIN_COLS], F32, kind="ExternalInput").ap()
        self.wuk = dt("wuk", [128, 4, 256], F32, kind="ExternalInput").ap()
        self.wuv = dt("wuv", [128, 2, 4, 128], F32, kind="ExternalInput").ap()
        self.wout = dt("wout", [D, D], F32, kind="ExternalInput").ap()
        self.cols = dt("cols", [128, 40], F32, kind="ExternalInput").ap()
        self.rows = dt("rows", [128, 896], F32, kind="ExternalInput").ap()
        self.bias = dt("biasblk", [128, 8, 2, 128], F32, kind="ExternalInput").ap()
        self.cmask = dt("cmask", [128, 3, 128], F32, kind="ExternalInput").ap()
        self.out = dt("out", [NSEQ, SEQ, D], F32, kind="ExternalOutput").ap()
        self.hs = dt("hs", [L, D], F32, kind="ExternalOutput").ap()
        self.dbg = dt("dbg", [128, 4096], F32, kind="ExternalOutput").ap() if dbg else None
        self.psum = [nc.alloc_psum_tensor("pb%d" % i, [128, 512], F32).ap() for i in range(8)]
        self.pdep = [Dep("pb%d" % i) for i in range(8)]
        self.out_dep = Dep("out")

    def sb(self, name, shape, dtype, at=None):
        n = 1
        for s in shape[1:]:
            n *= s
        nbytes = (n * _dsize(dtype) + 63) // 64 * 64
        off = self.cur if at is None else at
        t = self.nc.alloc_sbuf_tensor_at(name, list(shape), dtype, offset=off)
        if at is None:
            self.cur = off + nbytes
        assert off + nbytes <= 229376, (name, off + nbytes)
        return t.ap()

    def mm(self, out, lhsT, rhs, start, stop, r, w):
        self.P.add("pe", lambda e: e.matmul(out, lhsT, rhs, start=start, stop=stop, skip_group_check=True), r, w)

    def tr(self, out, in_, ident, r, w):
        self.P.add("pe", lambda e: e.transpose(out, in_, ident), r, w)

    def act(self, out, in_, func, r, w, bias=0.0, scale=1.0, accum=None):
        if accum is None:
            self.P.add("act", lambda e: e.activation(out, in_, func, bias=bias, scale=scale), r, w)
        else:
            self.P.add("act", lambda e: e.activation(out, in_, func, bias=bias, scale=scale, accum_out=accum), r, w)

    def ts(self, eng, out, in0, s1, s2, op0, op1, r, w, accum=None):
        if accum is None:
            self.P.add(eng, lambda e: e.tensor_scalar(out, in0, s1, s2, op0, op1), r, w)
        else:
            self.P.add(eng, lambda e: e.tensor_scalar(out, in0, s1, s2, op0, op1, accum_out=accum), r, w)

    def tt(self, eng, out, in0, in1, op, r, w):
        self.P.add(eng, lambda e: e.tensor_tensor(out, in0, in1, op), r, w)

    def stt(self, out, in0, scalar, in1, op0, op1, r, w):
        self.P.add("dve", lambda e: e.scalar_tensor_tensor(out, in0, scalar, in1, op0, op1), r, w)

    def cp(self, eng, out, in_, r, w):
        self.P.add(eng, lambda e: e.tensor_copy(out, in_), r, w)

    def red(self, out, in_, op, r, w, absval=False):
        self.P.add("dve", lambda e: e.tensor_reduce(out, in_, AX.X, op, apply_absolute_value=absval), r, w)

    def recip(self, out, in_, r, w):
        self.P.add("dve", lambda e: e.reciprocal(out, in_), r, w)

    def memset(self, eng, ap, val, w):
        self.P.add(eng, lambda e: e.memset(ap, val), (), w)

    def dma(self, out, in_, r, w):
        self.P.add("sp", lambda e: e.dma_start(out=out, in_=in_), r, w, dma=True)

    def stat(self):
        i = self.st_i
        self.st_i = (i + 1) % 8
        return 4 * i, self.st_dep[i]

    def rstd(self, np_, k, nc_, inv_n, deps):
        ssd, td, rsd = deps
        self.ts("dve", self.tmp[0:np_, k:k + nc_], self.ss[0:np_, k:k + nc_], inv_n, EPS, ALU.mult, ALU.add, [ssd], [td])
        self.tt("pool", self.rs[0:np_, k:k + nc_], self.tmp[0:np_, k:k + nc_], self.neghalf[0:np_, 0:nc_], ALU.pow,
                [td, self.nh_dep], [rsd])

    def build(self):
        nc = self.nc
        stage = self.stage
        with ExitStack() as stack:
            self._alloc()
            self._setup()
            for s in range(self.nseq):
                self._sequence(s)
            self._finish()
            self.P.emit(stack)
        return nc

    def _alloc(self):
        sb = self.sb
        self.cols_sb = sb("cols", [128, 40], F32)
        self.rows_sb = sb("rows", [128, 896], F32)
        self.ident_f = sb("identf", [128, 128], F32)
        self.ident = sb("ident", [128, 128], BF16)
        self.neghalf = sb("neghalf", [128, 16], F32)
        self.smallc = sb("smallc", [128, 32], F32)
        self.gqk = self.smallc[:, 6:7]
        self.gqb = [self.smallc[:, 2:3], self.smallc[:, 10:11]]
        self.lamn = self.smallc[:, 14:15]
        self.abias = sb("abias", [128, 8], F32)
        self.gsub = self.smallc[:, 18:19]
        self.sm = sb("small", [128, 272], F32)
        self.biasbf = sb("biasbf", [128, 8, 2, 128], BF16)
        self.masktok = sb("masktok", [128, 128], F32)
        self.iw_sb = sb("iw", [128, NT, 8], F32)
        self.ss = sb("ss", [128, 32], F32)
        self.tmp = sb("tmpst", [128, 32], F32)
        self.rs = sb("rs", [128, 32], F32)
        self.st_dep = [(Dep("ss%d" % i), Dep("tm%d" % i), Dep("rs%d" % i)) for i in range(8)]
        self.st_i = 0
        self.wukb = sb("wukb", [128, 4, 256], BF16)
        self.wuvb = sb("wuvb", [128, 2, 4, 128], BF16)
        self.maskT = sb("maskT", [128, 128], F32)
        self.pw = sb("pw", [128, NBIS + 1], F32)
        self.thc = self.smallc[:, 22:23]
        self.xn_s = [sb("xn_s%d" % i, [128, D], BF16) for i in range(2)]
        self.xn_s_dep = [Dep("xn_s%d" % i) for i in range(2)]
        self.junk = sb("junk", [128, D], BF16)
        self.junk_dep = Dep("junk")
        self.c_const = self.cur
        self.xnT = sb("xnT", [128, 8, L], BF16)
        self.xn_dep = [Dep("xnT%d" % t) for t in range(NT)]
        self.h_off = self.cur
        self.h = sb("h", [128, NT, D], F32)
        self.h_dep = [Dep("h%d" % t) for t in range(NT)]
        self.big_off = self.cur
        self.phase_off = self.cur

    def _setup(self):
        P = self.P
        cd = self.cdep = Dep("consts")
        self.dma(self.cols_sb, self.cols, [], [cd])
        rd = Dep("rows")
        self.dma(self.rows_sb, self.rows, [], [rd])
        idd = Dep("identf")
        self.dma(self.ident_f, self.cmask[:, 0, :], [], [idd])
        mk = Dep("masktok")
        self.dma(self.masktok, self.cmask[:, 2, :], [], [mk])
        self.masktok_dep = mk
        self.ident_dep = Dep("ident")
        self.cp("dve", self.ident, self.ident_f, [idd], [self.ident_dep])
        self.nh_dep = Dep("neghalf")
        self.memset("dve", self.neghalf, -0.5, [self.nh_dep])
        if self.stage <= 1:
            return
        self._ffn_alloc()
        C = self.cols_sb
        R = self.rows_sb
        sd = self.setup_dep = Dep("setup")
        smd = Dep("sm")
        mtd = Dep("maskT")
        self.dma(self.maskT, self.cmask[:, 1, :], [], [mtd])
        self.stt(self.gqk, C[:, 24:25], A_SCALE, C[:, 25:26], ALU.mult, ALU.mult, [cd], [sd])
        for cc in range(2):
            self.ts("dve", self.gqb[cc], C[:, 28 + cc:29 + cc], B_SCALE, None, ALU.mult, ALU.bypass, [cd], [sd])
        self.ts("dve", self.gsub, C[:, 30:31], 1.0 - LAM_INIT, None, ALU.mult, ALU.bypass, [cd], [sd])
        sm = self.sm
        self.tt("dve", sm[:, 0:64], R[:, 0:64], R[:, 64:128], ALU.mult, [rd], [smd])
        self.red(sm[:, 256:257], sm[:, 0:64], ALU.add, [smd], [smd])
        self.tt("dve", sm[:, 64:128], R[:, 128:192], R[:, 192:256], ALU.mult, [rd], [smd])
        self.red(sm[:, 257:258], sm[:, 64:128], ALU.add, [smd], [smd])
        self.act(sm[:, 258:260], sm[:, 256:258], AF.Exp, [smd], [smd])
        self.tt("dve", sm[:, 260:261], sm[:, 258:259], sm[:, 259:260], ALU.subtract, [smd], [smd])
        self.ts("dve", self.lamn, sm[:, 260:261], -1.0, -LAM_INIT, ALU.mult, ALU.add, [smd], [sd])
        self.tt("dve", sm[:, 0:64], R[:, 256:320], R[:, 320:384], ALU.mult, [rd, smd], [smd])
        self.red(sm[:, 261:262], sm[:, 0:64], ALU.max, [smd], [smd], absval=True)
        self.ts("dve", sm[:, 262:263], sm[:, 261:262], 64.0 * A_SCALE, None, ALU.mult, ALU.bypass, [smd], [smd])
        self.ts("dve", self.abias[:, 0:4], C[:, 31:35], sm[:, 262:263], None, ALU.subtract, ALU.bypass, [smd, cd], [sd])
        self.tt("dve", sm[:, 0:256], R[:, 384:640], R[:, 640:896], ALU.mult, [rd, smd], [smd])
        self.red(sm[:, 263:264], sm[:, 0:256], ALU.max, [smd], [smd], absval=True)
        self.ts("dve", sm[:, 264:265], sm[:, 263:264], 256.0 * B_SCALE, None, ALU.mult, ALU.bypass, [smd], [smd])
        self.ts("dve", self.abias[:, 4:8], C[:, 35:39], sm[:, 264:265], None, ALU.subtract, ALU.bypass, [smd, cd], [sd])
        st = self.stg[0].rearrange("p (h b c) -> p h b c", h=8, b=2)
        self.dma(st, self.bias, [], [self.stg_dep[0]])
        for hh in range(8):
            self.stt(self.biasbf[:, hh, 0, :], st[:, hh, 0, :], C[:, 31 + hh:32 + hh], self.maskT, ALU.subtract, ALU.add,
                     [self.stg_dep[0], cd, mtd], [sd])
            self.ts("dve", self.biasbf[:, hh, 1, :], st[:, hh, 1, :], C[:, 31 + hh:32 + hh], None, ALU.subtract, ALU.bypass,
                    [self.stg_dep[0], cd], [sd])
        s1 = self.stg[1].rearrange("p (h c) -> p h c", h=4)[:, :, 0:256]
        self.dma(s1, self.wuk, [], [self.stg_dep[1]])
        self.cp("pool", self.wukb, s1, [self.stg_dep[1]], [sd])
        s2 = self.stg[2][:, 0:1024].rearrange("p (a h c) -> p a h c", a=2, h=4)
        self.dma(s2, self.wuv, [], [self.stg_dep[2]])
        self.cp("pool", self.wuvb, s2, [self.stg_dep[2]], [sd])
        for k in range(NBIS + 1):
            self.memset("pool", self.pw[:, k:k + 1], 2.0 ** -(k + 1), [sd])
        self.memset("pool", self.thc, -1e29, [sd])

    def _sequence(self, s):
        self._load_h(s, from_x=True)
        self._norm_pass(0)
        self._ffn(0)
        if self.stage <= 1:
            self._store_out(s)
            return
        self._norm_pass(1)
        self.hs_dep = getattr(self, "hs_dep", None) or [Dep("hs%d" % t) for t in range(NT)]
        for t, (t0, n) in enumerate(TILES):
            self.dma(self.hs[t0:t0 + n, :], self.h[0:n, t, :], [self.h_dep[t]], [self.hs_dep[t]])
        self.P.barrier()
        self._attn_alloc()
        if not _os.environ.get("K_SKIP_PROJ"):
            self._proj()
        self.P.barrier()
        if self.stage >= 3:
            self._attnA()
            self.P.barrier()
        if self.stage >= 4:
            self._attnB()
            self.P.barrier()
        if self.dbg_stop:
            return
        if not _os.environ.get("K_NO_RELOAD"):
            self._load_h(s, from_x=False)
        if not _os.environ.get("K_NO_FFN2"):
            self._norm_pass(2)
            self._ffn(1)
        self._store_out(s)

    def _load_h(self, s, from_x):
        for t, (t0, n) in enumerate(TILES):
            hd = self.h_dep[t]
            if from_x:
                if t == 0:
                    self.dma(self.h[0:NMETA, 0, :], self.meta, [], [hd])
                    self.dma(self.h[NMETA:128, 0, :], self.x[s, 0:128 - NMETA, :], [], [hd])
                else:
                    self.dma(self.h[0:n, t, :], self.x[s, t0 - NMETA:t0 - NMETA + n, :], [], [hd])
            else:
                self.dma(self.h[0:n, t, :], self.hs[t0:t0 + n, :], [self.hs_dep[t]], [hd])

    def _store_out(self, s):
        for t, (t0, n) in enumerate(TILES):
            if t == 0:
                self.dma(self.out[s, 0:128 - NMETA, :], self.h[NMETA:128, 0, :], [self.h_dep[0]], [self.out_dep])
            else:
                self.dma(self.out[s, t0 - NMETA:t0 - NMETA + n, :], self.h[0:n, t, :], [self.h_dep[t]], [self.out_dep])

    def _norm_pass(self, which):
        slots = {}

        def stats(t):
            t0, n = TILES[t]
            k, (ssd, td, rsd) = self.stat()
            slots[t] = (k, rsd)
            ss = self.ss[0:n, k:k + 1]
            self.act(self.junk[0:n, :], self.h[0:n, t, :], AF.Square, [self.h_dep[t]], [self.junk_dep, ssd], accum=ss)
            self.ts("dve", self.tmp[0:n, k:k + 1], ss, 1.0 / D, EPS, ALU.mult, ALU.add, [ssd], [td])
            self.tt("pool", self.rs[0:n, k:k + 1], self.tmp[0:n, k:k + 1], self.neghalf[0:n, 0:1], ALU.pow,
                    [td, self.nh_dep], [rsd])

        stats(0)
        stats(1)
        for t, (t0, n) in enumerate(TILES):
            if t + 2 < NT:
                stats(t + 2)
            b = t % 2
            k, rsd = slots[t]
            self.ts("dve", self.xn_s[b][0:n, :], self.h[0:n, t, :], self.rs[0:n, k:k + 1], None, ALU.mult, ALU.bypass,
                    [self.h_dep[t], rsd], [self.xn_s_dep[b]])
            pb = self.psum[b].bitcast(BF16)
            for kk in range(8):
                self.tr(pb[:, kk * 128:kk * 128 + n], self.xn_s[b][0:n, kk * 128:(kk + 1) * 128],
                        self.ident[0:n, 0:n], [self.xn_s_dep[b], self.ident_dep], [self.pdep[b]])
            src = pb.rearrange("p (k c) -> p k c", k=8)[:, :, 0:n]
            self.P.add("act", (lambda e, o=self.xnT[:, :, t0:t0 + n], i=src: e.activation(o, i, AF.Copy)),
                       [self.pdep[b]], [self.xn_dep[t]])

    def _ffn_alloc(self):
        if hasattr(self, "ffn_alloced"):
            return
        self.ffn_alloced = True
        self.cur = self.phase_off
        sb = self.sb
        self.stg = [sb("stg%d" % i, [128, 2048], F32) for i in range(3)]
        self.stg_dep = [Dep("stg%d" % i) for i in range(3)]
        self.stg_i = 0
        self.wgb = [sb("wgb%d" % i, [128, 8, 256], BF16) for i in range(2)]
        self.wub = [sb("wub%d" % i, [128, 8, 256], BF16) for i in range(2)]
        self.wgb_dep = [Dep("wgb%d" % i) for i in range(2)]
        self.wub_dep = [Dep("wub%d" % i) for i in range(2)]
        self.wdb = [sb("wdb%d" % i, [128, 2, D], BF16) for i in range(4)]
        self.wdb_dep = [Dep("wdb%d" % i) for i in range(4)]
        self.actb = sb("actb", [128, 4, L], BF16)
        self.actb_dep = [[Dep("act%d_%d" % (f, g)) for g in range(len(TGS))] for f in range(4)]
        self.sg = [sb("sg%d" % i, [128, 512], BF16) for i in range(2)]
        self.sg_dep = [Dep("sg%d" % i) for i in range(2)]
        self.ffn_end = self.cur
        self.slab_ctr = 0
        self.gu_ctr = 0
        self.dn_ctr = 0

    def _stage_slot(self):
        i = self.stg_i
        self.stg_i = (i + 1) % 3
        return i

    def _ffn(self, which):
        self._ffn_alloc()
        wg, wu, wd = self.wg[which], self.wu[which], self.wd[which]
        gcol = {0: 0, 1: 16}[which]
        gain = self.cols_sb[:, gcol:gcol + 8]
        slabs = list(range(11))
        groups = [slabs[i:i + 2] for i in range(0, 11, 2)]
        for grp in groups:
            for li, sl in enumerate(grp):
                c0 = sl * 256
                sw = self.slab_ctr % 2
                dslot = self.slab_ctr % 4
                self.slab_ctr += 1
                for (src, dst, ddep) in ((wg, self.wgb[sw], self.wgb_dep[sw]), (wu, self.wub[sw], self.wub_dep[sw])):
                    si = self._stage_slot()
                    st3 = self.stg[si].rearrange("p (k c) -> p k c", k=8)
                    self.dma(st3, src[:, c0:c0 + 256].rearrange("(k p) c -> p k c", p=128), [], [self.stg_dep[si]])
                    self.tt("pool", dst, st3, gain.unsqueeze(2).to_broadcast([128, 8, 256]), ALU.mult,
                            [self.stg_dep[si], self.cdep], [ddep])
                si = self._stage_slot()
                st3 = self.stg[si].rearrange("p (k c) -> p k c", k=2)
                self.dma(st3, wd[c0:c0 + 256, :].rearrange("(k p) c -> p k c", p=128), [], [self.stg_dep[si]])
                self.cp("pool", self.wdb[dslot], st3, [self.stg_dep[si]], [self.wdb_dep[dslot]])
                grp_dslot = dslot
                for c in range(2):
                    fl = li * 2 + c
                    for g, (g0, gn) in enumerate(TGS):
                        pbuf = self.gu_ctr % 2
                        self.gu_ctr += 1
                        pg, pu = 2 + 2 * pbuf, 3 + 2 * pbuf
                        xdeps = [self.xn_dep[t] for t in range(NT) if TILES[t][0] >= g0 and TILES[t][0] < g0 + gn]
                        for k in range(8):
                            self.mm(self.psum[pg][:, 0:gn], self.wgb[sw][:, k, c * 128:(c + 1) * 128],
                                    self.xnT[:, k, g0:g0 + gn], k == 0, k == 7,
                                    [self.wgb_dep[sw]] + xdeps, [self.pdep[pg]])
                        for k in range(8):
                            self.mm(self.psum[pu][:, 0:gn], self.wub[sw][:, k, c * 128:(c + 1) * 128],
                                    self.xnT[:, k, g0:g0 + gn], k == 0, k == 7,
                                    [self.wub_dep[sw]] + xdeps, [self.pdep[pu]])
                        sgi = pbuf
                        self.act(self.sg[sgi][:, 0:gn], self.psum[pg][:, 0:gn], AF.Silu, [self.pdep[pg]], [self.sg_dep[sgi]])
                        self.tt("dve", self.actb[:, fl, g0:g0 + gn], self.sg[sgi][:, 0:gn], self.psum[pu][:, 0:gn], ALU.mult,
                                [self.sg_dep[sgi], self.pdep[pu]], [self.actb_dep[fl][g]])
            nfl = len(grp) * 2
            first_dslot = (self.slab_ctr - len(grp)) % 4
            for t, (t0, n) in enumerate(TILES):
                g = min(t // 4, 4)
                for half in range(2):
                    pd = 6 + (self.dn_ctr % 2)
                    self.dn_ctr += 1
                    for fl in range(nfl):
                        dslot = (first_dslot + fl // 2) % 4
                        self.mm(self.psum[pd][0:n, :], self.actb[:, fl, t0:t0 + n],
                                self.wdb[dslot][:, fl % 2, half * 512:(half + 1) * 512], fl == 0, fl == nfl - 1,
                                [self.actb_dep[fl][g], self.wdb_dep[dslot]], [self.pdep[pd]])
                    hv = self.h[0:n, t, half * 512:(half + 1) * 512]
                    self.stt(hv, self.psum[pd][0:n, :], 0.5, hv, ALU.mult, ALU.add,
                             [self.pdep[pd], self.h_dep[t]], [self.h_dep[t]])


    def _attn_alloc(self):
        if hasattr(self, "attn_alloced"):
            return
        self.attn_alloced = True
        sb = self.sb
        save = self.cur
        self.cur = self.h_off
        r1 = self.cur
        self.qT = sb("qT", [128, 4, L], BF16)
        self.kT = sb("kT", [128, 4, L], BF16)
        self.VA = sb("VA", [128, NT, 4, 129], BF16)
        r1_end = self.cur
        self.cur = r1
        self.qlT = sb("qlT", [128, 4, 2, 512], BF16)
        self.rr = [sb("rr%d" % i, [128, 512], F32) for i in range(2)]
        self.qn1k = sb("qn1k", [128, 1024], BF16)
        self.ocT = sb("ocT", [128, 8, 128], BF16)
        self.woutb = sb("woutb", [128, 8, D], BF16)
        self.hst2 = sb("hst2", [128, 2, D], F32)
        self.hst = [self.hst2[:, 0, :], self.hst2[:, 1, :]]
        self.obG = sb("obG", [128, 4, 512], BF16)
        self.cntj = sb("cntj", [128, L], BF16)
        assert self.cur <= r1_end, (self.cur, r1_end)
        self.cur = r1_end
        self.qbT = sb("qbT", [128, 4, L], BF16)
        self.cT = sb("cT", [128, 2, L], BF16)
        self.VB = sb("VB", [128, NT, 4, 129], BF16)
        self.iqT = sb("iqT", [128, 4, L], BF16)
        self.ikT = sb("ikT", [128, L], BF16)
        r3 = self.cur
        self.wst = [sb("wst%d" % i, [128, 8, 384], F32) for i in range(2)]
        self.wb = [sb("wb%d" % i, [128, 8, 384], BF16) for i in range(2)]
        r3_end = self.cur
        self.cur = r3
        self.oa = sb("oa", [128, NT, 512], BF16)
        self.PT = [sb("PT%d" % i, [128, 512], BF16) for i in range(4)]
        self.t1 = [sb("t1_%d" % i, [128, 128], F32) for i in range(2)]
        self.ov = [sb("ov_%d" % i, [128, 128], F32) for i in range(2)]
        self.bst = sb("bst", [128, 4 * NBIS + 8], F32)
        self.mTa = sb("mTa", [128, 12, 512], BF16)
        assert self.cur <= r3_end, (self.cur, r3_end)
        self.cur = max(r3_end, save)
        self.sq = [sb("sq%d" % i, [128, 256], F32) for i in range(2)]
        self.qn = [sb("qn%d" % i, [128, 256], BF16) for i in range(2)]
        save2 = self.cur
        self.cur = self.c_const
        self.acc = sb("acc", [128, L], F32)
        self.maskb = sb("maskb", [128, L], BF16)
        self.mT = sb("mT", [128, NT, 512], BF16)
        assert self.cur <= self.h_off, (self.cur, self.h_off)
        self.cur = save2
        D_ = Dep
        self.qT_dep = [[D_("qT%d_%d" % (h, t)) for t in range(NT)] for h in range(4)]
        self.kT_dep = [[D_("kT%d_%d" % (h, t)) for t in range(NT)] for h in range(4)]
        self.VA_dep = [D_("VA%d" % t) for t in range(NT)]
        self.VB_dep = [D_("VB%d" % t) for t in range(NT)]
        self.cT_dep = [D_("cT%d" % t) for t in range(NT)]
        self.qbT_dep = [D_("qbT%d" % g) for g in range(5)]
        self.iqT_dep = [D_("iqT%d" % g) for g in range(5)]
        self.ikT_dep = [D_("ikT%d" % g) for g in range(5)]
        self.iw_dep = [D_("iw%d" % t) for t in range(NT)]
        self.wst_dep = [D_("wst%d" % i) for i in range(2)]
        self.wb_dep = [D_("wb%d" % i) for i in range(2)]
        self.sq_dep = [D_("sq%d" % i) for i in range(2)]
        self.qn_dep = [D_("qn%d" % i) for i in range(2)]
        self.oa_dep = [D_("oa%d" % t) for t in range(NT)]
        self.PT_dep = [D_("PT%d" % i) for i in range(4)]
        self.t1_dep = [D_("t1_%d" % i) for i in range(2)]
        self.ov_dep = [D_("ov_%d" % i) for i in range(2)]
        self.qlT_dep = [D_("qlT%d" % i) for i in range(4)]
        self.rr_dep = [D_("rr%d" % i) for i in range(2)]
        self.qn1k_dep = D_("qn1k")
        self.ocT_dep = D_("ocT")
        self.woutb_dep = D_("woutb")
        self.hst_dep = [D_("hst%d" % i) for i in range(2)]
        self.obG_dep = [D_("obG%d" % i) for i in range(4)]
        self.cntj_dep = D_("cntj")
        self.acc_dep = D_("acc")
        self.maskb_dep = D_("maskb")
        self.mT_dep = [D_("mT%d" % i) for i in range(4)]
        self.mTa_dep = [D_("mTa%d" % i) for i in range(4)]
        self.bst_dep = D_("bst")
        self.ctr = 0

    def _load_slab(self, c0, ncol, gain):
        slot = self.ctr % 2
        self.ctr += 1
        st = self.wst[slot][:, :, 0:ncol]
        self.dma(st, self.win[:, c0:c0 + ncol].rearrange("(k p) c -> p k c", p=128), [], [self.wst_dep[slot]])
        self.tt("pool", self.wb[slot][:, :, 0:ncol], st, gain.unsqueeze(2).to_broadcast([128, 8, ncol]), ALU.mult,
                [self.wst_dep[slot], self.cdep], [self.wb_dep[slot]])
        return slot

    def _proj(self):
        gain = self.cols_sb[:, 8:16]
        C = self.cols_sb
        parts = _os.environ.get("K_PROJ_PARTS", "ms,A,CK,FM").split(",")
        if "ms" in parts:
            self.memset("pool", self.VA[:, :, :, 128:129], 1.0, self.VA_dep)
            self.memset("pool", self.VB[:, :, :, 128:129], 1.0, self.VB_dep)
        tctr = 0
        for sl in range(5):
            if (sl < 4 and "A" not in parts) or (sl == 4 and "CK" not in parts):
                continue
            ncol = 384 if sl < 4 else 264
            slot = self._load_slab(sl * 384, ncol, gain)

            def mmA(t, slot=slot, ncol=ncol):
                t0, n = TILES[t]
                pb = 2 + (t % 4)
                for k in range(8):
                    self.mm(self.psum[pb][0:n, 0:ncol], self.xnT[:, k, t0:t0 + n], self.wb[slot][:, k, 0:ncol], k == 0, k == 7,
                            [self.xn_dep[t], self.wb_dep[slot]], [self.pdep[pb]])

            for t in range(3):
                mmA(t)
            for t, (t0, n) in enumerate(TILES):
                if t + 3 < NT:
                    mmA(t + 3)
                pb = 2 + (t % 4)
                tb = t % 2
                b = t % 2
                ps = self.psum[pb]
                pT = self.psum[tb].bitcast(BF16)
                k4, sdeps = self.stat()
                ssd, td, rsd = sdeps
                if sl < 4:
                    h = sl
                    KA = _os.environ.get("K_A", "sq,red,rstd,qn,tr,evq,evk,va").split(",")
                    if "sq" in KA:
                        self.act(self.sq[b][0:n, :], ps[0:n, 0:256], AF.Square, [self.pdep[pb]], [self.sq_dep[b]])
                    if "red" in KA:
                        self.red(self.ss[0:n, k4:k4 + 4], self.sq[b][0:n, :].rearrange("p (g d) -> p g d", d=64), ALU.add,
                                 [self.sq_dep[b]], [ssd])
                    if "rstd" in KA:
                        self.rstd(n, k4, 4, 1.0 / 64, sdeps)
                    if "qn" in KA:
                        self.tt("dve", self.qn[b][0:n, :].rearrange("p (g d) -> p g d", d=64),
                                ps[0:n, 0:256].rearrange("p (g d) -> p g d", d=64),
                                self.rs[0:n, k4:k4 + 4].unsqueeze(2).to_broadcast([n, 4, 64]), ALU.mult,
                                [self.pdep[pb], rsd], [self.qn_dep[b]])
                    if "tr" in KA:
                        self.tr(pT[:, 0:n], self.qn[b][0:n, 0:128], self.ident[0:n, 0:n], [self.qn_dep[b], self.ident_dep],
                                [self.pdep[tb]])
                        self.tr(pT[:, 128:128 + n], self.qn[b][0:n, 128:256], self.ident[0:n, 0:n], [self.qn_dep[b], self.ident_dep],
                                [self.pdep[tb]])
                    if "evq" in KA:
                        qdst = self.junk[:, 0:n] if _os.environ.get("K_DEST") else self.qT[:, h, t0:t0 + n]
                        self.act(qdst, pT[:, 0:n], AF.Copy, [self.pdep[tb], self.setup_dep],
                                 [self.qT_dep[h][t]], scale=self.gqk)
                    if "evk" in KA:
                        kdst = self.junk[:, 128:128 + n] if _os.environ.get("K_DEST") else self.kT[:, h, t0:t0 + n]
                        self.act(kdst, pT[:, 128:128 + n], AF.Copy, [self.pdep[tb]], [self.kT_dep[h][t]])
                    if "va" in KA:
                        self.act(self.VA[0:n, t, h, 0:128], ps[0:n, 256:384], AF.Copy, [self.pdep[pb]], [self.VA_dep[t]])
                else:
                    self.act(self.sq[b][0:n, :], ps[0:n, 0:256], AF.Square, [self.pdep[pb]], [self.sq_dep[b], ssd],
                             accum=self.ss[0:n, k4:k4 + 1])
                    self.rstd(n, k4, 1, 1.0 / 256, sdeps)
                    self.ts("dve", self.qn[b][0:n, :], ps[0:n, 0:256], self.rs[0:n, k4:k4 + 1], None, ALU.mult, ALU.bypass,
                            [self.pdep[pb], rsd], [self.qn_dep[b]])
                    self.ts("dve", self.iw_sb[0:n, t, :], ps[0:n, 256:264], (64 ** -0.5) * (8 ** -0.5), None, ALU.mult, ALU.bypass,
                            [self.pdep[pb]], [self.iw_dep[t]])
                    for cc in range(2):
                        self.tr(pT[:, cc * 128:cc * 128 + n], self.qn[b][0:n, cc * 128:(cc + 1) * 128], self.ident[0:n, 0:n],
                                [self.qn_dep[b], self.ident_dep], [self.pdep[tb]])
                    self.ts("dve", self.cT[:, 0, t0:t0 + n], pT[:, 0:n], C[:, 26:27], None, ALU.mult, ALU.bypass,
                            [self.pdep[tb], self.cdep], [self.cT_dep[t]])
                    self.act(self.cT[:, 1, t0:t0 + n], pT[:, 128:128 + n], AF.Copy, [self.pdep[tb], self.cdep], [self.cT_dep[t]],
                             scale=C[:, 27:28])
                    vb = 6 + (t % 2)
                    for cc in range(2):
                        self.mm(self.psum[vb][0:n, :], self.cT[:, cc, t0:t0 + n],
                                self.wuvb[:, cc, :, :].rearrange("p h e -> p (h e)"), cc == 0, cc == 1,
                                [self.cT_dep[t], self.setup_dep], [self.pdep[vb]])
                    self.act(self.VB[0:n, t, :, 0:128], self.psum[vb][0:n, :].rearrange("p (h e) -> p h e", h=4), AF.Copy,
                             [self.pdep[vb]], [self.VB_dep[t]])
        ectr = 0
        for sl in range(3):
            if "FM" not in parts:
                continue
            slot = self._load_slab(1800 + sl * 384, 384, gain)
            for c in range(3):
                ch = sl * 3 + c
                for g, (g0, gn) in enumerate(TGS):
                    if ch < 4:
                        dst, ddep = self.qbT[:, ch, g0:g0 + gn], self.qbT_dep[g]
                    elif ch < 8:
                        dst, ddep = self.iqT[:, ch - 4, g0:g0 + gn], self.iqT_dep[g]
                    else:
                        dst, ddep = self.ikT[:, g0:g0 + gn], self.ikT_dep[g]
                    pb = 4 + (ectr % 2)
                    xdeps = [self.xn_dep[t] for t in range(NT) if g0 <= TILES[t][0] < g0 + gn]
                    for k in range(8):
                        self.mm(self.psum[pb][:, 0:gn], self.wb[slot][:, k, c * 128:(c + 1) * 128], self.xnT[:, k, g0:g0 + gn],
                                k == 0, k == 7, [self.wb_dep[slot]] + xdeps, [self.pdep[pb]])
                    if ectr % 2 == 0:
                        self.act(dst, self.psum[pb][:, 0:gn], AF.Copy, [self.pdep[pb]], [ddep])
                    else:
                        self.cp("dve", dst, self.psum[pb][:, 0:gn], [self.pdep[pb]], [ddep])
                    ectr += 1

    def _bias_blocks(self, ps, pbank, hh, j, tiles, c0):
        k0, kn = TILES[j]
        for blk, i in ((0, j), (1, j + 1)):
            if i in tiles:
                off = TILES[i][0] - c0
                ni = TILES[i][1]
                self.mm(ps[:, off:off + ni], self.ident[0:kn, 0:kn], self.biasbf[0:kn, hh, blk, 0:ni], False, True,
                        [self.ident_dep, self.setup_dep], [self.pdep[pbank]])

    def _pipe(self, n, stage1, stage2, depth=2):
        for k in range(min(depth, n)):
            stage1(k)
        for k in range(n):
            if k + depth < n:
                stage1(k + depth)
            stage2(k)

    def _attnA(self):
        for G, tiles in enumerate(QGS):
            q0 = TILES[tiles[0]][0]
            qn_ = sum(TILES[t][1] for t in tiles)
            for h in range(4):
                started = set()
                nblk = tiles[-1] + 1

                def stage1(j, h=h, tiles=tiles, q0=q0, qn_=qn_):
                    k0, kn = TILES[j]
                    c0 = max(q0, k0)
                    ncol = q0 + qn_ - c0
                    qdeps = [self.qT_dep[h][t] for t in tiles if TILES[t][0] + TILES[t][1] > c0]
                    for m in range(2):
                        bank = (j % 2) * 2 + m
                        ps = self.psum[bank][0:kn, 0:ncol]
                        self.mm(ps, self.kT[m * 64:(m + 1) * 64, h, k0:k0 + kn], self.qT[m * 64:(m + 1) * 64, h, c0:c0 + ncol],
                                True, True, [self.kT_dep[h][j]] + qdeps, [self.pdep[bank]])
                    for m in range(2):
                        bank = (j % 2) * 2 + m
                        ps = self.psum[bank][0:kn, 0:ncol]
                        self._bias_blocks(ps, bank, h, j, tiles, c0)
                        self.act(self.PT[bank][0:kn, 0:ncol], ps, AF.Exp, [self.pdep[bank], self.setup_dep], [self.PT_dep[bank]],
                                 bias=self.abias[0:kn, h:h + 1])

                def stage2(j, h=h, tiles=tiles, q0=q0, started=started):
                    k0, kn = TILES[j]
                    c0 = max(q0, k0)
                    for m in range(2):
                        pbuf = (j % 2) * 2 + m
                        for il, i in enumerate(tiles):
                            if i < j:
                                continue
                            off = TILES[i][0] - c0
                            ni = TILES[i][1]
                            slot = m * 4 + il
                            ab = 4 + slot // 3
                            o = (slot % 3) * 129
                            first = ab not in started
                            started.add(ab)
                            self.mm(self.psum[ab][0:ni, o:o + 129], self.PT[pbuf][0:kn, off:off + ni], self.VA[0:kn, j, h, :],
                                    first, j == i, [self.PT_dep[pbuf], self.VA_dep[j]], [self.pdep[ab]])

                self._pipe(nblk, stage1, stage2, depth=1)
                for il, i in enumerate(tiles):
                    ni = TILES[i][1]
                    b = il % 2
                    s0, s1 = il, 4 + il
                    b0, o0 = 4 + s0 // 3, (s0 % 3) * 129
                    b1, o1 = 4 + s1 // 3, (s1 % 3) * 129
                    k4, sdeps = self.stat()
                    ssd, td, rsd = sdeps
                    self.recip(self.rs[0:ni, k4 + 1:k4 + 2], self.psum[b0][0:ni, o0 + 128:o0 + 129], [self.pdep[b0]], [rsd])
                    self.recip(self.rs[0:ni, k4 + 2:k4 + 3], self.psum[b1][0:ni, o1 + 128:o1 + 129], [self.pdep[b1]], [rsd])
                    self.ts("dve", self.rs[0:ni, k4 + 3:k4 + 4], self.rs[0:ni, k4 + 2:k4 + 3], self.lamn[0:ni, 0:1], None,
                            ALU.mult, ALU.bypass, [rsd, self.setup_dep], [rsd])
                    self.ts("dve", self.t1[b][0:ni, :], self.psum[b1][0:ni, o1:o1 + 128], self.rs[0:ni, k4 + 3:k4 + 4], None,
                            ALU.mult, ALU.bypass, [self.pdep[b1], rsd], [self.t1_dep[b]])
                    self.stt(self.ov[b][0:ni, :], self.psum[b0][0:ni, o0:o0 + 128], self.rs[0:ni, k4 + 1:k4 + 2],
                             self.t1[b][0:ni, :], ALU.mult, ALU.add, [self.pdep[b0], rsd, self.t1_dep[b]], [self.ov_dep[b]])
                    self.act(self.junk[0:ni, 0:128], self.ov[b][0:ni, :], AF.Square, [self.ov_dep[b]], [self.junk_dep, ssd],
                             accum=self.ss[0:ni, k4:k4 + 1])
                    self.rstd(ni, k4, 1, 1.0 / 128, sdeps)
                    self.ts("dve", self.oa[0:ni, i, h * 128:(h + 1) * 128], self.ov[b][0:ni, :], self.rs[0:ni, k4:k4 + 1], None,
                            ALU.mult, ALU.bypass, [self.ov_dep[b], rsd], [self.oa_dep[i]])

    def _attnB(self):
        C = self.cols_sb
        for q in range(4):
            st = self.hst2
            hd = self.hst_dep
            self.dma(st, self.wout[q * 256:(q + 1) * 256, :].rearrange("(k p) c -> p k c", p=128), [hd[1]], [hd[0]])
            if q < 2:
                self.ts("pool", self.woutb[:, 2 * q:2 * q + 2, :], st, self.gsub[:, 0:1], None, ALU.mult, ALU.bypass,
                        [hd[0], hd[1], self.setup_dep], [self.woutb_dep])
            else:
                self.cp("pool", self.woutb[:, 2 * q:2 * q + 2, :], st, [hd[0], hd[1]], [self.woutb_dep])
        self.dctr = 0
        self.hctr = 0
        ng = len(QGS)
        for il in range(len(QGS[0])):
            self._b_idx1(0, il)
            self._b_idx2(0, il)
        for G in range(ng):
            self._b_qlat(G)
            nxt = G + 1 if G + 1 < ng - 1 else None
            for h in range(4):
                if nxt is not None and h < len(QGS[nxt]):
                    self._b_idx1(nxt, h)
                self._b_attn_mm(G, h)
                if nxt is not None and h < len(QGS[nxt]):
                    self._b_idx2(nxt, h)
                self._b_attn_norm(G, h)
            if G + 1 == ng - 1:
                for il in range(len(QGS[G + 1])):
                    self._b_idx1(G + 1, il)
                    self._b_idx2(G + 1, il)
            self._b_wout(G)

    def _b_ctx(self, G):
        tiles = QGS[G]
        q0 = TILES[tiles[0]][0]
        qn_ = sum(TILES[t][1] for t in tiles)
        use_a = G in (0, 2)
        mT = self.mTa if use_a else self.mT
        mT_dep = self.mTa_dep if use_a else self.mT_dep
        return tiles, q0, qn_, mT, mT_dep

    def _b_qlat(self, G):
        tiles, q0, qn_, mT, mT_dep = self._b_ctx(G)
        for il, i in enumerate(tiles):
            t0, ni = TILES[i]
            for h in range(4):
                pbk = h // 2
                self.mm(self.psum[pbk][0:ni, (h % 2) * 256:(h % 2) * 256 + 256], self.qbT[:, h, t0:t0 + ni], self.wukb[:, h, :],
                        True, True, [self.qbT_dep[G], self.setup_dep], [self.pdep[pbk]])
            k4, sdeps = self.stat()
            ssd, td, rsd = sdeps
            for pbk in range(2):
                self.act(self.junk[0:ni, pbk * 512:(pbk + 1) * 512], self.psum[pbk][0:ni, :], AF.Square, [self.pdep[pbk]],
                         [self.junk_dep])
            self.red(self.ss[0:ni, k4:k4 + 4], self.junk[0:ni, :].rearrange("p (g d) -> p g d", d=256), ALU.add,
                     [self.junk_dep], [ssd])
            self.rstd(ni, k4, 4, 1.0 / 256, sdeps)
            for pbk in range(2):
                self.tt("dve", self.qn1k[0:ni, pbk * 512:(pbk + 1) * 512].rearrange("p (g d) -> p g d", d=256),
                        self.psum[pbk][0:ni, :].rearrange("p (g d) -> p g d", d=256),
                        self.rs[0:ni, k4 + 2 * pbk:k4 + 2 * pbk + 2].unsqueeze(2).to_broadcast([ni, 2, 256]), ALU.mult,
                        [self.pdep[pbk], rsd], [self.qn1k_dep])
            pT = self.psum[2].bitcast(BF16)
            for ch in range(8):
                self.tr(pT[:, ch * 128:ch * 128 + ni], self.qn1k[0:ni, ch * 128:(ch + 1) * 128], self.ident[0:ni, 0:ni],
                        [self.qn1k_dep, self.ident_dep], [self.pdep[2]])
            pv = pT.rearrange("p (h c t) -> p h c t", h=4, c=2)
            self.ts("dve", self.qlT[:, :, 0, il * 128:il * 128 + ni], pv[:, :, 0, 0:ni], self.gqb[0], None, ALU.mult,
                    ALU.bypass, [self.pdep[2], self.setup_dep], [self.qlT_dep[il]])
            self.ts("dve", self.qlT[:, :, 1, il * 128:il * 128 + ni], pv[:, :, 1, 0:ni], self.gqb[1], None, ALU.mult,
                    ALU.bypass, [self.pdep[2], self.setup_dep], [self.qlT_dep[il]])

    def _b_idx1(self, G, il):
        tiles, q0, qn_, mT, mT_dep = self._b_ctx(G)
        i = tiles[il]
        dctr = self.dctr
        t0, ni = TILES[i]
        Lk = t0 + ni
        acc = self.acc
        for kc0 in range(0, Lk, 512):
            kcn = min(512, Lk - kc0)
            g = kc0 // 512
            for hh in range(8):
                pb = 3 + (dctr % 2)
                rb = dctr % 2
                dctr += 1
                pr = (hh % 2) * 64
                self.mm(self.psum[pb][0:ni, 0:kcn], self.iqT[pr:pr + 64, hh // 2, t0:t0 + ni], self.ikT[pr:pr + 64, kc0:kc0 + kcn],
                        True, True, [self.iqT_dep[G], self.ikT_dep[g]], [self.pdep[pb]])
                self.act(self.rr[rb][0:ni, 0:kcn], self.psum[pb][0:ni, 0:kcn], AF.Relu, [self.pdep[pb]], [self.rr_dep[rb]])
                if hh == 0:
                    self.ts("dve", acc[0:ni, kc0:kc0 + kcn], self.rr[rb][0:ni, 0:kcn], self.iw_sb[0:ni, i, 0:1], None,
                            ALU.mult, ALU.bypass, [self.rr_dep[rb], self.iw_dep[i]], [self.acc_dep])
                else:
                    self.stt(acc[0:ni, kc0:kc0 + kcn], self.rr[rb][0:ni, 0:kcn], self.iw_sb[0:ni, i, hh:hh + 1],
                             acc[0:ni, kc0:kc0 + kcn], ALU.mult, ALU.add, [self.rr_dep[rb], self.iw_dep[i], self.acc_dep],
                             [self.acc_dep])
        self.dctr = dctr

    def _b_idx2(self, G, il):
        tiles, q0, qn_, mT, mT_dep = self._b_ctx(G)
        i = tiles[il]
        t0, ni = TILES[i]
        Lk = t0 + ni
        acc = self.acc
        bs = self.bst
        bd = self.bst_dep
        if i >= 2:
            self.red(bs[0:ni, 0:1], acc[0:ni, 0:Lk], ALU.max, [self.acc_dep], [bd])
            self.red(bs[0:ni, 1:2], acc[0:ni, 0:TOPK], ALU.min, [self.acc_dep], [bd])
        self.tt("dve", acc[0:ni, t0:t0 + ni], acc[0:ni, t0:t0 + ni], self.masktok[0:ni, 0:ni], ALU.add,
                [self.acc_dep, self.masktok_dep], [self.acc_dep])
        if i >= 2:
            self.tt("dve", bs[0:ni, 2:3], bs[0:ni, 0:1], bs[0:ni, 1:2], ALU.subtract, [bd], [bd])
            W0 = 8
            self.ts("dve", bs[0:ni, W0:W0 + NBIS + 1], self.pw[0:ni, :], bs[0:ni, 2:3], None, ALU.mult, ALU.bypass,
                    [bd, self.setup_dep], [bd])
            M0 = W0 + NBIS + 1
            self.tt("dve", bs[0:ni, M0:M0 + 1], bs[0:ni, 1:2], bs[0:ni, W0:W0 + 1], ALU.add, [bd], [bd])
            C0 = M0 + NBIS + 1
            for k in range(NBIS):
                self.ts("dve", self.cntj[0:ni, 0:Lk], acc[0:ni, 0:Lk], bs[0:ni, M0 + k:M0 + k + 1], None, ALU.is_ge, ALU.add,
                        [self.acc_dep, bd], [self.cntj_dep, bd], accum=bs[0:ni, C0 + k:C0 + k + 1])
                self.ts("dve", bs[0:ni, 3:4], bs[0:ni, C0 + k:C0 + k + 1], TOPK - 0.5, bs[0:ni, W0 + k:W0 + k + 1],
                        ALU.is_ge, ALU.mult, [bd], [bd])
                wn = W0 + k + 1 if k < NBIS - 1 else W0 + k
                self.stt(bs[0:ni, M0 + k + 1:M0 + k + 2], bs[0:ni, 3:4], bs[0:ni, wn:wn + 1], bs[0:ni, M0 + k:M0 + k + 1],
                         ALU.subtract, ALU.add, [bd], [bd])
            theta = bs[0:ni, M0 + NBIS:M0 + NBIS + 1]
            thd = [bd]
        else:
            theta = self.thc[0:ni, 0:1]
            thd = [self.setup_dep]
        self.ts("dve", self.maskb[0:ni, 0:Lk], acc[0:ni, 0:Lk], theta, NEG, ALU.is_lt, ALU.mult, [self.acc_dep] + thd,
                [self.maskb_dep])
        for j0 in range(0, i + 1, 8):
            js = list(range(j0, min(j0 + 8, i + 1)))
            tb = 5
            pT = self.psum[tb].bitcast(BF16)
            for jj, j in enumerate(js):
                k0, kn = TILES[j]
                self.tr(pT[0:kn, jj * 128:jj * 128 + ni], self.maskb[0:ni, k0:k0 + kn], self.ident[0:ni, 0:ni],
                        [self.maskb_dep, self.ident_dep], [self.pdep[tb]])
            src = pT.rearrange("p (j t) -> p j t", j=8)[:, 0:len(js), 0:ni]
            self.cp("dve", mT[:, j0:j0 + len(js), il * 128:il * 128 + ni], src, [self.pdep[tb]], [mT_dep[il]])

    def _b_attn_mm(self, G, h):
        tiles, q0, qn_, mT, mT_dep = self._b_ctx(G)
        started = set()
        nblk = tiles[-1] + 1

        def stage1(j, h=h, tiles=tiles, q0=q0, qn_=qn_, mT=mT, mT_dep=mT_dep):
            k0, kn = TILES[j]
            c0 = max(q0, k0)
            ncol = q0 + qn_ - c0
            co = c0 - q0
            bank = j % 3
            ps = self.psum[bank][0:kn, 0:ncol]
            qd = [self.qlT_dep[il] for il, t in enumerate(tiles) if TILES[t][0] + TILES[t][1] > c0]
            md = [mT_dep[il] for il, t in enumerate(tiles) if TILES[t][0] + TILES[t][1] > c0]
            for cc in range(2):
                self.mm(ps, self.cT[:, cc, k0:k0 + kn], self.qlT[:, h, cc, co:co + ncol], cc == 0, False,
                        [self.cT_dep[j]] + qd, [self.pdep[bank]])
            self.mm(ps, self.ident[0:kn, 0:kn], mT[0:kn, j, co:co + ncol], False, True, [self.ident_dep] + md,
                    [self.pdep[bank]])
            self._bias_blocks(ps, bank, 4 + h, j, tiles, c0)
            self.act(self.PT[j % 4][0:kn, 0:ncol], ps, AF.Exp, [self.pdep[bank], self.setup_dep], [self.PT_dep[j % 4]],
                     bias=self.abias[0:kn, 4 + h:5 + h])

        def stage2(j, h=h, tiles=tiles, q0=q0, started=started):
            k0, kn = TILES[j]
            c0 = max(q0, k0)
            pbuf = j % 4
            for il, i in enumerate(tiles):
                if i < j:
                    continue
                off = TILES[i][0] - c0
                ni = TILES[i][1]
                ab = 6 + il // 3
                o = (il % 3) * 129
                first = ab not in started
                started.add(ab)
                self.mm(self.psum[ab][0:ni, o:o + 129], self.PT[pbuf][0:kn, off:off + ni], self.VB[0:kn, j, h, :],
                        first, j == i, [self.PT_dep[pbuf], self.VB_dep[j]], [self.pdep[ab]])

        self._pipe(nblk, stage1, stage2)

    def _b_attn_norm(self, G, h):
        tiles, q0, qn_, mT, mT_dep = self._b_ctx(G)
        for il, i in enumerate(tiles):
            ni = TILES[i][1]
            ab = 6 + il // 3
            o = (il % 3) * 129
            k4, sdeps = self.stat()
            ssd, td, rsd = sdeps
            self.recip(self.rs[0:ni, k4:k4 + 1], self.psum[ab][0:ni, o + 128:o + 129], [self.pdep[ab]], [rsd])
            self.ts("dve", self.obG[0:ni, il, h * 128:(h + 1) * 128], self.psum[ab][0:ni, o:o + 128], self.rs[0:ni, k4:k4 + 1],
                    None, ALU.mult, ALU.bypass, [self.pdep[ab], rsd], [self.obG_dep[il]])

    def _b_wout(self, G):
        tiles, q0, qn_, mT, mT_dep = self._b_ctx(G)
        hctr = self.hctr
        for il, i in enumerate(tiles):
            t0, ni = TILES[i]
            pT = self.psum[2].bitcast(BF16)
            for k in range(8):
                src = self.oa[0:ni, i, k * 128:(k + 1) * 128] if k < 4 else self.obG[0:ni, il, (k - 4) * 128:(k - 3) * 128]
                sd_ = self.oa_dep[i] if k < 4 else self.obG_dep[il]
                self.tr(pT[:, k * 128:k * 128 + ni], src, self.ident[0:ni, 0:ni], [sd_, self.ident_dep], [self.pdep[2]])
            self.act(self.ocT[:, :, 0:ni], pT.rearrange("p (k t) -> p k t", k=8)[:, :, 0:ni], AF.Copy, [self.pdep[2]],
                     [self.ocT_dep])
            hb = hctr % 2
            hctr += 1
            self.dma(self.hst[hb][0:ni, :], self.hs[t0:t0 + ni, :], [self.hs_dep[i]], [self.hst_dep[hb]])
            for half in range(2):
                pb = 3 + half
                for k in range(8):
                    self.mm(self.psum[pb][0:ni, :], self.ocT[:, k, 0:ni], self.woutb[:, k, half * 512:(half + 1) * 512],
                            k == 0, k == 7, [self.ocT_dep, self.woutb_dep], [self.pdep[pb]])
                hv = self.hst[hb][0:ni, half * 512:(half + 1) * 512]
                self.tt("dve", hv, self.psum[pb][0:ni, :], hv, ALU.add, [self.pdep[pb], self.hst_dep[hb]], [self.hst_dep[hb]])
            self.dma(self.hs[t0:t0 + ni, :], self.hst[hb][0:ni, :], [self.hst_dep[hb]], [self.hs_dep[i]])
        self.hctr = hctr

    def _finish(self):
        if _os.environ.get("K_DUMP"):
            dd = Dep("dump")
            self.P.barrier()
            self.dma(self.hs[0:128, 0:16], self.smallc[:, 0:16], [self.setup_dep], [dd])
            self.dma(self.hs[0:128, 16:24], self.abias, [self.setup_dep], [dd])
            self.P.add("sp", None, [dd], [])
        self.P.add("sp", None, [self.out_dep], [])
        if self.dbg is not None:
            pass


def _bucket(n):
    n = np.maximum(n, 0)
    nf = np.maximum(n, 1).astype(np.float32)
    large = 16 + (np.log(nf / np.float32(16)) / np.float32(math.log(128 / 16)) * np.float32(16)).astype(np.int32)
    large = np.minimum(large, 31)
    return np.where(n < 16, n, large)


def _prep_shared(inp):
    f = lambda a: np.ascontiguousarray(np.asarray(a, dtype=np.float32))
    sh = {}
    sh["meta"] = f(inp["meta_tokens"])
    for i, nm in ((1, "ffn1"), (2, "ffn2")):
        sh["w%dg" % i] = f(inp[nm + "_w_gate"][0])
        sh["w%du" % i] = f(inp[nm + "_w_up"][0])
        sh["w%dd" % i] = f(inp[nm + "_w_down"][0])
    w_in = f(inp["w_in"][0])
    qa, ka, va, qb, ckv, iq, ik, iw = np.split(w_in, np.cumsum([512, 512, 512, 512, 256, 512, 64, 8])[:-1], axis=1)
    parts = []
    for h in range(4):
        parts += [qa[:, h * 128:(h + 1) * 128], ka[:, h * 128:(h + 1) * 128], va[:, h * 128:(h + 1) * 128]]
    parts += [ckv, iw]
    parts += [qb, iq, ik, ik]
    sh["win"] = np.ascontiguousarray(np.concatenate(parts, axis=1))
    assert sh["win"].shape[1] == WIN_COLS
    sh["wuk"] = np.ascontiguousarray(f(inp["b_w_uk"][0]).transpose(1, 0, 2))
    sh["wuv"] = np.ascontiguousarray(f(inp["b_w_uv"][0]).reshape(4, 2, 128, 128).transpose(2, 1, 0, 3))
    sh["wout"] = f(inp["w_out"][0])
    cols = np.zeros((128, 40), np.float32)
    cols[:, 0:8] = f(inp["ffn1_norm"][0]).reshape(8, 128).T
    cols[:, 8:16] = f(inp["mix_norm"][0]).reshape(8, 128).T
    cols[:, 16:24] = f(inp["ffn2_norm"][0]).reshape(8, 128).T
    cols[:, 24] = np.tile(f(inp["a_q_norm"][0]), 2)
    cols[:, 25] = np.tile(f(inp["a_k_norm"][0]), 2)
    cols[:, 26:28] = f(inp["b_kv_norm"][0]).reshape(2, 128).T
    cols[:, 28:30] = f(inp["b_q_norm"][0]).reshape(2, 128).T
    cols[:, 30] = f(inp["a_subln"][0])
    rb = f(inp["rel_bias"])
    cols[:, 31:39] = np.broadcast_to(rb[31], (128, 8))
    sh["cols"] = cols
    rows = np.zeros((128, 896), np.float32)
    rows[:, 0:64] = f(inp["a_lambda_q1"][0])[None]
    rows[:, 64:128] = f(inp["a_lambda_k1"][0])[None]
    rows[:, 128:192] = f(inp["a_lambda_q2"][0])[None]
    rows[:, 192:256] = f(inp["a_lambda_k2"][0])[None]
    rows[:, 256:320] = f(inp["a_q_norm"][0])[None]
    rows[:, 320:384] = f(inp["a_k_norm"][0])[None]
    rows[:, 384:640] = f(inp["b_q_norm"][0])[None]
    rows[:, 640:896] = f(inp["b_kv_norm"][0])[None]
    sh["rows"] = rows
    tk = np.arange(128)[:, None]
    tq = np.arange(128)[None, :]
    bb = np.zeros((128, 8, 2, 128), np.float32)
    for blk in range(2):
        idx = _bucket(tq - tk + 128 * blk)
        bb[:, :, blk, :] = rb[idx].transpose(0, 2, 1)
    sh["biasblk"] = bb
    cm = np.zeros((128, 3, 128), np.float32)
    cm[:, 0, :] = np.eye(128, dtype=np.float32)
    cm[:, 1, :] = np.where(tq >= tk, 0.0, NEG)
    cm[:, 2, :] = np.where(tq <= tk, 0.0, -1e30)
    sh["cmask"] = cm
    return sh


_CACHE = {}


def kernel(**inputs):
    x = np.asarray(inputs["x"], dtype=np.float32)
    sh = _prep_shared(inputs)
    if "nc" not in _CACHE:
        _CACHE["nc"] = Builder().build()
    nc = _CACHE["nc"]
    in_maps = []
    for c in range(NCORES):
        m = dict(sh)
        m["x"] = np.ascontiguousarray(x[c * NSEQ:(c + 1) * NSEQ])
        in_maps.append(m)
    res = run_bass_kernel_spmd(nc, in_maps, core_ids=list(range(NCORES)))
    out = np.concatenate([np.asarray(r["out"]) for r in res.results], axis=0)
    return out.astype(np.float32)
```
